# Optimizing a Trainium2 kernel written in Bass

```python
import math
import jax
import jax.numpy as jnp
from jax import lax
import numpy as np

D_MODEL = 1024
BATCH = 8
SEQ = 4096
DEPTH = 4

GRID_W = 64
CTX_LEN = 256
N_BRANCH = 4
BRANCH_W = D_MODEL // 4
NA_HEADS = 4
NA_HD = BRANCH_W // NA_HEADS
NA_KH = 8
NA_KW = 16
MLA_HEADS = 4
MLA_Q_LORA = D_MODEL // 4
MLA_KV_LORA = D_MODEL // 8
MLA_NOPE = 64
MLA_ROPE = 32
MLA_V = BRANCH_W // MLA_HEADS
S5_GROUP_CH = 16
S5_GROUPS = BRANCH_W // S5_GROUP_CH
S5_STATE = 64
DIFF_HEADS = 4
DIFF_HD = BRANCH_W // (2 * DIFF_HEADS)
D_FF = 3584
N_EXPERTS = 8
TOP_K = 2
EXPERT_BLOCK = 128
N_DENSE = (DEPTH + 1) // 2
N_MOE = DEPTH // 2
Q_BLOCK = 128
ROPE_BASE = 10000.0
LN_EPS = 1e-5
RMS_EPS = 1e-6
ALPHA = (2 * DEPTH) ** 0.25
BETA = (8 * DEPTH) ** -0.25
NA_SCALE = NA_HD ** -0.5
MLA_SCALE = (MLA_NOPE + MLA_ROPE) ** -0.5
DIFF_SCALE = DIFF_HD ** -0.5
IN_SIZES = (BRANCH_W, BRANCH_W, BRANCH_W, MLA_Q_LORA, MLA_KV_LORA, MLA_ROPE, BRANCH_W, 2 * DIFF_HEADS * DIFF_HD, 2 * DIFF_HEADS * DIFF_HD, 2 * DIFF_HEADS * DIFF_HD)
IN_COLS = sum(IN_SIZES)

kernel_name = 'hybrid_diffusion_trunk'


def _split_cols(z, sizes):
    parts, start = [], 0
    for size in sizes:
        parts.append(z[..., start:start + size])
        start += size
    return parts


def _heads(z, n_heads):
    b, t, _ = z.shape
    return z.reshape(b, t, n_heads, -1).transpose(0, 2, 1, 3)


def _merge_heads(z):
    b, h, t, dh = z.shape
    return z.transpose(0, 2, 1, 3).reshape(b, t, h * dh)


def _layer_norm(z, g, b):
    zf = z.astype(jnp.float32)
    zc = zf - jnp.mean(zf, axis=-1, keepdims=True)
    var = jnp.mean(zc * zc, axis=-1, keepdims=True)
    return (zc * lax.rsqrt(var + LN_EPS) * g.astype(jnp.float32) + b.astype(jnp.float32)).astype(z.dtype)


def _rms_norm(z, g):
    zf = z.astype(jnp.float32)
    return (zf * lax.rsqrt(jnp.mean(zf * zf, axis=-1, keepdims=True) + RMS_EPS) * g.astype(jnp.float32)).astype(z.dtype)


def _softmax(s):
    return jax.nn.softmax(s.astype(jnp.float32), axis=-1)


def _rope_2d(z, prow, pcol):
    half = z.shape[-1] // 2
    nf = half // 2
    inv = ROPE_BASE ** (-jnp.arange(nf, dtype=jnp.float32) / nf)

    def rot(za, p):
        ang = p.astype(jnp.float32)[:, None] * inv[None, :]
        cos, sin = jnp.cos(ang), jnp.sin(ang)
        z1 = za[..., :nf].astype(jnp.float32)
        z2 = za[..., nf:].astype(jnp.float32)
        return jnp.concatenate([z1 * cos - z2 * sin, z1 * sin + z2 * cos], axis=-1)

    return jnp.concatenate([rot(z[..., :half], prow), rot(z[..., half:], pcol)], axis=-1).astype(z.dtype)


def _sweep_query_blocks(fn, *qs):
    b, h, t, _ = qs[0].shape
    nb = t // Q_BLOCK
    blocks = tuple(q.reshape(b, h, nb, Q_BLOCK, q.shape[-1]).transpose(2, 0, 1, 3, 4) for q in qs)
    out = lax.map(lambda blk: fn(*blk), blocks)
    return out.transpose(1, 2, 0, 3, 4).reshape(b, h, t, out.shape[-1])


def _dense_attention(q, k, v, scale):
    p = _softmax(jnp.einsum('bhqd,bhkd->bhqk', q, k) * scale)
    return jnp.einsum('bhqk,bhkd->bhqd', p.astype(v.dtype), v)


def _neighbourhood_attention(q, k, v, kc, vc, rpb):
    b, h, t, dh = q.shape
    rows = t // GRID_W
    kh = min(NA_KH, rows)
    kw = NA_KW
    q = q.reshape(b, h, rows, GRID_W, dh)
    k = k.reshape(b, h, rows, GRID_W, dh)
    v = v.reshape(b, h, rows, GRID_W, dh)
    col = jnp.arange(GRID_W)
    col_idx = jnp.clip(col - kw // 2, 0, GRID_W - kw)[:, None] + jnp.arange(kw)[None, :]
    col_off = col_idx - col[:, None] + (NA_KW - 1)

    def row_block(r):
        r0 = jnp.clip(r - kh // 2, 0, rows - kh)
        qr = lax.dynamic_index_in_dim(q, r, axis=2, keepdims=False)
        kg = lax.dynamic_slice_in_dim(k, r0, kh, axis=2)[:, :, :, col_idx]
        vg = lax.dynamic_slice_in_dim(v, r0, kh, axis=2)[:, :, :, col_idx]
        row_off = r0 + jnp.arange(kh) - r + (NA_KH - 1)
        bias = rpb[:, row_off][:, :, col_off].transpose(0, 2, 1, 3)
        s_loc = (jnp.einsum('bhqd,bhrqcd->bhqrc', qr, kg) * NA_SCALE + bias[None]).reshape(b, h, GRID_W, kh * kw)
        s_ctx = jnp.einsum('bhqd,bhld->bhql', qr, kc) * NA_SCALE
        p = _softmax(jnp.concatenate([s_loc, s_ctx], axis=-1)).astype(v.dtype)
        p_loc = p[..., :kh * kw].reshape(b, h, GRID_W, kh, kw)
        return jnp.einsum('bhqrc,bhrqcd->bhqd', p_loc, vg) + jnp.einsum('bhql,bhld->bhqd', p[..., kh * kw:], vc)

    out = lax.map(row_block, jnp.arange(rows))
    return out.transpose(1, 2, 0, 3, 4).reshape(b, h, t, dh)


def _mla_qkv(q_lat, kv_lat, k_rope, q_norm, kv_norm, w_uq, w_ukv, pos):
    b, t, _ = q_lat.shape
    q = (_rms_norm(q_lat, q_norm) @ w_uq).reshape(b, t, MLA_HEADS, MLA_NOPE + MLA_ROPE).transpose(0, 2, 1, 3)
    kv = (_rms_norm(kv_lat, kv_norm) @ w_ukv).reshape(b, t, MLA_HEADS, MLA_NOPE + MLA_V).transpose(0, 2, 1, 3)
    q_nope, q_rope = q[..., :MLA_NOPE], q[..., MLA_NOPE:]
    k_nope, v = kv[..., :MLA_NOPE], kv[..., MLA_NOPE:]
    if pos is not None:
        q_rope = _rope_2d(q_rope, *pos)
        k_rope = _rope_2d(k_rope, *pos)
    return q_nope, q_rope, k_nope, k_rope, v


def _mla_attend(qn, qr, kn, kr, v):
    s = jnp.einsum('bhqd,bhkd->bhqk', qn, kn) + jnp.einsum('bhqr,bkr->bhqk', qr, kr)
    p = _softmax(s * MLA_SCALE)
    return jnp.einsum('bhqk,bhkd->bhqd', p.astype(v.dtype), v)


def _diag_scan(a_bar, bu, h0, reverse):
    if h0 is not None:
        edge = bu.shape[1] - 1 if reverse else 0
        bu = bu.at[:, edge].add(a_bar * h0)
    a = jnp.broadcast_to(a_bar, bu.shape)

    def combine(e1, e2):
        a1, b1 = e1
        a2, b2 = e2
        return a1 * a2, a2 * b1 + b2

    return lax.associative_scan(combine, (a, bu), axis=1, reverse=reverse)[1]


def _s5_mixer(u, uc, lam_re, lam_im, log_dt, b_re, b_im, c_re, c_im, d_skip, w_glu, ctx_out):
    def groups(z):
        return z.astype(jnp.float32).reshape(z.shape[0], z.shape[1], S5_GROUPS, S5_GROUP_CH).astype(jnp.complex64)

    ug, ucg = groups(u), groups(uc)
    ys, ycs = [], []
    for direction in range(2):
        rev = direction == 1
        lam = lax.complex(lam_re[direction].astype(jnp.float32), lam_im[direction].astype(jnp.float32))
        dt = jnp.exp(log_dt[direction].astype(jnp.float32))[:, None]
        a_bar = jnp.exp(lam * dt)
        b_mat = lax.complex(b_re[direction].astype(jnp.float32), b_im[direction].astype(jnp.float32))
        b_bar = ((a_bar - 1.0) / lam)[..., None] * b_mat
        c_mat = lax.complex(c_re[direction].astype(jnp.float32), c_im[direction].astype(jnp.float32))
        st_c = _diag_scan(a_bar, jnp.einsum('gpc,btgc->btgp', b_bar, ucg), None, rev)
        h0 = st_c[:, 0] if rev else st_c[:, -1]
        st = _diag_scan(a_bar, jnp.einsum('gpc,btgc->btgp', b_bar, ug), h0, rev)
        ys.append(jnp.real(jnp.einsum('gcp,btgp->btgc', c_mat, st)))
        if ctx_out:
            ycs.append(jnp.real(jnp.einsum('gcp,btgp->btgc', c_mat, st_c)))

    def finish(y_dirs, z):
        y = (y_dirs[0] + y_dirs[1]).reshape(z.shape) + d_skip.astype(jnp.float32) * z.astype(jnp.float32)
        val, gate = jnp.split(y.astype(z.dtype) @ w_glu, 2, axis=-1)
        return val * jax.nn.sigmoid(gate)

    return finish(ys, u), (finish(ycs, uc) if ctx_out else None)


def _diff_qkv(zq, zk, zv, pos):
    b, t, _ = zq.shape
    q = zq.reshape(b, t, DIFF_HEADS, 2, DIFF_HD).transpose(0, 2, 3, 1, 4)
    k = zk.reshape(b, t, DIFF_HEADS, 2, DIFF_HD).transpose(0, 2, 3, 1, 4)
    if pos is not None:
        q = _rope_2d(q, *pos)
        k = _rope_2d(k, *pos)
    return q[:, :, 0], q[:, :, 1], k[:, :, 0], k[:, :, 1], _heads(zv, DIFF_HEADS)


def _diff_attend(q1, q2, k1, k2, v, lam):
    p1 = _softmax(jnp.einsum('bhqd,bhkd->bhqk', q1, k1) * DIFF_SCALE)
    p2 = _softmax(jnp.einsum('bhqd,bhkd->bhqk', q2, k2) * DIFF_SCALE)
    return jnp.einsum('bhqk,bhkd->bhqd', (p1 - lam * p2).astype(v.dtype), v)


def _diff_finish(o, subln, lam_init):
    return _merge_heads(_rms_norm(o, subln) * (1.0 - lam_init))


def _gated_merge(h, outs, w_branch, w_gate, b_gate):
    d = h.shape[-1]
    m = None
    for i, o in enumerate(outs):
        gate = jax.nn.sigmoid(h @ w_gate[:, i * d:(i + 1) * d] + b_gate[i * d:(i + 1) * d])
        term = gate * (o @ w_branch[i])
        m = term if m is None else m + term
    return m


def _token_mixing(h, hc, pos, w_in, na_rpb, mla_q_norm, mla_kv_norm, mla_w_uq, mla_w_ukv,
                  s5_lam_re, s5_lam_im, s5_log_dt, s5_b_re, s5_b_im, s5_c_re, s5_c_im, s5_d, s5_w_glu,
                  diff_lam_q1, diff_lam_k1, diff_lam_q2, diff_lam_k2, diff_subln,
                  w_branch, w_gate, b_gate, w_out, lam_init, ctx_out):
    na_q, na_k, na_v, m_q, m_kv, m_kr, s5_u, d_q, d_k, d_v = _split_cols(h @ w_in, IN_SIZES)
    na_qc, na_kc, na_vc, m_qc, m_kvc, m_krc, s5_uc, d_qc, d_kc, d_vc = _split_cols(hc @ w_in, IN_SIZES)
    outs, outs_c = [], []

    kac, vac = _heads(na_kc, NA_HEADS), _heads(na_vc, NA_HEADS)
    o = _neighbourhood_attention(_heads(na_q, NA_HEADS), _heads(na_k, NA_HEADS), _heads(na_v, NA_HEADS), kac, vac, na_rpb)
    outs.append(_merge_heads(o))
    if ctx_out:
        outs_c.append(_merge_heads(_dense_attention(_heads(na_qc, NA_HEADS), kac, vac, NA_SCALE)))

    qn, qr, kn, kr, vb = _mla_qkv(m_q, m_kv, m_kr, mla_q_norm, mla_kv_norm, mla_w_uq, mla_w_ukv, pos)
    qnc, qrc, knc, krc, vbc = _mla_qkv(m_qc, m_kvc, m_krc, mla_q_norm, mla_kv_norm, mla_w_uq, mla_w_ukv, None)
    kn_all = jnp.concatenate([kn, knc], axis=2)
    kr_all = jnp.concatenate([kr, krc], axis=1)
    vb_all = jnp.concatenate([vb, vbc], axis=2)
    o = _sweep_query_blocks(lambda a, r: _mla_attend(a, r, kn_all, kr_all, vb_all), qn, qr)
    outs.append(_merge_heads(o))
    if ctx_out:
        outs_c.append(_merge_heads(_mla_attend(qnc, qrc, knc, krc, vbc)))

    o, oc = _s5_mixer(s5_u, s5_uc, s5_lam_re, s5_lam_im, s5_log_dt, s5_b_re, s5_b_im, s5_c_re, s5_c_im, s5_d, s5_w_glu, ctx_out)
    outs.append(o)
    if ctx_out:
        outs_c.append(oc)

    lam = (jnp.exp(jnp.sum(diff_lam_q1.astype(jnp.float32) * diff_lam_k1.astype(jnp.float32)))
           - jnp.exp(jnp.sum(diff_lam_q2.astype(jnp.float32) * diff_lam_k2.astype(jnp.float32))) + lam_init)
    q1, q2, k1, k2, vd = _diff_qkv(d_q, d_k, d_v, pos)
    q1c, q2c, k1c, k2c, vdc = _diff_qkv(d_qc, d_kc, d_vc, None)
    k1a = jnp.concatenate([k1, k1c], axis=2)
    k2a = jnp.concatenate([k2, k2c], axis=2)
    vda = jnp.concatenate([vd, vdc], axis=2)
    o = _sweep_query_blocks(lambda a, bq: _diff_attend(a, bq, k1a, k2a, vda, lam), q1, q2)
    outs.append(_diff_finish(o, diff_subln, lam_init))
    if ctx_out:
        outs_c.append(_diff_finish(_diff_attend(q1c, q2c, k1c, k2c, vdc, lam), diff_subln, lam_init))

    y = _gated_merge(h, outs, w_branch, w_gate, b_gate) @ w_out
    yc = (_gated_merge(hc, outs_c, w_branch, w_gate, b_gate) @ w_out) if ctx_out else None
    return y, yc


def _swiglu(h, w1, w3, w2):
    return (jax.nn.silu(h @ w1) * (h @ w3)) @ w2


def _moe_swiglu(h, w_router, w1, w3, w2):
    t, d = h.shape
    top_v, top_i = lax.top_k((h @ w_router).astype(jnp.float32), TOP_K)
    gates = jax.nn.softmax(top_v, axis=-1)
    flat_e = top_i.reshape(-1)
    n_assign = t * TOP_K
    order = jnp.argsort(flat_e)
    sorted_e = flat_e[order]
    tok = (order // TOP_K).astype(jnp.int32)
    counts = jnp.bincount(flat_e, length=N_EXPERTS)
    padded = (counts + EXPERT_BLOCK - 1) // EXPERT_BLOCK * EXPERT_BLOCK
    pad_end = jnp.cumsum(padded)
    pad_start = pad_end - padded
    start = jnp.cumsum(counts) - counts
    dest = pad_start[sorted_e] + jnp.arange(n_assign) - start[sorted_e]
    n_blocks = -(-n_assign // EXPERT_BLOCK) + N_EXPERTS
    buf_tok = jnp.full((n_blocks * EXPERT_BLOCK,), t, jnp.int32).at[dest].set(tok)
    block_e = jnp.minimum(jnp.searchsorted(pad_end, jnp.arange(n_blocks) * EXPERT_BLOCK, side='right'), N_EXPERTS - 1)
    hp = jnp.concatenate([h, jnp.zeros((1, d), h.dtype)], axis=0)
    xb = hp[buf_tok].reshape(n_blocks, EXPERT_BLOCK, d)
    yb = lax.map(lambda a: _swiglu(a[0], w1[a[1]], w3[a[1]], w2[a[1]]), (xb, block_e))
    y_assign = yb.reshape(-1, d)[dest] * gates.reshape(-1)[order][:, None].astype(h.dtype)
    return jax.ops.segment_sum(y_assign, tok, num_segments=t)


def _channel_mixer(z, layer, ffn_w1, ffn_w3, ffn_w2, moe_router, moe_w1, moe_w3, moe_w2):
    j = layer // 2
    if layer % 2 == 0:
        return _swiglu(z, ffn_w1[j], ffn_w3[j], ffn_w2[j])
    flat = z.reshape(-1, z.shape[-1])
    return _moe_swiglu(flat, moe_router[j], moe_w1[j], moe_w3[j], moe_w2[j]).reshape(z.shape)


def setup_inputs(seed: int = 0) -> dict:
    key = jax.random.key(seed)
    ks = list(jax.random.split(key, 48))
    f32 = jnp.float32

    def nrm(shape, scale):
        return jax.random.normal(ks.pop(), shape, f32) * scale

    def gain(shape):
        return 1.0 + nrm(shape, 0.02)

    d = D_MODEL
    n_idx = jnp.arange(S5_STATE, dtype=f32)
    return {
        'x': nrm((BATCH, SEQ, d), 1.0),
        'c': nrm((BATCH, d), 1.0),
        'ctx': nrm((BATCH, CTX_LEN, d), 1.0),
        'c_ctx': nrm((d,), 1.0),
        'w_ada': nrm((DEPTH, d, 6 * d), d ** -0.5),
        'b_ada': nrm((DEPTH, 6 * d), 0.02),
        'w_in': nrm((DEPTH, d, IN_COLS), d ** -0.5),
        'na_rpb': nrm((DEPTH, NA_HEADS, 2 * NA_KH - 1, 2 * NA_KW - 1), 0.05),
        'mla_q_norm': gain((DEPTH, MLA_Q_LORA)),
        'mla_kv_norm': gain((DEPTH, MLA_KV_LORA)),
        'mla_w_uq': nrm((DEPTH, MLA_Q_LORA, MLA_HEADS * (MLA_NOPE + MLA_ROPE)), MLA_Q_LORA ** -0.5),
        'mla_w_ukv': nrm((DEPTH, MLA_KV_LORA, MLA_HEADS * (MLA_NOPE + MLA_V)), MLA_KV_LORA ** -0.5),
        's5_lam_re': -0.5 + nrm((DEPTH, 2, S5_GROUPS, S5_STATE), 0.01),
        's5_lam_im': math.pi * n_idx + nrm((DEPTH, 2, S5_GROUPS, S5_STATE), 0.01),
        's5_log_dt': jax.random.uniform(ks.pop(), (DEPTH, 2, S5_GROUPS), f32, math.log(1e-3), math.log(1e-1)),
        's5_b_re': nrm((DEPTH, 2, S5_GROUPS, S5_STATE, S5_GROUP_CH), (2 * S5_GROUP_CH) ** -0.5),
        's5_b_im': nrm((DEPTH, 2, S5_GROUPS, S5_STATE, S5_GROUP_CH), (2 * S5_GROUP_CH) ** -0.5),
        's5_c_re': nrm((DEPTH, 2, S5_GROUPS, S5_GROUP_CH, S5_STATE), S5_STATE ** -0.5),
        's5_c_im': nrm((DEPTH, 2, S5_GROUPS, S5_GROUP_CH, S5_STATE), S5_STATE ** -0.5),
        's5_d': nrm((DEPTH, BRANCH_W), 0.5),
        's5_w_glu': nrm((DEPTH, BRANCH_W, 2 * BRANCH_W), BRANCH_W ** -0.5),
        'diff_lam_q1': nrm((DEPTH, DIFF_HD), 0.1),
        'diff_lam_k1': nrm((DEPTH, DIFF_HD), 0.1),
        'diff_lam_q2': nrm((DEPTH, DIFF_HD), 0.1),
        'diff_lam_k2': nrm((DEPTH, DIFF_HD), 0.1),
        'diff_subln': gain((DEPTH, 2 * DIFF_HD)),
        'w_branch': nrm((DEPTH, N_BRANCH, BRANCH_W, d), BETA * BRANCH_W ** -0.5),
        'w_gate': nrm((DEPTH, d, N_BRANCH * d), d ** -0.5),
        'b_gate': nrm((DEPTH, N_BRANCH * d), 0.02),
        'w_out': nrm((DEPTH, d, d), BETA * d ** -0.5),
        'ln1_g': gain((DEPTH, d)),
        'ln1_b': nrm((DEPTH, d), 0.02),
        'ln2_g': gain((DEPTH, d)),
        'ln2_b': nrm((DEPTH, d), 0.02),
        'ffn_w1': nrm((N_DENSE, d, D_FF), d ** -0.5),
        'ffn_w3': nrm((N_DENSE, d, D_FF), d ** -0.5),
        'ffn_w2': nrm((N_DENSE, D_FF, d), BETA * D_FF ** -0.5),
        'moe_router': nrm((N_MOE, d, N_EXPERTS), d ** -0.5),
        'moe_w1': nrm((N_MOE, N_EXPERTS, d, D_FF), d ** -0.5),
        'moe_w3': nrm((N_MOE, N_EXPERTS, d, D_FF), d ** -0.5),
        'moe_w2': nrm((N_MOE, N_EXPERTS, D_FF, d), BETA * D_FF ** -0.5),
    }


def reference(x, c, ctx, c_ctx, w_ada, b_ada, w_in, na_rpb, mla_q_norm, mla_kv_norm, mla_w_uq, mla_w_ukv,
              s5_lam_re, s5_lam_im, s5_log_dt, s5_b_re, s5_b_im, s5_c_re, s5_c_im, s5_d, s5_w_glu,
              diff_lam_q1, diff_lam_k1, diff_lam_q2, diff_lam_k2, diff_subln,
              w_branch, w_gate, b_gate, w_out, ln1_g, ln1_b, ln2_g, ln2_b,
              ffn_w1, ffn_w3, ffn_w2, moe_router, moe_w1, moe_w3, moe_w2):
    b, s, d = x.shape
    t = jnp.arange(s)
    pos = (t // GRID_W, t % GRID_W)
    cond = jax.nn.silu(c)
    cond_ctx = jax.nn.silu(c_ctx)[None, :]
    xc = ctx
    for layer in range(DEPTH):
        ctx_out = layer < DEPTH - 1
        lam_init = 0.8 - 0.6 * math.exp(-0.3 * layer)
        mod = jnp.split((cond @ w_ada[layer] + b_ada[layer])[:, None, :], 6, axis=-1)
        mod_c = jnp.split((cond_ctx @ w_ada[layer] + b_ada[layer])[:, None, :], 6, axis=-1)
        y, yc = _token_mixing(x * (1.0 + mod[1]) + mod[0], xc * (1.0 + mod_c[1]) + mod_c[0], pos,
                              w_in[layer], na_rpb[layer], mla_q_norm[layer], mla_kv_norm[layer],
                              mla_w_uq[layer], mla_w_ukv[layer],
                              s5_lam_re[layer], s5_lam_im[layer], s5_log_dt[layer], s5_b_re[layer], s5_b_im[layer],
                              s5_c_re[layer], s5_c_im[layer], s5_d[layer], s5_w_glu[layer],
                              diff_lam_q1[layer], diff_lam_k1[layer], diff_lam_q2[layer], diff_lam_k2[layer],
                              diff_subln[layer], w_branch[layer], w_gate[layer], b_gate[layer], w_out[layer],
                              lam_init, ctx_out)
        x = _layer_norm(ALPHA * x + mod[2] * y, ln1_g[layer], ln1_b[layer])
        f = _channel_mixer(x * (1.0 + mod[4]) + mod[3], layer, ffn_w1, ffn_w3, ffn_w2, moe_router, moe_w1, moe_w3, moe_w2)
        x = _layer_norm(ALPHA * x + mod[5] * f, ln2_g[layer], ln2_b[layer])
        if ctx_out:
            xc = _layer_norm(ALPHA * xc + mod_c[2] * yc, ln1_g[layer], ln1_b[layer])
            fc = _channel_mixer(xc * (1.0 + mod_c[4]) + mod_c[3], layer, ffn_w1, ffn_w3, ffn_w2, moe_router, moe_w1, moe_w3, moe_w2)
            xc = _layer_norm(ALPHA * xc + mod_c[5] * fc, ln2_g[layer], ln2_b[layer])
    return x
```

```python
import math
import contextlib
import numpy as np
import ml_dtypes
import concourse.bass as bass
import concourse.mybir as mybir
from concourse.bass_utils import run_bass_kernel_spmd

F32 = mybir.dt.float32
BF16 = mybir.dt.bfloat16
ALU = mybir.AluOpType
AF = mybir.ActivationFunctionType
AX = mybir.AxisListType

D = 1024
SEQ = 4096
CTX = 256
T = SEQ + CTX
NT = T // 128
NLT = SEQ // 128
DEPTH = 4
IN_COLS = 2208
D_FF = 3584
NEXP = 8
ALPHA = (2 * DEPTH) ** 0.25
LN_EPS = 1e-5
RMS_EPS = 1e-6
NA_SCALE = 64 ** -0.5
MLA_SCALE = 96 ** -0.5
DIFF_SCALE = 32 ** -0.5
C_NAQ, C_NAK, C_S5U, C_MQ, C_MK, C_DQ, C_DK = 0, 2, 4, 6, 10, 14, 18
NFM = 22


class KB:
    def __init__(self, nc, es):
        self.nc = nc
        self.es = es
        self.eng = {'pe': nc.tensor, 'act': nc.scalar, 'dve': nc.vector, 'pool': nc.gpsimd, 'sp': nc.sync}
        self.sem = {}
        self.cnt = {}
        for e in ('pe', 'act', 'dve', 'pool'):
            self.sem[e] = es.enter_context(nc.semaphore('sem_' + e))
            self.cnt[e] = 0
        self.KD = 8
        self.dsem = {}
        self.dcnt = {}
        for q in ('sp', 'pool'):
            self.dsem[q] = [es.enter_context(nc.semaphore('dsem_%s%d' % (q, i))) for i in range(self.KD)]
            self.dcnt[q] = 0
        self.waited = {e: {} for e in self.eng}
        self.semobj = {}
        for e in self.sem:
            self.semobj[e] = self.sem[e]
        for q in self.dsem:
            for i, s in enumerate(self.dsem[q]):
                self.semobj[(q, i)] = s

    def _wait(self, e, tok):
        key, val = tok
        if self.waited[e].get(key, 0) >= val:
            return
        self.eng[e].wait_ge(self.semobj[key], val)
        self.waited[e][key] = val

    def _deps(self, e, reads, writes):
        deps = {}

        def add(tok):
            if tok is None:
                return
            k, v = tok
            if e == 'pe' and k == 'pe':
                return
            if deps.get(k, 0) < v:
                deps[k] = v
        for t in reads:
            add(t.w)
        for t in writes:
            add(t.w)
            for k, v in t.r.items():
                add((k, v))
        for k, v in deps.items():
            self._wait(e, (k, v))

    def _mark(self, tok, reads, writes):
        k, v = tok
        for t in reads:
            if t.r.get(k, 0) < v:
                t.r[k] = v
        for t in writes:
            t.w = tok
            t.r = {}

    def op(self, e, fn, reads=(), writes=()):
        self._deps(e, reads, writes)
        inst = fn(self.eng[e])
        self.cnt[e] += 1
        inst.then_inc(self.sem[e], 1)
        self._mark((e, self.cnt[e]), reads, writes)

    def dma(self, q, out, in_, reads=(), writes=()):
        self._deps(q, reads, writes)
        n = self.dcnt[q]
        k = n % self.KD
        gen = n // self.KD + 1
        if gen > 1:
            self._wait(q, ((q, k), 16 * (gen - 1)))
        self.eng[q].dma_start(out=out, in_=in_).then_inc(self.dsem[q][k], 16)
        self.dcnt[q] += 1
        self._mark(((q, k), 16 * gen), reads, writes)

    def barrier(self):
        toks = [(e, self.cnt[e]) for e in self.cnt if self.cnt[e] > 0]
        for q in self.dsem:
            n = self.dcnt[q]
            for k in range(self.KD):
                uses = (n - k + self.KD - 1) // self.KD if n > k else 0
                if uses > 0:
                    toks.append(((q, k), 16 * uses))
        for e in self.eng:
            for tok in toks:
                self._wait(e, tok)


class Tl:
    def __init__(self, t):
        self.t = t
        self.w = None
        self.r = {}

    def __getitem__(self, key):
        return self.t[key]


def build_program(nlayers=DEPTH, debug=None):
    nc = bass.Bass("TRN2", target_bir_lowering=False)
    es0 = contextlib.ExitStack()

    def din(name, shape, dt=F32):
        return nc.dram_tensor(name, list(shape), dt, kind="ExternalInput").ap()

    def dscr(name, shape, dt):
        return nc.dram_tensor(name, list(shape), dt, kind="Internal").ap()

    I = {}
    I['x'] = din('x', [SEQ, D]); I['ctx'] = din('ctx', [CTX, D]); I['c'] = din('c', [1, D]); I['c_ctx'] = din('c_ctx', [1, D])
    I['w_ada'] = din('w_ada', [DEPTH, D, 6 * D]); I['b_ada'] = din('b_ada', [DEPTH, 6 * D])
    I['w_in'] = din('w_in', [DEPTH, D, IN_COLS])
    I['rpbT'] = din('rpbT', [DEPTH, 4, 15, 64, 64])
    I['mla_q_norm'] = din('mla_q_norm', [DEPTH, 256]); I['mla_kv_norm'] = din('mla_kv_norm', [DEPTH, 128])
    I['mla_w_uq'] = din('mla_w_uq', [DEPTH, 256, 384]); I['mla_w_ukv'] = din('mla_w_ukv', [DEPTH, 128, 512])
    for nm in ('s5_lam_re', 's5_lam_im'):
        I[nm] = din(nm, [DEPTH, 2, 16, 64])
    I['s5_log_dt'] = din('s5_log_dt', [DEPTH, 2, 16])
    for nm in ('s5_b_re', 's5_b_im'):
        I[nm] = din(nm, [DEPTH, 2, 16, 64, 16])
    for nm in ('s5_c_re', 's5_c_im'):
        I[nm] = din(nm, [DEPTH, 2, 16, 16, 64])
    I['s5_d'] = din('s5_d', [DEPTH, 256]); I['s5_w_glu'] = din('s5_w_glu', [DEPTH, 256, 512])
    for nm in ('diff_lam_q1', 'diff_lam_k1', 'diff_lam_q2', 'diff_lam_k2'):
        I[nm] = din(nm, [DEPTH, 32])
    I['diff_subln'] = din('diff_subln', [DEPTH, 64])
    I['w_branch'] = din('w_branch', [DEPTH, 4, 256, D]); I['w_gate'] = din('w_gate', [DEPTH, D, 4 * D]); I['b_gate'] = din('b_gate', [DEPTH, 4 * D])
    I['w_out'] = din('w_out', [DEPTH, D, D])
    for nm in ('ln1_g', 'ln1_b', 'ln2_g', 'ln2_b'):
        I[nm] = din(nm, [DEPTH, D])
    for nm in ('ffn_w1', 'ffn_w3'):
        I[nm] = din(nm, [2, D, D_FF])
    I['ffn_w2'] = din('ffn_w2', [2, D_FF, D])
    I['moe_router'] = din('moe_router', [2, D, NEXP])
    for nm in ('moe_w1', 'moe_w3'):
        I[nm] = din(nm, [2, NEXP, D, D_FF])
    I['moe_w2'] = din('moe_w2', [2, NEXP, D_FF, D])
    I['ident'] = din('ident', [128, 128]); I['ropec'] = din('ropec', [128, NT, 16]); I['ropes'] = din('ropes', [128, NT, 16])
    I['iota128'] = din('iota128', [1, 128]); I['iotaT'] = din('iotaT', [2, T]); I['cv'] = din('cv', [2, NT]); I['maskC'] = din('maskC', [4, 128, 128]); I['maskB'] = din('maskB', [4, 128, 128])

    OUT = nc.dram_tensor('out', [SEQ, D], F32, kind="ExternalOutput").ap()
    dbg_out = {}

    S = {}
    S['xcur'] = dscr('xcur', [NT, 128, D], F32)
    S['x1'] = dscr('x1s', [NT, 128, D], F32)
    S['hT'] = dscr('hTs', [NT, 128, 8, 128], BF16)
    S['h2T'] = dscr('h2Ts', [128, 8, T], BF16)
    S['FM'] = dscr('FMs', [NFM, 128, T], BF16)
    S['VV'] = dscr('VVs', [NT, 128, 12, 128], BF16)
    S['oT'] = dscr('oTs', [4, 2, 128, T], BF16)
    S['mod'] = dscr('mods', [2, 6 * D], F32)
    D_ = {k: Tl(v) for k, v in S.items()}

    with es0:
        kb = KB(nc, es0)
        ucnt = [0]

        def sb(es, name, shape, dt):
            ucnt[0] += 1
            return Tl(es.enter_context(nc.sbuf_tensor('%s_u%d' % (name, ucnt[0]), list(shape), dt)))

        def ps(es, name, shape, dt):
            return Tl(es.enter_context(nc.psum_tensor(name, list(shape), dt)))

        ident_f = sb(es0, 'ident_f', [128, 128], F32)
        ident = sb(es0, 'ident_b', [128, 128], BF16)
        ones_b = sb(es0, 'ones_b', [128, 128], BF16)
        condT = sb(es0, 'condT', [128, 8, 2], BF16)
        ctmp = sb(es0, 'ctmp', [128, 8, 2], F32)
        kb.dma('sp', ident_f[:], I['ident'][:], writes=[ident_f])
        kb.op('dve', lambda e: e.tensor_copy(out=ident[:], in_=ident_f[:]), reads=[ident_f], writes=[ident])
        kb.op('dve', lambda e: e.memset(ones_b[:], 1.0), writes=[ones_b])
        with nc.allow_non_contiguous_dma(reason="tiny cond vector"):
            kb.dma('sp', ctmp[:, :, 0], I['c'][0].rearrange("(k p) -> p k", p=128), writes=[ctmp])
            kb.dma('sp', ctmp[:, :, 1], I['c_ctx'][0].rearrange("(k p) -> p k", p=128), writes=[ctmp])
        kb.op('act', lambda e: e.activation(out=condT[:], in_=ctmp[:], func=AF.Silu), reads=[ctmp], writes=[condT])

        pbig = [ps(es0, 'pbig%d' % i, [128, 1024], F32) for i in range(2)]
        pbank = [ps(es0, 'pb%d' % i, [128, 512], F32) for i in range(4)]
        pbank += [Tl(pbig[0].t[:, 0:512]), Tl(pbig[0].t[:, 512:1024])]
        ptr = [Tl(pbig[1].t[:, 0:512].bitcast(BF16)), Tl(pbig[1].t[:, 512:1024].bitcast(BF16))]

        for layer in range(nlayers):
            ctx_out = layer < DEPTH - 1
            lam_init = 0.8 - 0.6 * math.exp(-0.3 * layer)
            with contextlib.ExitStack() as es:
                wst = [sb(es, 'wada%d' % i, [128, 8, 512], BF16) for i in range(2)]
                modsb = sb(es, 'modsb', [2, 6 * D], F32)
                bada = sb(es, 'bada', [2, 6 * D], F32)
                kb.dma('sp', bada[:], I['b_ada'][layer:layer + 1, :].partition_broadcast(2) if False else I['b_ada'][layer:layer + 1, :].to_broadcast([2, 6 * D]), writes=[bada])
                for cc in range(12):
                    w = wst[cc % 2]
                    kb.dma('pool', w[:], I['w_ada'][layer, :, cc * 512:(cc + 1) * 512].rearrange("(k p) n -> p k n", p=128), writes=[w])
                    pb = pbank[cc % 2]
                    for kc in range(8):
                        kb.op('pe', lambda e, kc=kc, w=w, pb=pb: e.matmul(pb[0:2, :], lhsT=condT[:, kc, :], rhs=w[:, kc, :], start=(kc == 0), stop=(kc == 7)),
                              reads=[condT, w], writes=[pb])
                    kb.op('dve', lambda e, cc=cc, pb=pb: e.tensor_tensor(out=modsb[:, cc * 512:(cc + 1) * 512], in0=pb[0:2, :], in1=bada[:, cc * 512:(cc + 1) * 512], op=ALU.add),
                          reads=[pb, bada], writes=[modsb])
                kb.dma('sp', S['mod'][:], modsb[:], reads=[modsb], writes=[D_['mod']])
                if debug == 'M':
                    dbg_out['mod'] = nc.dram_tensor('dbg_mod', [2, 6 * D], F32, kind="ExternalOutput").ap()
                    kb.dma('sp', dbg_out['mod'][:], modsb[:], reads=[modsb])
                kb.barrier()
            if debug == 'M':
                break
            with contextlib.ExitStack() as es:
                win = sb(es, 'win', [128, 8, IN_COLS], BF16)
                wuq = sb(es, 'wuq', [128, 2, 384], BF16)
                wukv = sb(es, 'wukv', [128, 512], BF16)
                kb.dma('pool', win[:], I['w_in'][layer].rearrange("(k p) n -> p k n", p=128), writes=[win])
                kb.dma('pool', wuq[:], I['mla_w_uq'][layer].rearrange("(k p) n -> p k n", p=128), writes=[wuq])
                kb.dma('pool', wukv[:], I['mla_w_ukv'][layer], writes=[wukv])
                modb = sb(es, 'modbA', [128, 2, 2, D], F32)
                for kind in range(2):
                    for j in range(2):
                        kb.dma('sp', modb[:, kind, j, :], S['mod'][kind:kind + 1, j * D:(j + 1) * D].to_broadcast([128, D]), reads=[D_['mod']], writes=[modb])
                kb.op('dve', lambda e: e.tensor_scalar_add(out=modb[:, :, 1, :], in0=modb[:, :, 1, :], scalar1=1.0), reads=[modb], writes=[modb])
                qg = sb(es, 'qg', [128, 256], F32); kvg = sb(es, 'kvg', [128, 128], F32)
                kb.dma('sp', qg[:], I['mla_q_norm'][layer:layer + 1, :].to_broadcast([128, 256]), writes=[qg])
                kb.dma('sp', kvg[:], I['mla_kv_norm'][layer:layer + 1, :].to_broadcast([128, 128]), writes=[kvg])
                rc = sb(es, 'rc', [128, NT, 16], F32); rs = sb(es, 'rs', [128, NT, 16], F32)
                kb.dma('sp', rc[:], I['ropec'][:], writes=[rc]); kb.dma('sp', rs[:], I['ropes'][:], writes=[rs])
                epsq = sb(es, 'epsq', [128, 1], F32)
                kb.op('dve', lambda e: e.memset(epsq[:], RMS_EPS), writes=[epsq])
                xt = [sb(es, 'xt%d' % i, [128, D], F32) for i in range(2)]
                htmp = sb(es, 'htmp', [128, D], F32)
                hb = sb(es, 'hb', [128, D], BF16)
                hTt = [sb(es, 'hTt%d' % i, [128, 8, 128], BF16) for i in range(2)]
                z = sb(es, 'z', [128, IN_COLS], F32)
                zb = sb(es, 'zb', [128, IN_COLS], BF16)
                fm = [sb(es, 'fm%d' % i, [128, NFM, 256], BF16) for i in range(2)]
                vv = [sb(es, 'vv%d' % i, [128, 12, 128], BF16) for i in range(2)]
                for i in range(2):
                    kb.op('pool', lambda e, i=i: e.memset(vv[i][:], 1.0), writes=[vv[i]])
                    kb.op('pool', lambda e, i=i: e.memset(fm[i][:], 0.0), writes=[fm[i]])
                ss = sb(es, 'ss', [128, 4], F32)
                junk = sb(es, 'junk', [128, 256], F32)
                qn = sb(es, 'qn', [128, 384], BF16)
                qnT = sb(es, 'qnT', [128, 3, 128], BF16)
                qf = sb(es, 'qf', [128, 384], F32)
                kvf = sb(es, 'kvf', [128, 512], F32)
                Qb = sb(es, 'Qb', [128, 4, 96], BF16)
                Kb = sb(es, 'Kb', [128, 4, 96], BF16)
                krr = sb(es, 'krr', [128, 32], F32)
                dqk = sb(es, 'dqk', [128, 512], BF16)
                rt = [sb(es, 'rt%d' % i, [128, 256], F32) for i in range(4)]
                ptoggle = [0]

                def rope(src_ap, dst_ap, G, ti, reads, writes):
                    sv = src_ap.rearrange("p (g h two f) -> p g h two f", g=G, h=2, two=2, f=8)
                    dv = dst_ap.rearrange("p (g h two f) -> p g h two f", g=G, h=2, two=2, f=8)
                    cb = rc[:, ti, :].rearrange("p (h f) -> p h f", h=2).unsqueeze(1).to_broadcast([128, G, 2, 8])
                    sn = rs[:, ti, :].rearrange("p (h f) -> p h f", h=2).unsqueeze(1).to_broadcast([128, G, 2, 8])
                    tv = [r_[:, 0:G * 16].rearrange("p (g h f) -> p g h f", g=G, h=2, f=8) for r_ in rt]
                    z1 = sv[:, :, :, 0, :]; z2 = sv[:, :, :, 1, :]
                    kb.op('dve', lambda e: e.tensor_tensor(out=tv[0], in0=z1, in1=cb, op=ALU.mult), reads=reads + [rc], writes=[rt[0]])
                    kb.op('dve', lambda e: e.tensor_tensor(out=tv[1], in0=z2, in1=sn, op=ALU.mult), reads=reads + [rs], writes=[rt[1]])
                    kb.op('dve', lambda e: e.tensor_tensor(out=tv[2], in0=z1, in1=sn, op=ALU.mult), reads=reads + [rs], writes=[rt[2]])
                    kb.op('dve', lambda e: e.tensor_tensor(out=tv[3], in0=z2, in1=cb, op=ALU.mult), reads=reads + [rc], writes=[rt[3]])
                    kb.op('dve', lambda e: e.tensor_tensor(out=dv[:, :, :, 0, :], in0=tv[0], in1=tv[1], op=ALU.subtract), reads=[rt[0], rt[1]], writes=writes)
                    kb.op('dve', lambda e: e.tensor_tensor(out=dv[:, :, :, 1, :], in0=tv[2], in1=tv[3], op=ALU.add), reads=[rt[2], rt[3]], writes=writes)

                def transposes(items, fmt, j):
                    for b0 in range(0, len(items), 8):
                        batch = items[b0:b0 + 8]
                        pt = ptr[ptoggle[0] % 2]; ptoggle[0] += 1
                        for i, (st, sap, n, ch) in enumerate(batch):
                            kb.op('pe', lambda e, i=i, sap=sap, n=n, pt=pt: e.transpose(out=pt[0:n, i * 128:(i + 1) * 128], in_=sap, identity=ident[:]),
                                  reads=[st, ident], writes=[pt])
                        for i, (st, sap, n, ch) in enumerate(batch):
                            eng = 'act' if (i % 2 == 0) else 'pool_'
                            if eng == 'act':
                                kb.op('act', lambda e, i=i, n=n, ch=ch, pt=pt: e.copy(out=fmt[0:n, ch, j * 128:(j + 1) * 128], in_=pt[0:n, i * 128:(i + 1) * 128]), reads=[pt], writes=[fmt])
                            else:
                                kb.op('dve', lambda e, i=i, n=n, ch=ch, pt=pt: e.tensor_copy(out=fmt[0:n, ch, j * 128:(j + 1) * 128], in_=pt[0:n, i * 128:(i + 1) * 128]), reads=[pt], writes=[fmt])

                for ti in range(NT):
                    kind = 0 if ti < NLT else 1
                    g, j = ti // 2, ti % 2
                    fmt = fm[g % 2]; vt = vv[ti % 2]; x_t = xt[ti % 2]; hT_t = hTt[ti % 2]
                    if layer == 0:
                        src = I['x'][ti * 128:(ti + 1) * 128, :] if kind == 0 else I['ctx'][(ti - NLT) * 128:(ti - NLT + 1) * 128, :]
                        kb.dma('sp', x_t[:], src, writes=[x_t])
                    else:
                        kb.dma('sp', x_t[:], S['xcur'][ti], reads=[D_['xcur']], writes=[x_t])
                    kb.op('dve', lambda e: e.tensor_tensor(out=htmp[:], in0=x_t[:], in1=modb[:, kind, 1, :], op=ALU.mult), reads=[x_t, modb], writes=[htmp])
                    kb.op('dve', lambda e: e.tensor_tensor(out=hb[:], in0=htmp[:], in1=modb[:, kind, 0, :], op=ALU.add), reads=[htmp, modb], writes=[hb])
                    pt = ptr[ptoggle[0] % 2]; ptoggle[0] += 1
                    for kc in range(8):
                        kb.op('pe', lambda e, kc=kc, pt=pt: e.transpose(out=pt[:, kc * 128:(kc + 1) * 128], in_=hb[:, kc * 128:(kc + 1) * 128], identity=ident[:]), reads=[hb, ident], writes=[pt])
                    kb.op('act', lambda e, pt=pt: e.copy(out=hT_t[:].rearrange("p k t -> p (k t)"), in_=pt[:]), reads=[pt], writes=[hT_t])
                    kb.dma('sp', S['hT'][ti], hT_t[:], reads=[hT_t], writes=[D_['hT']])
                    for cg in range(5):
                        c0 = cg * 512; n = min(512, IN_COLS - c0)
                        pb = pbank[cg % 4]
                        for kc in range(8):
                            kb.op('pe', lambda e, kc=kc, pb=pb, c0=c0, n=n: e.matmul(pb[:, 0:n], lhsT=hT_t[:, kc, :], rhs=win[:, kc, c0:c0 + n], start=(kc == 0), stop=(kc == 7)),
                                  reads=[hT_t, win], writes=[pb])
                        kb.op('act', lambda e, pb=pb, c0=c0, n=n: e.copy(out=z[:, c0:c0 + n], in_=pb[:, 0:n]), reads=[pb], writes=[z])
                    kb.op('pool', lambda e: e.tensor_copy(out=zb[:], in_=z[:]), reads=[z], writes=[zb])
                    kb.op('pool', lambda e: e.tensor_copy(out=vt[:, 0:4, 0:64], in_=z[:, 512:768].rearrange("p (h d) -> p h d", h=4)), reads=[z], writes=[vt])
                    kb.op('pool', lambda e: e.tensor_copy(out=vt[:, 8:12, 0:64], in_=z[:, 1952:2208].rearrange("p (h d) -> p h d", h=4)), reads=[z], writes=[vt])
                    kb.op('act', lambda e: e.activation(out=junk[:, 0:256], in_=z[:, 768:1024], func=AF.Square, accum_out=ss[:, 0:1]), reads=[z], writes=[junk, ss])
                    kb.op('act', lambda e: e.activation(out=junk[:, 0:128], in_=z[:, 1024:1152], func=AF.Square, accum_out=ss[:, 1:2]), reads=[z], writes=[junk, ss])
                    kb.op('act', lambda e: e.activation(out=ss[:, 2:3], in_=ss[:, 0:1], func=AF.Sqrt, scale=1.0 / 256, bias=epsq[:, 0:1]), reads=[ss, epsq], writes=[ss])
                    kb.op('act', lambda e: e.activation(out=ss[:, 3:4], in_=ss[:, 1:2], func=AF.Sqrt, scale=1.0 / 128, bias=epsq[:, 0:1]), reads=[ss, epsq], writes=[ss])
                    kb.op('dve', lambda e: e.reciprocal(out=ss[:, 2:4], in_=ss[:, 2:4]), reads=[ss], writes=[ss])
                    kb.op('dve', lambda e: e.scalar_tensor_tensor(out=qn[:, 0:256], in0=z[:, 768:1024], scalar=ss[:, 2:3], in1=qg[:], op0=ALU.mult, op1=ALU.mult), reads=[z, ss, qg], writes=[qn])
                    kb.op('dve', lambda e: e.scalar_tensor_tensor(out=qn[:, 256:384], in0=z[:, 1024:1152], scalar=ss[:, 3:4], in1=kvg[:], op0=ALU.mult, op1=ALU.mult), reads=[z, ss, kvg], writes=[qn])
                    pt = ptr[ptoggle[0] % 2]; ptoggle[0] += 1
                    for c3 in range(3):
                        kb.op('pe', lambda e, c3=c3, pt=pt: e.transpose(out=pt[:, c3 * 128:(c3 + 1) * 128], in_=qn[:, c3 * 128:(c3 + 1) * 128], identity=ident[:]), reads=[qn, ident], writes=[pt])
                    kb.op('act', lambda e, pt=pt: e.copy(out=qnT[:].rearrange("p k t -> p (k t)"), in_=pt[:, 0:384]), reads=[pt], writes=[qnT])
                    pq = pbank[4]; pk = pbank[5]
                    for c2 in range(2):
                        kb.op('pe', lambda e, c2=c2: e.matmul(pq[:, 0:384], lhsT=qnT[:, c2, :], rhs=wuq[:, c2, :], start=(c2 == 0), stop=(c2 == 1)), reads=[qnT, wuq], writes=[pq])
                    kb.op('pe', lambda e: e.matmul(pk[:, :], lhsT=qnT[:, 2, :], rhs=wukv[:], start=True, stop=True), reads=[qnT, wukv], writes=[pk])
                    kb.op('act', lambda e: e.copy(out=qf[:], in_=pq[:, 0:384]), reads=[pq], writes=[qf])
                    kb.op('act', lambda e: e.copy(out=kvf[:], in_=pk[:]), reads=[pk], writes=[kvf])
                    qf3 = qf[:].rearrange("p (h d) -> p h d", h=4); kv3 = kvf[:].rearrange("p (h d) -> p h d", h=4)
                    kb.op('pool', lambda e: e.tensor_copy(out=Qb[:, :, 0:64], in_=qf3[:, :, 0:64]), reads=[qf], writes=[Qb])
                    kb.op('pool', lambda e: e.tensor_copy(out=Kb[:, :, 0:64], in_=kv3[:, :, 0:64]), reads=[kvf], writes=[Kb])
                    kb.op('pool', lambda e: e.tensor_copy(out=vt[:, 4:8, 0:64], in_=kv3[:, :, 64:128]), reads=[kvf], writes=[vt])
                    for h in range(4):
                        rope(qf[:, h * 96 + 64:h * 96 + 96], Qb[:, h, 64:96], 1, ti, [qf], [Qb])
                    rope(z[:, 1152:1184], krr[:, :], 1, ti, [z], [krr])
                    kb.op('pool', lambda e: e.tensor_copy(out=Kb[:, :, 64:96], in_=krr[:].unsqueeze(1).to_broadcast([128, 4, 32])), reads=[krr], writes=[Kb])
                    rope(z[:, 1440:1696], dqk[:, 0:256], 8, ti, [z], [dqk])
                    rope(z[:, 1696:1952], dqk[:, 256:512], 8, ti, [z], [dqk])
                    items = []
                    for c2 in range(2):
                        items.append((zb, zb[:, c2 * 128:(c2 + 1) * 128], 128, C_NAQ + c2))
                        items.append((zb, zb[:, 256 + c2 * 128:256 + (c2 + 1) * 128], 128, C_NAK + c2))
                        items.append((zb, zb[:, 1184 + c2 * 128:1184 + (c2 + 1) * 128], 128, C_S5U + c2))
                    for h in range(4):
                        items.append((Qb, Qb[:, h, :], 96, C_MQ + h))
                        items.append((Kb, Kb[:, h, :], 96, C_MK + h))
                        items.append((dqk, dqk[:, h * 64:(h + 1) * 64], 64, C_DQ + h))
                        items.append((dqk, dqk[:, 256 + h * 64:256 + (h + 1) * 64], 64, C_DK + h))
                    transposes(items, fmt, j)
                    kb.dma('sp', S['VV'][ti], vt[:], reads=[vt], writes=[D_['VV']])
                    if j == 1:
                        kb.dma('sp', S['FM'][:, :, g * 256:(g + 1) * 256].rearrange("c p t -> p c t"), fmt[:], reads=[fmt], writes=[D_['FM']])
                if debug == 'A':
                    kb.barrier()
                    for nm, shp, dt in (('FM', [NFM, 128, T], BF16), ('VV', [NT, 128, 12, 128], BF16), ('hT', [NT, 128, 8, 128], BF16)):
                        dbg_out[nm] = nc.dram_tensor('dbg_' + nm, shp, dt, kind="ExternalOutput").ap()
                        kb.dma('sp', dbg_out[nm], S[nm], reads=[D_[nm]])
                kb.barrier()
            if debug == 'A':
                break
            with contextlib.ExitStack() as es:
                KT = sb(es, 'KT', [128, T], BF16); QT = sb(es, 'QT', [128, T], BF16)
                V = sb(es, 'Vt', [128, NT, 128], BF16)
                rd = sb(es, 'rd', [128, 512], F32)
                onb = sb(es, 'onb', [128, 512], BF16)
                o1 = sb(es, 'o1', [128, 512], F32); o2 = sb(es, 'o2', [128, 512], F32); osq = sb(es, 'osq', [128, 512], BF16)
                rs2 = sb(es, 'rs2', [128, 512], F32)
                Wt = sb(es, 'Wt', [128, 4, 8, 512], BF16)
                Grev = sb(es, 'Grev', [128, 4, 15, 64], BF16)
                graw = sb(es, 'graw', [128, 4, 15, 64], F32)
                lamv = sb(es, 'lamv', [128, 4, 32], F32); lamt = sb(es, 'lamt', [128, 8], F32)
                gsub = sb(es, 'gsub', [128, 1], F32); epsd = sb(es, 'epsd', [128, 1], F32)
                ecnt = [0]
                for i4, nm in enumerate(('diff_lam_q1', 'diff_lam_k1', 'diff_lam_q2', 'diff_lam_k2')):
                    kb.dma('sp', lamv[:, i4, :], I[nm][layer:layer + 1, :].to_broadcast([128, 32]), writes=[lamv])
                kb.op('dve', lambda e: e.tensor_tensor(out=lamv[:, 0, :], in0=lamv[:, 0, :], in1=lamv[:, 1, :], op=ALU.mult), reads=[lamv], writes=[lamv])
                kb.op('dve', lambda e: e.tensor_tensor(out=lamv[:, 2, :], in0=lamv[:, 2, :], in1=lamv[:, 3, :], op=ALU.mult), reads=[lamv], writes=[lamv])
                kb.op('dve', lambda e: e.reduce_sum(out=lamt[:, 0:1], in_=lamv[:, 0, :], axis=AX.X), reads=[lamv], writes=[lamt])
                kb.op('dve', lambda e: e.reduce_sum(out=lamt[:, 1:2], in_=lamv[:, 2, :], axis=AX.X), reads=[lamv], writes=[lamt])
                kb.op('act', lambda e: e.activation(out=lamt[:, 2:4], in_=lamt[:, 0:2], func=AF.Exp), reads=[lamt], writes=[lamt])
                kb.op('dve', lambda e: e.tensor_tensor(out=lamt[:, 4:5], in0=lamt[:, 3:4], in1=lamt[:, 2:3], op=ALU.subtract), reads=[lamt], writes=[lamt])
                kb.op('dve', lambda e: e.tensor_scalar_add(out=lamt[:, 5:6], in0=lamt[:, 4:5], scalar1=-lam_init), reads=[lamt], writes=[lamt])
                with nc.allow_non_contiguous_dma(reason="tiny"):
                    kb.dma('sp', gsub[0:64, :], I['diff_subln'][layer].rearrange("(p o) -> p o", o=1), writes=[gsub])
                kb.op('dve', lambda e: e.tensor_scalar_mul(out=gsub[0:64, :], in0=gsub[0:64, :], scalar1=(1.0 - lam_init)), reads=[gsub], writes=[gsub])
                kb.op('dve', lambda e: e.memset(epsd[:], RMS_EPS), writes=[epsd])
                for half in range(2):
                    kb.dma('sp', graw[half * 64:(half + 1) * 64], I['rpbT'][layer].rearrange("h r k q -> k h r q"), writes=[graw])
                for m in range(15):
                    kb.op('act', lambda e, m=m: e.activation(out=Grev[:, :, m, :], in_=graw[:, :, 14 - m, :], func=AF.Exp), reads=[graw], writes=[Grev])

                def build_W(jq):
                    R0 = 8 * jq; KR0 = min(max(R0 - 4, 0), 48)
                    kb.op('pool', lambda e: e.memset(Wt[:], 0.0), writes=[Wt])
                    for i in range(8):
                        for half in range(2):
                            kr = KR0 + 2 * i + half
                            al = []
                            for a in range(8):
                                r0 = min(max(R0 + a - 4, 0), 56)
                                if r0 <= kr <= r0 + 7:
                                    al.append(a)
                            if not al:
                                continue
                            a0, a1 = al[0], al[-1] + 1
                            m0 = (R0 + a0) - kr + 7
                            kb.op('pool', lambda e, i=i, half=half, a0=a0, a1=a1, m0=m0: e.tensor_copy(
                                out=Wt[half * 64:(half + 1) * 64, :, i, a0 * 64:a1 * 64].rearrange("p h (a q) -> p h a q", q=64),
                                in_=Grev[half * 64:(half + 1) * 64, :, m0:m0 + (a1 - a0), :]), reads=[Grev], writes=[Wt])
                    return KR0 // 2

                E2 = [sb(es, 'E2_%d' % i, [128, 1024], BF16) for i in range(3)]

                def attn_block(kts, kr0, kd, q0, nq, scale, pso, wfn=None):
                    pairs = [kts[i:i + 2] for i in range(0, len(kts), 2)]
                    npair = len(pairs)
                    base = ecnt[0]; ecnt[0] += npair

                    def smm(pi):
                        sc = pbig[(base + pi) % 2]
                        for j, kt in enumerate(pairs[pi]):
                            kb.op('pe', lambda e, j=j, kt=kt: e.matmul(sc[:, j * 512:j * 512 + nq], lhsT=KT[kr0:kr0 + kd, kt * 128:(kt + 1) * 128], rhs=QT[kr0:kr0 + kd, q0:q0 + nq], start=True, stop=True),
                                  reads=[KT, QT], writes=[sc])
                    smm(0)
                    for pi, pr in enumerate(pairs):
                        sc = pbig[(base + pi) % 2]; Et = E2[(base + pi) % 3]
                        w = len(pr)
                        if nq == 512:
                            kb.op('act', lambda e, sc=sc, Et=Et, w=w: e.activation(out=Et[:, 0:w * 512], in_=sc[:, 0:w * 512], func=AF.Exp, scale=scale), reads=[sc], writes=[Et])
                        else:
                            kb.op('act', lambda e, sc=sc, Et=Et, w=w: e.activation(out=Et[:, :].rearrange("p (j n) -> p j n", n=512)[:, 0:w, 0:nq],
                                  in_=sc[:, :].rearrange("p (j n) -> p j n", n=512)[:, 0:w, 0:nq], func=AF.Exp, scale=scale), reads=[sc], writes=[Et])
                        if pi + 1 < npair:
                            smm(pi + 1)
                        for j, kt in enumerate(pr):
                            idx = pi * 2 + j
                            wm = wfn(idx) if wfn is not None else None
                            if wm is not None:
                                kb.op('dve', lambda e, Et=Et, wm=wm, j=j: e.tensor_tensor(out=Et[:, j * 512:j * 512 + nq], in0=Et[:, j * 512:j * 512 + nq], in1=wm, op=ALU.mult), reads=[Et, Wt], writes=[Et])
                        for j, kt in enumerate(pr):
                            idx = pi * 2 + j
                            kb.op('pe', lambda e, kt=kt, Et=Et, idx=idx, j=j: e.matmul(pso[:, 0:nq], lhsT=V[:, kt, :], rhs=Et[:, j * 512:j * 512 + nq], start=(idx == 0), stop=(idx == len(kts) - 1)),
                                  reads=[V, Et], writes=[pso])

                def norm_store(pso, nq, dst_tile, dst_ap, dt_out_tile=None):
                    kb.op('dve', lambda e: e.reciprocal(out=rd[64:128, 0:nq], in_=pso[64:128, 0:nq]), reads=[pso], writes=[rd])
                    kb.op('dve', lambda e: e.tensor_tensor(out=dst_ap, in0=pso[0:64, 0:nq], in1=rd[64:128, 0:nq], op=ALU.mult), reads=[pso, rd], writes=[dst_tile])

                qchunks = [(j * 512, 512, True) for j in range(8)] + ([(SEQ, 256, False)] if ctx_out else [])
                all_kt = list(range(NT)); ctx_kt = [32, 33]
                pcnt = [0]
                kt0_holder = [0]
                for br in (0, 1):
                    for h in range(4):
                        if br == 0:
                            if h % 2 == 0:
                                kb.dma('sp', KT[:], S['FM'][C_NAK + h // 2], reads=[D_['FM']], writes=[KT])
                                kb.dma('sp', QT[:], S['FM'][C_NAQ + h // 2], reads=[D_['FM']], writes=[QT])
                            kr0, kd, scale, vidx = (h % 2) * 64, 64, NA_SCALE, h
                        else:
                            kb.dma('sp', KT[:], S['FM'][C_MK + h], reads=[D_['FM']], writes=[KT])
                            kb.dma('sp', QT[:], S['FM'][C_MQ + h], reads=[D_['FM']], writes=[QT])
                            kr0, kd, scale, vidx = 0, 96, MLA_SCALE, 4 + h
                        kb.dma('sp', V[:], S['VV'][:, :, vidx, :].rearrange("t p c -> p t c"), reads=[D_['VV']], writes=[V])
                        for (q0, nq, lat) in qchunks:
                            pso = pbank[2 + pcnt[0] % 2]; pcnt[0] += 1
                            if br == 0 and lat:
                                jq = q0 // 512
                                if jq in (0, 1, 7):
                                    pass
                                kt0 = build_W_cached(jq) if False else None
                            if br == 0 and lat:
                                jq = q0 // 512
                                if (jq in (0, 1, 7)):
                                    kt0_holder[0] = build_W(jq) if True else 0
                                else:
                                    kt0_holder[0] = min(max(8 * jq - 4, 0), 48) // 2
                                kts = list(range(kt0_holder[0], kt0_holder[0] + 8)) + ctx_kt
                                wfn = (lambda idx, h=h: Wt[:, h, idx, :] if idx < 8 else None)
                                attn_block(kts, kr0, kd, q0, nq, scale, pso, wfn)
                            else:
                                attn_block(all_kt if lat else ctx_kt, kr0, kd, q0, nq, scale, pso)
                            norm_store(pso, nq, onb, onb[0:64, 0:nq])
                            kb.dma('sp', S['oT'][br, h // 2, (h % 2) * 64:(h % 2) * 64 + 64, q0:q0 + nq], onb[0:64, 0:nq], reads=[onb], writes=[D_['oT']])
                for h in range(4):
                    kb.dma('sp', KT[:], S['FM'][C_DK + h], reads=[D_['FM']], writes=[KT])
                    kb.dma('sp', QT[:], S['FM'][C_DQ + h], reads=[D_['FM']], writes=[QT])
                    kb.dma('sp', V[:], S['VV'][:, :, 8 + h, :].rearrange("t p c -> p t c"), reads=[D_['VV']], writes=[V])
                    for (q0, nq, lat) in qchunks:
                        kts = all_kt if lat else ctx_kt
                        attn_block(kts, 0, 32, q0, nq, DIFF_SCALE, pbank[2])
                        attn_block(kts, 32, 32, q0, nq, DIFF_SCALE, pbank[3])
                        norm_store(pbank[2], nq, o1, o1[0:64, 0:nq])
                        norm_store(pbank[3], nq, o2, o2[0:64, 0:nq])
                        kb.op('dve', lambda e: e.scalar_tensor_tensor(out=o1[0:64, 0:nq], in0=o2[0:64, 0:nq], scalar=lamt[0:64, 5:6], in1=o1[0:64, 0:nq], op0=ALU.mult, op1=ALU.add),
                              reads=[o1, o2, lamt], writes=[o1])
                        kb.op('pool', lambda e: e.tensor_tensor(out=osq[0:64, 0:nq], in0=o1[0:64, 0:nq], in1=o1[0:64, 0:nq], op=ALU.mult), reads=[o1], writes=[osq])
                        kb.op('pe', lambda e: e.matmul(pbank[0][0:64, 0:nq], lhsT=ones_b[0:64, 0:64], rhs=osq[0:64, 0:nq], start=True, stop=True), reads=[ones_b, osq], writes=[pbank[0]])
                        kb.op('act', lambda e: e.activation(out=rs2[0:64, 0:nq], in_=pbank[0][0:64, 0:nq], func=AF.Sqrt, scale=1.0 / 64, bias=epsd[0:64, 0:1]), reads=[pbank[0], epsd], writes=[rs2])
                        kb.op('dve', lambda e: e.reciprocal(out=rs2[0:64, 0:nq], in_=rs2[0:64, 0:nq]), reads=[rs2], writes=[rs2])
                        kb.op('dve', lambda e: e.scalar_tensor_tensor(out=onb[0:64, 0:nq], in0=o1[0:64, 0:nq], scalar=gsub[0:64, 0:1], in1=rs2[0:64, 0:nq], op0=ALU.mult, op1=ALU.mult),
                              reads=[o1, gsub, rs2], writes=[onb])
                        kb.dma('sp', S['oT'][3, h // 2, (h % 2) * 64:(h % 2) * 64 + 64, q0:q0 + nq], onb[0:64, 0:nq], reads=[onb], writes=[D_['oT']])
                if debug == 'B':
                    kb.barrier()
                    dbg_out['oT'] = nc.dram_tensor('dbg_oT', [4, 2, 128, T], BF16, kind="ExternalOutput").ap()
                    kb.dma('sp', dbg_out['oT'], S['oT'], reads=[D_['oT']])
                kb.barrier()
            if debug == 'B':
                break
            TWO_PI = 2.0 * math.pi
            with contextlib.ExitStack() as es:
                uTn = sb(es, 'uTn', [128, T], BF16)
                iot = sb(es, 'iot', [128, T], F32)
                kb.dma('sp', iot[:, :], I['iotaT'][0:1, :].to_broadcast([128, T]), writes=[iot])
                P = sb(es, 's5p', [128, 24, 16], F32)
                LRE, LIM, DT, LR, TH, RR, M1, SN, CS, ARE, AIM, NRE, NIM, DEN, CRE, CIM, TMP = range(17)
                with nc.allow_non_contiguous_dma(reason="small s5 params"):
                    kb.dma('sp', P[:, LRE, :].rearrange("p (d k) -> p d k", d=2), I['s5_lam_re'][layer].rearrange("d (k two) p -> (two p) d k", two=2), writes=[P])
                    kb.dma('sp', P[:, LIM, :].rearrange("p (d k) -> p d k", d=2), I['s5_lam_im'][layer].rearrange("d (k two) p -> (two p) d k", two=2), writes=[P])
                    for two in range(2):
                        kb.dma('sp', P[two * 64:(two + 1) * 64, DT, :].rearrange("p (d k) -> p d k", d=2),
                               I['s5_log_dt'][layer].rearrange("d (k two) -> two d k", two=2)[two:two + 1].to_broadcast([64, 2, 8]), writes=[P])
                def sm(fn, *a, **k):
                    kb.op('dve', fn, reads=[P], writes=[P])
                kb.op('act', lambda e: e.activation(out=P[:, DT, :], in_=P[:, DT, :], func=AF.Exp), reads=[P], writes=[P])
                sm(lambda e: e.tensor_tensor(out=P[:, LR, :], in0=P[:, LRE, :], in1=P[:, DT, :], op=ALU.mult))
                sm(lambda e: e.tensor_tensor(out=P[:, TH, :], in0=P[:, LIM, :], in1=P[:, DT, :], op=ALU.mult))
                kb.op('act', lambda e: e.activation(out=P[:, RR, :], in_=P[:, LR, :], func=AF.Exp), reads=[P], writes=[P])
                sm(lambda e: e.tensor_scalar(out=P[:, TMP, :], in0=P[:, TH, :], scalar1=1.0 / TWO_PI, scalar2=12582912.0, op0=ALU.mult, op1=ALU.add))
                sm(lambda e: e.tensor_scalar_add(out=P[:, TMP, :], in0=P[:, TMP, :], scalar1=-12582912.0))
                sm(lambda e: e.scalar_tensor_tensor(out=P[:, M1, :], in0=P[:, TMP, :], scalar=-TWO_PI, in1=P[:, TH, :], op0=ALU.mult, op1=ALU.add))
                sm(lambda e: e.tensor_scalar(out=P[:, M1, :], in0=P[:, M1, :], scalar1=-3.14159, scalar2=3.14159, op0=ALU.max, op1=ALU.min))
                kb.op('act', lambda e: e.activation(out=P[:, SN, :], in_=P[:, M1, :], func=AF.Sin), reads=[P], writes=[P])
                sm(lambda e: e.tensor_scalar_add(out=P[:, CS, :], in0=P[:, TH, :], scalar1=0.5 * math.pi))
                sm(lambda e: e.tensor_scalar(out=P[:, TMP, :], in0=P[:, CS, :], scalar1=1.0 / TWO_PI, scalar2=12582912.0, op0=ALU.mult, op1=ALU.add))
                sm(lambda e: e.tensor_scalar_add(out=P[:, TMP, :], in0=P[:, TMP, :], scalar1=-12582912.0))
                sm(lambda e: e.scalar_tensor_tensor(out=P[:, M1, :], in0=P[:, TMP, :], scalar=-TWO_PI, in1=P[:, CS, :], op0=ALU.mult, op1=ALU.add))
                sm(lambda e: e.tensor_scalar(out=P[:, M1, :], in0=P[:, M1, :], scalar1=-3.14159, scalar2=3.14159, op0=ALU.max, op1=ALU.min))
                kb.op('act', lambda e: e.activation(out=P[:, CS, :], in_=P[:, M1, :], func=AF.Sin), reads=[P], writes=[P])
                sm(lambda e: e.tensor_tensor(out=P[:, ARE, :], in0=P[:, RR, :], in1=P[:, CS, :], op=ALU.mult))
                sm(lambda e: e.tensor_tensor(out=P[:, AIM, :], in0=P[:, RR, :], in1=P[:, SN, :], op=ALU.mult))
                sm(lambda e: e.tensor_scalar_add(out=P[:, ARE, :], in0=P[:, ARE, :], scalar1=-1.0))
                sm(lambda e: e.tensor_tensor(out=P[:, NRE, :], in0=P[:, ARE, :], in1=P[:, LRE, :], op=ALU.mult))
                sm(lambda e: e.tensor_tensor(out=P[:, TMP, :], in0=P[:, AIM, :], in1=P[:, LIM, :], op=ALU.mult))
                sm(lambda e: e.tensor_tensor(out=P[:, NRE, :], in0=P[:, NRE, :], in1=P[:, TMP, :], op=ALU.add))
                sm(lambda e: e.tensor_tensor(out=P[:, NIM, :], in0=P[:, AIM, :], in1=P[:, LRE, :], op=ALU.mult))
                sm(lambda e: e.tensor_tensor(out=P[:, TMP, :], in0=P[:, ARE, :], in1=P[:, LIM, :], op=ALU.mult))
                sm(lambda e: e.tensor_tensor(out=P[:, NIM, :], in0=P[:, NIM, :], in1=P[:, TMP, :], op=ALU.subtract))
                sm(lambda e: e.tensor_tensor(out=P[:, DEN, :], in0=P[:, LRE, :], in1=P[:, LRE, :], op=ALU.mult))
                sm(lambda e: e.tensor_tensor(out=P[:, TMP, :], in0=P[:, LIM, :], in1=P[:, LIM, :], op=ALU.mult))
                sm(lambda e: e.tensor_tensor(out=P[:, DEN, :], in0=P[:, DEN, :], in1=P[:, TMP, :], op=ALU.add))
                sm(lambda e: e.reciprocal(out=P[:, DEN, :], in_=P[:, DEN, :]))
                sm(lambda e: e.tensor_tensor(out=P[:, CRE, :], in0=P[:, NRE, :], in1=P[:, DEN, :], op=ALU.mult))
                sm(lambda e: e.tensor_tensor(out=P[:, CIM, :], in0=P[:, NIM, :], in1=P[:, DEN, :], op=ALU.mult))
                BbT = sb(es, 'BbT', [128, 16, 2, 128], BF16); CT = sb(es, 'CT', [128, 16, 2, 128], BF16)
                with contextlib.ExitStack() as es2:
                    braw = sb(es2, 'braw', [128, 2, 16, 16], F32)
                    bbar = sb(es2, 'bbar', [128, 2, 16, 16], F32)
                    btmp = sb(es2, 'btmp', [128, 16, 16], F32)
                    craw = sb(es2, 'craw', [128, 2, 4, 64], F32)
                    mB = sb(es2, 'mB', [128, 4, 128], F32); mC = sb(es2, 'mC', [128, 4, 128], F32)
                    kb.dma('sp', mB[:], I['maskB'].rearrange("b p q -> p b q"), writes=[mB]); kb.dma('sp', mC[:], I['maskC'].rearrange("b p q -> p b q"), writes=[mC])
                    with nc.allow_non_contiguous_dma(reason="s5 small"):
                        for ri, nm in enumerate(('s5_b_re', 's5_b_im')):
                            for dd in range(2):
                                kb.dma('sp', braw[:, ri, dd * 8:(dd + 1) * 8, :], I[nm][layer, dd].rearrange("(k two) p c -> (two p) k c", two=2), writes=[braw])
                        for ri, nm in enumerate(('s5_c_re', 's5_c_im')):
                            for dd in range(2):
                                kb.dma('sp', craw[:, ri, dd * 2:(dd + 1) * 2, :], I[nm][layer, dd].rearrange("(gh gl) c p -> (gl c) gh p", gl=8), writes=[craw])
                    cre_b = P[:, CRE, :].unsqueeze(2).to_broadcast([128, 16, 16]); cim_b = P[:, CIM, :].unsqueeze(2).to_broadcast([128, 16, 16])
                    kb.op('dve', lambda e: e.tensor_tensor(out=bbar[:, 0], in0=braw[:, 0], in1=cre_b, op=ALU.mult), reads=[braw, P], writes=[bbar])
                    kb.op('dve', lambda e: e.tensor_tensor(out=btmp[:], in0=braw[:, 1], in1=cim_b, op=ALU.mult), reads=[braw, P], writes=[btmp])
                    kb.op('dve', lambda e: e.tensor_tensor(out=bbar[:, 0], in0=bbar[:, 0], in1=btmp[:], op=ALU.subtract), reads=[bbar, btmp], writes=[bbar])
                    kb.op('dve', lambda e: e.tensor_tensor(out=bbar[:, 1], in0=braw[:, 1], in1=cre_b, op=ALU.mult), reads=[braw, P], writes=[bbar])
                    kb.op('dve', lambda e: e.tensor_tensor(out=btmp[:], in0=braw[:, 0], in1=cim_b, op=ALU.mult), reads=[braw, P], writes=[btmp])
                    kb.op('dve', lambda e: e.tensor_tensor(out=bbar[:, 1], in0=bbar[:, 1], in1=btmp[:], op=ALU.add), reads=[bbar, btmp], writes=[bbar])
                    pad = [sb(es2, 'pad%d' % i, [128, 128], BF16) for i in range(2)]
                    pc = [0]
                    for tile in range(16):
                        dd, k = tile // 8, tile % 8
                        for ri in range(2):
                            pd = pad[pc[0] % 2]; pt = ptr[pc[0] % 2]; pc[0] += 1
                            kb.op('dve', lambda e, pd=pd, tile=tile, ri=ri, k=k: e.tensor_tensor(out=pd[:].rearrange("p (a c) -> p a c", c=16), in0=bbar[:, ri, tile, :].unsqueeze(1).to_broadcast([128, 8, 16]),
                                  in1=mB[:, k % 4, :].rearrange("p (a c) -> p a c", c=16), op=ALU.mult), reads=[bbar, mB], writes=[pd])
                            kb.op('pe', lambda e, pd=pd, pt=pt: e.transpose(out=pt[:, 0:128], in_=pd[:], identity=ident[:]), reads=[pd, ident], writes=[pt])
                            kb.op('act', lambda e, pt=pt, tile=tile, ri=ri: e.copy(out=BbT[:, tile, ri, :], in_=pt[:, 0:128]), reads=[pt], writes=[BbT])
                            pd = pad[pc[0] % 2]; pt = ptr[pc[0] % 2]; pc[0] += 1
                            kb.op('dve', lambda e, pd=pd, dd=dd, ri=ri, k=k: e.scalar_tensor_tensor(out=pd[:].rearrange("p (a c) -> p a c", c=64), in0=craw[:, ri, dd * 2 + k // 4, :].unsqueeze(1).to_broadcast([128, 2, 64]),
                                  scalar=(1.0 if ri == 0 else -1.0), in1=mC[:, k % 4, :].rearrange("p (a c) -> p a c", c=64), op0=ALU.mult, op1=ALU.mult), reads=[craw, mC], writes=[pd])
                            kb.op('pe', lambda e, pd=pd, pt=pt: e.transpose(out=pt[:, 0:128], in_=pd[:], identity=ident[:]), reads=[pd, ident], writes=[pt])
                            kb.op('act', lambda e, pt=pt, tile=tile, ri=ri: e.copy(out=CT[:, tile, ri, :], in_=pt[:, 0:128]), reads=[pt], writes=[CT])
                    kb.barrier()
                yacc = sb(es, 'yacc', [128, 2, T], F32)
                dsk = sb(es, 'dsk', [128, 2], F32)
                with nc.allow_non_contiguous_dma(reason="tiny"):
                    kb.dma('sp', dsk[:], I['s5_d'][layer].rearrange("(c p) -> p c", p=128), writes=[dsk])
                for c2 in range(2):
                    kb.dma('sp', uTn[:], S['FM'][C_S5U + c2], reads=[D_['FM']], writes=[uTn])
                    kb.op('pool', lambda e, c2=c2: e.tensor_scalar_mul(out=yacc[:, c2, :], in0=uTn[:, :], scalar1=dsk[:, c2:c2 + 1]), reads=[uTn, dsk], writes=[yacc])
                cur_c = [1]
                cosT = sb(es, 'cosT', [128, T], F32); sinT = sb(es, 'sinT', [128, T], F32)
                dre = sb(es, 'dre', [128, T], F32); dim_ = sb(es, 'dim', [128, T], F32)
                gre = sb(es, 'gre', [128, T], F32); gim = sb(es, 'gim', [128, T], F32)
                hreb = sb(es, 'hreb', [128, T], BF16); himb = sb(es, 'himb', [128, T], BF16)
                lat_chunks = [(j * 512, 512) for j in range(8)]
                for tile in range(16):
                    dd, k = tile // 8, tile % 8
                    cch = k // 4
                    seq = ([(SEQ, 256)] + lat_chunks) if dd == 0 else (lat_chunks + [(SEQ, 256)])
                    if cur_c[0] != cch:
                        kb.dma('sp', uTn[:], S['FM'][C_S5U + cch], reads=[D_['FM']], writes=[uTn])
                        cur_c[0] = cch
                    for (buf, off) in ((sinT, 0.0), (cosT, 0.5)):
                        kb.op('pool', lambda e, buf=buf, off=off: e.tensor_scalar(out=buf[:], in0=(iot[:, :] if dd == 0 else iot[:, ::-1]), scalar1=P[:, TH, tile:tile + 1], scalar2=off * math.pi, op0=ALU.mult, op1=ALU.add), reads=[iot, P], writes=[buf])
                        kb.op('dve', lambda e, buf=buf: e.tensor_scalar(out=dre[:], in0=buf[:], scalar1=1.0 / TWO_PI, scalar2=12582912.0, op0=ALU.mult, op1=ALU.add), reads=[buf], writes=[dre])
                        kb.op('dve', lambda e: e.tensor_scalar_add(out=dre[:], in0=dre[:], scalar1=-12582912.0), reads=[dre], writes=[dre])
                        kb.op('dve', lambda e, buf=buf: e.scalar_tensor_tensor(out=buf[:], in0=dre[:], scalar=-TWO_PI, in1=buf[:], op0=ALU.mult, op1=ALU.add), reads=[dre, buf], writes=[buf])
                        kb.op('dve', lambda e, buf=buf: e.tensor_scalar(out=buf[:], in0=buf[:], scalar1=-3.14159, scalar2=3.14159, op0=ALU.max, op1=ALU.min), reads=[buf], writes=[buf])
                        kb.op('act', lambda e, buf=buf: e.activation(out=buf[:], in_=buf[:], func=AF.Sin), reads=[buf], writes=[buf])
                    so = 0
                    for ci, (to, n) in enumerate(seq):
                        pr = pbank[0 + (ci % 2) * 2]; pi_ = pbank[1 + (ci % 2) * 2]
                        kb.op('pe', lambda e, pr=pr, to=to, n=n: e.matmul(pr[:, 0:n], lhsT=BbT[:, tile, 0, :], rhs=uTn[:, to:to + n], start=True, stop=True), reads=[BbT, uTn], writes=[pr])
                        kb.op('pe', lambda e, pi_=pi_, to=to, n=n: e.matmul(pi_[:, 0:n], lhsT=BbT[:, tile, 1, :], rhs=uTn[:, to:to + n], start=True, stop=True), reads=[BbT, uTn], writes=[pi_])
                        sl = slice(so, so + n)
                        kb.op('act', lambda e, pr=pr, sl=sl, n=n: e.copy(out=gre[:, sl], in_=pr[:, 0:n]), reads=[pr], writes=[gre])
                        kb.op('act', lambda e, pi_=pi_, sl=sl, n=n: e.copy(out=gim[:, sl], in_=pi_[:, 0:n]), reads=[pi_], writes=[gim])
                        so += n
                    kb.op('dve', lambda e: e.tensor_tensor(out=dre[:], in0=gre[:], in1=cosT[:], op=ALU.mult), reads=[gre, cosT], writes=[dre])
                    kb.op('pool', lambda e: e.tensor_tensor(out=dim_[:], in0=gim[:], in1=cosT[:], op=ALU.mult), reads=[gim, cosT], writes=[dim_])
                    kb.op('dve', lambda e: e.tensor_tensor(out=gim[:], in0=gim[:], in1=sinT[:], op=ALU.mult), reads=[gim, sinT], writes=[gim])
                    kb.op('dve', lambda e: e.tensor_tensor(out=gre[:], in0=gre[:], in1=sinT[:], op=ALU.mult), reads=[gre, sinT], writes=[gre])
                    kb.op('dve', lambda e: e.tensor_tensor(out=dre[:], in0=dre[:], in1=gim[:], op=ALU.add), reads=[dre, gim], writes=[dre])
                    kb.op('dve', lambda e: e.tensor_tensor(out=dim_[:], in0=dim_[:], in1=gre[:], op=ALU.subtract), reads=[dim_, gre], writes=[dim_])
                    rb = P[:, RR, tile:tile + 1].to_broadcast([128, T])
                    if dd == 0:
                        kb.op('dve', lambda e: e.tensor_tensor_scan(out=gre[:], data0=rb, data1=dre[:], initial=0.0, op0=ALU.mult, op1=ALU.add), reads=[dre, P], writes=[gre])
                        kb.op('dve', lambda e: e.tensor_tensor_scan(out=gim[:], data0=rb, data1=dim_[:], initial=0.0, op0=ALU.mult, op1=ALU.add), reads=[dim_, P], writes=[gim])
                    else:
                        kb.op('dve', lambda e: e.tensor_tensor_scan(out=gre[:, ::-1], data0=rb, data1=dre[:, ::-1], initial=0.0, op0=ALU.mult, op1=ALU.add), reads=[dre, P], writes=[gre])
                        kb.op('dve', lambda e: e.tensor_tensor_scan(out=gim[:, ::-1], data0=rb, data1=dim_[:, ::-1], initial=0.0, op0=ALU.mult, op1=ALU.add), reads=[dim_, P], writes=[gim])
                    kb.op('pool', lambda e: e.tensor_tensor(out=dre[:], in0=gre[:], in1=cosT[:], op=ALU.mult), reads=[gre, cosT], writes=[dre])
                    kb.op('dve', lambda e: e.tensor_tensor(out=dim_[:], in0=gim[:], in1=cosT[:], op=ALU.mult), reads=[gim, cosT], writes=[dim_])
                    kb.op('dve', lambda e: e.tensor_tensor(out=gim[:], in0=gim[:], in1=sinT[:], op=ALU.mult), reads=[gim, sinT], writes=[gim])
                    kb.op('dve', lambda e: e.tensor_tensor(out=gre[:], in0=gre[:], in1=sinT[:], op=ALU.mult), reads=[gre, sinT], writes=[gre])
                    kb.op('dve', lambda e: e.tensor_tensor(out=hreb[:], in0=dre[:], in1=gim[:], op=ALU.subtract), reads=[dre, gim], writes=[hreb])
                    kb.op('dve', lambda e: e.tensor_tensor(out=himb[:], in0=gre[:], in1=dim_[:], op=ALU.add), reads=[gre, dim_], writes=[himb])
                    so = 0
                    for ci, (to, n) in enumerate(seq):
                        sl = slice(so, so + n)
                        py = pbank[4 + ci % 2]
                        kb.op('pe', lambda e, py=py, sl=sl, n=n: e.matmul(py[:, 0:n], lhsT=CT[:, tile, 0, :], rhs=hreb[:, sl], start=True, stop=False), reads=[CT, hreb], writes=[py])
                        kb.op('pe', lambda e, py=py, sl=sl, n=n: e.matmul(py[:, 0:n], lhsT=CT[:, tile, 1, :], rhs=himb[:, sl], start=False, stop=True), reads=[CT, himb], writes=[py])
                        kb.op('dve', lambda e, py=py, to=to, n=n: e.tensor_tensor(out=yacc[:, cch, to:to + n], in0=py[:, 0:n], in1=yacc[:, cch, to:to + n], op=ALU.add), reads=[py, yacc], writes=[yacc])
                        so += n
                wglu = sb(es, 'wglu', [128, 2, 512], BF16)
                kb.dma('pool', wglu[:], I['s5_w_glu'][layer].rearrange("(c p) n -> p c n", p=128), writes=[wglu])
                yb = sb(es, 'yb', [128, 2, 512], BF16)
                sg = sb(es, 'sg', [128, 512], F32)
                og = sb(es, 'og', [128, 2, 512], BF16)
                for (to, n) in lat_chunks + [(SEQ, 256)]:
                    kb.op('pool', lambda e, to=to, n=n: e.tensor_copy(out=yb[:, :, 0:n], in_=yacc[:, :, to:to + n]), reads=[yacc], writes=[yb])
                    for oc in range(2):
                        pv = pbank[0]; pg = pbank[1]
                        for c2 in range(2):
                            kb.op('pe', lambda e, c2=c2, oc=oc, n=n: e.matmul(pv[:, 0:n], lhsT=wglu[:, c2, oc * 128:(oc + 1) * 128], rhs=yb[:, c2, 0:n], start=(c2 == 0), stop=(c2 == 1)), reads=[wglu, yb], writes=[pv])
                        for c2 in range(2):
                            kb.op('pe', lambda e, c2=c2, oc=oc, n=n: e.matmul(pg[:, 0:n], lhsT=wglu[:, c2, 256 + oc * 128:256 + (oc + 1) * 128], rhs=yb[:, c2, 0:n], start=(c2 == 0), stop=(c2 == 1)), reads=[wglu, yb], writes=[pg])
                        kb.op('act', lambda e, n=n: e.activation(out=sg[:, 0:n], in_=pg[:, 0:n], func=AF.Sigmoid), reads=[pg], writes=[sg])
                        kb.op('dve', lambda e, oc=oc, n=n: e.tensor_tensor(out=og[:, oc, 0:n], in0=pv[:, 0:n], in1=sg[:, 0:n], op=ALU.mult), reads=[pv, sg], writes=[og])
                    kb.dma('sp', S['oT'][2, :, :, to:to + n].rearrange("c p t -> p c t"), og[:, :, 0:n], reads=[og], writes=[D_['oT']])
                if debug == 'C':
                    kb.barrier()
                    dbg_out['oT'] = nc.dram_tensor('dbg_oT', [4, 2, 128, T], BF16, kind="ExternalOutput").ap()
                    kb.dma('sp', dbg_out['oT'], S['oT'], reads=[D_['oT']])
                kb.barrier()
            if debug == 'C':
                break
            with contextlib.ExitStack() as es:
                wg = sb(es, 'wg', [128, 8, 4 * D], BF16)
                wbr = sb(es, 'wbr', [128, 4, 2, D], BF16)
                wo = sb(es, 'wo', [128, 8, D], BF16)
                for kc in range(8):
                    kb.dma('pool', wg[:, kc, :], I['w_gate'][layer, kc * 128:(kc + 1) * 128, :], writes=[wg])
                kb.dma('pool', wbr[:], I['w_branch'][layer].rearrange("b (c p) n -> p b c n", p=128), writes=[wbr])
                kb.dma('pool', wo[:], I['w_out'][layer].rearrange("(k p) n -> p k n", p=128), writes=[wo])
                bg = sb(es, 'bg', [128, 4 * D], F32)
                kb.dma('sp', bg[:], I['b_gate'][layer:layer + 1, :].to_broadcast([128, 4 * D]), writes=[bg])
                md = sb(es, 'mdD', [128, 2, 3, D], F32)
                for kind in range(2):
                    for jj, mi in enumerate((2, 3, 4)):
                        kb.dma('sp', md[:, kind, jj, :], S['mod'][kind:kind + 1, mi * D:(mi + 1) * D].to_broadcast([128, D]), reads=[D_['mod']], writes=[md])
                kb.op('dve', lambda e: e.tensor_scalar_add(out=md[:, :, 2, :], in0=md[:, :, 2, :], scalar1=1.0), reads=[md], writes=[md])
                lng = sb(es, 'lng', [128, 2, D], F32)
                kb.dma('sp', lng[:, 0, :], I['ln1_g'][layer:layer + 1, :].to_broadcast([128, D]), writes=[lng])
                kb.dma('sp', lng[:, 1, :], I['ln1_b'][layer:layer + 1, :].to_broadcast([128, D]), writes=[lng])
                epsl = sb(es, 'epsl', [128, 1], F32)
                kb.op('dve', lambda e: e.memset(epsl[:], LN_EPS), writes=[epsl])
                hTm = [sb(es, 'hTm%d' % i, [128, 8, 128], BF16) for i in range(2)]
                oTm = [sb(es, 'oTm%d' % i, [128, 4, 2, 128], BF16) for i in range(2)]
                xm = [sb(es, 'xm%d' % i, [128, D], F32) for i in range(2)]
                gt = sb(es, 'gt', [128, D], F32); macc = sb(es, 'macc', [128, D], F32); tt = sb(es, 'tt', [128, D], F32)
                mbb = sb(es, 'mbb', [128, D], BF16); mT = sb(es, 'mT', [128, 8, 128], BF16)
                st = sb(es, 'stD', [128, 8], F32); jk = sb(es, 'jkD', [128, D], F32)
                h2b = sb(es, 'h2b', [128, D], BF16); h2T = sb(es, 'h2Tt', [128, 8, 128], BF16)
                pcd = [0]

                def layer_norm(r_t, g_ap, b_ap, out_t):
                    kb.op('act', lambda e: e.activation(out=jk[:], in_=r_t[:], func=AF.Identity, accum_out=st[:, 0:1]), reads=[r_t], writes=[jk, st])
                    kb.op('act', lambda e: e.activation(out=jk[:], in_=r_t[:], func=AF.Square, accum_out=st[:, 1:2]), reads=[r_t], writes=[jk, st])
                    kb.op('dve', lambda e: e.tensor_scalar_mul(out=st[:, 2:4], in0=st[:, 0:2], scalar1=1.0 / D), reads=[st], writes=[st])
                    kb.op('dve', lambda e: e.tensor_tensor(out=st[:, 4:5], in0=st[:, 2:3], in1=st[:, 2:3], op=ALU.mult), reads=[st], writes=[st])
                    kb.op('dve', lambda e: e.tensor_tensor(out=st[:, 5:6], in0=st[:, 3:4], in1=st[:, 4:5], op=ALU.subtract), reads=[st], writes=[st])
                    kb.op('act', lambda e: e.activation(out=st[:, 6:7], in_=st[:, 5:6], func=AF.Sqrt, bias=epsl[:, 0:1]), reads=[st, epsl], writes=[st])
                    kb.op('dve', lambda e: e.reciprocal(out=st[:, 6:7], in_=st[:, 6:7]), reads=[st], writes=[st])
                    kb.op('dve', lambda e: e.tensor_scalar(out=out_t[:], in0=r_t[:], scalar1=st[:, 2:3], scalar2=st[:, 6:7], op0=ALU.subtract, op1=ALU.mult), reads=[r_t, st], writes=[out_t])
                    kb.op('dve', lambda e: e.tensor_tensor(out=out_t[:], in0=out_t[:], in1=g_ap, op=ALU.mult), reads=[out_t, lng], writes=[out_t])
                    kb.op('dve', lambda e: e.tensor_tensor(out=out_t[:], in0=out_t[:], in1=b_ap, op=ALU.add), reads=[out_t, lng], writes=[out_t])

                for ti in range(NT):
                    kind = 0 if ti < NLT else 1
                    hT_t = hTm[ti % 2]; oT_t = oTm[ti % 2]; x_t = xm[ti % 2]
                    kb.dma('sp', hT_t[:], S['hT'][ti], reads=[D_['hT']], writes=[hT_t])
                    kb.dma('sp', oT_t[:], S['oT'][:, :, :, ti * 128:(ti + 1) * 128].rearrange("b c p t -> p b c t"), reads=[D_['oT']], writes=[oT_t])
                    if layer == 0:
                        src = I['x'][ti * 128:(ti + 1) * 128, :] if kind == 0 else I['ctx'][(ti - NLT) * 128:(ti - NLT + 1) * 128, :]
                        kb.dma('sp', x_t[:], src, writes=[x_t])
                    else:
                        kb.dma('sp', x_t[:], S['xcur'][ti], reads=[D_['xcur']], writes=[x_t])
                    for br in range(4):
                        for hf in range(2):
                            pgt = pbank[hf]
                            for kc in range(8):
                                kb.op('pe', lambda e, kc=kc, pgt=pgt, br=br, hf=hf: e.matmul(pgt[:, :], lhsT=hT_t[:, kc, :], rhs=wg[:, kc, br * D + hf * 512:br * D + (hf + 1) * 512], start=(kc == 0), stop=(kc == 7)),
                                      reads=[hT_t, wg], writes=[pgt])
                            kb.op('dve', lambda e, pgt=pgt, br=br, hf=hf: e.tensor_tensor(out=gt[:, hf * 512:(hf + 1) * 512], in0=pgt[:, :], in1=bg[:, br * D + hf * 512:br * D + (hf + 1) * 512], op=ALU.add), reads=[pgt, bg], writes=[gt])
                        kb.op('act', lambda e: e.activation(out=gt[:], in_=gt[:], func=AF.Sigmoid), reads=[gt], writes=[gt])
                        for hf in range(2):
                            pbt = pbank[2 + hf]
                            for c2 in range(2):
                                kb.op('pe', lambda e, c2=c2, pbt=pbt, br=br, hf=hf: e.matmul(pbt[:, :], lhsT=oT_t[:, br, c2, :], rhs=wbr[:, br, c2, hf * 512:(hf + 1) * 512], start=(c2 == 0), stop=(c2 == 1)),
                                      reads=[oT_t, wbr], writes=[pbt])
                            dst = macc if br == 0 else tt
                            kb.op('dve', lambda e, pbt=pbt, hf=hf, dst=dst: e.tensor_tensor(out=dst[:, hf * 512:(hf + 1) * 512], in0=pbt[:, :], in1=gt[:, hf * 512:(hf + 1) * 512], op=ALU.mult), reads=[pbt, gt], writes=[dst])
                        if br > 0:
                            kb.op('pool', lambda e: e.tensor_tensor(out=macc[:], in0=macc[:], in1=tt[:], op=ALU.add), reads=[macc, tt], writes=[macc])
                    kb.op('pool', lambda e: e.tensor_copy(out=mbb[:], in_=macc[:]), reads=[macc], writes=[mbb])
                    pt = ptr[pcd[0] % 2]; pcd[0] += 1
                    for kc in range(8):
                        kb.op('pe', lambda e, kc=kc, pt=pt: e.transpose(out=pt[:, kc * 128:(kc + 1) * 128], in_=mbb[:, kc * 128:(kc + 1) * 128], identity=ident[:]), reads=[mbb, ident], writes=[pt])
                    kb.op('act', lambda e, pt=pt: e.copy(out=mT[:].rearrange("p k t -> p (k t)"), in_=pt[:]), reads=[pt], writes=[mT])
                    for hf in range(2):
                        py = pbank[4 + hf]
                        for kc in range(8):
                            kb.op('pe', lambda e, kc=kc, py=py, hf=hf: e.matmul(py[:, :], lhsT=mT[:, kc, :], rhs=wo[:, kc, hf * 512:(hf + 1) * 512], start=(kc == 0), stop=(kc == 7)), reads=[mT, wo], writes=[py])
                        kb.op('dve', lambda e, py=py, hf=hf: e.tensor_tensor(out=tt[:, hf * 512:(hf + 1) * 512], in0=py[:, :], in1=md[:, kind, 0, hf * 512:(hf + 1) * 512], op=ALU.mult), reads=[py, md], writes=[tt])
                    kb.op('dve', lambda e: e.scalar_tensor_tensor(out=tt[:], in0=x_t[:], scalar=ALPHA, in1=tt[:], op0=ALU.mult, op1=ALU.add), reads=[x_t, tt], writes=[tt])
                    layer_norm(tt, lng[:, 0, :], lng[:, 1, :], macc)
                    kb.dma('sp', S['x1'][ti], macc[:], reads=[macc], writes=[D_['x1']])
                    kb.op('dve', lambda e: e.tensor_tensor(out=tt[:], in0=macc[:], in1=md[:, kind, 2, :], op=ALU.mult), reads=[macc, md], writes=[tt])
                    kb.op('dve', lambda e: e.tensor_tensor(out=h2b[:], in0=tt[:], in1=md[:, kind, 1, :], op=ALU.add), reads=[tt, md], writes=[h2b])
                    pt = ptr[pcd[0] % 2]; pcd[0] += 1
                    for kc in range(8):
                        kb.op('pe', lambda e, kc=kc, pt=pt: e.transpose(out=pt[:, kc * 128:(kc + 1) * 128], in_=h2b[:, kc * 128:(kc + 1) * 128], identity=ident[:]), reads=[h2b, ident], writes=[pt])
                    kb.op('act', lambda e, pt=pt: e.copy(out=h2T[:].rearrange("p k t -> p (k t)"), in_=pt[:]), reads=[pt], writes=[h2T])
                    kb.dma('sp', S['h2T'][:, :, ti * 128:(ti + 1) * 128], h2T[:], reads=[h2T], writes=[D_['h2T']])
                kb.barrier()
            with contextlib.ExitStack() as es:
                is_moe = (layer % 2 == 1)
                jl = layer // 2
                nexp = NEXP if is_moe else 1
                TB = 1024
                hblk = sb(es, 'hblk', [128, 8, TB], BF16)
                acc = sb(es, 'accE', [128, 8, D], F32)
                w1c = [sb(es, 'w1c%d' % i, [128, 8, 512], BF16) for i in range(2)]
                w3c = [sb(es, 'w3c%d' % i, [128, 8, 512], BF16) for i in range(2)]
                w2c = [sb(es, 'w2c%d' % i, [128, 4, D], BF16) for i in range(2)]
                gT = [sb(es, 'gT%d' % i, [128, 4, TB], BF16) for i in range(2)]
                sil = sb(es, 'sil', [128, 512], F32)
                gates = sb(es, 'gates', [128, 8, NEXP], F32)
                wr = sb(es, 'wr', [128, 8, NEXP], BF16)
                lg = sb(es, 'lg', [128, 4, NEXP], F32); sc = sb(es, 'scE', [128, 8], F32)
                md5 = sb(es, 'md5', [128, 2, D], F32)
                for kind in range(2):
                    kb.dma('sp', md5[:, kind, :], S['mod'][kind:kind + 1, 5 * D:6 * D].to_broadcast([128, D]), reads=[D_['mod']], writes=[md5])
                lng = sb(es, 'lng2', [128, 2, D], F32)
                kb.dma('sp', lng[:, 0, :], I['ln2_g'][layer:layer + 1, :].to_broadcast([128, D]), writes=[lng])
                kb.dma('sp', lng[:, 1, :], I['ln2_b'][layer:layer + 1, :].to_broadcast([128, D]), writes=[lng])
                epsl = sb(es, 'epsl2', [128, 1], F32)
                kb.op('dve', lambda e: e.memset(epsl[:], LN_EPS), writes=[epsl])
                x1t = [sb(es, 'x1t%d' % i, [128, D], F32) for i in range(2)]
                rr = sb(es, 'rrE', [128, D], F32); xo = [sb(es, 'xoE%d' % i, [128, D], F32) for i in range(2)]
                st = sb(es, 'stE', [128, 8], F32); jk = sb(es, 'jkE', [128, D], F32)
                if is_moe:
                    kb.dma('pool', wr[:], I['moe_router'][jl].rearrange("(k p) n -> p k n", p=128), writes=[wr])
                wcnt = [0]
                blocks = [(b * TB, min(TB, T - b * TB)) for b in range((T + TB - 1) // TB)]
                for (t0, nb) in blocks:
                    ntl = nb // 128
                    kb.dma('sp', hblk[:, :, 0:nb], S['h2T'][:, :, t0:t0 + nb], reads=[D_['h2T']], writes=[hblk])
                    kb.op('pool', lambda e: e.memset(acc[:], 0.0), writes=[acc])
                    if is_moe:
                        for s_ in range(ntl):
                            pl = pbank[4 + s_ % 2]
                            for kc in range(8):
                                kb.op('pe', lambda e, kc=kc, pl=pl, s_=s_: e.matmul(pl[:, 0:NEXP], lhsT=hblk[:, kc, s_ * 128:(s_ + 1) * 128], rhs=wr[:, kc, :], start=(kc == 0), stop=(kc == 7)), reads=[hblk, wr], writes=[pl])
                            kb.op('dve', lambda e, pl=pl: e.tensor_copy(out=lg[:, 0, :], in_=pl[:, 0:NEXP]), reads=[pl], writes=[lg])
                            kb.op('dve', lambda e: e.reduce_max(out=sc[:, 0:1], in_=lg[:, 0, :], axis=AX.X), reads=[lg], writes=[sc])
                            kb.op('dve', lambda e: e.tensor_scalar(out=lg[:, 1, :], in0=lg[:, 0, :], scalar1=sc[:, 0:1], scalar2=None, op0=ALU.is_equal), reads=[lg, sc], writes=[lg])
                            kb.op('dve', lambda e: e.scalar_tensor_tensor(out=lg[:, 2, :], in0=lg[:, 1, :], scalar=-1e30, in1=lg[:, 0, :], op0=ALU.mult, op1=ALU.add), reads=[lg], writes=[lg])
                            kb.op('dve', lambda e: e.reduce_max(out=sc[:, 1:2], in_=lg[:, 2, :], axis=AX.X), reads=[lg], writes=[sc])
                            kb.op('dve', lambda e: e.tensor_scalar(out=lg[:, 3, :], in0=lg[:, 2, :], scalar1=sc[:, 1:2], scalar2=None, op0=ALU.is_equal), reads=[lg, sc], writes=[lg])
                            kb.op('dve', lambda e: e.tensor_tensor(out=sc[:, 2:3], in0=sc[:, 1:2], in1=sc[:, 0:1], op=ALU.subtract), reads=[sc], writes=[sc])
                            kb.op('act', lambda e: e.activation(out=sc[:, 3:4], in_=sc[:, 2:3], func=AF.Exp), reads=[sc], writes=[sc])
                            kb.op('dve', lambda e: e.tensor_scalar_add(out=sc[:, 4:5], in0=sc[:, 3:4], scalar1=1.0), reads=[sc], writes=[sc])
                            kb.op('dve', lambda e: e.reciprocal(out=sc[:, 4:5], in_=sc[:, 4:5]), reads=[sc], writes=[sc])
                            kb.op('dve', lambda e: e.tensor_tensor(out=sc[:, 5:6], in0=sc[:, 3:4], in1=sc[:, 4:5], op=ALU.mult), reads=[sc], writes=[sc])
                            kb.op('dve', lambda e: e.tensor_scalar_mul(out=lg[:, 1, :], in0=lg[:, 1, :], scalar1=sc[:, 4:5]), reads=[lg, sc], writes=[lg])
                            kb.op('dve', lambda e, s_=s_: e.scalar_tensor_tensor(out=gates[:, s_, :], in0=lg[:, 3, :], scalar=sc[:, 5:6], in1=lg[:, 1, :], op0=ALU.mult, op1=ALU.add), reads=[lg, sc], writes=[gates])
                    for ex in range(nexp):
                        if is_moe:
                            W1 = I['moe_w1'][jl, ex]; W3 = I['moe_w3'][jl, ex]; W2 = I['moe_w2'][jl, ex]
                        else:
                            W1 = I['ffn_w1'][jl]; W3 = I['ffn_w3'][jl]; W2 = I['ffn_w2'][jl]
                        for fc in range(D_FF // 512):
                            a1 = w1c[wcnt[0] % 2]; a3 = w3c[wcnt[0] % 2]; a2 = w2c[wcnt[0] % 2]; g_ = gT[wcnt[0] % 2]; wcnt[0] += 1
                            kb.dma('pool', a1[:], W1[:, fc * 512:(fc + 1) * 512].rearrange("(k p) n -> p k n", p=128), writes=[a1])
                            kb.dma('pool', a3[:], W3[:, fc * 512:(fc + 1) * 512].rearrange("(k p) n -> p k n", p=128), writes=[a3])
                            kb.dma('pool', a2[:], W2[fc * 512:(fc + 1) * 512, :].rearrange("(f p) n -> p f n", p=128), writes=[a2])
                            for f in range(4):
                                for th in range((nb + 511) // 512):
                                    c0 = th * 512; n = min(512, nb - c0)
                                    p1 = pbank[0 + (f * 2 + th) % 2 * 2]; p3 = pbank[1 + (f * 2 + th) % 2 * 2]
                                    for kc in range(8):
                                        kb.op('pe', lambda e, kc=kc, p1=p1, a1=a1, f=f, c0=c0, n=n: e.matmul(p1[:, 0:n], lhsT=a1[:, kc, f * 128:(f + 1) * 128], rhs=hblk[:, kc, c0:c0 + n], start=(kc == 0), stop=(kc == 7)), reads=[a1, hblk], writes=[p1])
                                    for kc in range(8):
                                        kb.op('pe', lambda e, kc=kc, p3=p3, a3=a3, f=f, c0=c0, n=n: e.matmul(p3[:, 0:n], lhsT=a3[:, kc, f * 128:(f + 1) * 128], rhs=hblk[:, kc, c0:c0 + n], start=(kc == 0), stop=(kc == 7)), reads=[a3, hblk], writes=[p3])
                                    kb.op('act', lambda e, p1=p1, n=n: e.activation(out=sil[:, 0:n], in_=p1[:, 0:n], func=AF.Silu), reads=[p1], writes=[sil])
                                    kb.op('dve', lambda e, p3=p3, g_=g_, f=f, c0=c0, n=n: e.tensor_tensor(out=g_[:, f, c0:c0 + n], in0=p3[:, 0:n], in1=sil[:, 0:n], op=ALU.mult), reads=[p3, sil], writes=[g_])
                            for s_ in range(ntl):
                                for hf in range(2):
                                    py = pbank[4 + (s_ * 2 + hf) % 2]
                                    for f in range(4):
                                        kb.op('pe', lambda e, f=f, py=py, g_=g_, a2=a2, s_=s_, hf=hf: e.matmul(py[:, :], lhsT=g_[:, f, s_ * 128:(s_ + 1) * 128], rhs=a2[:, f, hf * 512:(hf + 1) * 512], start=(f == 0), stop=(f == 3)), reads=[g_, a2], writes=[py])
                                    if is_moe:
                                        kb.op('dve', lambda e, py=py, s_=s_, hf=hf, ex=ex: e.scalar_tensor_tensor(out=acc[:, s_, hf * 512:(hf + 1) * 512], in0=py[:, :], scalar=gates[:, s_, ex:ex + 1], in1=acc[:, s_, hf * 512:(hf + 1) * 512], op0=ALU.mult, op1=ALU.add), reads=[py, gates, acc], writes=[acc])
                                    else:
                                        kb.op('dve', lambda e, py=py, s_=s_, hf=hf: e.tensor_tensor(out=acc[:, s_, hf * 512:(hf + 1) * 512], in0=py[:, :], in1=acc[:, s_, hf * 512:(hf + 1) * 512], op=ALU.add), reads=[py, acc], writes=[acc])
                    for s_ in range(ntl):
                        ti = t0 // 128 + s_
                        kind = 0 if ti < NLT else 1
                        if layer == DEPTH - 1 and kind == 1:
                            continue
                        x1_ = x1t[ti % 2]; xo_ = xo[ti % 2]
                        kb.dma('sp', x1_[:], S['x1'][ti], reads=[D_['x1']], writes=[x1_])
                        kb.op('dve', lambda e, s_=s_, kind=kind: e.tensor_tensor(out=rr[:], in0=acc[:, s_, :], in1=md5[:, kind, :], op=ALU.mult), reads=[acc, md5], writes=[rr])
                        kb.op('dve', lambda e, x1_=x1_: e.scalar_tensor_tensor(out=rr[:], in0=x1_[:], scalar=ALPHA, in1=rr[:], op0=ALU.mult, op1=ALU.add), reads=[x1_, rr], writes=[rr])
                        kb.op('act', lambda e: e.activation(out=jk[:], in_=rr[:], func=AF.Identity, accum_out=st[:, 0:1]), reads=[rr], writes=[jk, st])
                        kb.op('act', lambda e: e.activation(out=jk[:], in_=rr[:], func=AF.Square, accum_out=st[:, 1:2]), reads=[rr], writes=[jk, st])
                        kb.op('dve', lambda e: e.tensor_scalar_mul(out=st[:, 2:4], in0=st[:, 0:2], scalar1=1.0 / D), reads=[st], writes=[st])
                        kb.op('dve', lambda e: e.tensor_tensor(out=st[:, 4:5], in0=st[:, 2:3], in1=st[:, 2:3], op=ALU.mult), reads=[st], writes=[st])
                        kb.op('dve', lambda e: e.tensor_tensor(out=st[:, 5:6], in0=st[:, 3:4], in1=st[:, 4:5], op=ALU.subtract), reads=[st], writes=[st])
                        kb.op('act', lambda e: e.activation(out=st[:, 6:7], in_=st[:, 5:6], func=AF.Sqrt, bias=epsl[:, 0:1]), reads=[st, epsl], writes=[st])
                        kb.op('dve', lambda e: e.reciprocal(out=st[:, 6:7], in_=st[:, 6:7]), reads=[st], writes=[st])
                        kb.op('dve', lambda e, xo_=xo_: e.tensor_scalar(out=xo_[:], in0=rr[:], scalar1=st[:, 2:3], scalar2=st[:, 6:7], op0=ALU.subtract, op1=ALU.mult), reads=[rr, st], writes=[xo_])
                        kb.op('dve', lambda e, xo_=xo_: e.tensor_tensor(out=xo_[:], in0=xo_[:], in1=lng[:, 0, :], op=ALU.mult), reads=[xo_, lng], writes=[xo_])
                        kb.op('dve', lambda e, xo_=xo_: e.tensor_tensor(out=xo_[:], in0=xo_[:], in1=lng[:, 1, :], op=ALU.add), reads=[xo_, lng], writes=[xo_])
                        if layer == DEPTH - 1:
                            kb.dma('sp', OUT[ti * 128:(ti + 1) * 128, :], xo_[:], reads=[xo_])
                        else:
                            kb.dma('sp', S['xcur'][ti], xo_[:], reads=[xo_], writes=[D_['xcur']])
                if debug == 'L':
                    kb.barrier()
                    dbg_out['xcur'] = nc.dram_tensor('dbg_xcur', [NT, 128, D], F32, kind="ExternalOutput").ap()
                    kb.dma('sp', dbg_out['xcur'], S['xcur'], reads=[D_['xcur']])
                kb.barrier()

        kb.barrier()
    return nc, dbg_out


def host_consts():
    c = {}
    c['ident'] = np.eye(128, dtype=np.float32)
    t = np.arange(SEQ)
    prow, pcol = t // 64, t % 64
    inv = (10000.0 ** (-np.arange(8, dtype=np.float32) / 8)).astype(np.float32)
    cosr = np.ones((T, 16), np.float32); sinr = np.zeros((T, 16), np.float32)
    ar = prow[:, None].astype(np.float32) * inv[None, :]
    ac = pcol[:, None].astype(np.float32) * inv[None, :]
    cosr[:SEQ, 0:8] = np.cos(ar); cosr[:SEQ, 8:16] = np.cos(ac)
    sinr[:SEQ, 0:8] = np.sin(ar); sinr[:SEQ, 8:16] = np.sin(ac)
    c['ropec'] = np.ascontiguousarray(cosr.reshape(NT, 128, 16).transpose(1, 0, 2))
    c['ropes'] = np.ascontiguousarray(sinr.reshape(NT, 128, 16).transpose(1, 0, 2))
    c['iota128'] = np.arange(128, dtype=np.float32)[None, :]
    c['iotaT'] = np.stack([np.arange(T, dtype=np.float32), (T - 1) - np.arange(T, dtype=np.float32)])
    cc = np.arange(NT, dtype=np.float32)
    c['cv'] = np.stack([127.0 - 128.0 * cc, 1.0 + 128.0 * cc]).astype(np.float32)
    mC = np.zeros((4, 128, 128), np.float32)
    for b in range(4):
        for co in range(128):
            for st in range(128):
                if co // 16 == b * 2 + st // 64:
                    mC[b, co, st] = 1.0
    c['maskC'] = mC
    c['maskB'] = np.ascontiguousarray(mC.transpose(0, 2, 1))
    return c


def rpb_toeplitz(rpb):
    kc = np.arange(64)[:, None]; qc = np.arange(64)[None, :]
    c0 = np.clip(qc - 8, 0, 48)
    inwin = (kc >= c0) & (kc <= c0 + 15)
    idx = np.clip(kc - qc + 15, 0, 30)
    g = rpb[:, :, :, idx]
    return np.where(inwin[None, None, None], g, np.float32(-30000.0)).astype(np.float32)


_CACHE = {}


def make_in_maps(inputs, ncores=8):
    consts = host_consts()
    shared = {}
    for k, v in inputs.items():
        if k in ('x', 'c', 'ctx', 'c_ctx', 'na_rpb'):
            continue
        shared[k] = np.ascontiguousarray(np.asarray(v, dtype=np.float32))
    shared['rpbT'] = rpb_toeplitz(np.asarray(inputs['na_rpb'], dtype=np.float32))
    shared['c_ctx'] = np.asarray(inputs['c_ctx'], np.float32).reshape(1, D)
    shared.update(consts)
    maps = []
    for b in range(ncores):
        m = dict(shared)
        m['x'] = np.ascontiguousarray(np.asarray(inputs['x'][b], np.float32))
        m['ctx'] = np.ascontiguousarray(np.asarray(inputs['ctx'][b], np.float32))
        m['c'] = np.asarray(inputs['c'][b], np.float32).reshape(1, D)
        maps.append(m)
    return maps


def kernel(**inputs):
    if 'nc' not in _CACHE:
        _CACHE['nc'] = build_program()[0]
    nc = _CACHE['nc']
    maps = make_in_maps(inputs, 8)
    res = run_bass_kernel_spmd(nc, maps, core_ids=list(range(8)))
    return np.stack([np.asarray(r['out'], dtype=np.float32) for r in res.results], axis=0)
```

```python
import math
import contextlib
import numpy as np
import ml_dtypes
import concourse.bass as bass
import concourse.mybir as mybir
from concourse.bass_utils import run_bass_kernel_spmd

F32 = mybir.dt.float32
BF16 = mybir.dt.bfloat16
ALU = mybir.AluOpType
AF = mybir.ActivationFunctionType
AX = mybir.AxisListType

D = 1024
SEQ = 4096
CTX = 256
T = SEQ + CTX
NT = T // 128
NLT = SEQ // 128
DEPTH = 4
IN_COLS = 2208
D_FF = 3584
NEXP = 8
ALPHA = (2 * DEPTH) ** 0.25
LN_EPS = 1e-5
RMS_EPS = 1e-6
NA_SCALE = 64 ** -0.5
MLA_SCALE = 96 ** -0.5
DIFF_SCALE = 32 ** -0.5
C_NAQ, C_NAK, C_S5U, C_MQ, C_MK, C_DQ, C_DK = 0, 2, 4, 6, 10, 14, 18
NFM = 22


class KB:
    def __init__(self, nc, es):
        self.nc = nc
        self.es = es
        self.eng = {'pe': nc.tensor, 'act': nc.scalar, 'dve': nc.vector, 'pool': nc.gpsimd, 'sp': nc.sync}
        self.sem = {}
        self.cnt = {}
        for e in ('pe', 'act', 'dve', 'pool'):
            self.sem[e] = es.enter_context(nc.semaphore('sem_' + e))
            self.cnt[e] = 0
        self.KD = 8
        self.dsem = {}
        self.dcnt = {}
        for q in ('sp', 'pool'):
            self.dsem[q] = [es.enter_context(nc.semaphore('dsem_%s%d' % (q, i))) for i in range(self.KD)]
            self.dcnt[q] = 0
        self.waited = {e: {} for e in self.eng}
        self.semobj = {}
        for e in self.sem:
            self.semobj[e] = self.sem[e]
        for q in self.dsem:
            for i, s in enumerate(self.dsem[q]):
                self.semobj[(q, i)] = s

    def _wait(self, e, tok):
        key, val = tok
        if self.waited[e].get(key, 0) >= val:
            return
        self.eng[e].wait_ge(self.semobj[key], val)
        self.waited[e][key] = val

    def _deps(self, e, reads, writes):
        deps = {}

        def add(tok):
            if tok is None:
                return
            k, v = tok
            if e == 'pe' and k == 'pe':
                return
            if deps.get(k, 0) < v:
                deps[k] = v
        for t in reads:
            add(t.w)
        for t in writes:
            add(t.w)
            for k, v in t.r.items():
                add((k, v))
        for k, v in deps.items():
            self._wait(e, (k, v))

    def _mark(self, tok, reads, writes):
        k, v = tok
        for t in reads:
            if t.r.get(k, 0) < v:
                t.r[k] = v
        for t in writes:
            t.w = tok
            t.r = {}

    def op(self, e, fn, reads=(), writes=()):
        self._deps(e, reads, writes)
        inst = fn(self.eng[e])
        self.cnt[e] += 1
        inst.then_inc(self.sem[e], 1)
        self._mark((e, self.cnt[e]), reads, writes)

    def dma(self, q, out, in_, reads=(), writes=()):
        self._deps(q, reads, writes)
        n = self.dcnt[q]
        k = n % self.KD
        gen = n // self.KD + 1
        if gen > 1:
            self._wait(q, ((q, k), 16 * (gen - 1)))
        self.eng[q].dma_start(out=out, in_=in_).then_inc(self.dsem[q][k], 16)
        self.dcnt[q] += 1
        self._mark(((q, k), 16 * gen), reads, writes)

    def barrier(self):
        toks = [(e, self.cnt[e]) for e in self.cnt if self.cnt[e] > 0]
        for q in self.dsem:
            n = self.dcnt[q]
            for k in range(self.KD):
                uses = (n - k + self.KD - 1) // self.KD if n > k else 0
                if uses > 0:
                    toks.append(((q, k), 16 * uses))
        for e in self.eng:
            for tok in toks:
                self._wait(e, tok)


class Tl:
    def __init__(self, t):
        self.t = t
        self.w = None
        self.r = {}

    def __getitem__(self, key):
        return self.t[key]


def build_program(nlayers=DEPTH, debug=None):
    nc = bass.Bass("TRN2", target_bir_lowering=False)
    es0 = contextlib.ExitStack()

    def din(name, shape, dt=F32):
        return nc.dram_tensor(name, list(shape), dt, kind="ExternalInput").ap()

    def dscr(name, shape, dt):
        return nc.dram_tensor(name, list(shape), dt, kind="Internal").ap()

    I = {}
    I['x'] = din('x', [SEQ, D]); I['ctx'] = din('ctx', [CTX, D]); I['c'] = din('c', [1, D]); I['c_ctx'] = din('c_ctx', [1, D])
    I['w_ada'] = din('w_ada', [DEPTH, D, 6 * D]); I['b_ada'] = din('b_ada', [DEPTH, 6 * D])
    I['w_in'] = din('w_in', [DEPTH, D, IN_COLS])
    I['rpbT'] = din('rpbT', [DEPTH, 4, 15, 64, 64])
    I['mla_q_norm'] = din('mla_q_norm', [DEPTH, 256]); I['mla_kv_norm'] = din('mla_kv_norm', [DEPTH, 128])
    I['mla_w_uq'] = din('mla_w_uq', [DEPTH, 256, 384]); I['mla_w_ukv'] = din('mla_w_ukv', [DEPTH, 128, 512])
    for nm in ('s5_lam_re', 's5_lam_im'):
        I[nm] = din(nm, [DEPTH, 2, 16, 64])
    I['s5_log_dt'] = din('s5_log_dt', [DEPTH, 2, 16])
    for nm in ('s5_b_re', 's5_b_im'):
        I[nm] = din(nm, [DEPTH, 2, 16, 64, 16])
    for nm in ('s5_c_re', 's5_c_im'):
        I[nm] = din(nm, [DEPTH, 2, 16, 16, 64])
    I['s5_d'] = din('s5_d', [DEPTH, 256]); I['s5_w_glu'] = din('s5_w_glu', [DEPTH, 256, 512])
    for nm in ('diff_lam_q1', 'diff_lam_k1', 'diff_lam_q2', 'diff_lam_k2'):
        I[nm] = din(nm, [DEPTH, 32])
    I['diff_subln'] = din('diff_subln', [DEPTH, 64])
    I['w_branch'] = din('w_branch', [DEPTH, 4, 256, D]); I['w_gate'] = din('w_gate', [DEPTH, D, 4 * D]); I['b_gate'] = din('b_gate', [DEPTH, 4 * D])
    I['w_out'] = din('w_out', [DEPTH, D, D])
    for nm in ('ln1_g', 'ln1_b', 'ln2_g', 'ln2_b'):
        I[nm] = din(nm, [DEPTH, D])
    for nm in ('ffn_w1', 'ffn_w3'):
        I[nm] = din(nm, [2, D, D_FF])
    I['ffn_w2'] = din('ffn_w2', [2, D_FF, D])
    I['moe_router'] = din('moe_router', [2, D, NEXP])
    for nm in ('moe_w1', 'moe_w3'):
        I[nm] = din(nm, [2, NEXP, D, D_FF])
    I['moe_w2'] = din('moe_w2', [2, NEXP, D_FF, D])
    I['ident'] = din('ident', [128, 128]); I['ropec'] = din('ropec', [128, NT, 16]); I['ropes'] = din('ropes', [128, NT, 16])
    I['iota128'] = din('iota128', [1, 128]); I['m32'] = din('m32', [128, 6]); I['iotaT'] = din('iotaT', [2, T]); I['cv'] = din('cv', [2, NT]); I['maskC'] = din('maskC', [4, 128, 128]); I['maskB'] = din('maskB', [4, 128, 128])

    OUT = nc.dram_tensor('out', [SEQ, D], F32, kind="ExternalOutput").ap()
    dbg_out = {}

    S = {}
    S['xcur'] = dscr('xcur', [NT, 128, D], F32)
    S['x1'] = dscr('x1s', [NT, 128, D], F32)
    S['hT'] = dscr('hTs', [NT, 128, 8, 128], BF16)
    S['h2T'] = dscr('h2Ts', [128, 8, T], BF16)
    S['FM'] = dscr('FMs', [NFM, 128, T], BF16)
    S['VV'] = dscr('VVs', [NT, 128, 12, 128], BF16)
    S['oT'] = dscr('oTs', [4, 2, 128, T], BF16)
    S['mod'] = dscr('mods', [2, 6 * D], F32)
    D_ = {k: Tl(v) for k, v in S.items()}

    with es0:
        kb = KB(nc, es0)
        ucnt = [0]

        def sb(es, name, shape, dt):
            ucnt[0] += 1
            return Tl(es.enter_context(nc.sbuf_tensor('%s_u%d' % (name, ucnt[0]), list(shape), dt)))

        def ps(es, name, shape, dt):
            return Tl(es.enter_context(nc.psum_tensor(name, list(shape), dt)))

        ident_f = sb(es0, 'ident_f', [128, 128], F32)
        ident = sb(es0, 'ident_b', [128, 128], BF16)
        ones_b = sb(es0, 'ones_b', [128, 128], BF16)
        condT = sb(es0, 'condT', [128, 8, 2], BF16)
        ctmp = sb(es0, 'ctmp', [128, 8, 2], F32)
        kb.dma('sp', ident_f[:], I['ident'][:], writes=[ident_f])
        kb.op('dve', lambda e: e.tensor_copy(out=ident[:], in_=ident_f[:]), reads=[ident_f], writes=[ident])
        kb.op('dve', lambda e: e.memset(ones_b[:], 1.0), writes=[ones_b])
        with nc.allow_non_contiguous_dma(reason="tiny cond vector"):
            kb.dma('sp', ctmp[:, :, 0], I['c'][0].rearrange("(k p) -> p k", p=128), writes=[ctmp])
            kb.dma('sp', ctmp[:, :, 1], I['c_ctx'][0].rearrange("(k p) -> p k", p=128), writes=[ctmp])
        kb.op('act', lambda e: e.activation(out=condT[:], in_=ctmp[:], func=AF.Silu), reads=[ctmp], writes=[condT])

        pbig = [ps(es0, 'pbig%d' % i, [128, 1024], F32) for i in range(2)]
        pbank = [ps(es0, 'pb%d' % i, [128, 512], F32) for i in range(4)]
        pbank += [Tl(pbig[0].t[:, 0:512]), Tl(pbig[0].t[:, 512:1024])]
        ptr = [Tl(pbig[1].t[:, 0:512].bitcast(BF16)), Tl(pbig[1].t[:, 512:1024].bitcast(BF16))]

        for layer in range(nlayers):
            ctx_out = layer < DEPTH - 1
            lam_init = 0.8 - 0.6 * math.exp(-0.3 * layer)
            with contextlib.ExitStack() as es:
                wst = [sb(es, 'wada%d' % i, [128, 8, 512], BF16) for i in range(2)]
                modsb = sb(es, 'modsb', [2, 6 * D], F32)
                bada = sb(es, 'bada', [2, 6 * D], F32)
                kb.dma('sp', bada[:], I['b_ada'][layer:layer + 1, :].partition_broadcast(2) if False else I['b_ada'][layer:layer + 1, :].to_broadcast([2, 6 * D]), writes=[bada])
                for cc in range(12):
                    w = wst[cc % 2]
                    kb.dma('pool', w[:], I['w_ada'][layer, :, cc * 512:(cc + 1) * 512].rearrange("(k p) n -> p k n", p=128), writes=[w])
                    pb = pbank[cc % 2]
                    for kc in range(8):
                        kb.op('pe', lambda e, kc=kc, w=w, pb=pb: e.matmul(pb[0:2, :], lhsT=condT[:, kc, :], rhs=w[:, kc, :], start=(kc == 0), stop=(kc == 7)),
                              reads=[condT, w], writes=[pb])
                    kb.op('dve', lambda e, cc=cc, pb=pb: e.tensor_tensor(out=modsb[:, cc * 512:(cc + 1) * 512], in0=pb[0:2, :], in1=bada[:, cc * 512:(cc + 1) * 512], op=ALU.add),
                          reads=[pb, bada], writes=[modsb])
                kb.dma('sp', S['mod'][:], modsb[:], reads=[modsb], writes=[D_['mod']])
                if debug == 'M':
                    dbg_out['mod'] = nc.dram_tensor('dbg_mod', [2, 6 * D], F32, kind="ExternalOutput").ap()
                    kb.dma('sp', dbg_out['mod'][:], modsb[:], reads=[modsb])
                kb.barrier()
            if debug == 'M':
                break
            with contextlib.ExitStack() as es:
                win = sb(es, 'win', [128, 8, IN_COLS], BF16)
                wuq = sb(es, 'wuq', [128, 2, 384], BF16)
                wukv = sb(es, 'wukv', [128, 512], BF16)
                kb.dma('pool', win[:], I['w_in'][layer].rearrange("(k p) n -> p k n", p=128), writes=[win])
                kb.dma('pool', wuq[:], I['mla_w_uq'][layer].rearrange("(k p) n -> p k n", p=128), writes=[wuq])
                kb.dma('pool', wukv[:], I['mla_w_ukv'][layer], writes=[wukv])
                modb = sb(es, 'modbA', [128, 2, 2, D], F32)
                for kind in range(2):
                    for j in range(2):
                        kb.dma('sp', modb[:, kind, j, :], S['mod'][kind:kind + 1, j * D:(j + 1) * D].to_broadcast([128, D]), reads=[D_['mod']], writes=[modb])
                kb.op('dve', lambda e: e.tensor_scalar_add(out=modb[:, :, 1, :], in0=modb[:, :, 1, :], scalar1=1.0), reads=[modb], writes=[modb])
                qg = sb(es, 'qg', [128, 256], F32); kvg = sb(es, 'kvg', [128, 128], F32)
                kb.dma('sp', qg[:], I['mla_q_norm'][layer:layer + 1, :].to_broadcast([128, 256]), writes=[qg])
                kb.dma('sp', kvg[:], I['mla_kv_norm'][layer:layer + 1, :].to_broadcast([128, 128]), writes=[kvg])
                rc = sb(es, 'rc', [128, NT, 16], F32); rs = sb(es, 'rs', [128, NT, 16], F32)
                kb.dma('sp', rc[:], I['ropec'][:], writes=[rc]); kb.dma('sp', rs[:], I['ropes'][:], writes=[rs])
                epsq = sb(es, 'epsq', [128, 1], F32)
                kb.op('dve', lambda e: e.memset(epsq[:], RMS_EPS), writes=[epsq])
                xt = [sb(es, 'xt%d' % i, [128, D], F32) for i in range(2)]
                htmp = sb(es, 'htmp', [128, D], F32)
                hb = sb(es, 'hb', [128, D], BF16)
                hTt = [sb(es, 'hTt%d' % i, [128, 8, 128], BF16) for i in range(2)]
                z = sb(es, 'z', [128, IN_COLS], F32)
                zb = sb(es, 'zb', [128, IN_COLS], BF16)
                fm = [sb(es, 'fm%d' % i, [128, NFM, 256], BF16) for i in range(2)]
                vv = [sb(es, 'vv%d' % i, [128, 12, 128], BF16) for i in range(2)]
                for i in range(2):
                    kb.op('pool', lambda e, i=i: e.memset(vv[i][:], 1.0), writes=[vv[i]])
                    kb.op('pool', lambda e, i=i: e.memset(fm[i][:], 0.0), writes=[fm[i]])
                ss = sb(es, 'ss', [128, 4], F32)
                junk = sb(es, 'junk', [128, 256], F32)
                qn = sb(es, 'qn', [128, 384], BF16)
                qnT = sb(es, 'qnT', [128, 3, 128], BF16)
                qf = sb(es, 'qf', [128, 384], F32)
                kvf = sb(es, 'kvf', [128, 512], F32)
                Qb = sb(es, 'Qb', [128, 4, 96], BF16)
                Kb = sb(es, 'Kb', [128, 4, 96], BF16)
                krr = sb(es, 'krr', [128, 32], F32)
                dqk = sb(es, 'dqk', [128, 512], BF16)
                rt = [sb(es, 'rt%d' % i, [128, 256], F32) for i in range(4)]
                ptoggle = [0]

                def rope(src_ap, dst_ap, G, ti, reads, writes):
                    sv = src_ap.rearrange("p (g h two f) -> p g h two f", g=G, h=2, two=2, f=8)
                    dv = dst_ap.rearrange("p (g h two f) -> p g h two f", g=G, h=2, two=2, f=8)
                    cb = rc[:, ti, :].rearrange("p (h f) -> p h f", h=2).unsqueeze(1).to_broadcast([128, G, 2, 8])
                    sn = rs[:, ti, :].rearrange("p (h f) -> p h f", h=2).unsqueeze(1).to_broadcast([128, G, 2, 8])
                    tv = [r_[:, 0:G * 16].rearrange("p (g h f) -> p g h f", g=G, h=2, f=8) for r_ in rt]
                    z1 = sv[:, :, :, 0, :]; z2 = sv[:, :, :, 1, :]
                    kb.op('dve', lambda e: e.tensor_tensor(out=tv[0], in0=z1, in1=cb, op=ALU.mult), reads=reads + [rc], writes=[rt[0]])
                    kb.op('dve', lambda e: e.tensor_tensor(out=tv[1], in0=z2, in1=sn, op=ALU.mult), reads=reads + [rs], writes=[rt[1]])
                    kb.op('dve', lambda e: e.tensor_tensor(out=tv[2], in0=z1, in1=sn, op=ALU.mult), reads=reads + [rs], writes=[rt[2]])
                    kb.op('dve', lambda e: e.tensor_tensor(out=tv[3], in0=z2, in1=cb, op=ALU.mult), reads=reads + [rc], writes=[rt[3]])
                    kb.op('dve', lambda e: e.tensor_tensor(out=dv[:, :, :, 0, :], in0=tv[0], in1=tv[1], op=ALU.subtract), reads=[rt[0], rt[1]], writes=writes)
                    kb.op('dve', lambda e: e.tensor_tensor(out=dv[:, :, :, 1, :], in0=tv[2], in1=tv[3], op=ALU.add), reads=[rt[2], rt[3]], writes=writes)

                def transposes(items, fmt, j):
                    for b0 in range(0, len(items), 8):
                        batch = items[b0:b0 + 8]
                        pt = ptr[ptoggle[0] % 2]; ptoggle[0] += 1
                        for i, (st, sap, n, ch) in enumerate(batch):
                            kb.op('pe', lambda e, i=i, sap=sap, n=n, pt=pt: e.transpose(out=pt[0:n, i * 128:(i + 1) * 128], in_=sap, identity=ident[:]),
                                  reads=[st, ident], writes=[pt])
                        for i, (st, sap, n, ch) in enumerate(batch):
                            eng = 'act' if (i % 2 == 0) else 'pool_'
                            if eng == 'act':
                                kb.op('act', lambda e, i=i, n=n, ch=ch, pt=pt: e.copy(out=fmt[0:n, ch, j * 128:(j + 1) * 128], in_=pt[0:n, i * 128:(i + 1) * 128]), reads=[pt], writes=[fmt])
                            else:
                                kb.op('dve', lambda e, i=i, n=n, ch=ch, pt=pt: e.tensor_copy(out=fmt[0:n, ch, j * 128:(j + 1) * 128], in_=pt[0:n, i * 128:(i + 1) * 128]), reads=[pt], writes=[fmt])

                for ti in range(NT):
                    kind = 0 if ti < NLT else 1
                    g, j = ti // 2, ti % 2
                    fmt = fm[g % 2]; vt = vv[ti % 2]; x_t = xt[ti % 2]; hT_t = hTt[ti % 2]
                    if layer == 0:
                        src = I['x'][ti * 128:(ti + 1) * 128, :] if kind == 0 else I['ctx'][(ti - NLT) * 128:(ti - NLT + 1) * 128, :]
                        kb.dma('sp', x_t[:], src, writes=[x_t])
                    else:
                        kb.dma('sp', x_t[:], S['xcur'][ti], reads=[D_['xcur']], writes=[x_t])
                    kb.op('dve', lambda e: e.tensor_tensor(out=htmp[:], in0=x_t[:], in1=modb[:, kind, 1, :], op=ALU.mult), reads=[x_t, modb], writes=[htmp])
                    kb.op('dve', lambda e: e.tensor_tensor(out=hb[:], in0=htmp[:], in1=modb[:, kind, 0, :], op=ALU.add), reads=[htmp, modb], writes=[hb])
                    pt = ptr[ptoggle[0] % 2]; ptoggle[0] += 1
                    for kc in range(8):
                        kb.op('pe', lambda e, kc=kc, pt=pt: e.transpose(out=pt[:, kc * 128:(kc + 1) * 128], in_=hb[:, kc * 128:(kc + 1) * 128], identity=ident[:]), reads=[hb, ident], writes=[pt])
                    kb.op('act', lambda e, pt=pt: e.copy(out=hT_t[:].rearrange("p k t -> p (k t)"), in_=pt[:]), reads=[pt], writes=[hT_t])
                    kb.dma('sp', S['hT'][ti], hT_t[:], reads=[hT_t], writes=[D_['hT']])
                    for cg in range(5):
                        c0 = cg * 512; n = min(512, IN_COLS - c0)
                        pb = pbank[cg % 4]
                        for kc in range(8):
                            kb.op('pe', lambda e, kc=kc, pb=pb, c0=c0, n=n: e.matmul(pb[:, 0:n], lhsT=hT_t[:, kc, :], rhs=win[:, kc, c0:c0 + n], start=(kc == 0), stop=(kc == 7)),
                                  reads=[hT_t, win], writes=[pb])
                        kb.op('act', lambda e, pb=pb, c0=c0, n=n: e.copy(out=z[:, c0:c0 + n], in_=pb[:, 0:n]), reads=[pb], writes=[z])
                    kb.op('pool', lambda e: e.tensor_copy(out=zb[:], in_=z[:]), reads=[z], writes=[zb])
                    kb.op('pool', lambda e: e.tensor_copy(out=vt[:, 0:4, 0:64], in_=z[:, 512:768].rearrange("p (h d) -> p h d", h=4)), reads=[z], writes=[vt])
                    kb.op('pool', lambda e: e.tensor_copy(out=vt[:, 8:12, 0:64], in_=z[:, 1952:2208].rearrange("p (h d) -> p h d", h=4)), reads=[z], writes=[vt])
                    kb.op('act', lambda e: e.activation(out=junk[:, 0:256], in_=z[:, 768:1024], func=AF.Square, accum_out=ss[:, 0:1]), reads=[z], writes=[junk, ss])
                    kb.op('act', lambda e: e.activation(out=junk[:, 0:128], in_=z[:, 1024:1152], func=AF.Square, accum_out=ss[:, 1:2]), reads=[z], writes=[junk, ss])
                    kb.op('act', lambda e: e.activation(out=ss[:, 2:3], in_=ss[:, 0:1], func=AF.Sqrt, scale=1.0 / 256, bias=epsq[:, 0:1]), reads=[ss, epsq], writes=[ss])
                    kb.op('act', lambda e: e.activation(out=ss[:, 3:4], in_=ss[:, 1:2], func=AF.Sqrt, scale=1.0 / 128, bias=epsq[:, 0:1]), reads=[ss, epsq], writes=[ss])
                    kb.op('dve', lambda e: e.reciprocal(out=ss[:, 2:4], in_=ss[:, 2:4]), reads=[ss], writes=[ss])
                    kb.op('dve', lambda e: e.scalar_tensor_tensor(out=qn[:, 0:256], in0=z[:, 768:1024], scalar=ss[:, 2:3], in1=qg[:], op0=ALU.mult, op1=ALU.mult), reads=[z, ss, qg], writes=[qn])
                    kb.op('dve', lambda e: e.scalar_tensor_tensor(out=qn[:, 256:384], in0=z[:, 1024:1152], scalar=ss[:, 3:4], in1=kvg[:], op0=ALU.mult, op1=ALU.mult), reads=[z, ss, kvg], writes=[qn])
                    pt = ptr[ptoggle[0] % 2]; ptoggle[0] += 1
                    for c3 in range(3):
                        kb.op('pe', lambda e, c3=c3, pt=pt: e.transpose(out=pt[:, c3 * 128:(c3 + 1) * 128], in_=qn[:, c3 * 128:(c3 + 1) * 128], identity=ident[:]), reads=[qn, ident], writes=[pt])
                    kb.op('act', lambda e, pt=pt: e.copy(out=qnT[:].rearrange("p k t -> p (k t)"), in_=pt[:, 0:384]), reads=[pt], writes=[qnT])
                    pq = pbank[4]; pk = pbank[5]
                    for c2 in range(2):
                        kb.op('pe', lambda e, c2=c2: e.matmul(pq[:, 0:384], lhsT=qnT[:, c2, :], rhs=wuq[:, c2, :], start=(c2 == 0), stop=(c2 == 1)), reads=[qnT, wuq], writes=[pq])
                    kb.op('pe', lambda e: e.matmul(pk[:, :], lhsT=qnT[:, 2, :], rhs=wukv[:], start=True, stop=True), reads=[qnT, wukv], writes=[pk])
                    kb.op('act', lambda e: e.copy(out=qf[:], in_=pq[:, 0:384]), reads=[pq], writes=[qf])
                    kb.op('act', lambda e: e.copy(out=kvf[:], in_=pk[:]), reads=[pk], writes=[kvf])
                    qf3 = qf[:].rearrange("p (h d) -> p h d", h=4); kv3 = kvf[:].rearrange("p (h d) -> p h d", h=4)
                    kb.op('pool', lambda e: e.tensor_copy(out=Qb[:, :, 0:64], in_=qf3[:, :, 0:64]), reads=[qf], writes=[Qb])
                    kb.op('pool', lambda e: e.tensor_copy(out=Kb[:, :, 0:64], in_=kv3[:, :, 0:64]), reads=[kvf], writes=[Kb])
                    kb.op('pool', lambda e: e.tensor_copy(out=vt[:, 4:8, 0:64], in_=kv3[:, :, 64:128]), reads=[kvf], writes=[vt])
                    for h in range(4):
                        rope(qf[:, h * 96 + 64:h * 96 + 96], Qb[:, h, 64:96], 1, ti, [qf], [Qb])
                    rope(z[:, 1152:1184], krr[:, :], 1, ti, [z], [krr])
                    kb.op('pool', lambda e: e.tensor_copy(out=Kb[:, :, 64:96], in_=krr[:].unsqueeze(1).to_broadcast([128, 4, 32])), reads=[krr], writes=[Kb])
                    rope(z[:, 1440:1696], dqk[:, 0:256], 8, ti, [z], [dqk])
                    rope(z[:, 1696:1952], dqk[:, 256:512], 8, ti, [z], [dqk])
                    items = []
                    for c2 in range(2):
                        items.append((zb, zb[:, c2 * 128:(c2 + 1) * 128], 128, C_NAQ + c2))
                        items.append((zb, zb[:, 256 + c2 * 128:256 + (c2 + 1) * 128], 128, C_NAK + c2))
                        items.append((zb, zb[:, 1184 + c2 * 128:1184 + (c2 + 1) * 128], 128, C_S5U + c2))
                    for h in range(4):
                        items.append((Qb, Qb[:, h, :], 96, C_MQ + h))
                        items.append((Kb, Kb[:, h, :], 96, C_MK + h))
                    for c2 in range(2):
                        items.append((dqk, dqk[:, c2 * 128:(c2 + 1) * 128], 128, C_DQ + c2))
                        items.append((dqk, dqk[:, 256 + c2 * 128:256 + (c2 + 1) * 128], 128, C_DK + c2))
                    transposes(items, fmt, j)
                    kb.dma('sp', S['VV'][ti], vt[:], reads=[vt], writes=[D_['VV']])
                    if j == 1:
                        kb.dma('sp', S['FM'][:, :, g * 256:(g + 1) * 256].rearrange("c p t -> p c t"), fmt[:], reads=[fmt], writes=[D_['FM']])
                if debug == 'A':
                    kb.barrier()
                    for nm, shp, dt in (('FM', [NFM, 128, T], BF16), ('VV', [NT, 128, 12, 128], BF16), ('hT', [NT, 128, 8, 128], BF16)):
                        dbg_out[nm] = nc.dram_tensor('dbg_' + nm, shp, dt, kind="ExternalOutput").ap()
                        kb.dma('sp', dbg_out[nm], S[nm], reads=[D_[nm]])
                kb.barrier()
            if debug == 'A':
                break
            with contextlib.ExitStack() as es:
                KT = sb(es, 'KT', [128, T], BF16); QT = sb(es, 'QT', [128, T], BF16)
                V = sb(es, 'Vt', [128, NT, 128], BF16)
                rd = sb(es, 'rd', [128, 512], F32)
                onb = sb(es, 'onb', [128, 512], BF16)
                o1 = sb(es, 'o1', [128, 512], F32); o2 = sb(es, 'o2', [128, 512], F32); osq = sb(es, 'osq', [128, 512], BF16)
                rs2 = sb(es, 'rs2', [128, 512], F32)
                Wt3 = [sb(es, 'Wt3_%d' % i, [128, 4, 8, 512], BF16) for i in range(3)]
                QM = [sb(es, 'QM%d' % i, [128, T], BF16) for i in range(4)]
                m32 = sb(es, 'm32', [128, 6], F32)
                kb.dma('sp', m32[:], I['m32'][:], writes=[m32])
                Grev = sb(es, 'Grev', [128, 4, 15, 64], BF16)
                lamv = sb(es, 'lamv', [128, 4, 32], F32); lamt = sb(es, 'lamt', [128, 8], F32)
                gsub = sb(es, 'gsub', [128, 1], F32); epsd = sb(es, 'epsd', [128, 1], F32)
                ecnt = [0]
                for i4, nm in enumerate(('diff_lam_q1', 'diff_lam_k1', 'diff_lam_q2', 'diff_lam_k2')):
                    kb.dma('sp', lamv[:, i4, :], I[nm][layer:layer + 1, :].to_broadcast([128, 32]), writes=[lamv])
                kb.op('dve', lambda e: e.tensor_tensor(out=lamv[:, 0, :], in0=lamv[:, 0, :], in1=lamv[:, 1, :], op=ALU.mult), reads=[lamv], writes=[lamv])
                kb.op('dve', lambda e: e.tensor_tensor(out=lamv[:, 2, :], in0=lamv[:, 2, :], in1=lamv[:, 3, :], op=ALU.mult), reads=[lamv], writes=[lamv])
                kb.op('dve', lambda e: e.reduce_sum(out=lamt[:, 0:1], in_=lamv[:, 0, :], axis=AX.X), reads=[lamv], writes=[lamt])
                kb.op('dve', lambda e: e.reduce_sum(out=lamt[:, 1:2], in_=lamv[:, 2, :], axis=AX.X), reads=[lamv], writes=[lamt])
                kb.op('act', lambda e: e.activation(out=lamt[:, 2:4], in_=lamt[:, 0:2], func=AF.Exp), reads=[lamt], writes=[lamt])
                kb.op('dve', lambda e: e.tensor_tensor(out=lamt[:, 4:5], in0=lamt[:, 3:4], in1=lamt[:, 2:3], op=ALU.subtract), reads=[lamt], writes=[lamt])
                kb.op('dve', lambda e: e.tensor_scalar_add(out=lamt[:, 5:6], in0=lamt[:, 4:5], scalar1=-lam_init), reads=[lamt], writes=[lamt])
                with nc.allow_non_contiguous_dma(reason="tiny"):
                    kb.dma('sp', gsub[0:64, :], I['diff_subln'][layer].rearrange("(p o) -> p o", o=1), writes=[gsub])
                kb.op('dve', lambda e: e.tensor_scalar_mul(out=gsub[0:64, :], in0=gsub[0:64, :], scalar1=(1.0 - lam_init)), reads=[gsub], writes=[gsub])
                kb.op('dve', lambda e: e.memset(epsd[:], RMS_EPS), writes=[epsd])
                with contextlib.ExitStack() as es2:
                    graw = sb(es2, 'graw', [128, 4, 15, 64], F32)
                    for half in range(2):
                        kb.dma('sp', graw[half * 64:(half + 1) * 64], I['rpbT'][layer].rearrange("h r k q -> k h r q"), writes=[graw])
                    for m in range(15):
                        kb.op('act', lambda e, m=m: e.activation(out=Grev[:, :, m, :], in_=graw[:, :, 14 - m, :], func=AF.Exp), reads=[graw], writes=[Grev])
                    kb.barrier()

                def build_W(jq, Wt):
                    R0 = 8 * jq; KR0 = min(max(R0 - 4, 0), 48)
                    kb.op('pool', lambda e: e.memset(Wt[:], 0.0), writes=[Wt])
                    for i in range(8):
                        for half in range(2):
                            kr = KR0 + 2 * i + half
                            al = []
                            for a in range(8):
                                r0 = min(max(R0 + a - 4, 0), 56)
                                if r0 <= kr <= r0 + 7:
                                    al.append(a)
                            if not al:
                                continue
                            a0, a1 = al[0], al[-1] + 1
                            m0 = (R0 + a0) - kr + 7
                            kb.op('pool', lambda e, i=i, half=half, a0=a0, a1=a1, m0=m0: e.tensor_copy(
                                out=Wt[half * 64:(half + 1) * 64, :, i, a0 * 64:a1 * 64].rearrange("p h (a q) -> p h a q", q=64),
                                in_=Grev[half * 64:(half + 1) * 64, :, m0:m0 + (a1 - a0), :]), reads=[Grev], writes=[Wt])
                for ci_, jq_ in enumerate((0, 1, 7)):
                    build_W(jq_, Wt3[ci_])

                E2 = [sb(es, 'E2_%d' % i, [128, 1024], BF16) for i in range(3)]

                def attn_block(kts, kr0, kd, q0, nq, scale, pso, wfn=None, Qs=None, Wsrc=None):
                    Qs = QT if Qs is None else Qs
                    pairs = [kts[i:i + 2] for i in range(0, len(kts), 2)]
                    npair = len(pairs)
                    base = ecnt[0]; ecnt[0] += npair

                    def smm(pi):
                        sc = pbig[(base + pi) % 2]
                        for j, kt in enumerate(pairs[pi]):
                            kb.op('pe', lambda e, j=j, kt=kt: e.matmul(sc[:, j * 512:j * 512 + nq], lhsT=KT[kr0:kr0 + kd, kt * 128:(kt + 1) * 128], rhs=Qs[kr0:kr0 + kd, q0:q0 + nq], start=True, stop=True),
                                  reads=[KT, Qs], writes=[sc])
                    smm(0)
                    for pi, pr in enumerate(pairs):
                        sc = pbig[(base + pi) % 2]; Et = E2[(base + pi) % 3]
                        w = len(pr)
                        if nq == 512:
                            kb.op('act', lambda e, sc=sc, Et=Et, w=w: e.activation(out=Et[:, 0:w * 512], in_=sc[:, 0:w * 512], func=AF.Exp, scale=scale), reads=[sc], writes=[Et])
                        else:
                            kb.op('act', lambda e, sc=sc, Et=Et, w=w: e.activation(out=Et[:, :].rearrange("p (j n) -> p j n", n=512)[:, 0:w, 0:nq],
                                  in_=sc[:, :].rearrange("p (j n) -> p j n", n=512)[:, 0:w, 0:nq], func=AF.Exp, scale=scale), reads=[sc], writes=[Et])
                        if pi + 1 < npair:
                            smm(pi + 1)
                        for j, kt in enumerate(pr):
                            idx = pi * 2 + j
                            wm = wfn(idx) if wfn is not None else None
                            if wm is not None:
                                kb.op('dve', lambda e, Et=Et, wm=wm, j=j: e.tensor_tensor(out=Et[:, j * 512:j * 512 + nq], in0=Et[:, j * 512:j * 512 + nq], in1=wm, op=ALU.mult), reads=[Et, Wsrc], writes=[Et])
                        for j, kt in enumerate(pr):
                            idx = pi * 2 + j
                            kb.op('pe', lambda e, kt=kt, Et=Et, idx=idx, j=j: e.matmul(pso[:, 0:nq], lhsT=V[:, kt, :], rhs=Et[:, j * 512:j * 512 + nq], start=(idx == 0), stop=(idx == len(kts) - 1)),
                                  reads=[V, Et], writes=[pso])

                def norm_store(pso, nq, dst_tile, dst_ap, dt_out_tile=None):
                    kb.op('dve', lambda e: e.reciprocal(out=rd[64:128, 0:nq], in_=pso[64:128, 0:nq]), reads=[pso], writes=[rd])
                    kb.op('dve', lambda e: e.tensor_tensor(out=dst_ap, in0=pso[0:64, 0:nq], in1=rd[64:128, 0:nq], op=ALU.mult), reads=[pso, rd], writes=[dst_tile])

                qchunks = [(j * 512, 512, True) for j in range(8)] + ([(SEQ, 256, False)] if ctx_out else [])
                all_kt = list(range(NT)); ctx_kt = [32, 33]
                pcnt = [0]
                kt0_holder = [0]
                for h in range(4):
                    kb.dma('sp', KT[:], S['FM'][C_MK + h], reads=[D_['FM']], writes=[KT])
                    kb.dma('sp', QT[:], S['FM'][C_MQ + h], reads=[D_['FM']], writes=[QT])
                    kb.dma('sp', V[:], S['VV'][:, :, 4 + h, :].rearrange("t p c -> p t c"), reads=[D_['VV']], writes=[V])
                    for (q0, nq, lat) in qchunks:
                        pso = pbank[2 + pcnt[0] % 2]; pcnt[0] += 1
                        attn_block(all_kt if lat else ctx_kt, 0, 96, q0, nq, MLA_SCALE, pso)
                        norm_store(pso, nq, onb, onb[0:64, 0:nq])
                        kb.dma('sp', S['oT'][1, h // 2, (h % 2) * 64:(h % 2) * 64 + 64, q0:q0 + nq], onb[0:64, 0:nq], reads=[onb], writes=[D_['oT']])
                for c2 in range(2):
                    kb.dma('sp', KT[:], S['FM'][C_DK + c2], reads=[D_['FM']], writes=[KT])
                    kb.dma('sp', QT[:], S['FM'][C_DQ + c2], reads=[D_['FM']], writes=[QT])
                    for i4 in range(4):
                        kb.op('dve', lambda e, i4=i4: e.tensor_scalar_mul(out=QM[i4][:], in0=QT[:], scalar1=m32[:, i4:i4 + 1]), reads=[QT, m32], writes=[QM[i4]])
                    for hh in range(2):
                        h = 2 * c2 + hh
                        kb.dma('sp', V[:], S['VV'][:, :, 8 + h, :].rearrange("t p c -> p t c"), reads=[D_['VV']], writes=[V])
                        for (q0, nq, lat) in qchunks:
                            kts = all_kt if lat else ctx_kt
                            attn_block(kts, 0, 128, q0, nq, DIFF_SCALE, pbank[2], Qs=QM[2 * hh])
                            attn_block(kts, 0, 128, q0, nq, DIFF_SCALE, pbank[3], Qs=QM[2 * hh + 1])
                            norm_store(pbank[2], nq, o1, o1[0:64, 0:nq])
                            norm_store(pbank[3], nq, o2, o2[0:64, 0:nq])
                            kb.op('dve', lambda e: e.scalar_tensor_tensor(out=o1[0:64, 0:nq], in0=o2[0:64, 0:nq], scalar=lamt[0:64, 5:6], in1=o1[0:64, 0:nq], op0=ALU.mult, op1=ALU.add),
                                  reads=[o1, o2, lamt], writes=[o1])
                            kb.op('dve', lambda e: e.tensor_tensor(out=osq[0:64, 0:nq], in0=o1[0:64, 0:nq], in1=o1[0:64, 0:nq], op=ALU.mult), reads=[o1], writes=[osq])
                            kb.op('pe', lambda e: e.matmul(pbank[0][0:64, 0:nq], lhsT=ones_b[0:64, 0:64], rhs=osq[0:64, 0:nq], start=True, stop=True), reads=[ones_b, osq], writes=[pbank[0]])
                            kb.op('act', lambda e: e.activation(out=rs2[0:64, 0:nq], in_=pbank[0][0:64, 0:nq], func=AF.Sqrt, scale=1.0 / 64, bias=epsd[0:64, 0:1]), reads=[pbank[0], epsd], writes=[rs2])
                            kb.op('dve', lambda e: e.reciprocal(out=rs2[0:64, 0:nq], in_=rs2[0:64, 0:nq]), reads=[rs2], writes=[rs2])
                            kb.op('dve', lambda e: e.scalar_tensor_tensor(out=onb[0:64, 0:nq], in0=o1[0:64, 0:nq], scalar=gsub[0:64, 0:1], in1=rs2[0:64, 0:nq], op0=ALU.mult, op1=ALU.mult),
                                  reads=[o1, gsub, rs2], writes=[onb])
                            kb.dma('sp', S['oT'][3, h // 2, (h % 2) * 64:(h % 2) * 64 + 64, q0:q0 + nq], onb[0:64, 0:nq], reads=[onb], writes=[D_['oT']])
                for c2 in range(2):
                    kb.dma('sp', KT[:], S['FM'][C_NAK + c2], reads=[D_['FM']], writes=[KT])
                    kb.dma('sp', QT[:], S['FM'][C_NAQ + c2], reads=[D_['FM']], writes=[QT])
                    for hh in range(2):
                        kb.op('dve', lambda e, hh=hh: e.tensor_scalar_mul(out=QM[hh][:], in0=QT[:], scalar1=m32[:, 4 + hh:5 + hh]), reads=[QT, m32], writes=[QM[hh]])
                    for hh in range(2):
                        h = 2 * c2 + hh
                        kb.dma('sp', V[:], S['VV'][:, :, h, :].rearrange("t p c -> p t c"), reads=[D_['VV']], writes=[V])
                        for (q0, nq, lat) in qchunks:
                            pso = pbank[2 + pcnt[0] % 2]; pcnt[0] += 1
                            if lat:
                                jq = q0 // 512
                                Wc = Wt3[0] if jq == 0 else (Wt3[2] if jq == 7 else Wt3[1])
                                kt0 = min(max(8 * jq - 4, 0), 48) // 2
                                kts = list(range(kt0, kt0 + 8)) + ctx_kt
                                wfn = (lambda idx, h=h, Wc=Wc: Wc[:, h, idx, :] if idx < 8 else None)
                                attn_block(kts, 0, 128, q0, nq, NA_SCALE, pso, wfn, Qs=QM[hh], Wsrc=Wc)
                            else:
                                attn_block(ctx_kt, 0, 128, q0, nq, NA_SCALE, pso, Qs=QM[hh])
                            norm_store(pso, nq, onb, onb[0:64, 0:nq])
                            kb.dma('sp', S['oT'][0, h // 2, (h % 2) * 64:(h % 2) * 64 + 64, q0:q0 + nq], onb[0:64, 0:nq], reads=[onb], writes=[D_['oT']])
                if debug == 'B':
                    kb.barrier()
                    dbg_out['oT'] = nc.dram_tensor('dbg_oT', [4, 2, 128, T], BF16, kind="ExternalOutput").ap()
                    kb.dma('sp', dbg_out['oT'], S['oT'], reads=[D_['oT']])
                kb.barrier()
            if debug == 'B':
                break
            TWO_PI = 2.0 * math.pi
            with contextlib.ExitStack() as es:
                uTn = sb(es, 'uTn', [128, T], BF16)
                iot = sb(es, 'iot', [128, T], F32)
                kb.dma('sp', iot[:, :], I['iotaT'][0:1, :].to_broadcast([128, T]), writes=[iot])
                P = sb(es, 's5p', [128, 24, 16], F32)
                LRE, LIM, DT, LR, TH, RR, M1, SN, CS, ARE, AIM, NRE, NIM, DEN, CRE, CIM, TMP = range(17)
                with nc.allow_non_contiguous_dma(reason="small s5 params"):
                    kb.dma('sp', P[:, LRE, :].rearrange("p (d k) -> p d k", d=2), I['s5_lam_re'][layer].rearrange("d (k two) p -> (two p) d k", two=2), writes=[P])
                    kb.dma('sp', P[:, LIM, :].rearrange("p (d k) -> p d k", d=2), I['s5_lam_im'][layer].rearrange("d (k two) p -> (two p) d k", two=2), writes=[P])
                    for two in range(2):
                        kb.dma('sp', P[two * 64:(two + 1) * 64, DT, :].rearrange("p (d k) -> p d k", d=2),
                               I['s5_log_dt'][layer].rearrange("d (k two) -> two d k", two=2)[two:two + 1].to_broadcast([64, 2, 8]), writes=[P])
                def sm(fn, *a, **k):
                    kb.op('dve', fn, reads=[P], writes=[P])
                kb.op('act', lambda e: e.activation(out=P[:, DT, :], in_=P[:, DT, :], func=AF.Exp), reads=[P], writes=[P])
                sm(lambda e: e.tensor_tensor(out=P[:, LR, :], in0=P[:, LRE, :], in1=P[:, DT, :], op=ALU.mult))
                sm(lambda e: e.tensor_tensor(out=P[:, TH, :], in0=P[:, LIM, :], in1=P[:, DT, :], op=ALU.mult))
                kb.op('act', lambda e: e.activation(out=P[:, RR, :], in_=P[:, LR, :], func=AF.Exp), reads=[P], writes=[P])
                sm(lambda e: e.tensor_scalar(out=P[:, TMP, :], in0=P[:, TH, :], scalar1=1.0 / TWO_PI, scalar2=12582912.0, op0=ALU.mult, op1=ALU.add))
                sm(lambda e: e.tensor_scalar_add(out=P[:, TMP, :], in0=P[:, TMP, :], scalar1=-12582912.0))
                sm(lambda e: e.scalar_tensor_tensor(out=P[:, M1, :], in0=P[:, TMP, :], scalar=-TWO_PI, in1=P[:, TH, :], op0=ALU.mult, op1=ALU.add))
                sm(lambda e: e.tensor_scalar(out=P[:, M1, :], in0=P[:, M1, :], scalar1=-3.14159, scalar2=3.14159, op0=ALU.max, op1=ALU.min))
                kb.op('act', lambda e: e.activation(out=P[:, SN, :], in_=P[:, M1, :], func=AF.Sin), reads=[P], writes=[P])
                sm(lambda e: e.tensor_scalar_add(out=P[:, CS, :], in0=P[:, TH, :], scalar1=0.5 * math.pi))
                sm(lambda e: e.tensor_scalar(out=P[:, TMP, :], in0=P[:, CS, :], scalar1=1.0 / TWO_PI, scalar2=12582912.0, op0=ALU.mult, op1=ALU.add))
                sm(lambda e: e.tensor_scalar_add(out=P[:, TMP, :], in0=P[:, TMP, :], scalar1=-12582912.0))
                sm(lambda e: e.scalar_tensor_tensor(out=P[:, M1, :], in0=P[:, TMP, :], scalar=-TWO_PI, in1=P[:, CS, :], op0=ALU.mult, op1=ALU.add))
                sm(lambda e: e.tensor_scalar(out=P[:, M1, :], in0=P[:, M1, :], scalar1=-3.14159, scalar2=3.14159, op0=ALU.max, op1=ALU.min))
                kb.op('act', lambda e: e.activation(out=P[:, CS, :], in_=P[:, M1, :], func=AF.Sin), reads=[P], writes=[P])
                sm(lambda e: e.tensor_tensor(out=P[:, ARE, :], in0=P[:, RR, :], in1=P[:, CS, :], op=ALU.mult))
                sm(lambda e: e.tensor_tensor(out=P[:, AIM, :], in0=P[:, RR, :], in1=P[:, SN, :], op=ALU.mult))
                sm(lambda e: e.tensor_scalar_add(out=P[:, ARE, :], in0=P[:, ARE, :], scalar1=-1.0))
                sm(lambda e: e.tensor_tensor(out=P[:, NRE, :], in0=P[:, ARE, :], in1=P[:, LRE, :], op=ALU.mult))
                sm(lambda e: e.tensor_tensor(out=P[:, TMP, :], in0=P[:, AIM, :], in1=P[:, LIM, :], op=ALU.mult))
                sm(lambda e: e.tensor_tensor(out=P[:, NRE, :], in0=P[:, NRE, :], in1=P[:, TMP, :], op=ALU.add))
                sm(lambda e: e.tensor_tensor(out=P[:, NIM, :], in0=P[:, AIM, :], in1=P[:, LRE, :], op=ALU.mult))
                sm(lambda e: e.tensor_tensor(out=P[:, TMP, :], in0=P[:, ARE, :], in1=P[:, LIM, :], op=ALU.mult))
                sm(lambda e: e.tensor_tensor(out=P[:, NIM, :], in0=P[:, NIM, :], in1=P[:, TMP, :], op=ALU.subtract))
                sm(lambda e: e.tensor_tensor(out=P[:, DEN, :], in0=P[:, LRE, :], in1=P[:, LRE, :], op=ALU.mult))
                sm(lambda e: e.tensor_tensor(out=P[:, TMP, :], in0=P[:, LIM, :], in1=P[:, LIM, :], op=ALU.mult))
                sm(lambda e: e.tensor_tensor(out=P[:, DEN, :], in0=P[:, DEN, :], in1=P[:, TMP, :], op=ALU.add))
                sm(lambda e: e.reciprocal(out=P[:, DEN, :], in_=P[:, DEN, :]))
                sm(lambda e: e.tensor_tensor(out=P[:, CRE, :], in0=P[:, NRE, :], in1=P[:, DEN, :], op=ALU.mult))
                sm(lambda e: e.tensor_tensor(out=P[:, CIM, :], in0=P[:, NIM, :], in1=P[:, DEN, :], op=ALU.mult))
                BbT = sb(es, 'BbT', [128, 16, 2, 128], BF16); CT = sb(es, 'CT', [128, 16, 2, 128], BF16)
                with contextlib.ExitStack() as es2:
                    braw = sb(es2, 'braw', [128, 2, 16, 16], F32)
                    bbar = sb(es2, 'bbar', [128, 2, 16, 16], F32)
                    btmp = sb(es2, 'btmp', [128, 16, 16], F32)
                    craw = sb(es2, 'craw', [128, 2, 4, 64], F32)
                    mB = sb(es2, 'mB', [128, 4, 128], F32); mC = sb(es2, 'mC', [128, 4, 128], F32)
                    kb.dma('sp', mB[:], I['maskB'].rearrange("b p q -> p b q"), writes=[mB]); kb.dma('sp', mC[:], I['maskC'].rearrange("b p q -> p b q"), writes=[mC])
                    with nc.allow_non_contiguous_dma(reason="s5 small"):
                        for ri, nm in enumerate(('s5_b_re', 's5_b_im')):
                            for dd in range(2):
                                kb.dma('sp', braw[:, ri, dd * 8:(dd + 1) * 8, :], I[nm][layer, dd].rearrange("(k two) p c -> (two p) k c", two=2), writes=[braw])
                        for ri, nm in enumerate(('s5_c_re', 's5_c_im')):
                            for dd in range(2):
                                kb.dma('sp', craw[:, ri, dd * 2:(dd + 1) * 2, :], I[nm][layer, dd].rearrange("(gh gl) c p -> (gl c) gh p", gl=8), writes=[craw])
                    cre_b = P[:, CRE, :].unsqueeze(2).to_broadcast([128, 16, 16]); cim_b = P[:, CIM, :].unsqueeze(2).to_broadcast([128, 16, 16])
                    kb.op('dve', lambda e: e.tensor_tensor(out=bbar[:, 0], in0=braw[:, 0], in1=cre_b, op=ALU.mult), reads=[braw, P], writes=[bbar])
                    kb.op('dve', lambda e: e.tensor_tensor(out=btmp[:], in0=braw[:, 1], in1=cim_b, op=ALU.mult), reads=[braw, P], writes=[btmp])
                    kb.op('dve', lambda e: e.tensor_tensor(out=bbar[:, 0], in0=bbar[:, 0], in1=btmp[:], op=ALU.subtract), reads=[bbar, btmp], writes=[bbar])
                    kb.op('dve', lambda e: e.tensor_tensor(out=bbar[:, 1], in0=braw[:, 1], in1=cre_b, op=ALU.mult), reads=[braw, P], writes=[bbar])
                    kb.op('dve', lambda e: e.tensor_tensor(out=btmp[:], in0=braw[:, 0], in1=cim_b, op=ALU.mult), reads=[braw, P], writes=[btmp])
                    kb.op('dve', lambda e: e.tensor_tensor(out=bbar[:, 1], in0=bbar[:, 1], in1=btmp[:], op=ALU.add), reads=[bbar, btmp], writes=[bbar])
                    pad = [sb(es2, 'pad%d' % i, [128, 128], BF16) for i in range(2)]
                    pc = [0]
                    for tile in range(16):
                        dd, k = tile // 8, tile % 8
                        for ri in range(2):
                            pd = pad[pc[0] % 2]; pt = ptr[pc[0] % 2]; pc[0] += 1
                            kb.op('dve', lambda e, pd=pd, tile=tile, ri=ri, k=k: e.tensor_tensor(out=pd[:].rearrange("p (a c) -> p a c", c=16), in0=bbar[:, ri, tile, :].unsqueeze(1).to_broadcast([128, 8, 16]),
                                  in1=mB[:, k % 4, :].rearrange("p (a c) -> p a c", c=16), op=ALU.mult), reads=[bbar, mB], writes=[pd])
                            kb.op('pe', lambda e, pd=pd, pt=pt: e.transpose(out=pt[:, 0:128], in_=pd[:], identity=ident[:]), reads=[pd, ident], writes=[pt])
                            kb.op('act', lambda e, pt=pt, tile=tile, ri=ri: e.copy(out=BbT[:, tile, ri, :], in_=pt[:, 0:128]), reads=[pt], writes=[BbT])
                            pd = pad[pc[0] % 2]; pt = ptr[pc[0] % 2]; pc[0] += 1
                            kb.op('dve', lambda e, pd=pd, dd=dd, ri=ri, k=k: e.scalar_tensor_tensor(out=pd[:].rearrange("p (a c) -> p a c", c=64), in0=craw[:, ri, dd * 2 + k // 4, :].unsqueeze(1).to_broadcast([128, 2, 64]),
                                  scalar=(1.0 if ri == 0 else -1.0), in1=mC[:, k % 4, :].rearrange("p (a c) -> p a c", c=64), op0=ALU.mult, op1=ALU.mult), reads=[craw, mC], writes=[pd])
                            kb.op('pe', lambda e, pd=pd, pt=pt: e.transpose(out=pt[:, 0:128], in_=pd[:], identity=ident[:]), reads=[pd, ident], writes=[pt])
                            kb.op('act', lambda e, pt=pt, tile=tile, ri=ri: e.copy(out=CT[:, tile, ri, :], in_=pt[:, 0:128]), reads=[pt], writes=[CT])
                    kb.barrier()
                yacc = sb(es, 'yacc', [128, 2, T], F32)
                dsk = sb(es, 'dsk', [128, 2], F32)
                with nc.allow_non_contiguous_dma(reason="tiny"):
                    kb.dma('sp', dsk[:], I['s5_d'][layer].rearrange("(c p) -> p c", p=128), writes=[dsk])
                for c2 in range(2):
                    kb.dma('sp', uTn[:], S['FM'][C_S5U + c2], reads=[D_['FM']], writes=[uTn])
                    kb.op('pool', lambda e, c2=c2: e.tensor_scalar_mul(out=yacc[:, c2, :], in0=uTn[:, :], scalar1=dsk[:, c2:c2 + 1]), reads=[uTn, dsk], writes=[yacc])
                cur_c = [1]
                cosT = sb(es, 'cosT', [128, T], F32); sinT = sb(es, 'sinT', [128, T], F32)
                dre = sb(es, 'dre', [128, T], F32); dim_ = sb(es, 'dim', [128, T], F32)
                gre = sb(es, 'gre', [128, T], F32); gim = sb(es, 'gim', [128, T], F32)
                hreb = sb(es, 'hreb', [128, T], BF16); himb = sb(es, 'himb', [128, T], BF16)
                lat_chunks = [(j * 512, 512) for j in range(8)]
                for tile in range(16):
                    dd, k = tile // 8, tile % 8
                    cch = k // 4
                    seq = ([(SEQ, 256)] + lat_chunks) if dd == 0 else (lat_chunks + [(SEQ, 256)])
                    if cur_c[0] != cch:
                        kb.dma('sp', uTn[:], S['FM'][C_S5U + cch], reads=[D_['FM']], writes=[uTn])
                        cur_c[0] = cch
                    for (buf, off) in ((sinT, 0.0), (cosT, 0.5)):
                        kb.op('pool', lambda e, buf=buf, off=off: e.tensor_scalar(out=buf[:], in0=(iot[:, :] if dd == 0 else iot[:, ::-1]), scalar1=P[:, TH, tile:tile + 1], scalar2=off * math.pi, op0=ALU.mult, op1=ALU.add), reads=[iot, P], writes=[buf])
                        kb.op('dve', lambda e, buf=buf: e.tensor_scalar(out=dre[:], in0=buf[:], scalar1=1.0 / TWO_PI, scalar2=12582912.0, op0=ALU.mult, op1=ALU.add), reads=[buf], writes=[dre])
                        kb.op('dve', lambda e: e.tensor_scalar_add(out=dre[:], in0=dre[:], scalar1=-12582912.0), reads=[dre], writes=[dre])
                        kb.op('dve', lambda e, buf=buf: e.scalar_tensor_tensor(out=buf[:], in0=dre[:], scalar=-TWO_PI, in1=buf[:], op0=ALU.mult, op1=ALU.add), reads=[dre, buf], writes=[buf])
                        kb.op('dve', lambda e, buf=buf: e.tensor_scalar(out=buf[:], in0=buf[:], scalar1=-3.14159, scalar2=3.14159, op0=ALU.max, op1=ALU.min), reads=[buf], writes=[buf])
                        kb.op('act', lambda e, buf=buf: e.activation(out=buf[:], in_=buf[:], func=AF.Sin), reads=[buf], writes=[buf])
                    so = 0
                    for ci, (to, n) in enumerate(seq):
                        pr = pbank[0 + (ci % 2) * 2]; pi_ = pbank[1 + (ci % 2) * 2]
                        kb.op('pe', lambda e, pr=pr, to=to, n=n: e.matmul(pr[:, 0:n], lhsT=BbT[:, tile, 0, :], rhs=uTn[:, to:to + n], start=True, stop=True), reads=[BbT, uTn], writes=[pr])
                        kb.op('pe', lambda e, pi_=pi_, to=to, n=n: e.matmul(pi_[:, 0:n], lhsT=BbT[:, tile, 1, :], rhs=uTn[:, to:to + n], start=True, stop=True), reads=[BbT, uTn], writes=[pi_])
                        sl = slice(so, so + n)
                        kb.op('act', lambda e, pr=pr, sl=sl, n=n: e.copy(out=gre[:, sl], in_=pr[:, 0:n]), reads=[pr], writes=[gre])
                        kb.op('act', lambda e, pi_=pi_, sl=sl, n=n: e.copy(out=gim[:, sl], in_=pi_[:, 0:n]), reads=[pi_], writes=[gim])
                        so += n
                    kb.op('dve', lambda e: e.tensor_tensor(out=dre[:], in0=gre[:], in1=cosT[:], op=ALU.mult), reads=[gre, cosT], writes=[dre])
                    kb.op('pool', lambda e: e.tensor_tensor(out=dim_[:], in0=gim[:], in1=cosT[:], op=ALU.mult), reads=[gim, cosT], writes=[dim_])
                    kb.op('dve', lambda e: e.tensor_tensor(out=gim[:], in0=gim[:], in1=sinT[:], op=ALU.mult), reads=[gim, sinT], writes=[gim])
                    kb.op('dve', lambda e: e.tensor_tensor(out=gre[:], in0=gre[:], in1=sinT[:], op=ALU.mult), reads=[gre, sinT], writes=[gre])
                    kb.op('dve', lambda e: e.tensor_tensor(out=dre[:], in0=dre[:], in1=gim[:], op=ALU.add), reads=[dre, gim], writes=[dre])
                    kb.op('dve', lambda e: e.tensor_tensor(out=dim_[:], in0=dim_[:], in1=gre[:], op=ALU.subtract), reads=[dim_, gre], writes=[dim_])
                    rb = P[:, RR, tile:tile + 1].to_broadcast([128, T])
                    if dd == 0:
                        kb.op('dve', lambda e: e.tensor_tensor_scan(out=gre[:], data0=rb, data1=dre[:], initial=0.0, op0=ALU.mult, op1=ALU.add), reads=[dre, P], writes=[gre])
                        kb.op('dve', lambda e: e.tensor_tensor_scan(out=gim[:], data0=rb, data1=dim_[:], initial=0.0, op0=ALU.mult, op1=ALU.add), reads=[dim_, P], writes=[gim])
                    else:
                        kb.op('dve', lambda e: e.tensor_tensor_scan(out=gre[:, ::-1], data0=rb, data1=dre[:, ::-1], initial=0.0, op0=ALU.mult, op1=ALU.add), reads=[dre, P], writes=[gre])
                        kb.op('dve', lambda e: e.tensor_tensor_scan(out=gim[:, ::-1], data0=rb, data1=dim_[:, ::-1], initial=0.0, op0=ALU.mult, op1=ALU.add), reads=[dim_, P], writes=[gim])
                    kb.op('pool', lambda e: e.tensor_tensor(out=dre[:], in0=gre[:], in1=cosT[:], op=ALU.mult), reads=[gre, cosT], writes=[dre])
                    kb.op('dve', lambda e: e.tensor_tensor(out=dim_[:], in0=gim[:], in1=cosT[:], op=ALU.mult), reads=[gim, cosT], writes=[dim_])
                    kb.op('dve', lambda e: e.tensor_tensor(out=gim[:], in0=gim[:], in1=sinT[:], op=ALU.mult), reads=[gim, sinT], writes=[gim])
                    kb.op('dve', lambda e: e.tensor_tensor(out=gre[:], in0=gre[:], in1=sinT[:], op=ALU.mult), reads=[gre, sinT], writes=[gre])
                    kb.op('dve', lambda e: e.tensor_tensor(out=hreb[:], in0=dre[:], in1=gim[:], op=ALU.subtract), reads=[dre, gim], writes=[hreb])
                    kb.op('dve', lambda e: e.tensor_tensor(out=himb[:], in0=gre[:], in1=dim_[:], op=ALU.add), reads=[gre, dim_], writes=[himb])
                    so = 0
                    for ci, (to, n) in enumerate(seq):
                        sl = slice(so, so + n)
                        py = pbank[4 + ci % 2]
                        kb.op('pe', lambda e, py=py, sl=sl, n=n: e.matmul(py[:, 0:n], lhsT=CT[:, tile, 0, :], rhs=hreb[:, sl], start=True, stop=False), reads=[CT, hreb], writes=[py])
                        kb.op('pe', lambda e, py=py, sl=sl, n=n: e.matmul(py[:, 0:n], lhsT=CT[:, tile, 1, :], rhs=himb[:, sl], start=False, stop=True), reads=[CT, himb], writes=[py])
                        kb.op('dve', lambda e, py=py, to=to, n=n: e.tensor_tensor(out=yacc[:, cch, to:to + n], in0=py[:, 0:n], in1=yacc[:, cch, to:to + n], op=ALU.add), reads=[py, yacc], writes=[yacc])
                        so += n
                wglu = sb(es, 'wglu', [128, 2, 512], BF16)
                kb.dma('pool', wglu[:], I['s5_w_glu'][layer].rearrange("(c p) n -> p c n", p=128), writes=[wglu])
                yb = sb(es, 'yb', [128, 2, 512], BF16)
                sg = sb(es, 'sg', [128, 512], F32)
                og = sb(es, 'og', [128, 2, 512], BF16)
                for (to, n) in lat_chunks + [(SEQ, 256)]:
                    kb.op('pool', lambda e, to=to, n=n: e.tensor_copy(out=yb[:, :, 0:n], in_=yacc[:, :, to:to + n]), reads=[yacc], writes=[yb])
                    for oc in range(2):
                        pv = pbank[0]; pg = pbank[1]
                        for c2 in range(2):
                            kb.op('pe', lambda e, c2=c2, oc=oc, n=n: e.matmul(pv[:, 0:n], lhsT=wglu[:, c2, oc * 128:(oc + 1) * 128], rhs=yb[:, c2, 0:n], start=(c2 == 0), stop=(c2 == 1)), reads=[wglu, yb], writes=[pv])
                        for c2 in range(2):
                            kb.op('pe', lambda e, c2=c2, oc=oc, n=n: e.matmul(pg[:, 0:n], lhsT=wglu[:, c2, 256 + oc * 128:256 + (oc + 1) * 128], rhs=yb[:, c2, 0:n], start=(c2 == 0), stop=(c2 == 1)), reads=[wglu, yb], writes=[pg])
                        kb.op('act', lambda e, n=n: e.activation(out=sg[:, 0:n], in_=pg[:, 0:n], func=AF.Sigmoid), reads=[pg], writes=[sg])
                        kb.op('dve', lambda e, oc=oc, n=n: e.tensor_tensor(out=og[:, oc, 0:n], in0=pv[:, 0:n], in1=sg[:, 0:n], op=ALU.mult), reads=[pv, sg], writes=[og])
                    kb.dma('sp', S['oT'][2, :, :, to:to + n].rearrange("c p t -> p c t"), og[:, :, 0:n], reads=[og], writes=[D_['oT']])
                if debug == 'C':
                    kb.barrier()
                    dbg_out['oT'] = nc.dram_tensor('dbg_oT', [4, 2, 128, T], BF16, kind="ExternalOutput").ap()
                    kb.dma('sp', dbg_out['oT'], S['oT'], reads=[D_['oT']])
                kb.barrier()
            if debug == 'C':
                break
            with contextlib.ExitStack() as es:
                wg = sb(es, 'wg', [128, 8, 4 * D], BF16)
                wbr = sb(es, 'wbr', [128, 4, 2, D], BF16)
                wo = sb(es, 'wo', [128, 8, D], BF16)
                for kc in range(8):
                    kb.dma('pool', wg[:, kc, :], I['w_gate'][layer, kc * 128:(kc + 1) * 128, :], writes=[wg])
                kb.dma('pool', wbr[:], I['w_branch'][layer].rearrange("b (c p) n -> p b c n", p=128), writes=[wbr])
                kb.dma('pool', wo[:], I['w_out'][layer].rearrange("(k p) n -> p k n", p=128), writes=[wo])
                bg = sb(es, 'bg', [128, 4 * D], F32)
                kb.dma('sp', bg[:], I['b_gate'][layer:layer + 1, :].to_broadcast([128, 4 * D]), writes=[bg])
                md = sb(es, 'mdD', [128, 2, 3, D], F32)
                for kind in range(2):
                    for jj, mi in enumerate((2, 3, 4)):
                        kb.dma('sp', md[:, kind, jj, :], S['mod'][kind:kind + 1, mi * D:(mi + 1) * D].to_broadcast([128, D]), reads=[D_['mod']], writes=[md])
                kb.op('dve', lambda e: e.tensor_scalar_add(out=md[:, :, 2, :], in0=md[:, :, 2, :], scalar1=1.0), reads=[md], writes=[md])
                lng = sb(es, 'lng', [128, 2, D], F32)
                kb.dma('sp', lng[:, 0, :], I['ln1_g'][layer:layer + 1, :].to_broadcast([128, D]), writes=[lng])
                kb.dma('sp', lng[:, 1, :], I['ln1_b'][layer:layer + 1, :].to_broadcast([128, D]), writes=[lng])
                epsl = sb(es, 'epsl', [128, 1], F32)
                kb.op('dve', lambda e: e.memset(epsl[:], LN_EPS), writes=[epsl])
                hTm = [sb(es, 'hTm%d' % i, [128, 8, 128], BF16) for i in range(2)]
                oTm = [sb(es, 'oTm%d' % i, [128, 4, 2, 128], BF16) for i in range(2)]
                xm = [sb(es, 'xm%d' % i, [128, D], F32) for i in range(2)]
                gt = sb(es, 'gt', [128, D], F32); macc = sb(es, 'macc', [128, D], F32); tt = sb(es, 'tt', [128, D], F32)
                mbb = sb(es, 'mbb', [128, D], BF16); mT = sb(es, 'mT', [128, 8, 128], BF16)
                st = sb(es, 'stD', [128, 8], F32); jk = sb(es, 'jkD', [128, D], F32)
                h2b = sb(es, 'h2b', [128, D], BF16); h2T = sb(es, 'h2Tt', [128, 8, 128], BF16)
                pcd = [0]

                def layer_norm(r_t, g_ap, b_ap, out_t):
                    kb.op('act', lambda e: e.activation(out=jk[:], in_=r_t[:], func=AF.Identity, accum_out=st[:, 0:1]), reads=[r_t], writes=[jk, st])
                    kb.op('act', lambda e: e.activation(out=jk[:], in_=r_t[:], func=AF.Square, accum_out=st[:, 1:2]), reads=[r_t], writes=[jk, st])
                    kb.op('dve', lambda e: e.tensor_scalar_mul(out=st[:, 2:4], in0=st[:, 0:2], scalar1=1.0 / D), reads=[st], writes=[st])
                    kb.op('dve', lambda e: e.tensor_tensor(out=st[:, 4:5], in0=st[:, 2:3], in1=st[:, 2:3], op=ALU.mult), reads=[st], writes=[st])
                    kb.op('dve', lambda e: e.tensor_tensor(out=st[:, 5:6], in0=st[:, 3:4], in1=st[:, 4:5], op=ALU.subtract), reads=[st], writes=[st])
                    kb.op('act', lambda e: e.activation(out=st[:, 6:7], in_=st[:, 5:6], func=AF.Sqrt, bias=epsl[:, 0:1]), reads=[st, epsl], writes=[st])
                    kb.op('dve', lambda e: e.reciprocal(out=st[:, 6:7], in_=st[:, 6:7]), reads=[st], writes=[st])
                    kb.op('dve', lambda e: e.tensor_scalar(out=out_t[:], in0=r_t[:], scalar1=st[:, 2:3], scalar2=st[:, 6:7], op0=ALU.subtract, op1=ALU.mult), reads=[r_t, st], writes=[out_t])
                    kb.op('dve', lambda e: e.tensor_tensor(out=out_t[:], in0=out_t[:], in1=g_ap, op=ALU.mult), reads=[out_t, lng], writes=[out_t])
                    kb.op('dve', lambda e: e.tensor_tensor(out=out_t[:], in0=out_t[:], in1=b_ap, op=ALU.add), reads=[out_t, lng], writes=[out_t])

                for ti in range(NT):
                    kind = 0 if ti < NLT else 1
                    hT_t = hTm[ti % 2]; oT_t = oTm[ti % 2]; x_t = xm[ti % 2]
                    kb.dma('sp', hT_t[:], S['hT'][ti], reads=[D_['hT']], writes=[hT_t])
                    kb.dma('sp', oT_t[:], S['oT'][:, :, :, ti * 128:(ti + 1) * 128].rearrange("b c p t -> p b c t"), reads=[D_['oT']], writes=[oT_t])
                    if layer == 0:
                        src = I['x'][ti * 128:(ti + 1) * 128, :] if kind == 0 else I['ctx'][(ti - NLT) * 128:(ti - NLT + 1) * 128, :]
                        kb.dma('sp', x_t[:], src, writes=[x_t])
                    else:
                        kb.dma('sp', x_t[:], S['xcur'][ti], reads=[D_['xcur']], writes=[x_t])
                    for br in range(4):
                        for hf in range(2):
                            pgt = pbank[hf]
                            for kc in range(8):
                                kb.op('pe', lambda e, kc=kc, pgt=pgt, br=br, hf=hf: e.matmul(pgt[:, :], lhsT=hT_t[:, kc, :], rhs=wg[:, kc, br * D + hf * 512:br * D + (hf + 1) * 512], start=(kc == 0), stop=(kc == 7)),
                                      reads=[hT_t, wg], writes=[pgt])
                            kb.op('dve', lambda e, pgt=pgt, br=br, hf=hf: e.tensor_tensor(out=gt[:, hf * 512:(hf + 1) * 512], in0=pgt[:, :], in1=bg[:, br * D + hf * 512:br * D + (hf + 1) * 512], op=ALU.add), reads=[pgt, bg], writes=[gt])
                        kb.op('act', lambda e: e.activation(out=gt[:], in_=gt[:], func=AF.Sigmoid), reads=[gt], writes=[gt])
                        for hf in range(2):
                            pbt = pbank[2 + hf]
                            for c2 in range(2):
                                kb.op('pe', lambda e, c2=c2, pbt=pbt, br=br, hf=hf: e.matmul(pbt[:, :], lhsT=oT_t[:, br, c2, :], rhs=wbr[:, br, c2, hf * 512:(hf + 1) * 512], start=(c2 == 0), stop=(c2 == 1)),
                                      reads=[oT_t, wbr], writes=[pbt])
                            dst = macc if br == 0 else tt
                            kb.op('dve', lambda e, pbt=pbt, hf=hf, dst=dst: e.tensor_tensor(out=dst[:, hf * 512:(hf + 1) * 512], in0=pbt[:, :], in1=gt[:, hf * 512:(hf + 1) * 512], op=ALU.mult), reads=[pbt, gt], writes=[dst])
                        if br > 0:
                            kb.op('pool', lambda e: e.tensor_tensor(out=macc[:], in0=macc[:], in1=tt[:], op=ALU.add), reads=[macc, tt], writes=[macc])
                    kb.op('pool', lambda e: e.tensor_copy(out=mbb[:], in_=macc[:]), reads=[macc], writes=[mbb])
                    pt = ptr[pcd[0] % 2]; pcd[0] += 1
                    for kc in range(8):
                        kb.op('pe', lambda e, kc=kc, pt=pt: e.transpose(out=pt[:, kc * 128:(kc + 1) * 128], in_=mbb[:, kc * 128:(kc + 1) * 128], identity=ident[:]), reads=[mbb, ident], writes=[pt])
                    kb.op('act', lambda e, pt=pt: e.copy(out=mT[:].rearrange("p k t -> p (k t)"), in_=pt[:]), reads=[pt], writes=[mT])
                    for hf in range(2):
                        py = pbank[4 + hf]
                        for kc in range(8):
                            kb.op('pe', lambda e, kc=kc, py=py, hf=hf: e.matmul(py[:, :], lhsT=mT[:, kc, :], rhs=wo[:, kc, hf * 512:(hf + 1) * 512], start=(kc == 0), stop=(kc == 7)), reads=[mT, wo], writes=[py])
                        kb.op('dve', lambda e, py=py, hf=hf: e.tensor_tensor(out=tt[:, hf * 512:(hf + 1) * 512], in0=py[:, :], in1=md[:, kind, 0, hf * 512:(hf + 1) * 512], op=ALU.mult), reads=[py, md], writes=[tt])
                    kb.op('dve', lambda e: e.scalar_tensor_tensor(out=tt[:], in0=x_t[:], scalar=ALPHA, in1=tt[:], op0=ALU.mult, op1=ALU.add), reads=[x_t, tt], writes=[tt])
                    layer_norm(tt, lng[:, 0, :], lng[:, 1, :], macc)
                    kb.dma('sp', S['x1'][ti], macc[:], reads=[macc], writes=[D_['x1']])
                    kb.op('dve', lambda e: e.tensor_tensor(out=tt[:], in0=macc[:], in1=md[:, kind, 2, :], op=ALU.mult), reads=[macc, md], writes=[tt])
                    kb.op('dve', lambda e: e.tensor_tensor(out=h2b[:], in0=tt[:], in1=md[:, kind, 1, :], op=ALU.add), reads=[tt, md], writes=[h2b])
                    pt = ptr[pcd[0] % 2]; pcd[0] += 1
                    for kc in range(8):
                        kb.op('pe', lambda e, kc=kc, pt=pt: e.transpose(out=pt[:, kc * 128:(kc + 1) * 128], in_=h2b[:, kc * 128:(kc + 1) * 128], identity=ident[:]), reads=[h2b, ident], writes=[pt])
                    kb.op('act', lambda e, pt=pt: e.copy(out=h2T[:].rearrange("p k t -> p (k t)"), in_=pt[:]), reads=[pt], writes=[h2T])
                    kb.dma('sp', S['h2T'][:, :, ti * 128:(ti + 1) * 128], h2T[:], reads=[h2T], writes=[D_['h2T']])
                kb.barrier()
            with contextlib.ExitStack() as es:
                is_moe = (layer % 2 == 1)
                jl = layer // 2
                nexp = NEXP if is_moe else 1
                TB = 1024
                hblk = sb(es, 'hblk', [128, 8, TB], BF16)
                acc = sb(es, 'accE', [128, 8, D], F32)
                w1c = [sb(es, 'w1c%d' % i, [128, 8, 512], BF16) for i in range(2)]
                w3c = [sb(es, 'w3c%d' % i, [128, 8, 512], BF16) for i in range(2)]
                w2c = [sb(es, 'w2c%d' % i, [128, 4, D], BF16) for i in range(2)]
                gT = [sb(es, 'gT%d' % i, [128, 4, TB], BF16) for i in range(2)]
                sil = sb(es, 'sil', [128, 512], F32)
                gates = sb(es, 'gates', [128, 8, NEXP], F32)
                wr = sb(es, 'wr', [128, 8, NEXP], BF16)
                lg = sb(es, 'lg', [128, 4, NEXP], F32); sc = sb(es, 'scE', [128, 8], F32)
                md5 = sb(es, 'md5', [128, 2, D], F32)
                for kind in range(2):
                    kb.dma('sp', md5[:, kind, :], S['mod'][kind:kind + 1, 5 * D:6 * D].to_broadcast([128, D]), reads=[D_['mod']], writes=[md5])
                lng = sb(es, 'lng2', [128, 2, D], F32)
                kb.dma('sp', lng[:, 0, :], I['ln2_g'][layer:layer + 1, :].to_broadcast([128, D]), writes=[lng])
                kb.dma('sp', lng[:, 1, :], I['ln2_b'][layer:layer + 1, :].to_broadcast([128, D]), writes=[lng])
                epsl = sb(es, 'epsl2', [128, 1], F32)
                kb.op('dve', lambda e: e.memset(epsl[:], LN_EPS), writes=[epsl])
                x1t = [sb(es, 'x1t%d' % i, [128, D], F32) for i in range(2)]
                rr = sb(es, 'rrE', [128, D], F32); xo = [sb(es, 'xoE%d' % i, [128, D], F32) for i in range(2)]
                st = sb(es, 'stE', [128, 8], F32); jk = sb(es, 'jkE', [128, D], F32)
                if is_moe:
                    kb.dma('pool', wr[:], I['moe_router'][jl].rearrange("(k p) n -> p k n", p=128), writes=[wr])
                wcnt = [0]
                blocks = [(b * TB, min(TB, T - b * TB)) for b in range((T + TB - 1) // TB)]
                for (t0, nb) in blocks:
                    ntl = nb // 128
                    kb.dma('sp', hblk[:, :, 0:nb], S['h2T'][:, :, t0:t0 + nb], reads=[D_['h2T']], writes=[hblk])
                    kb.op('pool', lambda e: e.memset(acc[:], 0.0), writes=[acc])
                    if is_moe:
                        for s_ in range(ntl):
                            pl = pbank[4 + s_ % 2]
                            for kc in range(8):
                                kb.op('pe', lambda e, kc=kc, pl=pl, s_=s_: e.matmul(pl[:, 0:NEXP], lhsT=hblk[:, kc, s_ * 128:(s_ + 1) * 128], rhs=wr[:, kc, :], start=(kc == 0), stop=(kc == 7)), reads=[hblk, wr], writes=[pl])
                            kb.op('dve', lambda e, pl=pl: e.tensor_copy(out=lg[:, 0, :], in_=pl[:, 0:NEXP]), reads=[pl], writes=[lg])
                            kb.op('dve', lambda e: e.reduce_max(out=sc[:, 0:1], in_=lg[:, 0, :], axis=AX.X), reads=[lg], writes=[sc])
                            kb.op('dve', lambda e: e.tensor_scalar(out=lg[:, 1, :], in0=lg[:, 0, :], scalar1=sc[:, 0:1], scalar2=None, op0=ALU.is_equal), reads=[lg, sc], writes=[lg])
                            kb.op('dve', lambda e: e.scalar_tensor_tensor(out=lg[:, 2, :], in0=lg[:, 1, :], scalar=-1e30, in1=lg[:, 0, :], op0=ALU.mult, op1=ALU.add), reads=[lg], writes=[lg])
                            kb.op('dve', lambda e: e.reduce_max(out=sc[:, 1:2], in_=lg[:, 2, :], axis=AX.X), reads=[lg], writes=[sc])
                            kb.op('dve', lambda e: e.tensor_scalar(out=lg[:, 3, :], in0=lg[:, 2, :], scalar1=sc[:, 1:2], scalar2=None, op0=ALU.is_equal), reads=[lg, sc], writes=[lg])
                            kb.op('dve', lambda e: e.tensor_tensor(out=sc[:, 2:3], in0=sc[:, 1:2], in1=sc[:, 0:1], op=ALU.subtract), reads=[sc], writes=[sc])
                            kb.op('act', lambda e: e.activation(out=sc[:, 3:4], in_=sc[:, 2:3], func=AF.Exp), reads=[sc], writes=[sc])
                            kb.op('dve', lambda e: e.tensor_scalar_add(out=sc[:, 4:5], in0=sc[:, 3:4], scalar1=1.0), reads=[sc], writes=[sc])
                            kb.op('dve', lambda e: e.reciprocal(out=sc[:, 4:5], in_=sc[:, 4:5]), reads=[sc], writes=[sc])
                            kb.op('dve', lambda e: e.tensor_tensor(out=sc[:, 5:6], in0=sc[:, 3:4], in1=sc[:, 4:5], op=ALU.mult), reads=[sc], writes=[sc])
                            kb.op('dve', lambda e: e.tensor_scalar_mul(out=lg[:, 1, :], in0=lg[:, 1, :], scalar1=sc[:, 4:5]), reads=[lg, sc], writes=[lg])
                            kb.op('dve', lambda e, s_=s_: e.scalar_tensor_tensor(out=gates[:, s_, :], in0=lg[:, 3, :], scalar=sc[:, 5:6], in1=lg[:, 1, :], op0=ALU.mult, op1=ALU.add), reads=[lg, sc], writes=[gates])
                    for ex in range(nexp):
                        if is_moe:
                            W1 = I['moe_w1'][jl, ex]; W3 = I['moe_w3'][jl, ex]; W2 = I['moe_w2'][jl, ex]
                        else:
                            W1 = I['ffn_w1'][jl]; W3 = I['ffn_w3'][jl]; W2 = I['ffn_w2'][jl]
                        for fc in range(D_FF // 512):
                            a1 = w1c[wcnt[0] % 2]; a3 = w3c[wcnt[0] % 2]; a2 = w2c[wcnt[0] % 2]; g_ = gT[wcnt[0] % 2]; wcnt[0] += 1
                            kb.dma('pool', a1[:], W1[:, fc * 512:(fc + 1) * 512].rearrange("(k p) n -> p k n", p=128), writes=[a1])
                            kb.dma('pool', a3[:], W3[:, fc * 512:(fc + 1) * 512].rearrange("(k p) n -> p k n", p=128), writes=[a3])
                            kb.dma('pool', a2[:], W2[fc * 512:(fc + 1) * 512, :].rearrange("(f p) n -> p f n", p=128), writes=[a2])
                            for f in range(4):
                                for th in range((nb + 511) // 512):
                                    c0 = th * 512; n = min(512, nb - c0)
                                    p1 = pbank[0 + (f * 2 + th) % 2 * 2]; p3 = pbank[1 + (f * 2 + th) % 2 * 2]
                                    for kc in range(8):
                                        kb.op('pe', lambda e, kc=kc, p1=p1, a1=a1, f=f, c0=c0, n=n: e.matmul(p1[:, 0:n], lhsT=a1[:, kc, f * 128:(f + 1) * 128], rhs=hblk[:, kc, c0:c0 + n], start=(kc == 0), stop=(kc == 7)), reads=[a1, hblk], writes=[p1])
                                    for kc in range(8):
                                        kb.op('pe', lambda e, kc=kc, p3=p3, a3=a3, f=f, c0=c0, n=n: e.matmul(p3[:, 0:n], lhsT=a3[:, kc, f * 128:(f + 1) * 128], rhs=hblk[:, kc, c0:c0 + n], start=(kc == 0), stop=(kc == 7)), reads=[a3, hblk], writes=[p3])
                                    kb.op('act', lambda e, p1=p1, n=n: e.activation(out=sil[:, 0:n], in_=p1[:, 0:n], func=AF.Silu), reads=[p1], writes=[sil])
                                    kb.op('dve', lambda e, p3=p3, g_=g_, f=f, c0=c0, n=n: e.tensor_tensor(out=g_[:, f, c0:c0 + n], in0=p3[:, 0:n], in1=sil[:, 0:n], op=ALU.mult), reads=[p3, sil], writes=[g_])
                            for s_ in range(ntl):
                                for hf in range(2):
                                    py = pbank[4 + (s_ * 2 + hf) % 2]
                                    for f in range(4):
                                        kb.op('pe', lambda e, f=f, py=py, g_=g_, a2=a2, s_=s_, hf=hf: e.matmul(py[:, :], lhsT=g_[:, f, s_ * 128:(s_ + 1) * 128], rhs=a2[:, f, hf * 512:(hf + 1) * 512], start=(f == 0), stop=(f == 3)), reads=[g_, a2], writes=[py])
                                    if is_moe:
                                        kb.op('dve', lambda e, py=py, s_=s_, hf=hf, ex=ex: e.scalar_tensor_tensor(out=acc[:, s_, hf * 512:(hf + 1) * 512], in0=py[:, :], scalar=gates[:, s_, ex:ex + 1], in1=acc[:, s_, hf * 512:(hf + 1) * 512], op0=ALU.mult, op1=ALU.add), reads=[py, gates, acc], writes=[acc])
                                    else:
                                        kb.op('dve', lambda e, py=py, s_=s_, hf=hf: e.tensor_tensor(out=acc[:, s_, hf * 512:(hf + 1) * 512], in0=py[:, :], in1=acc[:, s_, hf * 512:(hf + 1) * 512], op=ALU.add), reads=[py, acc], writes=[acc])
                    for s_ in range(ntl):
                        ti = t0 // 128 + s_
                        kind = 0 if ti < NLT else 1
                        if layer == DEPTH - 1 and kind == 1:
                            continue
                        x1_ = x1t[ti % 2]; xo_ = xo[ti % 2]
                        kb.dma('sp', x1_[:], S['x1'][ti], reads=[D_['x1']], writes=[x1_])
                        kb.op('dve', lambda e, s_=s_, kind=kind: e.tensor_tensor(out=rr[:], in0=acc[:, s_, :], in1=md5[:, kind, :], op=ALU.mult), reads=[acc, md5], writes=[rr])
                        kb.op('dve', lambda e, x1_=x1_: e.scalar_tensor_tensor(out=rr[:], in0=x1_[:], scalar=ALPHA, in1=rr[:], op0=ALU.mult, op1=ALU.add), reads=[x1_, rr], writes=[rr])
                        kb.op('act', lambda e: e.activation(out=jk[:], in_=rr[:], func=AF.Identity, accum_out=st[:, 0:1]), reads=[rr], writes=[jk, st])
                        kb.op('act', lambda e: e.activation(out=jk[:], in_=rr[:], func=AF.Square, accum_out=st[:, 1:2]), reads=[rr], writes=[jk, st])
                        kb.op('dve', lambda e: e.tensor_scalar_mul(out=st[:, 2:4], in0=st[:, 0:2], scalar1=1.0 / D), reads=[st], writes=[st])
                        kb.op('dve', lambda e: e.tensor_tensor(out=st[:, 4:5], in0=st[:, 2:3], in1=st[:, 2:3], op=ALU.mult), reads=[st], writes=[st])
                        kb.op('dve', lambda e: e.tensor_tensor(out=st[:, 5:6], in0=st[:, 3:4], in1=st[:, 4:5], op=ALU.subtract), reads=[st], writes=[st])
                        kb.op('act', lambda e: e.activation(out=st[:, 6:7], in_=st[:, 5:6], func=AF.Sqrt, bias=epsl[:, 0:1]), reads=[st, epsl], writes=[st])
                        kb.op('dve', lambda e: e.reciprocal(out=st[:, 6:7], in_=st[:, 6:7]), reads=[st], writes=[st])
                        kb.op('dve', lambda e, xo_=xo_: e.tensor_scalar(out=xo_[:], in0=rr[:], scalar1=st[:, 2:3], scalar2=st[:, 6:7], op0=ALU.subtract, op1=ALU.mult), reads=[rr, st], writes=[xo_])
                        kb.op('dve', lambda e, xo_=xo_: e.tensor_tensor(out=xo_[:], in0=xo_[:], in1=lng[:, 0, :], op=ALU.mult), reads=[xo_, lng], writes=[xo_])
                        kb.op('dve', lambda e, xo_=xo_: e.tensor_tensor(out=xo_[:], in0=xo_[:], in1=lng[:, 1, :], op=ALU.add), reads=[xo_, lng], writes=[xo_])
                        if layer == DEPTH - 1:
                            kb.dma('sp', OUT[ti * 128:(ti + 1) * 128, :], xo_[:], reads=[xo_])
                        else:
                            kb.dma('sp', S['xcur'][ti], xo_[:], reads=[xo_], writes=[D_['xcur']])
                if debug == 'L':
                    kb.barrier()
                    dbg_out['xcur'] = nc.dram_tensor('dbg_xcur', [NT, 128, D], F32, kind="ExternalOutput").ap()
                    kb.dma('sp', dbg_out['xcur'], S['xcur'], reads=[D_['xcur']])
                kb.barrier()

        kb.barrier()
    return nc, dbg_out


def host_consts():
    c = {}
    c['ident'] = np.eye(128, dtype=np.float32)
    t = np.arange(SEQ)
    prow, pcol = t // 64, t % 64
    inv = (10000.0 ** (-np.arange(8, dtype=np.float32) / 8)).astype(np.float32)
    cosr = np.ones((T, 16), np.float32); sinr = np.zeros((T, 16), np.float32)
    ar = prow[:, None].astype(np.float32) * inv[None, :]
    ac = pcol[:, None].astype(np.float32) * inv[None, :]
    cosr[:SEQ, 0:8] = np.cos(ar); cosr[:SEQ, 8:16] = np.cos(ac)
    sinr[:SEQ, 0:8] = np.sin(ar); sinr[:SEQ, 8:16] = np.sin(ac)
    c['ropec'] = np.ascontiguousarray(cosr.reshape(NT, 128, 16).transpose(1, 0, 2))
    c['ropes'] = np.ascontiguousarray(sinr.reshape(NT, 128, 16).transpose(1, 0, 2))
    c['iota128'] = np.arange(128, dtype=np.float32)[None, :]
    m32 = np.zeros((128, 6), np.float32)
    for p_ in range(128):
        m32[p_, p_ // 32] = 1.0
        m32[p_, 4 + p_ // 64] = 1.0
    c['m32'] = m32
    c['iotaT'] = np.stack([np.arange(T, dtype=np.float32), (T - 1) - np.arange(T, dtype=np.float32)])
    cc = np.arange(NT, dtype=np.float32)
    c['cv'] = np.stack([127.0 - 128.0 * cc, 1.0 + 128.0 * cc]).astype(np.float32)
    mC = np.zeros((4, 128, 128), np.float32)
    for b in range(4):
        for co in range(128):
            for st in range(128):
                if co // 16 == b * 2 + st // 64:
                    mC[b, co, st] = 1.0
    c['maskC'] = mC
    c['maskB'] = np.ascontiguousarray(mC.transpose(0, 2, 1))
    return c


def rpb_toeplitz(rpb):
    kc = np.arange(64)[:, None]; qc = np.arange(64)[None, :]
    c0 = np.clip(qc - 8, 0, 48)
    inwin = (kc >= c0) & (kc <= c0 + 15)
    idx = np.clip(kc - qc + 15, 0, 30)
    g = rpb[:, :, :, idx]
    return np.where(inwin[None, None, None], g, np.float32(-30000.0)).astype(np.float32)


_CACHE = {}


def make_in_maps(inputs, ncores=8):
    consts = host_consts()
    shared = {}
    for k, v in inputs.items():
        if k in ('x', 'c', 'ctx', 'c_ctx', 'na_rpb'):
            continue
        shared[k] = np.ascontiguousarray(np.asarray(v, dtype=np.float32))
    shared['rpbT'] = rpb_toeplitz(np.asarray(inputs['na_rpb'], dtype=np.float32))
    shared['c_ctx'] = np.asarray(inputs['c_ctx'], np.float32).reshape(1, D)
    shared.update(consts)
    maps = []
    for b in range(ncores):
        m = dict(shared)
        m['x'] = np.ascontiguousarray(np.asarray(inputs['x'][b], np.float32))
        m['ctx'] = np.ascontiguousarray(np.asarray(inputs['ctx'][b], np.float32))
        m['c'] = np.asarray(inputs['c'][b], np.float32).reshape(1, D)
        maps.append(m)
    return maps


def kernel(**inputs):
    if 'nc' not in _CACHE:
        _CACHE['nc'] = build_program()[0]
    nc = _CACHE['nc']
    maps = make_in_maps(inputs, 8)
    res = run_bass_kernel_spmd(nc, maps, core_ids=list(range(8)))
    return np.stack([np.asarray(r['out'], dtype=np.float32) for r in res.results], axis=0)
```

```python
import math
import contextlib
import numpy as np
import ml_dtypes
import concourse.bass as bass
import concourse.mybir as mybir
from concourse.bass_utils import run_bass_kernel_spmd

F32 = mybir.dt.float32
BF16 = mybir.dt.bfloat16
ALU = mybir.AluOpType
AF = mybir.ActivationFunctionType
AX = mybir.AxisListType

D = 1024
SEQ = 4096
CTX = 256
T = SEQ + CTX
NT = T // 128
NLT = SEQ // 128
DEPTH = 4
IN_COLS = 2208
D_FF = 3584
NEXP = 8
ALPHA = (2 * DEPTH) ** 0.25
LN_EPS = 1e-5
RMS_EPS = 1e-6
NA_SCALE = 64 ** -0.5
MLA_SCALE = 96 ** -0.5
DIFF_SCALE = 32 ** -0.5
C_NAQ, C_NAK, C_S5U, C_MQ, C_MK, C_DQ, C_DK = 0, 2, 4, 6, 10, 14, 18
NFM = 22
NBLK = 25
NSLOT = NBLK * 512
I32 = mybir.dt.int32


class KB:
    def __init__(self, nc, es):
        self.nc = nc
        self.es = es
        self.eng = {'pe': nc.tensor, 'act': nc.scalar, 'dve': nc.vector, 'pool': nc.gpsimd, 'sp': nc.sync}
        self.sem = {}
        self.cnt = {}
        for e in ('pe', 'act', 'dve', 'pool'):
            self.sem[e] = es.enter_context(nc.semaphore('sem_' + e))
            self.cnt[e] = 0
        self.KD = 8
        self.dsem = {}
        self.dcnt = {}
        for q in ('sp', 'pool'):
            self.dsem[q] = [es.enter_context(nc.semaphore('dsem_%s%d' % (q, i))) for i in range(self.KD)]
            self.dcnt[q] = 0
        self.waited = {e: {} for e in self.eng}
        self.semobj = {}
        for e in self.sem:
            self.semobj[e] = self.sem[e]
        for q in self.dsem:
            for i, s in enumerate(self.dsem[q]):
                self.semobj[(q, i)] = s

    def _wait(self, e, tok):
        key, val = tok
        if self.waited[e].get(key, 0) >= val:
            return
        self.eng[e].wait_ge(self.semobj[key], val)
        self.waited[e][key] = val

    def _deps(self, e, reads, writes):
        deps = {}

        def add(tok):
            if tok is None:
                return
            k, v = tok
            if e == 'pe' and k == 'pe':
                return
            if deps.get(k, 0) < v:
                deps[k] = v
        for t in reads:
            add(t.w)
        for t in writes:
            add(t.w)
            for k, v in t.r.items():
                add((k, v))
        for k, v in deps.items():
            self._wait(e, (k, v))

    def _mark(self, tok, reads, writes):
        k, v = tok
        for t in reads:
            if t.r.get(k, 0) < v:
                t.r[k] = v
        for t in writes:
            t.w = tok
            t.r = {}

    def op(self, e, fn, reads=(), writes=()):
        self._deps(e, reads, writes)
        inst = fn(self.eng[e])
        self.cnt[e] += 1
        inst.then_inc(self.sem[e], 1)
        self._mark((e, self.cnt[e]), reads, writes)

    def dma(self, q, out, in_, reads=(), writes=()):
        self._deps(q, reads, writes)
        n = self.dcnt[q]
        k = n % self.KD
        gen = n // self.KD + 1
        if gen > 1:
            self._wait(q, ((q, k), 16 * (gen - 1)))
        self.eng[q].dma_start(out=out, in_=in_).then_inc(self.dsem[q][k], 16)
        self.dcnt[q] += 1
        self._mark(((q, k), 16 * gen), reads, writes)

    def idma(self, out, out_offset, in_, in_offset, reads=(), writes=()):
        q = 'pool'
        self._deps(q, reads, writes)
        n = self.dcnt[q]
        k = n % self.KD
        gen = n // self.KD + 1
        if gen > 1:
            self._wait(q, ((q, k), 16 * (gen - 1)))
        self.eng[q].indirect_dma_start(out=out, out_offset=out_offset, in_=in_, in_offset=in_offset).then_inc(self.dsem[q][k], 16)
        self.dcnt[q] += 1
        self._mark(((q, k), 16 * gen), reads, writes)

    def barrier(self):
        toks = [(e, self.cnt[e]) for e in self.cnt if self.cnt[e] > 0]
        for q in self.dsem:
            n = self.dcnt[q]
            for k in range(self.KD):
                uses = (n - k + self.KD - 1) // self.KD if n > k else 0
                if uses > 0:
                    toks.append(((q, k), 16 * uses))
        for e in self.eng:
            for tok in toks:
                self._wait(e, tok)


class Tl:
    def __init__(self, t):
        self.t = t
        self.w = None
        self.r = {}

    def __getitem__(self, key):
        return self.t[key]


def build_program(nlayers=DEPTH, debug=None):
    nc = bass.Bass("TRN2", target_bir_lowering=False)
    es0 = contextlib.ExitStack()

    def din(name, shape, dt=F32):
        return nc.dram_tensor(name, list(shape), dt, kind="ExternalInput").ap()

    def dscr(name, shape, dt):
        return nc.dram_tensor(name, list(shape), dt, kind="Internal").ap()

    I = {}
    I['x'] = din('x', [SEQ, D]); I['ctx'] = din('ctx', [CTX, D]); I['c'] = din('c', [1, D]); I['c_ctx'] = din('c_ctx', [1, D])
    I['w_ada'] = din('w_ada', [DEPTH, D, 6 * D]); I['b_ada'] = din('b_ada', [DEPTH, 6 * D])
    I['w_in'] = din('w_in', [DEPTH, D, IN_COLS])
    I['rpbT'] = din('rpbT', [DEPTH, 4, 15, 64, 64])
    I['mla_q_norm'] = din('mla_q_norm', [DEPTH, 256]); I['mla_kv_norm'] = din('mla_kv_norm', [DEPTH, 128])
    I['mla_w_uq'] = din('mla_w_uq', [DEPTH, 256, 384]); I['mla_w_ukv'] = din('mla_w_ukv', [DEPTH, 128, 512])
    for nm in ('s5_lam_re', 's5_lam_im'):
        I[nm] = din(nm, [DEPTH, 2, 16, 64])
    I['s5_log_dt'] = din('s5_log_dt', [DEPTH, 2, 16])
    for nm in ('s5_b_re', 's5_b_im'):
        I[nm] = din(nm, [DEPTH, 2, 16, 64, 16])
    for nm in ('s5_c_re', 's5_c_im'):
        I[nm] = din(nm, [DEPTH, 2, 16, 16, 64])
    I['s5_d'] = din('s5_d', [DEPTH, 256]); I['s5_w_glu'] = din('s5_w_glu', [DEPTH, 256, 512])
    for nm in ('diff_lam_q1', 'diff_lam_k1', 'diff_lam_q2', 'diff_lam_k2'):
        I[nm] = din(nm, [DEPTH, 32])
    I['diff_subln'] = din('diff_subln', [DEPTH, 64])
    I['w_branch'] = din('w_branch', [DEPTH, 4, 256, D]); I['w_gate'] = din('w_gate', [DEPTH, D, 4 * D]); I['b_gate'] = din('b_gate', [DEPTH, 4 * D])
    I['w_out'] = din('w_out', [DEPTH, D, D])
    for nm in ('ln1_g', 'ln1_b', 'ln2_g', 'ln2_b'):
        I[nm] = din(nm, [DEPTH, D])
    for nm in ('ffn_w1', 'ffn_w3'):
        I[nm] = din(nm, [2, D, D_FF])
    I['ffn_w2'] = din('ffn_w2', [2, D_FF, D])
    I['moe_router'] = din('moe_router', [2, D, NEXP])
    for nm in ('moe_w1', 'moe_w3'):
        I[nm] = din(nm, [2, NEXP, D, D_FF])
    I['moe_w2'] = din('moe_w2', [2, NEXP, D_FF, D])
    I['ident'] = din('ident', [128, 128]); I['ropec'] = din('ropec', [128, NT, 16]); I['ropes'] = din('ropes', [128, NT, 16])
    I['iota128'] = din('iota128', [1, 128]); I['base1'] = din('base1', [128, 56]); I['base2'] = din('base2', [128, 28]); I['utri'] = din('utri', [128, 128]); I['bstart'] = din('bstart', [1, NBLK]); I['m32'] = din('m32', [128, 6]); I['iotaT'] = din('iotaT', [2, T]); I['cv'] = din('cv', [2, NT]); I['maskC'] = din('maskC', [4, 128, 128]); I['maskB'] = din('maskB', [4, 128, 128])

    OUT = nc.dram_tensor('out', [SEQ, D], F32, kind="ExternalOutput").ap()
    dbg_out = {}

    S = {}
    S['xcur'] = dscr('xcur', [NT, 128, D], F32)
    S['x1'] = dscr('x1s', [NT, 128, D], F32)
    S['hT'] = dscr('hTs', [NT, 128, 8, 128], BF16)
    S['h2T'] = dscr('h2Ts', [128, 8, T], BF16)
    S['FM'] = dscr('FMs', [NFM, 128, T], BF16)
    S['VV'] = dscr('VVs', [NT, 128, 12, 128], BF16)
    S['oT'] = dscr('oTs', [4, 2, 128, T], BF16)
    S['mod'] = dscr('mods', [2, 6 * D], F32)
    S['h2tm'] = dscr('h2tms', [NT, 128, D], BF16)
    S['Xs'] = dscr('Xss', [NSLOT, D], BF16)
    S['Ys'] = dscr('Yss', [NSLOT, D], F32)
    D_ = {k: Tl(v) for k, v in S.items()}

    with es0:
        kb = KB(nc, es0)
        ucnt = [0]

        def sb(es, name, shape, dt):
            ucnt[0] += 1
            return Tl(es.enter_context(nc.sbuf_tensor('%s_u%d' % (name, ucnt[0]), list(shape), dt)))

        def ps(es, name, shape, dt):
            return Tl(es.enter_context(nc.psum_tensor(name, list(shape), dt)))

        ident_f = sb(es0, 'ident_f', [128, 128], F32)
        ident = sb(es0, 'ident_b', [128, 128], BF16)
        ones_b = sb(es0, 'ones_b', [128, 128], BF16)
        condT = sb(es0, 'condT', [128, 8, 2], BF16)
        ctmp = sb(es0, 'ctmp', [128, 8, 2], F32)
        kb.dma('sp', ident_f[:], I['ident'][:], writes=[ident_f])
        kb.op('dve', lambda e: e.tensor_copy(out=ident[:], in_=ident_f[:]), reads=[ident_f], writes=[ident])
        kb.op('dve', lambda e: e.memset(ones_b[:], 1.0), writes=[ones_b])
        with nc.allow_non_contiguous_dma(reason="tiny cond vector"):
            kb.dma('sp', ctmp[:, :, 0], I['c'][0].rearrange("(k p) -> p k", p=128), writes=[ctmp])
            kb.dma('sp', ctmp[:, :, 1], I['c_ctx'][0].rearrange("(k p) -> p k", p=128), writes=[ctmp])
        kb.op('act', lambda e: e.activation(out=condT[:], in_=ctmp[:], func=AF.Silu), reads=[ctmp], writes=[condT])

        pbig = [ps(es0, 'pbig%d' % i, [128, 1024], F32) for i in range(2)]
        pbank = [ps(es0, 'pb%d' % i, [128, 512], F32) for i in range(4)]
        pbank += [Tl(pbig[0].t[:, 0:512]), Tl(pbig[0].t[:, 512:1024])]
        ptr = [Tl(pbig[1].t[:, 0:512].bitcast(BF16)), Tl(pbig[1].t[:, 512:1024].bitcast(BF16))]

        for layer in range(nlayers):
            ctx_out = layer < DEPTH - 1
            lam_init = 0.8 - 0.6 * math.exp(-0.3 * layer)
            with contextlib.ExitStack() as es:
                wst = [sb(es, 'wada%d' % i, [128, 8, 512], BF16) for i in range(2)]
                modsb = sb(es, 'modsb', [2, 6 * D], F32)
                bada = sb(es, 'bada', [2, 6 * D], F32)
                kb.dma('sp', bada[:], I['b_ada'][layer:layer + 1, :].partition_broadcast(2) if False else I['b_ada'][layer:layer + 1, :].to_broadcast([2, 6 * D]), writes=[bada])
                for cc in range(12):
                    w = wst[cc % 2]
                    kb.dma('pool', w[:], I['w_ada'][layer, :, cc * 512:(cc + 1) * 512].rearrange("(k p) n -> p k n", p=128), writes=[w])
                    pb = pbank[cc % 2]
                    for kc in range(8):
                        kb.op('pe', lambda e, kc=kc, w=w, pb=pb: e.matmul(pb[0:2, :], lhsT=condT[:, kc, :], rhs=w[:, kc, :], start=(kc == 0), stop=(kc == 7)),
                              reads=[condT, w], writes=[pb])
                    kb.op('dve', lambda e, cc=cc, pb=pb: e.tensor_tensor(out=modsb[:, cc * 512:(cc + 1) * 512], in0=pb[0:2, :], in1=bada[:, cc * 512:(cc + 1) * 512], op=ALU.add),
                          reads=[pb, bada], writes=[modsb])
                kb.dma('sp', S['mod'][:], modsb[:], reads=[modsb], writes=[D_['mod']])
                if debug == 'M':
                    dbg_out['mod'] = nc.dram_tensor('dbg_mod', [2, 6 * D], F32, kind="ExternalOutput").ap()
                    kb.dma('sp', dbg_out['mod'][:], modsb[:], reads=[modsb])
                kb.barrier()
            if debug == 'M':
                break
            with contextlib.ExitStack() as es:
                win = sb(es, 'win', [128, 8, IN_COLS], BF16)
                wuq = sb(es, 'wuq', [128, 2, 384], BF16)
                wukv = sb(es, 'wukv', [128, 512], BF16)
                kb.dma('pool', win[:], I['w_in'][layer].rearrange("(k p) n -> p k n", p=128), writes=[win])
                kb.dma('pool', wuq[:], I['mla_w_uq'][layer].rearrange("(k p) n -> p k n", p=128), writes=[wuq])
                kb.dma('pool', wukv[:], I['mla_w_ukv'][layer], writes=[wukv])
                modb = sb(es, 'modbA', [128, 2, 2, D], F32)
                for kind in range(2):
                    for j in range(2):
                        kb.dma('sp', modb[:, kind, j, :], S['mod'][kind:kind + 1, j * D:(j + 1) * D].to_broadcast([128, D]), reads=[D_['mod']], writes=[modb])
                kb.op('dve', lambda e: e.tensor_scalar_add(out=modb[:, :, 1, :], in0=modb[:, :, 1, :], scalar1=1.0), reads=[modb], writes=[modb])
                qg = sb(es, 'qg', [128, 256], F32); kvg = sb(es, 'kvg', [128, 128], F32)
                kb.dma('sp', qg[:], I['mla_q_norm'][layer:layer + 1, :].to_broadcast([128, 256]), writes=[qg])
                kb.dma('sp', kvg[:], I['mla_kv_norm'][layer:layer + 1, :].to_broadcast([128, 128]), writes=[kvg])
                rc = sb(es, 'rc', [128, NT, 16], F32); rs = sb(es, 'rs', [128, NT, 16], F32)
                kb.dma('sp', rc[:], I['ropec'][:], writes=[rc]); kb.dma('sp', rs[:], I['ropes'][:], writes=[rs])
                epsq = sb(es, 'epsq', [128, 1], F32)
                kb.op('dve', lambda e: e.memset(epsq[:], RMS_EPS), writes=[epsq])
                xt = [sb(es, 'xt%d' % i, [128, D], F32) for i in range(2)]
                htmp = sb(es, 'htmp', [128, D], F32)
                hb = sb(es, 'hb', [128, D], BF16)
                hTt = [sb(es, 'hTt%d' % i, [128, 8, 128], BF16) for i in range(2)]
                z = sb(es, 'z', [128, IN_COLS], F32)
                zb = sb(es, 'zb', [128, IN_COLS], BF16)
                fm = [sb(es, 'fm%d' % i, [128, NFM, 256], BF16) for i in range(2)]
                vv = [sb(es, 'vv%d' % i, [128, 12, 128], BF16) for i in range(2)]
                for i in range(2):
                    kb.op('pool', lambda e, i=i: e.memset(vv[i][:], 1.0), writes=[vv[i]])
                    kb.op('pool', lambda e, i=i: e.memset(fm[i][:], 0.0), writes=[fm[i]])
                ss = sb(es, 'ss', [128, 4], F32)
                junk = sb(es, 'junk', [128, 256], F32)
                qn = sb(es, 'qn', [128, 384], BF16)
                qnT = sb(es, 'qnT', [128, 3, 128], BF16)
                qf = sb(es, 'qf', [128, 384], F32)
                kvf = sb(es, 'kvf', [128, 512], F32)
                Qb = sb(es, 'Qb', [128, 4, 96], BF16)
                Kb = sb(es, 'Kb', [128, 4, 96], BF16)
                krr = sb(es, 'krr', [128, 32], F32)
                dqk = sb(es, 'dqk', [128, 512], BF16)
                rt = [sb(es, 'rt%d' % i, [128, 256], F32) for i in range(4)]
                ptoggle = [0]

                def rope(src_ap, dst_ap, G, ti, reads, writes):
                    sv = src_ap.rearrange("p (g h two f) -> p g h two f", g=G, h=2, two=2, f=8)
                    dv = dst_ap.rearrange("p (g h two f) -> p g h two f", g=G, h=2, two=2, f=8)
                    cb = rc[:, ti, :].rearrange("p (h f) -> p h f", h=2).unsqueeze(1).to_broadcast([128, G, 2, 8])
                    sn = rs[:, ti, :].rearrange("p (h f) -> p h f", h=2).unsqueeze(1).to_broadcast([128, G, 2, 8])
                    tv = [r_[:, 0:G * 16].rearrange("p (g h f) -> p g h f", g=G, h=2, f=8) for r_ in rt]
                    z1 = sv[:, :, :, 0, :]; z2 = sv[:, :, :, 1, :]
                    kb.op('dve', lambda e: e.tensor_tensor(out=tv[0], in0=z1, in1=cb, op=ALU.mult), reads=reads + [rc], writes=[rt[0]])
                    kb.op('dve', lambda e: e.tensor_tensor(out=tv[1], in0=z2, in1=sn, op=ALU.mult), reads=reads + [rs], writes=[rt[1]])
                    kb.op('dve', lambda e: e.tensor_tensor(out=tv[2], in0=z1, in1=sn, op=ALU.mult), reads=reads + [rs], writes=[rt[2]])
                    kb.op('dve', lambda e: e.tensor_tensor(out=tv[3], in0=z2, in1=cb, op=ALU.mult), reads=reads + [rc], writes=[rt[3]])
                    kb.op('dve', lambda e: e.tensor_tensor(out=dv[:, :, :, 0, :], in0=tv[0], in1=tv[1], op=ALU.subtract), reads=[rt[0], rt[1]], writes=writes)
                    kb.op('dve', lambda e: e.tensor_tensor(out=dv[:, :, :, 1, :], in0=tv[2], in1=tv[3], op=ALU.add), reads=[rt[2], rt[3]], writes=writes)

                def transposes(items, fmt, j):
                    for b0 in range(0, len(items), 8):
                        batch = items[b0:b0 + 8]
                        pt = ptr[ptoggle[0] % 2]; ptoggle[0] += 1
                        for i, (st, sap, n, ch) in enumerate(batch):
                            kb.op('pe', lambda e, i=i, sap=sap, n=n, pt=pt: e.transpose(out=pt[0:n, i * 128:(i + 1) * 128], in_=sap, identity=ident[:]),
                                  reads=[st, ident], writes=[pt])
                        for i, (st, sap, n, ch) in enumerate(batch):
                            eng = 'act' if (i % 2 == 0) else 'pool_'
                            if eng == 'act':
                                kb.op('act', lambda e, i=i, n=n, ch=ch, pt=pt: e.copy(out=fmt[0:n, ch, j * 128:(j + 1) * 128], in_=pt[0:n, i * 128:(i + 1) * 128]), reads=[pt], writes=[fmt])
                            else:
                                kb.op('dve', lambda e, i=i, n=n, ch=ch, pt=pt: e.tensor_copy(out=fmt[0:n, ch, j * 128:(j + 1) * 128], in_=pt[0:n, i * 128:(i + 1) * 128]), reads=[pt], writes=[fmt])

                for ti in range(NT):
                    kind = 0 if ti < NLT else 1
                    g, j = ti // 2, ti % 2
                    fmt = fm[g % 2]; vt = vv[ti % 2]; x_t = xt[ti % 2]; hT_t = hTt[ti % 2]
                    if layer == 0:
                        src = I['x'][ti * 128:(ti + 1) * 128, :] if kind == 0 else I['ctx'][(ti - NLT) * 128:(ti - NLT + 1) * 128, :]
                        kb.dma('sp', x_t[:], src, writes=[x_t])
                    else:
                        kb.dma('sp', x_t[:], S['xcur'][ti], reads=[D_['xcur']], writes=[x_t])
                    kb.op('dve', lambda e: e.tensor_tensor(out=htmp[:], in0=x_t[:], in1=modb[:, kind, 1, :], op=ALU.mult), reads=[x_t, modb], writes=[htmp])
                    kb.op('dve', lambda e: e.tensor_tensor(out=hb[:], in0=htmp[:], in1=modb[:, kind, 0, :], op=ALU.add), reads=[htmp, modb], writes=[hb])
                    pt = ptr[ptoggle[0] % 2]; ptoggle[0] += 1
                    for kc in range(8):
                        kb.op('pe', lambda e, kc=kc, pt=pt: e.transpose(out=pt[:, kc * 128:(kc + 1) * 128], in_=hb[:, kc * 128:(kc + 1) * 128], identity=ident[:]), reads=[hb, ident], writes=[pt])
                    kb.op('act', lambda e, pt=pt: e.copy(out=hT_t[:].rearrange("p k t -> p (k t)"), in_=pt[:]), reads=[pt], writes=[hT_t])
                    kb.dma('sp', S['hT'][ti], hT_t[:], reads=[hT_t], writes=[D_['hT']])
                    for cg in range(5):
                        c0 = cg * 512; n = min(512, IN_COLS - c0)
                        pb = pbank[cg % 4]
                        for kc in range(8):
                            kb.op('pe', lambda e, kc=kc, pb=pb, c0=c0, n=n: e.matmul(pb[:, 0:n], lhsT=hT_t[:, kc, :], rhs=win[:, kc, c0:c0 + n], start=(kc == 0), stop=(kc == 7)),
                                  reads=[hT_t, win], writes=[pb])
                        kb.op('act', lambda e, pb=pb, c0=c0, n=n: e.copy(out=z[:, c0:c0 + n], in_=pb[:, 0:n]), reads=[pb], writes=[z])
                    kb.op('pool', lambda e: e.tensor_copy(out=zb[:], in_=z[:]), reads=[z], writes=[zb])
                    kb.op('pool', lambda e: e.tensor_copy(out=vt[:, 0:4, 0:64], in_=z[:, 512:768].rearrange("p (h d) -> p h d", h=4)), reads=[z], writes=[vt])
                    kb.op('pool', lambda e: e.tensor_copy(out=vt[:, 8:12, 0:64], in_=z[:, 1952:2208].rearrange("p (h d) -> p h d", h=4)), reads=[z], writes=[vt])
                    kb.op('act', lambda e: e.activation(out=junk[:, 0:256], in_=z[:, 768:1024], func=AF.Square, accum_out=ss[:, 0:1]), reads=[z], writes=[junk, ss])
                    kb.op('act', lambda e: e.activation(out=junk[:, 0:128], in_=z[:, 1024:1152], func=AF.Square, accum_out=ss[:, 1:2]), reads=[z], writes=[junk, ss])
                    kb.op('act', lambda e: e.activation(out=ss[:, 2:3], in_=ss[:, 0:1], func=AF.Sqrt, scale=1.0 / 256, bias=epsq[:, 0:1]), reads=[ss, epsq], writes=[ss])
                    kb.op('act', lambda e: e.activation(out=ss[:, 3:4], in_=ss[:, 1:2], func=AF.Sqrt, scale=1.0 / 128, bias=epsq[:, 0:1]), reads=[ss, epsq], writes=[ss])
                    kb.op('dve', lambda e: e.reciprocal(out=ss[:, 2:4], in_=ss[:, 2:4]), reads=[ss], writes=[ss])
                    kb.op('dve', lambda e: e.scalar_tensor_tensor(out=qn[:, 0:256], in0=z[:, 768:1024], scalar=ss[:, 2:3], in1=qg[:], op0=ALU.mult, op1=ALU.mult), reads=[z, ss, qg], writes=[qn])
                    kb.op('dve', lambda e: e.scalar_tensor_tensor(out=qn[:, 256:384], in0=z[:, 1024:1152], scalar=ss[:, 3:4], in1=kvg[:], op0=ALU.mult, op1=ALU.mult), reads=[z, ss, kvg], writes=[qn])
                    pt = ptr[ptoggle[0] % 2]; ptoggle[0] += 1
                    for c3 in range(3):
                        kb.op('pe', lambda e, c3=c3, pt=pt: e.transpose(out=pt[:, c3 * 128:(c3 + 1) * 128], in_=qn[:, c3 * 128:(c3 + 1) * 128], identity=ident[:]), reads=[qn, ident], writes=[pt])
                    kb.op('act', lambda e, pt=pt: e.copy(out=qnT[:].rearrange("p k t -> p (k t)"), in_=pt[:, 0:384]), reads=[pt], writes=[qnT])
                    pq = pbank[4]; pk = pbank[5]
                    for c2 in range(2):
                        kb.op('pe', lambda e, c2=c2: e.matmul(pq[:, 0:384], lhsT=qnT[:, c2, :], rhs=wuq[:, c2, :], start=(c2 == 0), stop=(c2 == 1)), reads=[qnT, wuq], writes=[pq])
                    kb.op('pe', lambda e: e.matmul(pk[:, :], lhsT=qnT[:, 2, :], rhs=wukv[:], start=True, stop=True), reads=[qnT, wukv], writes=[pk])
                    kb.op('act', lambda e: e.copy(out=qf[:], in_=pq[:, 0:384]), reads=[pq], writes=[qf])
                    kb.op('act', lambda e: e.copy(out=kvf[:], in_=pk[:]), reads=[pk], writes=[kvf])
                    qf3 = qf[:].rearrange("p (h d) -> p h d", h=4); kv3 = kvf[:].rearrange("p (h d) -> p h d", h=4)
                    kb.op('pool', lambda e: e.tensor_copy(out=Qb[:, :, 0:64], in_=qf3[:, :, 0:64]), reads=[qf], writes=[Qb])
                    kb.op('pool', lambda e: e.tensor_copy(out=Kb[:, :, 0:64], in_=kv3[:, :, 0:64]), reads=[kvf], writes=[Kb])
                    kb.op('pool', lambda e: e.tensor_copy(out=vt[:, 4:8, 0:64], in_=kv3[:, :, 64:128]), reads=[kvf], writes=[vt])
                    for h in range(4):
                        rope(qf[:, h * 96 + 64:h * 96 + 96], Qb[:, h, 64:96], 1, ti, [qf], [Qb])
                    rope(z[:, 1152:1184], krr[:, :], 1, ti, [z], [krr])
                    kb.op('pool', lambda e: e.tensor_copy(out=Kb[:, :, 64:96], in_=krr[:].unsqueeze(1).to_broadcast([128, 4, 32])), reads=[krr], writes=[Kb])
                    rope(z[:, 1440:1696], dqk[:, 0:256], 8, ti, [z], [dqk])
                    rope(z[:, 1696:1952], dqk[:, 256:512], 8, ti, [z], [dqk])
                    items = []
                    for c2 in range(2):
                        items.append((zb, zb[:, c2 * 128:(c2 + 1) * 128], 128, C_NAQ + c2))
                        items.append((zb, zb[:, 256 + c2 * 128:256 + (c2 + 1) * 128], 128, C_NAK + c2))
                        items.append((zb, zb[:, 1184 + c2 * 128:1184 + (c2 + 1) * 128], 128, C_S5U + c2))
                    for h in range(4):
                        items.append((Qb, Qb[:, h, :], 96, C_MQ + h))
                        items.append((Kb, Kb[:, h, :], 96, C_MK + h))
                    for c2 in range(2):
                        items.append((dqk, dqk[:, c2 * 128:(c2 + 1) * 128], 128, C_DQ + c2))
                        items.append((dqk, dqk[:, 256 + c2 * 128:256 + (c2 + 1) * 128], 128, C_DK + c2))
                    transposes(items, fmt, j)
                    kb.dma('sp', S['VV'][ti], vt[:], reads=[vt], writes=[D_['VV']])
                    if j == 1:
                        kb.dma('sp', S['FM'][:, :, g * 256:(g + 1) * 256].rearrange("c p t -> p c t"), fmt[:], reads=[fmt], writes=[D_['FM']])
                if debug == 'A':
                    kb.barrier()
                    for nm, shp, dt in (('FM', [NFM, 128, T], BF16), ('VV', [NT, 128, 12, 128], BF16), ('hT', [NT, 128, 8, 128], BF16)):
                        dbg_out[nm] = nc.dram_tensor('dbg_' + nm, shp, dt, kind="ExternalOutput").ap()
                        kb.dma('sp', dbg_out[nm], S[nm], reads=[D_[nm]])
                kb.barrier()
            if debug == 'A':
                break
            with contextlib.ExitStack() as es:
                KT = sb(es, 'KT', [128, T], BF16); QT = sb(es, 'QT', [128, T], BF16)
                V = sb(es, 'Vt', [128, NT, 128], BF16)
                rd = sb(es, 'rd', [128, 512], F32)
                onb = sb(es, 'onb', [128, 512], BF16)
                o1 = sb(es, 'o1', [128, 512], F32); o2 = sb(es, 'o2', [128, 512], F32); osq = sb(es, 'osq', [128, 512], BF16)
                rs2 = sb(es, 'rs2', [128, 512], F32)
                Wt3 = [sb(es, 'Wt3_%d' % i, [128, 4, 8, 512], BF16) for i in range(3)]
                QM = [sb(es, 'QM%d' % i, [128, T], BF16) for i in range(4)]
                m32 = sb(es, 'm32', [128, 6], F32)
                kb.dma('sp', m32[:], I['m32'][:], writes=[m32])
                Grev = sb(es, 'Grev', [128, 4, 15, 64], BF16)
                lamv = sb(es, 'lamv', [128, 4, 32], F32); lamt = sb(es, 'lamt', [128, 8], F32)
                gsub = sb(es, 'gsub', [128, 1], F32); epsd = sb(es, 'epsd', [128, 1], F32)
                ecnt = [0]
                for i4, nm in enumerate(('diff_lam_q1', 'diff_lam_k1', 'diff_lam_q2', 'diff_lam_k2')):
                    kb.dma('sp', lamv[:, i4, :], I[nm][layer:layer + 1, :].to_broadcast([128, 32]), writes=[lamv])
                kb.op('dve', lambda e: e.tensor_tensor(out=lamv[:, 0, :], in0=lamv[:, 0, :], in1=lamv[:, 1, :], op=ALU.mult), reads=[lamv], writes=[lamv])
                kb.op('dve', lambda e: e.tensor_tensor(out=lamv[:, 2, :], in0=lamv[:, 2, :], in1=lamv[:, 3, :], op=ALU.mult), reads=[lamv], writes=[lamv])
                kb.op('dve', lambda e: e.reduce_sum(out=lamt[:, 0:1], in_=lamv[:, 0, :], axis=AX.X), reads=[lamv], writes=[lamt])
                kb.op('dve', lambda e: e.reduce_sum(out=lamt[:, 1:2], in_=lamv[:, 2, :], axis=AX.X), reads=[lamv], writes=[lamt])
                kb.op('act', lambda e: e.activation(out=lamt[:, 2:4], in_=lamt[:, 0:2], func=AF.Exp), reads=[lamt], writes=[lamt])
                kb.op('dve', lambda e: e.tensor_tensor(out=lamt[:, 4:5], in0=lamt[:, 3:4], in1=lamt[:, 2:3], op=ALU.subtract), reads=[lamt], writes=[lamt])
                kb.op('dve', lambda e: e.tensor_scalar_add(out=lamt[:, 5:6], in0=lamt[:, 4:5], scalar1=-lam_init), reads=[lamt], writes=[lamt])
                with nc.allow_non_contiguous_dma(reason="tiny"):
                    kb.dma('sp', gsub[0:64, :], I['diff_subln'][layer].rearrange("(p o) -> p o", o=1), writes=[gsub])
                kb.op('dve', lambda e: e.tensor_scalar_mul(out=gsub[0:64, :], in0=gsub[0:64, :], scalar1=(1.0 - lam_init)), reads=[gsub], writes=[gsub])
                kb.op('dve', lambda e: e.memset(epsd[:], RMS_EPS), writes=[epsd])
                with contextlib.ExitStack() as es2:
                    graw = sb(es2, 'graw', [128, 4, 15, 64], F32)
                    for half in range(2):
                        kb.dma('sp', graw[half * 64:(half + 1) * 64], I['rpbT'][layer].rearrange("h r k q -> k h r q"), writes=[graw])
                    for m in range(15):
                        kb.op('act', lambda e, m=m: e.activation(out=Grev[:, :, m, :], in_=graw[:, :, 14 - m, :], func=AF.Exp), reads=[graw], writes=[Grev])
                    kb.barrier()

                def build_W(jq, Wt):
                    R0 = 8 * jq; KR0 = min(max(R0 - 4, 0), 48)
                    kb.op('pool', lambda e: e.memset(Wt[:], 0.0), writes=[Wt])
                    for i in range(8):
                        for half in range(2):
                            kr = KR0 + 2 * i + half
                            al = []
                            for a in range(8):
                                r0 = min(max(R0 + a - 4, 0), 56)
                                if r0 <= kr <= r0 + 7:
                                    al.append(a)
                            if not al:
                                continue
                            a0, a1 = al[0], al[-1] + 1
                            m0 = (R0 + a0) - kr + 7
                            kb.op('pool', lambda e, i=i, half=half, a0=a0, a1=a1, m0=m0: e.tensor_copy(
                                out=Wt[half * 64:(half + 1) * 64, :, i, a0 * 64:a1 * 64].rearrange("p h (a q) -> p h a q", q=64),
                                in_=Grev[half * 64:(half + 1) * 64, :, m0:m0 + (a1 - a0), :]), reads=[Grev], writes=[Wt])
                for ci_, jq_ in enumerate((0, 1, 7)):
                    build_W(jq_, Wt3[ci_])

                E2 = [sb(es, 'E2_%d' % i, [128, 1024], BF16) for i in range(3)]

                def attn_block(kts, kr0, kd, q0, nq, scale, pso, wfn=None, Qs=None, Wsrc=None):
                    Qs = QT if Qs is None else Qs
                    pairs = [kts[i:i + 2] for i in range(0, len(kts), 2)]
                    npair = len(pairs)
                    base = ecnt[0]; ecnt[0] += npair

                    def smm(pi):
                        sc = pbig[(base + pi) % 2]
                        for j, kt in enumerate(pairs[pi]):
                            kb.op('pe', lambda e, j=j, kt=kt: e.matmul(sc[:, j * 512:j * 512 + nq], lhsT=KT[kr0:kr0 + kd, kt * 128:(kt + 1) * 128], rhs=Qs[kr0:kr0 + kd, q0:q0 + nq], start=True, stop=True),
                                  reads=[KT, Qs], writes=[sc])
                    smm(0)
                    for pi, pr in enumerate(pairs):
                        sc = pbig[(base + pi) % 2]; Et = E2[(base + pi) % 3]
                        w = len(pr)
                        if nq == 512:
                            kb.op('act', lambda e, sc=sc, Et=Et, w=w: e.activation(out=Et[:, 0:w * 512], in_=sc[:, 0:w * 512], func=AF.Exp, scale=scale), reads=[sc], writes=[Et])
                        else:
                            kb.op('act', lambda e, sc=sc, Et=Et, w=w: e.activation(out=Et[:, :].rearrange("p (j n) -> p j n", n=512)[:, 0:w, 0:nq],
                                  in_=sc[:, :].rearrange("p (j n) -> p j n", n=512)[:, 0:w, 0:nq], func=AF.Exp, scale=scale), reads=[sc], writes=[Et])
                        if pi + 1 < npair:
                            smm(pi + 1)
                        for j, kt in enumerate(pr):
                            idx = pi * 2 + j
                            wm = wfn(idx) if wfn is not None else None
                            if wm is not None:
                                kb.op('dve', lambda e, Et=Et, wm=wm, j=j: e.tensor_tensor(out=Et[:, j * 512:j * 512 + nq], in0=Et[:, j * 512:j * 512 + nq], in1=wm, op=ALU.mult), reads=[Et, Wsrc], writes=[Et])
                        for j, kt in enumerate(pr):
                            idx = pi * 2 + j
                            kb.op('pe', lambda e, kt=kt, Et=Et, idx=idx, j=j: e.matmul(pso[:, 0:nq], lhsT=V[:, kt, :], rhs=Et[:, j * 512:j * 512 + nq], start=(idx == 0), stop=(idx == len(kts) - 1)),
                                  reads=[V, Et], writes=[pso])

                def norm_store(pso, nq, dst_tile, dst_ap, dt_out_tile=None):
                    kb.op('dve', lambda e: e.reciprocal(out=rd[64:128, 0:nq], in_=pso[64:128, 0:nq]), reads=[pso], writes=[rd])
                    kb.op('dve', lambda e: e.tensor_tensor(out=dst_ap, in0=pso[0:64, 0:nq], in1=rd[64:128, 0:nq], op=ALU.mult), reads=[pso, rd], writes=[dst_tile])

                qchunks = [(j * 512, 512, True) for j in range(8)] + ([(SEQ, 256, False)] if ctx_out else [])
                all_kt = list(range(NT)); ctx_kt = [32, 33]
                pcnt = [0]
                kt0_holder = [0]
                for h in range(4):
                    kb.dma('sp', KT[:], S['FM'][C_MK + h], reads=[D_['FM']], writes=[KT])
                    kb.dma('sp', QT[:], S['FM'][C_MQ + h], reads=[D_['FM']], writes=[QT])
                    kb.dma('sp', V[:], S['VV'][:, :, 4 + h, :].rearrange("t p c -> p t c"), reads=[D_['VV']], writes=[V])
                    for (q0, nq, lat) in qchunks:
                        pso = pbank[2 + pcnt[0] % 2]; pcnt[0] += 1
                        attn_block(all_kt if lat else ctx_kt, 0, 96, q0, nq, MLA_SCALE, pso)
                        norm_store(pso, nq, onb, onb[0:64, 0:nq])
                        kb.dma('sp', S['oT'][1, h // 2, (h % 2) * 64:(h % 2) * 64 + 64, q0:q0 + nq], onb[0:64, 0:nq], reads=[onb], writes=[D_['oT']])
                for c2 in range(2):
                    kb.dma('sp', KT[:], S['FM'][C_DK + c2], reads=[D_['FM']], writes=[KT])
                    kb.dma('sp', QT[:], S['FM'][C_DQ + c2], reads=[D_['FM']], writes=[QT])
                    for i4 in range(4):
                        kb.op('dve', lambda e, i4=i4: e.tensor_scalar_mul(out=QM[i4][:], in0=QT[:], scalar1=m32[:, i4:i4 + 1]), reads=[QT, m32], writes=[QM[i4]])
                    for hh in range(2):
                        h = 2 * c2 + hh
                        kb.dma('sp', V[:], S['VV'][:, :, 8 + h, :].rearrange("t p c -> p t c"), reads=[D_['VV']], writes=[V])
                        for (q0, nq, lat) in qchunks:
                            kts = all_kt if lat else ctx_kt
                            attn_block(kts, 0, 128, q0, nq, DIFF_SCALE, pbank[2], Qs=QM[2 * hh])
                            attn_block(kts, 0, 128, q0, nq, DIFF_SCALE, pbank[3], Qs=QM[2 * hh + 1])
                            norm_store(pbank[2], nq, o1, o1[0:64, 0:nq])
                            norm_store(pbank[3], nq, o2, o2[0:64, 0:nq])
                            kb.op('dve', lambda e: e.scalar_tensor_tensor(out=o1[0:64, 0:nq], in0=o2[0:64, 0:nq], scalar=lamt[0:64, 5:6], in1=o1[0:64, 0:nq], op0=ALU.mult, op1=ALU.add),
                                  reads=[o1, o2, lamt], writes=[o1])
                            kb.op('dve', lambda e: e.tensor_tensor(out=osq[0:64, 0:nq], in0=o1[0:64, 0:nq], in1=o1[0:64, 0:nq], op=ALU.mult), reads=[o1], writes=[osq])
                            kb.op('pe', lambda e: e.matmul(pbank[0][0:64, 0:nq], lhsT=ones_b[0:64, 0:64], rhs=osq[0:64, 0:nq], start=True, stop=True), reads=[ones_b, osq], writes=[pbank[0]])
                            kb.op('act', lambda e: e.activation(out=rs2[0:64, 0:nq], in_=pbank[0][0:64, 0:nq], func=AF.Sqrt, scale=1.0 / 64, bias=epsd[0:64, 0:1]), reads=[pbank[0], epsd], writes=[rs2])
                            kb.op('dve', lambda e: e.reciprocal(out=rs2[0:64, 0:nq], in_=rs2[0:64, 0:nq]), reads=[rs2], writes=[rs2])
                            kb.op('dve', lambda e: e.scalar_tensor_tensor(out=onb[0:64, 0:nq], in0=o1[0:64, 0:nq], scalar=gsub[0:64, 0:1], in1=rs2[0:64, 0:nq], op0=ALU.mult, op1=ALU.mult),
                                  reads=[o1, gsub, rs2], writes=[onb])
                            kb.dma('sp', S['oT'][3, h // 2, (h % 2) * 64:(h % 2) * 64 + 64, q0:q0 + nq], onb[0:64, 0:nq], reads=[onb], writes=[D_['oT']])
                for c2 in range(2):
                    kb.dma('sp', KT[:], S['FM'][C_NAK + c2], reads=[D_['FM']], writes=[KT])
                    kb.dma('sp', QT[:], S['FM'][C_NAQ + c2], reads=[D_['FM']], writes=[QT])
                    for hh in range(2):
                        kb.op('dve', lambda e, hh=hh: e.tensor_scalar_mul(out=QM[hh][:], in0=QT[:], scalar1=m32[:, 4 + hh:5 + hh]), reads=[QT, m32], writes=[QM[hh]])
                    for hh in range(2):
                        h = 2 * c2 + hh
                        kb.dma('sp', V[:], S['VV'][:, :, h, :].rearrange("t p c -> p t c"), reads=[D_['VV']], writes=[V])
                        for (q0, nq, lat) in qchunks:
                            pso = pbank[2 + pcnt[0] % 2]; pcnt[0] += 1
                            if lat:
                                jq = q0 // 512
                                Wc = Wt3[0] if jq == 0 else (Wt3[2] if jq == 7 else Wt3[1])
                                kt0 = min(max(8 * jq - 4, 0), 48) // 2
                                kts = list(range(kt0, kt0 + 8)) + ctx_kt
                                wfn = (lambda idx, h=h, Wc=Wc: Wc[:, h, idx, :] if idx < 8 else None)
                                attn_block(kts, 0, 128, q0, nq, NA_SCALE, pso, wfn, Qs=QM[hh], Wsrc=Wc)
                            else:
                                attn_block(ctx_kt, 0, 128, q0, nq, NA_SCALE, pso, Qs=QM[hh])
                            norm_store(pso, nq, onb, onb[0:64, 0:nq])
                            kb.dma('sp', S['oT'][0, h // 2, (h % 2) * 64:(h % 2) * 64 + 64, q0:q0 + nq], onb[0:64, 0:nq], reads=[onb], writes=[D_['oT']])
                if debug == 'B':
                    kb.barrier()
                    dbg_out['oT'] = nc.dram_tensor('dbg_oT', [4, 2, 128, T], BF16, kind="ExternalOutput").ap()
                    kb.dma('sp', dbg_out['oT'], S['oT'], reads=[D_['oT']])
                kb.barrier()
            if debug == 'B':
                break
            TWO_PI = 2.0 * math.pi
            with contextlib.ExitStack() as es:
                uTn = sb(es, 'uTn', [128, T], BF16)
                iot = sb(es, 'iot', [128, T], F32)
                kb.dma('sp', iot[:, :], I['iotaT'][0:1, :].to_broadcast([128, T]), writes=[iot])
                P = sb(es, 's5p', [128, 24, 16], F32)
                LRE, LIM, DT, LR, TH, RR, M1, SN, CS, ARE, AIM, NRE, NIM, DEN, CRE, CIM, TMP = range(17)
                with nc.allow_non_contiguous_dma(reason="small s5 params"):
                    kb.dma('sp', P[:, LRE, :].rearrange("p (d k) -> p d k", d=2), I['s5_lam_re'][layer].rearrange("d (k two) p -> (two p) d k", two=2), writes=[P])
                    kb.dma('sp', P[:, LIM, :].rearrange("p (d k) -> p d k", d=2), I['s5_lam_im'][layer].rearrange("d (k two) p -> (two p) d k", two=2), writes=[P])
                    for two in range(2):
                        kb.dma('sp', P[two * 64:(two + 1) * 64, DT, :].rearrange("p (d k) -> p d k", d=2),
                               I['s5_log_dt'][layer].rearrange("d (k two) -> two d k", two=2)[two:two + 1].to_broadcast([64, 2, 8]), writes=[P])
                def sm(fn, *a, **k):
                    kb.op('dve', fn, reads=[P], writes=[P])
                kb.op('act', lambda e: e.activation(out=P[:, DT, :], in_=P[:, DT, :], func=AF.Exp), reads=[P], writes=[P])
                sm(lambda e: e.tensor_tensor(out=P[:, LR, :], in0=P[:, LRE, :], in1=P[:, DT, :], op=ALU.mult))
                sm(lambda e: e.tensor_tensor(out=P[:, TH, :], in0=P[:, LIM, :], in1=P[:, DT, :], op=ALU.mult))
                kb.op('act', lambda e: e.activation(out=P[:, RR, :], in_=P[:, LR, :], func=AF.Exp), reads=[P], writes=[P])
                sm(lambda e: e.tensor_scalar(out=P[:, TMP, :], in0=P[:, TH, :], scalar1=1.0 / TWO_PI, scalar2=12582912.0, op0=ALU.mult, op1=ALU.add))
                sm(lambda e: e.tensor_scalar_add(out=P[:, TMP, :], in0=P[:, TMP, :], scalar1=-12582912.0))
                sm(lambda e: e.scalar_tensor_tensor(out=P[:, M1, :], in0=P[:, TMP, :], scalar=-TWO_PI, in1=P[:, TH, :], op0=ALU.mult, op1=ALU.add))
                sm(lambda e: e.tensor_scalar(out=P[:, M1, :], in0=P[:, M1, :], scalar1=-3.14159, scalar2=3.14159, op0=ALU.max, op1=ALU.min))
                kb.op('act', lambda e: e.activation(out=P[:, SN, :], in_=P[:, M1, :], func=AF.Sin), reads=[P], writes=[P])
                sm(lambda e: e.tensor_scalar_add(out=P[:, CS, :], in0=P[:, TH, :], scalar1=0.5 * math.pi))
                sm(lambda e: e.tensor_scalar(out=P[:, TMP, :], in0=P[:, CS, :], scalar1=1.0 / TWO_PI, scalar2=12582912.0, op0=ALU.mult, op1=ALU.add))
                sm(lambda e: e.tensor_scalar_add(out=P[:, TMP, :], in0=P[:, TMP, :], scalar1=-12582912.0))
                sm(lambda e: e.scalar_tensor_tensor(out=P[:, M1, :], in0=P[:, TMP, :], scalar=-TWO_PI, in1=P[:, CS, :], op0=ALU.mult, op1=ALU.add))
                sm(lambda e: e.tensor_scalar(out=P[:, M1, :], in0=P[:, M1, :], scalar1=-3.14159, scalar2=3.14159, op0=ALU.max, op1=ALU.min))
                kb.op('act', lambda e: e.activation(out=P[:, CS, :], in_=P[:, M1, :], func=AF.Sin), reads=[P], writes=[P])
                sm(lambda e: e.tensor_tensor(out=P[:, ARE, :], in0=P[:, RR, :], in1=P[:, CS, :], op=ALU.mult))
                sm(lambda e: e.tensor_tensor(out=P[:, AIM, :], in0=P[:, RR, :], in1=P[:, SN, :], op=ALU.mult))
                sm(lambda e: e.tensor_scalar_add(out=P[:, ARE, :], in0=P[:, ARE, :], scalar1=-1.0))
                sm(lambda e: e.tensor_tensor(out=P[:, NRE, :], in0=P[:, ARE, :], in1=P[:, LRE, :], op=ALU.mult))
                sm(lambda e: e.tensor_tensor(out=P[:, TMP, :], in0=P[:, AIM, :], in1=P[:, LIM, :], op=ALU.mult))
                sm(lambda e: e.tensor_tensor(out=P[:, NRE, :], in0=P[:, NRE, :], in1=P[:, TMP, :], op=ALU.add))
                sm(lambda e: e.tensor_tensor(out=P[:, NIM, :], in0=P[:, AIM, :], in1=P[:, LRE, :], op=ALU.mult))
                sm(lambda e: e.tensor_tensor(out=P[:, TMP, :], in0=P[:, ARE, :], in1=P[:, LIM, :], op=ALU.mult))
                sm(lambda e: e.tensor_tensor(out=P[:, NIM, :], in0=P[:, NIM, :], in1=P[:, TMP, :], op=ALU.subtract))
                sm(lambda e: e.tensor_tensor(out=P[:, DEN, :], in0=P[:, LRE, :], in1=P[:, LRE, :], op=ALU.mult))
                sm(lambda e: e.tensor_tensor(out=P[:, TMP, :], in0=P[:, LIM, :], in1=P[:, LIM, :], op=ALU.mult))
                sm(lambda e: e.tensor_tensor(out=P[:, DEN, :], in0=P[:, DEN, :], in1=P[:, TMP, :], op=ALU.add))
                sm(lambda e: e.reciprocal(out=P[:, DEN, :], in_=P[:, DEN, :]))
                sm(lambda e: e.tensor_tensor(out=P[:, CRE, :], in0=P[:, NRE, :], in1=P[:, DEN, :], op=ALU.mult))
                sm(lambda e: e.tensor_tensor(out=P[:, CIM, :], in0=P[:, NIM, :], in1=P[:, DEN, :], op=ALU.mult))
                BbT = sb(es, 'BbT', [128, 16, 2, 128], BF16); CT = sb(es, 'CT', [128, 16, 2, 128], BF16)
                with contextlib.ExitStack() as es2:
                    braw = sb(es2, 'braw', [128, 2, 16, 16], F32)
                    bbar = sb(es2, 'bbar', [128, 2, 16, 16], F32)
                    btmp = sb(es2, 'btmp', [128, 16, 16], F32)
                    craw = sb(es2, 'craw', [128, 2, 4, 64], F32)
                    mB = sb(es2, 'mB', [128, 4, 128], F32); mC = sb(es2, 'mC', [128, 4, 128], F32)
                    kb.dma('sp', mB[:], I['maskB'].rearrange("b p q -> p b q"), writes=[mB]); kb.dma('sp', mC[:], I['maskC'].rearrange("b p q -> p b q"), writes=[mC])
                    with nc.allow_non_contiguous_dma(reason="s5 small"):
                        for ri, nm in enumerate(('s5_b_re', 's5_b_im')):
                            for dd in range(2):
                                kb.dma('sp', braw[:, ri, dd * 8:(dd + 1) * 8, :], I[nm][layer, dd].rearrange("(k two) p c -> (two p) k c", two=2), writes=[braw])
                        for ri, nm in enumerate(('s5_c_re', 's5_c_im')):
                            for dd in range(2):
                                kb.dma('sp', craw[:, ri, dd * 2:(dd + 1) * 2, :], I[nm][layer, dd].rearrange("(gh gl) c p -> (gl c) gh p", gl=8), writes=[craw])
                    cre_b = P[:, CRE, :].unsqueeze(2).to_broadcast([128, 16, 16]); cim_b = P[:, CIM, :].unsqueeze(2).to_broadcast([128, 16, 16])
                    kb.op('dve', lambda e: e.tensor_tensor(out=bbar[:, 0], in0=braw[:, 0], in1=cre_b, op=ALU.mult), reads=[braw, P], writes=[bbar])
                    kb.op('dve', lambda e: e.tensor_tensor(out=btmp[:], in0=braw[:, 1], in1=cim_b, op=ALU.mult), reads=[braw, P], writes=[btmp])
                    kb.op('dve', lambda e: e.tensor_tensor(out=bbar[:, 0], in0=bbar[:, 0], in1=btmp[:], op=ALU.subtract), reads=[bbar, btmp], writes=[bbar])
                    kb.op('dve', lambda e: e.tensor_tensor(out=bbar[:, 1], in0=braw[:, 1], in1=cre_b, op=ALU.mult), reads=[braw, P], writes=[bbar])
                    kb.op('dve', lambda e: e.tensor_tensor(out=btmp[:], in0=braw[:, 0], in1=cim_b, op=ALU.mult), reads=[braw, P], writes=[btmp])
                    kb.op('dve', lambda e: e.tensor_tensor(out=bbar[:, 1], in0=bbar[:, 1], in1=btmp[:], op=ALU.add), reads=[bbar, btmp], writes=[bbar])
                    pad = [sb(es2, 'pad%d' % i, [128, 128], BF16) for i in range(2)]
                    pc = [0]
                    for tile in range(16):
                        dd, k = tile // 8, tile % 8
                        for ri in range(2):
                            pd = pad[pc[0] % 2]; pt = ptr[pc[0] % 2]; pc[0] += 1
                            kb.op('dve', lambda e, pd=pd, tile=tile, ri=ri, k=k: e.tensor_tensor(out=pd[:].rearrange("p (a c) -> p a c", c=16), in0=bbar[:, ri, tile, :].unsqueeze(1).to_broadcast([128, 8, 16]),
                                  in1=mB[:, k % 4, :].rearrange("p (a c) -> p a c", c=16), op=ALU.mult), reads=[bbar, mB], writes=[pd])
                            kb.op('pe', lambda e, pd=pd, pt=pt: e.transpose(out=pt[:, 0:128], in_=pd[:], identity=ident[:]), reads=[pd, ident], writes=[pt])
                            kb.op('act', lambda e, pt=pt, tile=tile, ri=ri: e.copy(out=BbT[:, tile, ri, :], in_=pt[:, 0:128]), reads=[pt], writes=[BbT])
                            pd = pad[pc[0] % 2]; pt = ptr[pc[0] % 2]; pc[0] += 1
                            kb.op('dve', lambda e, pd=pd, dd=dd, ri=ri, k=k: e.scalar_tensor_tensor(out=pd[:].rearrange("p (a c) -> p a c", c=64), in0=craw[:, ri, dd * 2 + k // 4, :].unsqueeze(1).to_broadcast([128, 2, 64]),
                                  scalar=(1.0 if ri == 0 else -1.0), in1=mC[:, k % 4, :].rearrange("p (a c) -> p a c", c=64), op0=ALU.mult, op1=ALU.mult), reads=[craw, mC], writes=[pd])
                            kb.op('pe', lambda e, pd=pd, pt=pt: e.transpose(out=pt[:, 0:128], in_=pd[:], identity=ident[:]), reads=[pd, ident], writes=[pt])
                            kb.op('act', lambda e, pt=pt, tile=tile, ri=ri: e.copy(out=CT[:, tile, ri, :], in_=pt[:, 0:128]), reads=[pt], writes=[CT])
                    kb.barrier()
                yacc = sb(es, 'yacc', [128, 2, T], F32)
                dsk = sb(es, 'dsk', [128, 2], F32)
                with nc.allow_non_contiguous_dma(reason="tiny"):
                    kb.dma('sp', dsk[:], I['s5_d'][layer].rearrange("(c p) -> p c", p=128), writes=[dsk])
                for c2 in range(2):
                    kb.dma('sp', uTn[:], S['FM'][C_S5U + c2], reads=[D_['FM']], writes=[uTn])
                    kb.op('pool', lambda e, c2=c2: e.tensor_scalar_mul(out=yacc[:, c2, :], in0=uTn[:, :], scalar1=dsk[:, c2:c2 + 1]), reads=[uTn, dsk], writes=[yacc])
                cur_c = [1]
                cosT = sb(es, 'cosT', [128, T], F32); sinT = sb(es, 'sinT', [128, T], F32)
                dre = sb(es, 'dre', [128, T], F32); dim_ = sb(es, 'dim', [128, T], F32)
                gre = sb(es, 'gre', [128, T], F32); gim = sb(es, 'gim', [128, T], F32)
                hreb = sb(es, 'hreb', [128, T], BF16); himb = sb(es, 'himb', [128, T], BF16)
                lat_chunks = [(j * 512, 512) for j in range(8)]
                for tile in range(16):
                    dd, k = tile // 8, tile % 8
                    cch = k // 4
                    seq = ([(SEQ, 256)] + lat_chunks) if dd == 0 else (lat_chunks + [(SEQ, 256)])
                    if cur_c[0] != cch:
                        kb.dma('sp', uTn[:], S['FM'][C_S5U + cch], reads=[D_['FM']], writes=[uTn])
                        cur_c[0] = cch
                    for (buf, off) in ((sinT, 0.0), (cosT, 0.5)):
                        kb.op('pool', lambda e, buf=buf, off=off: e.tensor_scalar(out=buf[:], in0=(iot[:, :] if dd == 0 else iot[:, ::-1]), scalar1=P[:, TH, tile:tile + 1], scalar2=off * math.pi, op0=ALU.mult, op1=ALU.add), reads=[iot, P], writes=[buf])
                        kb.op('dve', lambda e, buf=buf: e.tensor_scalar(out=dre[:], in0=buf[:], scalar1=1.0 / TWO_PI, scalar2=12582912.0, op0=ALU.mult, op1=ALU.add), reads=[buf], writes=[dre])
                        kb.op('dve', lambda e: e.tensor_scalar_add(out=dre[:], in0=dre[:], scalar1=-12582912.0), reads=[dre], writes=[dre])
                        kb.op('dve', lambda e, buf=buf: e.scalar_tensor_tensor(out=buf[:], in0=dre[:], scalar=-TWO_PI, in1=buf[:], op0=ALU.mult, op1=ALU.add), reads=[dre, buf], writes=[buf])
                        kb.op('dve', lambda e, buf=buf: e.tensor_scalar(out=buf[:], in0=buf[:], scalar1=-3.14159, scalar2=3.14159, op0=ALU.max, op1=ALU.min), reads=[buf], writes=[buf])
                        kb.op('act', lambda e, buf=buf: e.activation(out=buf[:], in_=buf[:], func=AF.Sin), reads=[buf], writes=[buf])
                    so = 0
                    for ci, (to, n) in enumerate(seq):
                        pr = pbank[0 + (ci % 2) * 2]; pi_ = pbank[1 + (ci % 2) * 2]
                        kb.op('pe', lambda e, pr=pr, to=to, n=n: e.matmul(pr[:, 0:n], lhsT=BbT[:, tile, 0, :], rhs=uTn[:, to:to + n], start=True, stop=True), reads=[BbT, uTn], writes=[pr])
                        kb.op('pe', lambda e, pi_=pi_, to=to, n=n: e.matmul(pi_[:, 0:n], lhsT=BbT[:, tile, 1, :], rhs=uTn[:, to:to + n], start=True, stop=True), reads=[BbT, uTn], writes=[pi_])
                        sl = slice(so, so + n)
                        kb.op('act', lambda e, pr=pr, sl=sl, n=n: e.copy(out=gre[:, sl], in_=pr[:, 0:n]), reads=[pr], writes=[gre])
                        kb.op('act', lambda e, pi_=pi_, sl=sl, n=n: e.copy(out=gim[:, sl], in_=pi_[:, 0:n]), reads=[pi_], writes=[gim])
                        so += n
                    kb.op('dve', lambda e: e.tensor_tensor(out=dre[:], in0=gre[:], in1=cosT[:], op=ALU.mult), reads=[gre, cosT], writes=[dre])
                    kb.op('pool', lambda e: e.tensor_tensor(out=dim_[:], in0=gim[:], in1=cosT[:], op=ALU.mult), reads=[gim, cosT], writes=[dim_])
                    kb.op('dve', lambda e: e.tensor_tensor(out=gim[:], in0=gim[:], in1=sinT[:], op=ALU.mult), reads=[gim, sinT], writes=[gim])
                    kb.op('dve', lambda e: e.tensor_tensor(out=gre[:], in0=gre[:], in1=sinT[:], op=ALU.mult), reads=[gre, sinT], writes=[gre])
                    kb.op('dve', lambda e: e.tensor_tensor(out=dre[:], in0=dre[:], in1=gim[:], op=ALU.add), reads=[dre, gim], writes=[dre])
                    kb.op('dve', lambda e: e.tensor_tensor(out=dim_[:], in0=dim_[:], in1=gre[:], op=ALU.subtract), reads=[dim_, gre], writes=[dim_])
                    rb = P[:, RR, tile:tile + 1].to_broadcast([128, T])
                    if dd == 0:
                        kb.op('dve', lambda e: e.tensor_tensor_scan(out=gre[:], data0=rb, data1=dre[:], initial=0.0, op0=ALU.mult, op1=ALU.add), reads=[dre, P], writes=[gre])
                        kb.op('dve', lambda e: e.tensor_tensor_scan(out=gim[:], data0=rb, data1=dim_[:], initial=0.0, op0=ALU.mult, op1=ALU.add), reads=[dim_, P], writes=[gim])
                    else:
                        kb.op('dve', lambda e: e.tensor_tensor_scan(out=gre[:, ::-1], data0=rb, data1=dre[:, ::-1], initial=0.0, op0=ALU.mult, op1=ALU.add), reads=[dre, P], writes=[gre])
                        kb.op('dve', lambda e: e.tensor_tensor_scan(out=gim[:, ::-1], data0=rb, data1=dim_[:, ::-1], initial=0.0, op0=ALU.mult, op1=ALU.add), reads=[dim_, P], writes=[gim])
                    kb.op('pool', lambda e: e.tensor_tensor(out=dre[:], in0=gre[:], in1=cosT[:], op=ALU.mult), reads=[gre, cosT], writes=[dre])
                    kb.op('dve', lambda e: e.tensor_tensor(out=dim_[:], in0=gim[:], in1=cosT[:], op=ALU.mult), reads=[gim, cosT], writes=[dim_])
                    kb.op('dve', lambda e: e.tensor_tensor(out=gim[:], in0=gim[:], in1=sinT[:], op=ALU.mult), reads=[gim, sinT], writes=[gim])
                    kb.op('dve', lambda e: e.tensor_tensor(out=gre[:], in0=gre[:], in1=sinT[:], op=ALU.mult), reads=[gre, sinT], writes=[gre])
                    kb.op('dve', lambda e: e.tensor_tensor(out=hreb[:], in0=dre[:], in1=gim[:], op=ALU.subtract), reads=[dre, gim], writes=[hreb])
                    kb.op('dve', lambda e: e.tensor_tensor(out=himb[:], in0=gre[:], in1=dim_[:], op=ALU.add), reads=[gre, dim_], writes=[himb])
                    so = 0
                    for ci, (to, n) in enumerate(seq):
                        sl = slice(so, so + n)
                        py = pbank[4 + ci % 2]
                        kb.op('pe', lambda e, py=py, sl=sl, n=n: e.matmul(py[:, 0:n], lhsT=CT[:, tile, 0, :], rhs=hreb[:, sl], start=True, stop=False), reads=[CT, hreb], writes=[py])
                        kb.op('pe', lambda e, py=py, sl=sl, n=n: e.matmul(py[:, 0:n], lhsT=CT[:, tile, 1, :], rhs=himb[:, sl], start=False, stop=True), reads=[CT, himb], writes=[py])
                        kb.op('dve', lambda e, py=py, to=to, n=n: e.tensor_tensor(out=yacc[:, cch, to:to + n], in0=py[:, 0:n], in1=yacc[:, cch, to:to + n], op=ALU.add), reads=[py, yacc], writes=[yacc])
                        so += n
                wglu = sb(es, 'wglu', [128, 2, 512], BF16)
                kb.dma('pool', wglu[:], I['s5_w_glu'][layer].rearrange("(c p) n -> p c n", p=128), writes=[wglu])
                yb = sb(es, 'yb', [128, 2, 512], BF16)
                sg = sb(es, 'sg', [128, 512], F32)
                og = sb(es, 'og', [128, 2, 512], BF16)
                for (to, n) in lat_chunks + [(SEQ, 256)]:
                    kb.op('pool', lambda e, to=to, n=n: e.tensor_copy(out=yb[:, :, 0:n], in_=yacc[:, :, to:to + n]), reads=[yacc], writes=[yb])
                    for oc in range(2):
                        pv = pbank[0]; pg = pbank[1]
                        for c2 in range(2):
                            kb.op('pe', lambda e, c2=c2, oc=oc, n=n: e.matmul(pv[:, 0:n], lhsT=wglu[:, c2, oc * 128:(oc + 1) * 128], rhs=yb[:, c2, 0:n], start=(c2 == 0), stop=(c2 == 1)), reads=[wglu, yb], writes=[pv])
                        for c2 in range(2):
                            kb.op('pe', lambda e, c2=c2, oc=oc, n=n: e.matmul(pg[:, 0:n], lhsT=wglu[:, c2, 256 + oc * 128:256 + (oc + 1) * 128], rhs=yb[:, c2, 0:n], start=(c2 == 0), stop=(c2 == 1)), reads=[wglu, yb], writes=[pg])
                        kb.op('act', lambda e, n=n: e.activation(out=sg[:, 0:n], in_=pg[:, 0:n], func=AF.Sigmoid), reads=[pg], writes=[sg])
                        kb.op('dve', lambda e, oc=oc, n=n: e.tensor_tensor(out=og[:, oc, 0:n], in0=pv[:, 0:n], in1=sg[:, 0:n], op=ALU.mult), reads=[pv, sg], writes=[og])
                    kb.dma('sp', S['oT'][2, :, :, to:to + n].rearrange("c p t -> p c t"), og[:, :, 0:n], reads=[og], writes=[D_['oT']])
                if debug == 'C':
                    kb.barrier()
                    dbg_out['oT'] = nc.dram_tensor('dbg_oT', [4, 2, 128, T], BF16, kind="ExternalOutput").ap()
                    kb.dma('sp', dbg_out['oT'], S['oT'], reads=[D_['oT']])
                kb.barrier()
            if debug == 'C':
                break
            with contextlib.ExitStack() as es:
                wg = sb(es, 'wg', [128, 8, 4 * D], BF16)
                wbr = sb(es, 'wbr', [128, 4, 2, D], BF16)
                wo = sb(es, 'wo', [128, 8, D], BF16)
                for kc in range(8):
                    kb.dma('pool', wg[:, kc, :], I['w_gate'][layer, kc * 128:(kc + 1) * 128, :], writes=[wg])
                kb.dma('pool', wbr[:], I['w_branch'][layer].rearrange("b (c p) n -> p b c n", p=128), writes=[wbr])
                kb.dma('pool', wo[:], I['w_out'][layer].rearrange("(k p) n -> p k n", p=128), writes=[wo])
                bg = sb(es, 'bg', [128, 4 * D], F32)
                kb.dma('sp', bg[:], I['b_gate'][layer:layer + 1, :].to_broadcast([128, 4 * D]), writes=[bg])
                md = sb(es, 'mdD', [128, 2, 3, D], F32)
                for kind in range(2):
                    for jj, mi in enumerate((2, 3, 4)):
                        kb.dma('sp', md[:, kind, jj, :], S['mod'][kind:kind + 1, mi * D:(mi + 1) * D].to_broadcast([128, D]), reads=[D_['mod']], writes=[md])
                kb.op('dve', lambda e: e.tensor_scalar_add(out=md[:, :, 2, :], in0=md[:, :, 2, :], scalar1=1.0), reads=[md], writes=[md])
                lng = sb(es, 'lng', [128, 2, D], F32)
                kb.dma('sp', lng[:, 0, :], I['ln1_g'][layer:layer + 1, :].to_broadcast([128, D]), writes=[lng])
                kb.dma('sp', lng[:, 1, :], I['ln1_b'][layer:layer + 1, :].to_broadcast([128, D]), writes=[lng])
                epsl = sb(es, 'epsl', [128, 1], F32)
                kb.op('dve', lambda e: e.memset(epsl[:], LN_EPS), writes=[epsl])
                hTm = [sb(es, 'hTm%d' % i, [128, 8, 128], BF16) for i in range(2)]
                oTm = [sb(es, 'oTm%d' % i, [128, 4, 2, 128], BF16) for i in range(2)]
                xm = [sb(es, 'xm%d' % i, [128, D], F32) for i in range(2)]
                gt = sb(es, 'gt', [128, D], F32); macc = sb(es, 'macc', [128, D], F32); tt = sb(es, 'tt', [128, D], F32)
                mbb = sb(es, 'mbb', [128, D], BF16); mT = sb(es, 'mT', [128, 8, 128], BF16)
                st = sb(es, 'stD', [128, 8], F32); jk = sb(es, 'jkD', [128, D], F32)
                h2b = sb(es, 'h2b', [128, D], BF16); h2T = sb(es, 'h2Tt', [128, 8, 128], BF16)
                pcd = [0]

                def layer_norm(r_t, g_ap, b_ap, out_t):
                    kb.op('act', lambda e: e.activation(out=jk[:], in_=r_t[:], func=AF.Identity, accum_out=st[:, 0:1]), reads=[r_t], writes=[jk, st])
                    kb.op('act', lambda e: e.activation(out=jk[:], in_=r_t[:], func=AF.Square, accum_out=st[:, 1:2]), reads=[r_t], writes=[jk, st])
                    kb.op('dve', lambda e: e.tensor_scalar_mul(out=st[:, 2:4], in0=st[:, 0:2], scalar1=1.0 / D), reads=[st], writes=[st])
                    kb.op('dve', lambda e: e.tensor_tensor(out=st[:, 4:5], in0=st[:, 2:3], in1=st[:, 2:3], op=ALU.mult), reads=[st], writes=[st])
                    kb.op('dve', lambda e: e.tensor_tensor(out=st[:, 5:6], in0=st[:, 3:4], in1=st[:, 4:5], op=ALU.subtract), reads=[st], writes=[st])
                    kb.op('act', lambda e: e.activation(out=st[:, 6:7], in_=st[:, 5:6], func=AF.Sqrt, bias=epsl[:, 0:1]), reads=[st, epsl], writes=[st])
                    kb.op('dve', lambda e: e.reciprocal(out=st[:, 6:7], in_=st[:, 6:7]), reads=[st], writes=[st])
                    kb.op('dve', lambda e: e.tensor_scalar(out=out_t[:], in0=r_t[:], scalar1=st[:, 2:3], scalar2=st[:, 6:7], op0=ALU.subtract, op1=ALU.mult), reads=[r_t, st], writes=[out_t])
                    kb.op('dve', lambda e: e.tensor_tensor(out=out_t[:], in0=out_t[:], in1=g_ap, op=ALU.mult), reads=[out_t, lng], writes=[out_t])
                    kb.op('dve', lambda e: e.tensor_tensor(out=out_t[:], in0=out_t[:], in1=b_ap, op=ALU.add), reads=[out_t, lng], writes=[out_t])

                for ti in range(NT):
                    kind = 0 if ti < NLT else 1
                    hT_t = hTm[ti % 2]; oT_t = oTm[ti % 2]; x_t = xm[ti % 2]
                    kb.dma('sp', hT_t[:], S['hT'][ti], reads=[D_['hT']], writes=[hT_t])
                    kb.dma('sp', oT_t[:], S['oT'][:, :, :, ti * 128:(ti + 1) * 128].rearrange("b c p t -> p b c t"), reads=[D_['oT']], writes=[oT_t])
                    if layer == 0:
                        src = I['x'][ti * 128:(ti + 1) * 128, :] if kind == 0 else I['ctx'][(ti - NLT) * 128:(ti - NLT + 1) * 128, :]
                        kb.dma('sp', x_t[:], src, writes=[x_t])
                    else:
                        kb.dma('sp', x_t[:], S['xcur'][ti], reads=[D_['xcur']], writes=[x_t])
                    for br in range(4):
                        for hf in range(2):
                            pgt = pbank[hf]
                            for kc in range(8):
                                kb.op('pe', lambda e, kc=kc, pgt=pgt, br=br, hf=hf: e.matmul(pgt[:, :], lhsT=hT_t[:, kc, :], rhs=wg[:, kc, br * D + hf * 512:br * D + (hf + 1) * 512], start=(kc == 0), stop=(kc == 7)),
                                      reads=[hT_t, wg], writes=[pgt])
                            kb.op('dve', lambda e, pgt=pgt, br=br, hf=hf: e.tensor_tensor(out=gt[:, hf * 512:(hf + 1) * 512], in0=pgt[:, :], in1=bg[:, br * D + hf * 512:br * D + (hf + 1) * 512], op=ALU.add), reads=[pgt, bg], writes=[gt])
                        kb.op('act', lambda e: e.activation(out=gt[:], in_=gt[:], func=AF.Sigmoid), reads=[gt], writes=[gt])
                        for hf in range(2):
                            pbt = pbank[2 + hf]
                            for c2 in range(2):
                                kb.op('pe', lambda e, c2=c2, pbt=pbt, br=br, hf=hf: e.matmul(pbt[:, :], lhsT=oT_t[:, br, c2, :], rhs=wbr[:, br, c2, hf * 512:(hf + 1) * 512], start=(c2 == 0), stop=(c2 == 1)),
                                      reads=[oT_t, wbr], writes=[pbt])
                            dst = macc if br == 0 else tt
                            kb.op('dve', lambda e, pbt=pbt, hf=hf, dst=dst: e.tensor_tensor(out=dst[:, hf * 512:(hf + 1) * 512], in0=pbt[:, :], in1=gt[:, hf * 512:(hf + 1) * 512], op=ALU.mult), reads=[pbt, gt], writes=[dst])
                        if br > 0:
                            kb.op('pool', lambda e: e.tensor_tensor(out=macc[:], in0=macc[:], in1=tt[:], op=ALU.add), reads=[macc, tt], writes=[macc])
                    kb.op('pool', lambda e: e.tensor_copy(out=mbb[:], in_=macc[:]), reads=[macc], writes=[mbb])
                    pt = ptr[pcd[0] % 2]; pcd[0] += 1
                    for kc in range(8):
                        kb.op('pe', lambda e, kc=kc, pt=pt: e.transpose(out=pt[:, kc * 128:(kc + 1) * 128], in_=mbb[:, kc * 128:(kc + 1) * 128], identity=ident[:]), reads=[mbb, ident], writes=[pt])
                    kb.op('act', lambda e, pt=pt: e.copy(out=mT[:].rearrange("p k t -> p (k t)"), in_=pt[:]), reads=[pt], writes=[mT])
                    for hf in range(2):
                        py = pbank[4 + hf]
                        for kc in range(8):
                            kb.op('pe', lambda e, kc=kc, py=py, hf=hf: e.matmul(py[:, :], lhsT=mT[:, kc, :], rhs=wo[:, kc, hf * 512:(hf + 1) * 512], start=(kc == 0), stop=(kc == 7)), reads=[mT, wo], writes=[py])
                        kb.op('dve', lambda e, py=py, hf=hf: e.tensor_tensor(out=tt[:, hf * 512:(hf + 1) * 512], in0=py[:, :], in1=md[:, kind, 0, hf * 512:(hf + 1) * 512], op=ALU.mult), reads=[py, md], writes=[tt])
                    kb.op('dve', lambda e: e.scalar_tensor_tensor(out=tt[:], in0=x_t[:], scalar=ALPHA, in1=tt[:], op0=ALU.mult, op1=ALU.add), reads=[x_t, tt], writes=[tt])
                    layer_norm(tt, lng[:, 0, :], lng[:, 1, :], macc)
                    kb.dma('sp', S['x1'][ti], macc[:], reads=[macc], writes=[D_['x1']])
                    kb.op('dve', lambda e: e.tensor_tensor(out=tt[:], in0=macc[:], in1=md[:, kind, 2, :], op=ALU.mult), reads=[macc, md], writes=[tt])
                    kb.op('dve', lambda e: e.tensor_tensor(out=h2b[:], in0=tt[:], in1=md[:, kind, 1, :], op=ALU.add), reads=[tt, md], writes=[h2b])
                    if layer % 2 == 1:
                        kb.dma('sp', S['h2tm'][ti], h2b[:], reads=[h2b], writes=[D_['h2tm']])
                    pt = ptr[pcd[0] % 2]; pcd[0] += 1
                    for kc in range(8):
                        kb.op('pe', lambda e, kc=kc, pt=pt: e.transpose(out=pt[:, kc * 128:(kc + 1) * 128], in_=h2b[:, kc * 128:(kc + 1) * 128], identity=ident[:]), reads=[h2b, ident], writes=[pt])
                    kb.op('act', lambda e, pt=pt: e.copy(out=h2T[:].rearrange("p k t -> p (k t)"), in_=pt[:]), reads=[pt], writes=[h2T])
                    kb.dma('sp', S['h2T'][:, :, ti * 128:(ti + 1) * 128], h2T[:], reads=[h2T], writes=[D_['h2T']])
                kb.barrier()
            if layer % 2 == 1:
              with contextlib.ExitStack() as es:
                jl = layer // 2
                hblk = sb(es, 'hblkS', [128, 8, 1024], BF16)
                wr = sb(es, 'wrS', [128, 8, NEXP], BF16)
                kb.dma('pool', wr[:], I['moe_router'][jl].rearrange("(k p) n -> p k n", p=128), writes=[wr])
                gk = sb(es, 'gkS', [128, NT, 2], F32)
                oh = sb(es, 'ohS', [128, 2, NT, NEXP], F32)
                mkb = sb(es, 'mkbS', [128, NT, NEXP], BF16)
                rank = sb(es, 'rankS', [128, NT, NEXP], F32)
                run = sb(es, 'runS', [128, NEXP], F32)
                lg = sb(es, 'lgS', [128, 4, NEXP], F32); sc = sb(es, 'scS', [128, 8], F32)
                utri = sb(es, 'utriS', [128, 128], BF16); utf = sb(es, 'utfS', [128, 128], F32)
                kb.dma('sp', utf[:], I['utri'][:], writes=[utf])
                kb.op('dve', lambda e: e.tensor_copy(out=utri[:], in_=utf[:]), reads=[utf], writes=[utri])
                bst = sb(es, 'bstS', [128, NBLK], F32)
                kb.dma('sp', bst[:], I['bstart'][0:1, :].to_broadcast([128, NBLK]), writes=[bst])
                kb.op('dve', lambda e: e.memset(run[:], 0.0), writes=[run])
                for (t0, nb) in [(b_ * 1024, min(1024, T - b_ * 1024)) for b_ in range((T + 1023) // 1024)]:
                    kb.dma('sp', hblk[:, :, 0:nb], S['h2T'][:, :, t0:t0 + nb], reads=[D_['h2T']], writes=[hblk])
                    for s_ in range(nb // 128):
                        ti = t0 // 128 + s_
                        pl = pbank[s_ % 2]
                        for kc in range(8):
                            kb.op('pe', lambda e, kc=kc, pl=pl, s_=s_: e.matmul(pl[:, 0:NEXP], lhsT=hblk[:, kc, s_ * 128:(s_ + 1) * 128], rhs=wr[:, kc, :], start=(kc == 0), stop=(kc == 7)), reads=[hblk, wr], writes=[pl])
                        kb.op('dve', lambda e, pl=pl: e.tensor_copy(out=lg[:, 0, :], in_=pl[:, 0:NEXP]), reads=[pl], writes=[lg])
                        kb.op('dve', lambda e: e.reduce_max(out=sc[:, 0:1], in_=lg[:, 0, :], axis=AX.X), reads=[lg], writes=[sc])
                        kb.op('dve', lambda e, ti=ti: e.tensor_scalar(out=oh[:, 0, ti, :], in0=lg[:, 0, :], scalar1=sc[:, 0:1], scalar2=None, op0=ALU.is_equal), reads=[lg, sc], writes=[oh])
                        kb.op('dve', lambda e, ti=ti: e.scalar_tensor_tensor(out=lg[:, 2, :], in0=oh[:, 0, ti, :], scalar=-1e30, in1=lg[:, 0, :], op0=ALU.mult, op1=ALU.add), reads=[lg, oh], writes=[lg])
                        kb.op('dve', lambda e: e.reduce_max(out=sc[:, 1:2], in_=lg[:, 2, :], axis=AX.X), reads=[lg], writes=[sc])
                        kb.op('dve', lambda e, ti=ti: e.tensor_scalar(out=oh[:, 1, ti, :], in0=lg[:, 2, :], scalar1=sc[:, 1:2], scalar2=None, op0=ALU.is_equal), reads=[lg, sc], writes=[oh])
                        kb.op('dve', lambda e: e.tensor_tensor(out=sc[:, 2:3], in0=sc[:, 1:2], in1=sc[:, 0:1], op=ALU.subtract), reads=[sc], writes=[sc])
                        kb.op('act', lambda e: e.activation(out=sc[:, 3:4], in_=sc[:, 2:3], func=AF.Exp), reads=[sc], writes=[sc])
                        kb.op('dve', lambda e: e.tensor_scalar_add(out=sc[:, 4:5], in0=sc[:, 3:4], scalar1=1.0), reads=[sc], writes=[sc])
                        kb.op('dve', lambda e, ti=ti: e.reciprocal(out=gk[:, ti, 0:1], in_=sc[:, 4:5]), reads=[sc], writes=[gk])
                        kb.op('dve', lambda e, ti=ti: e.tensor_tensor(out=gk[:, ti, 1:2], in0=sc[:, 3:4], in1=gk[:, ti, 0:1], op=ALU.mult), reads=[sc, gk], writes=[gk])
                        kb.op('dve', lambda e, ti=ti: e.tensor_tensor(out=mkb[:, ti, :], in0=oh[:, 0, ti, :], in1=oh[:, 1, ti, :], op=ALU.add), reads=[oh], writes=[mkb])
                        pr_ = pbank[2 + s_ % 2]
                        kb.op('pe', lambda e, pr_=pr_, ti=ti: e.matmul(pr_[:, 0:NEXP], lhsT=utri[:], rhs=mkb[:, ti, :], start=True, stop=True), reads=[utri, mkb], writes=[pr_])
                        kb.op('pe', lambda e, pr_=pr_, ti=ti: e.matmul(pr_[:, NEXP:2 * NEXP], lhsT=ones_b[:], rhs=mkb[:, ti, :], start=True, stop=True), reads=[ones_b, mkb], writes=[pr_])
                        kb.op('dve', lambda e, pr_=pr_, ti=ti: e.tensor_tensor(out=rank[:, ti, :], in0=pr_[:, 0:NEXP], in1=run[:], op=ALU.add), reads=[pr_, run], writes=[rank])
                        kb.op('dve', lambda e, pr_=pr_: e.tensor_tensor(out=run[:], in0=pr_[:, NEXP:2 * NEXP], in1=run[:], op=ALU.add), reads=[pr_, run], writes=[run])
                cw = sb(es, 'cwS', [128, 6, NEXP], F32)
                MAGIC = 12582912.0
                kb.op('dve', lambda e: e.tensor_scalar(out=cw[:, 3, :], in0=run[:], scalar1=511.0, scalar2=1.0 / 512, op0=ALU.add, op1=ALU.mult), reads=[run], writes=[cw])
                kb.op('dve', lambda e: e.tensor_scalar(out=cw[:, 3, :], in0=cw[:, 3, :], scalar1=(-0.5 + 1.0 / 1024), scalar2=MAGIC, op0=ALU.add, op1=ALU.add), reads=[cw], writes=[cw])
                kb.op('dve', lambda e: e.tensor_scalar(out=cw[:, 0, :], in0=cw[:, 3, :], scalar1=-MAGIC, scalar2=512.0, op0=ALU.add, op1=ALU.mult), reads=[cw], writes=[cw])
                kb.op('dve', lambda e: e.tensor_copy(out=cw[:, 1, 0:1], in_=cw[:, 0, 0:1]), reads=[cw], writes=[cw])
                for e_ in range(1, NEXP):
                    kb.op('dve', lambda e, e_=e_: e.tensor_tensor(out=cw[:, 1, e_:e_ + 1], in0=cw[:, 1, e_ - 1:e_], in1=cw[:, 0, e_:e_ + 1], op=ALU.add), reads=[cw], writes=[cw])
                kb.op('dve', lambda e: e.tensor_tensor(out=cw[:, 2, :], in0=cw[:, 1, :], in1=cw[:, 0, :], op=ALU.subtract), reads=[cw], writes=[cw])
                cmpb = sb(es, 'cmpbS', [128, NBLK, NEXP], F32)
                bef = sb(es, 'befS', [128, NBLK], F32); bei = sb(es, 'beiS', [128, NBLK], I32)
                kb.op('dve', lambda e: e.tensor_tensor(out=cmpb[:], in0=cw[:, 1, :].unsqueeze(1).to_broadcast([128, NBLK, NEXP]), in1=bst[:].unsqueeze(2).to_broadcast([128, NBLK, NEXP]), op=ALU.is_le), reads=[cw, bst], writes=[cmpb])
                kb.op('dve', lambda e: e.reduce_sum(out=bef[:], in_=cmpb[:], axis=AX.X), reads=[cmpb], writes=[bef])
                kb.op('dve', lambda e: e.tensor_scalar_min(out=bef[:], in0=bef[:], scalar1=float(NEXP - 1)), reads=[bef], writes=[bef])
                kb.op('dve', lambda e: e.tensor_copy(out=bei[:], in_=bef[:]), reads=[bef], writes=[bei])
                b1 = sb(es, 'b1S', [128, 56], F32); b2 = sb(es, 'b2S', [128, 28], F32)
                kb.dma('sp', b1[:], I['base1'][:], writes=[b1]); kb.dma('sp', b2[:], I['base2'][:], writes=[b2])
                ix1f = sb(es, 'ix1fS', [128, NBLK, 56], F32); ix2f = sb(es, 'ix2fS', [128, NBLK, 28], F32)
                ix1 = sb(es, 'ix1S', [128, NBLK, 56], I32); ix2 = sb(es, 'ix2S', [128, NBLK, 28], I32)
                kb.op('dve', lambda e: e.scalar_tensor_tensor(out=ix1f[:], in0=bef[:].unsqueeze(2).to_broadcast([128, NBLK, 56]), scalar=7168.0, in1=b1[:].unsqueeze(1).to_broadcast([128, NBLK, 56]), op0=ALU.mult, op1=ALU.add), reads=[bef, b1], writes=[ix1f])
                kb.op('dve', lambda e: e.scalar_tensor_tensor(out=ix2f[:], in0=bef[:].unsqueeze(2).to_broadcast([128, NBLK, 28]), scalar=3584.0, in1=b2[:].unsqueeze(1).to_broadcast([128, NBLK, 28]), op0=ALU.mult, op1=ALU.add), reads=[bef, b2], writes=[ix2f])
                if jl > 0:
                    kb.op('dve', lambda e: e.tensor_scalar_add(out=ix1f[:], in0=ix1f[:], scalar1=float(jl * NEXP * D * 7)), reads=[ix1f], writes=[ix1f])
                    kb.op('dve', lambda e: e.tensor_scalar_add(out=ix2f[:], in0=ix2f[:], scalar1=float(jl * NEXP * D_FF)), reads=[ix2f], writes=[ix2f])
                kb.op('dve', lambda e: e.tensor_copy(out=ix1[:], in_=ix1f[:]), reads=[ix1f], writes=[ix1])
                kb.op('dve', lambda e: e.tensor_copy(out=ix2[:], in_=ix2f[:]), reads=[ix2f], writes=[ix2])
                posf = sb(es, 'posfS', [128, 2, NT], F32); posi = sb(es, 'posiS', [128, 2, NT], I32)
                kb.op('dve', lambda e: e.tensor_tensor(out=rank[:], in0=rank[:], in1=cw[:, 2, :].unsqueeze(1).to_broadcast([128, NT, NEXP]), op=ALU.add), reads=[rank, cw], writes=[rank])
                for k2 in range(2):
                    kb.op('dve', lambda e, k2=k2: e.tensor_tensor(out=oh[:, k2], in0=oh[:, k2], in1=rank[:], op=ALU.mult), reads=[oh, rank], writes=[oh])
                    kb.op('dve', lambda e, k2=k2: e.reduce_sum(out=posf[:, k2, :], in_=oh[:, k2], axis=AX.X), reads=[oh], writes=[posf])
                kb.op('dve', lambda e: e.tensor_copy(out=posi[:], in_=posf[:]), reads=[posf], writes=[posi])
                zt = sb(es, 'ztS', [128, 4 * D], BF16)
                kb.op('pool', lambda e: e.memset(zt[:], 0.0), writes=[zt])
                for a_ in range(NSLOT // 512):
                    kb.dma('sp', S['Xs'][a_ * 512:(a_ + 1) * 512, :].rearrange("(p j) d -> p (j d)", j=4), zt[:], reads=[zt], writes=[D_['Xs']])
                h2t = [sb(es, 'h2tS%d' % i, [128, D], BF16) for i in range(2)]
                for ti in range(NT):
                    ht_ = h2t[ti % 2]
                    kb.dma('sp', ht_[:], S['h2tm'][ti], reads=[D_['h2tm']], writes=[ht_])
                    for k2 in range(2):
                        kb.idma(S['Xs'][:, :], bass.IndirectOffsetOnAxis(ap=posi[:, k2, ti:ti + 1].bitcast(mybir.dt.uint32), axis=0), ht_[:], None, reads=[ht_, posi, D_['Xs']], writes=[D_['Xs']])
                kb.barrier()
                es2 = es
                xb = sb(es2, 'xbS', [128, 4, D], BF16)
                hb2 = sb(es2, 'hb2S', [128, 8, 512], BF16)
                acc = sb(es2, 'accS', [128, 4, D], F32)
                w1c = [sb(es2, 'w1cS%d' % i, [128, 8, 512], BF16) for i in range(2)]
                w3c = [sb(es2, 'w3cS%d' % i, [128, 8, 512], BF16) for i in range(2)]
                w2c = [sb(es2, 'w2cS%d' % i, [128, 4, D], BF16) for i in range(2)]
                gT = [sb(es2, 'gTS%d' % i, [128, 4, 512], BF16) for i in range(2)]
                sil = sb(es2, 'silS', [128, 512], F32)
                W1v = I['moe_w1'].rearrange("j e k (c n) -> (j e k c) n", n=512); W3v = I['moe_w3'].rearrange("j e k (c n) -> (j e k c) n", n=512); W2v = I['moe_w2'].rearrange("j e f n -> (j e f) n")
                U32 = mybir.dt.uint32
                wcnt = [0]; pcs = [0]
                for b_ in range(NBLK):
                    kb.dma('sp', xb[:], S['Xs'][b_ * 512:(b_ + 1) * 512, :].rearrange("(s p) d -> p s d", p=128), reads=[D_['Xs']], writes=[xb])
                    for s_ in range(4):
                        pt = ptr[pcs[0] % 2]; pcs[0] += 1
                        for kc in range(8):
                            kb.op('pe', lambda e, kc=kc, pt=pt, s_=s_: e.transpose(out=pt[:, kc * 128:(kc + 1) * 128], in_=xb[:, s_, kc * 128:(kc + 1) * 128], identity=ident[:]), reads=[xb, ident], writes=[pt])
                        kb.op('act', lambda e, pt=pt, s_=s_: e.copy(out=hb2[:, :, s_ * 128:(s_ + 1) * 128], in_=pt[:].rearrange("p (k t) -> p k t", k=8)), reads=[pt], writes=[hb2])
                    kb.op('pool', lambda e: e.memset(acc[:], 0.0), writes=[acc])
                    for fc in range(D_FF // 512):
                        a1 = w1c[wcnt[0] % 2]; a3 = w3c[wcnt[0] % 2]; a2 = w2c[wcnt[0] % 2]; g_ = gT[wcnt[0] % 2]; wcnt[0] += 1
                        for kc in range(8):
                            kb.idma(a1[:, kc, :], None, W1v[:, :], bass.IndirectOffsetOnAxis(ap=ix1[:, b_, kc * 7 + fc:kc * 7 + fc + 1].bitcast(U32), axis=0), reads=[ix1], writes=[a1])
                            kb.idma(a3[:, kc, :], None, W3v[:, :], bass.IndirectOffsetOnAxis(ap=ix1[:, b_, kc * 7 + fc:kc * 7 + fc + 1].bitcast(U32), axis=0), reads=[ix1], writes=[a3])
                        for f in range(4):
                            kb.idma(a2[:, f, :], None, W2v[:, :], bass.IndirectOffsetOnAxis(ap=ix2[:, b_, fc * 4 + f:fc * 4 + f + 1].bitcast(U32), axis=0), reads=[ix2], writes=[a2])
                        for f in range(4):
                            p1 = pbank[0 + (f % 2) * 2]; p3 = pbank[1 + (f % 2) * 2]
                            for kc in range(8):
                                kb.op('pe', lambda e, kc=kc, p1=p1, a1=a1, f=f: e.matmul(p1[:, :], lhsT=a1[:, kc, f * 128:(f + 1) * 128], rhs=hb2[:, kc, :], start=(kc == 0), stop=(kc == 7)), reads=[a1, hb2], writes=[p1])
                            for kc in range(8):
                                kb.op('pe', lambda e, kc=kc, p3=p3, a3=a3, f=f: e.matmul(p3[:, :], lhsT=a3[:, kc, f * 128:(f + 1) * 128], rhs=hb2[:, kc, :], start=(kc == 0), stop=(kc == 7)), reads=[a3, hb2], writes=[p3])
                            kb.op('act', lambda e, p1=p1: e.activation(out=sil[:], in_=p1[:, :], func=AF.Silu), reads=[p1], writes=[sil])
                            kb.op('dve', lambda e, p3=p3, g_=g_, f=f: e.tensor_tensor(out=g_[:, f, :], in0=p3[:, :], in1=sil[:], op=ALU.mult), reads=[p3, sil], writes=[g_])
                        for s_ in range(4):
                            for hf in range(2):
                                py = pbank[4 + (s_ * 2 + hf) % 2]
                                for f in range(4):
                                    kb.op('pe', lambda e, f=f, py=py, g_=g_, a2=a2, s_=s_, hf=hf: e.matmul(py[:, :], lhsT=g_[:, f, s_ * 128:(s_ + 1) * 128], rhs=a2[:, f, hf * 512:(hf + 1) * 512], start=(f == 0), stop=(f == 3)), reads=[g_, a2], writes=[py])
                                kb.op('dve', lambda e, py=py, s_=s_, hf=hf: e.tensor_tensor(out=acc[:, s_, hf * 512:(hf + 1) * 512], in0=py[:, :], in1=acc[:, s_, hf * 512:(hf + 1) * 512], op=ALU.add), reads=[py, acc], writes=[acc])
                    kb.dma('sp', S['Ys'][b_ * 512:(b_ + 1) * 512, :].rearrange("(s p) d -> p s d", p=128), acc[:], reads=[acc], writes=[D_['Ys']])
                kb.barrier()
                md5 = sb(es2, 'md5S', [128, 2, D], F32)
                for kind in range(2):
                    kb.dma('sp', md5[:, kind, :], S['mod'][kind:kind + 1, 5 * D:6 * D].to_broadcast([128, D]), reads=[D_['mod']], writes=[md5])
                lng = sb(es2, 'lng2S', [128, 2, D], F32)
                kb.dma('sp', lng[:, 0, :], I['ln2_g'][layer:layer + 1, :].to_broadcast([128, D]), writes=[lng])
                kb.dma('sp', lng[:, 1, :], I['ln2_b'][layer:layer + 1, :].to_broadcast([128, D]), writes=[lng])
                epsl = sb(es2, 'epsl2S', [128, 1], F32)
                kb.op('dve', lambda e: e.memset(epsl[:], LN_EPS), writes=[epsl])
                y0 = [sb(es2, 'y0S%d' % i, [128, D], F32) for i in range(2)]; y1 = [sb(es2, 'y1S%d' % i, [128, D], F32) for i in range(2)]
                x1t = [sb(es2, 'x1tS%d' % i, [128, D], F32) for i in range(2)]
                rr = sb(es2, 'rrS', [128, D], F32); xo = [sb(es2, 'xoS%d' % i, [128, D], F32) for i in range(2)]
                st = sb(es2, 'stS', [128, 8], F32); jk = sb(es2, 'jkS', [128, D], F32)
                for ti in range(NT):
                    kind = 0 if ti < NLT else 1
                    if layer == DEPTH - 1 and kind == 1:
                        continue
                    x1_ = x1t[ti % 2]; xo_ = xo[ti % 2]; ya = y0[ti % 2]; yb_ = y1[ti % 2]
                    kb.dma('sp', x1_[:], S['x1'][ti], reads=[D_['x1']], writes=[x1_])
                    kb.idma(ya[:], None, S['Ys'][:, :], bass.IndirectOffsetOnAxis(ap=posi[:, 0, ti:ti + 1].bitcast(mybir.dt.uint32), axis=0), reads=[posi, D_['Ys']], writes=[ya])
                    kb.idma(yb_[:], None, S['Ys'][:, :], bass.IndirectOffsetOnAxis(ap=posi[:, 1, ti:ti + 1].bitcast(mybir.dt.uint32), axis=0), reads=[posi, D_['Ys']], writes=[yb_])
                    kb.op('dve', lambda e, ya=ya, ti=ti: e.tensor_scalar_mul(out=rr[:], in0=ya[:], scalar1=gk[:, ti, 0:1]), reads=[ya, gk], writes=[rr])
                    kb.op('dve', lambda e, yb_=yb_, ti=ti: e.scalar_tensor_tensor(out=rr[:], in0=yb_[:], scalar=gk[:, ti, 1:2], in1=rr[:], op0=ALU.mult, op1=ALU.add), reads=[yb_, gk, rr], writes=[rr])
                    kb.op('dve', lambda e, kind=kind: e.tensor_tensor(out=rr[:], in0=rr[:], in1=md5[:, kind, :], op=ALU.mult), reads=[rr, md5], writes=[rr])
                    kb.op('dve', lambda e, x1_=x1_: e.scalar_tensor_tensor(out=rr[:], in0=x1_[:], scalar=ALPHA, in1=rr[:], op0=ALU.mult, op1=ALU.add), reads=[x1_, rr], writes=[rr])
                    kb.op('act', lambda e: e.activation(out=jk[:], in_=rr[:], func=AF.Identity, accum_out=st[:, 0:1]), reads=[rr], writes=[jk, st])
                    kb.op('act', lambda e: e.activation(out=jk[:], in_=rr[:], func=AF.Square, accum_out=st[:, 1:2]), reads=[rr], writes=[jk, st])
                    kb.op('dve', lambda e: e.tensor_scalar_mul(out=st[:, 2:4], in0=st[:, 0:2], scalar1=1.0 / D), reads=[st], writes=[st])
                    kb.op('dve', lambda e: e.tensor_tensor(out=st[:, 4:5], in0=st[:, 2:3], in1=st[:, 2:3], op=ALU.mult), reads=[st], writes=[st])
                    kb.op('dve', lambda e: e.tensor_tensor(out=st[:, 5:6], in0=st[:, 3:4], in1=st[:, 4:5], op=ALU.subtract), reads=[st], writes=[st])
                    kb.op('act', lambda e: e.activation(out=st[:, 6:7], in_=st[:, 5:6], func=AF.Sqrt, bias=epsl[:, 0:1]), reads=[st, epsl], writes=[st])
                    kb.op('dve', lambda e: e.reciprocal(out=st[:, 6:7], in_=st[:, 6:7]), reads=[st], writes=[st])
                    kb.op('dve', lambda e, xo_=xo_: e.tensor_scalar(out=xo_[:], in0=rr[:], scalar1=st[:, 2:3], scalar2=st[:, 6:7], op0=ALU.subtract, op1=ALU.mult), reads=[rr, st], writes=[xo_])
                    kb.op('dve', lambda e, xo_=xo_: e.tensor_tensor(out=xo_[:], in0=xo_[:], in1=lng[:, 0, :], op=ALU.mult), reads=[xo_, lng], writes=[xo_])
                    kb.op('dve', lambda e, xo_=xo_: e.tensor_tensor(out=xo_[:], in0=xo_[:], in1=lng[:, 1, :], op=ALU.add), reads=[xo_, lng], writes=[xo_])
                    if layer == DEPTH - 1:
                        kb.dma('sp', OUT[ti * 128:(ti + 1) * 128, :], xo_[:], reads=[xo_])
                    else:
                        kb.dma('sp', S['xcur'][ti], xo_[:], reads=[xo_], writes=[D_['xcur']])
                kb.barrier()
            with contextlib.ExitStack() as es:
              if layer % 2 == 0:
                  is_moe = (layer % 2 == 1)
                  jl = layer // 2
                  nexp = NEXP if is_moe else 1
                  TB = 1536
                  hblk = sb(es, 'hblk', [128, 8, TB], BF16)
                  acc = sb(es, 'accE', [128, TB // 128, D], F32)
                  w1c = [sb(es, 'w1c%d' % i, [128, 8, 512], BF16) for i in range(2)]
                  w3c = [sb(es, 'w3c%d' % i, [128, 8, 512], BF16) for i in range(2)]
                  w2c = [sb(es, 'w2c%d' % i, [128, 4, D], BF16) for i in range(2)]
                  gT = [sb(es, 'gT%d' % i, [128, 4, TB], BF16) for i in range(2)]
                  sil = sb(es, 'sil', [128, 512], F32)
                  gates = sb(es, 'gates', [128, TB // 128, NEXP], F32)
                  wr = sb(es, 'wr', [128, 8, NEXP], BF16)
                  lg = sb(es, 'lg', [128, 4, NEXP], F32); sc = sb(es, 'scE', [128, 8], F32)
                  md5 = sb(es, 'md5', [128, 2, D], F32)
                  for kind in range(2):
                      kb.dma('sp', md5[:, kind, :], S['mod'][kind:kind + 1, 5 * D:6 * D].to_broadcast([128, D]), reads=[D_['mod']], writes=[md5])
                  lng = sb(es, 'lng2', [128, 2, D], F32)
                  kb.dma('sp', lng[:, 0, :], I['ln2_g'][layer:layer + 1, :].to_broadcast([128, D]), writes=[lng])
                  kb.dma('sp', lng[:, 1, :], I['ln2_b'][layer:layer + 1, :].to_broadcast([128, D]), writes=[lng])
                  epsl = sb(es, 'epsl2', [128, 1], F32)
                  kb.op('dve', lambda e: e.memset(epsl[:], LN_EPS), writes=[epsl])
                  x1t = [sb(es, 'x1t%d' % i, [128, D], F32) for i in range(2)]
                  rr = sb(es, 'rrE', [128, D], F32); xo = [sb(es, 'xoE%d' % i, [128, D], F32) for i in range(2)]
                  st = sb(es, 'stE', [128, 8], F32); jk = sb(es, 'jkE', [128, D], F32)
                  if is_moe:
                      kb.dma('pool', wr[:], I['moe_router'][jl].rearrange("(k p) n -> p k n", p=128), writes=[wr])
                  wcnt = [0]
                  blocks = [(b * TB, min(TB, T - b * TB)) for b in range((T + TB - 1) // TB)]
                  for (t0, nb) in blocks:
                      ntl = nb // 128
                      kb.dma('sp', hblk[:, :, 0:nb], S['h2T'][:, :, t0:t0 + nb], reads=[D_['h2T']], writes=[hblk])
                      kb.op('pool', lambda e: e.memset(acc[:], 0.0), writes=[acc])
                      if is_moe:
                          for s_ in range(ntl):
                              pl = pbank[4 + s_ % 2]
                              for kc in range(8):
                                  kb.op('pe', lambda e, kc=kc, pl=pl, s_=s_: e.matmul(pl[:, 0:NEXP], lhsT=hblk[:, kc, s_ * 128:(s_ + 1) * 128], rhs=wr[:, kc, :], start=(kc == 0), stop=(kc == 7)), reads=[hblk, wr], writes=[pl])
                              kb.op('dve', lambda e, pl=pl: e.tensor_copy(out=lg[:, 0, :], in_=pl[:, 0:NEXP]), reads=[pl], writes=[lg])
                              kb.op('dve', lambda e: e.reduce_max(out=sc[:, 0:1], in_=lg[:, 0, :], axis=AX.X), reads=[lg], writes=[sc])
                              kb.op('dve', lambda e: e.tensor_scalar(out=lg[:, 1, :], in0=lg[:, 0, :], scalar1=sc[:, 0:1], scalar2=None, op0=ALU.is_equal), reads=[lg, sc], writes=[lg])
                              kb.op('dve', lambda e: e.scalar_tensor_tensor(out=lg[:, 2, :], in0=lg[:, 1, :], scalar=-1e30, in1=lg[:, 0, :], op0=ALU.mult, op1=ALU.add), reads=[lg], writes=[lg])
                              kb.op('dve', lambda e: e.reduce_max(out=sc[:, 1:2], in_=lg[:, 2, :], axis=AX.X), reads=[lg], writes=[sc])
                              kb.op('dve', lambda e: e.tensor_scalar(out=lg[:, 3, :], in0=lg[:, 2, :], scalar1=sc[:, 1:2], scalar2=None, op0=ALU.is_equal), reads=[lg, sc], writes=[lg])
                              kb.op('dve', lambda e: e.tensor_tensor(out=sc[:, 2:3], in0=sc[:, 1:2], in1=sc[:, 0:1], op=ALU.subtract), reads=[sc], writes=[sc])
                              kb.op('act', lambda e: e.activation(out=sc[:, 3:4], in_=sc[:, 2:3], func=AF.Exp), reads=[sc], writes=[sc])
                              kb.op('dve', lambda e: e.tensor_scalar_add(out=sc[:, 4:5], in0=sc[:, 3:4], scalar1=1.0), reads=[sc], writes=[sc])
                              kb.op('dve', lambda e: e.reciprocal(out=sc[:, 4:5], in_=sc[:, 4:5]), reads=[sc], writes=[sc])
                              kb.op('dve', lambda e: e.tensor_tensor(out=sc[:, 5:6], in0=sc[:, 3:4], in1=sc[:, 4:5], op=ALU.mult), reads=[sc], writes=[sc])
                              kb.op('dve', lambda e: e.tensor_scalar_mul(out=lg[:, 1, :], in0=lg[:, 1, :], scalar1=sc[:, 4:5]), reads=[lg, sc], writes=[lg])
                              kb.op('dve', lambda e, s_=s_: e.scalar_tensor_tensor(out=gates[:, s_, :], in0=lg[:, 3, :], scalar=sc[:, 5:6], in1=lg[:, 1, :], op0=ALU.mult, op1=ALU.add), reads=[lg, sc], writes=[gates])
                      for ex in range(nexp):
                          if is_moe:
                              W1 = I['moe_w1'][jl, ex]; W3 = I['moe_w3'][jl, ex]; W2 = I['moe_w2'][jl, ex]
                          else:
                              W1 = I['ffn_w1'][jl]; W3 = I['ffn_w3'][jl]; W2 = I['ffn_w2'][jl]
                          for fc in range(D_FF // 512):
                              a1 = w1c[wcnt[0] % 2]; a3 = w3c[wcnt[0] % 2]; a2 = w2c[wcnt[0] % 2]; g_ = gT[wcnt[0] % 2]; wcnt[0] += 1
                              kb.dma('pool', a1[:], W1[:, fc * 512:(fc + 1) * 512].rearrange("(k p) n -> p k n", p=128), writes=[a1])
                              kb.dma('pool', a3[:], W3[:, fc * 512:(fc + 1) * 512].rearrange("(k p) n -> p k n", p=128), writes=[a3])
                              kb.dma('pool', a2[:], W2[fc * 512:(fc + 1) * 512, :].rearrange("(f p) n -> p f n", p=128), writes=[a2])
                              for f in range(4):
                                  for th in range((nb + 511) // 512):
                                      c0 = th * 512; n = min(512, nb - c0)
                                      p1 = pbank[0 + (f * 2 + th) % 2 * 2]; p3 = pbank[1 + (f * 2 + th) % 2 * 2]
                                      for kc in range(8):
                                          kb.op('pe', lambda e, kc=kc, p1=p1, a1=a1, f=f, c0=c0, n=n: e.matmul(p1[:, 0:n], lhsT=a1[:, kc, f * 128:(f + 1) * 128], rhs=hblk[:, kc, c0:c0 + n], start=(kc == 0), stop=(kc == 7)), reads=[a1, hblk], writes=[p1])
                                      for kc in range(8):
                                          kb.op('pe', lambda e, kc=kc, p3=p3, a3=a3, f=f, c0=c0, n=n: e.matmul(p3[:, 0:n], lhsT=a3[:, kc, f * 128:(f + 1) * 128], rhs=hblk[:, kc, c0:c0 + n], start=(kc == 0), stop=(kc == 7)), reads=[a3, hblk], writes=[p3])
                                      kb.op('act', lambda e, p1=p1, n=n: e.activation(out=sil[:, 0:n], in_=p1[:, 0:n], func=AF.Silu), reads=[p1], writes=[sil])
                                      kb.op('dve', lambda e, p3=p3, g_=g_, f=f, c0=c0, n=n: e.tensor_tensor(out=g_[:, f, c0:c0 + n], in0=p3[:, 0:n], in1=sil[:, 0:n], op=ALU.mult), reads=[p3, sil], writes=[g_])
                              for s_ in range(ntl):
                                  for hf in range(2):
                                      py = pbank[4 + (s_ * 2 + hf) % 2]
                                      for f in range(4):
                                          kb.op('pe', lambda e, f=f, py=py, g_=g_, a2=a2, s_=s_, hf=hf: e.matmul(py[:, :], lhsT=g_[:, f, s_ * 128:(s_ + 1) * 128], rhs=a2[:, f, hf * 512:(hf + 1) * 512], start=(f == 0), stop=(f == 3)), reads=[g_, a2], writes=[py])
                                      if is_moe:
                                          kb.op('dve', lambda e, py=py, s_=s_, hf=hf, ex=ex: e.scalar_tensor_tensor(out=acc[:, s_, hf * 512:(hf + 1) * 512], in0=py[:, :], scalar=gates[:, s_, ex:ex + 1], in1=acc[:, s_, hf * 512:(hf + 1) * 512], op0=ALU.mult, op1=ALU.add), reads=[py, gates, acc], writes=[acc])
                                      else:
                                          kb.op('dve', lambda e, py=py, s_=s_, hf=hf: e.tensor_tensor(out=acc[:, s_, hf * 512:(hf + 1) * 512], in0=py[:, :], in1=acc[:, s_, hf * 512:(hf + 1) * 512], op=ALU.add), reads=[py, acc], writes=[acc])
                      for s_ in range(ntl):
                          ti = t0 // 128 + s_
                          kind = 0 if ti < NLT else 1
                          if layer == DEPTH - 1 and kind == 1:
                              continue
                          x1_ = x1t[ti % 2]; xo_ = xo[ti % 2]
                          kb.dma('sp', x1_[:], S['x1'][ti], reads=[D_['x1']], writes=[x1_])
                          kb.op('dve', lambda e, s_=s_, kind=kind: e.tensor_tensor(out=rr[:], in0=acc[:, s_, :], in1=md5[:, kind, :], op=ALU.mult), reads=[acc, md5], writes=[rr])
                          kb.op('dve', lambda e, x1_=x1_: e.scalar_tensor_tensor(out=rr[:], in0=x1_[:], scalar=ALPHA, in1=rr[:], op0=ALU.mult, op1=ALU.add), reads=[x1_, rr], writes=[rr])
                          kb.op('act', lambda e: e.activation(out=jk[:], in_=rr[:], func=AF.Identity, accum_out=st[:, 0:1]), reads=[rr], writes=[jk, st])
                          kb.op('act', lambda e: e.activation(out=jk[:], in_=rr[:], func=AF.Square, accum_out=st[:, 1:2]), reads=[rr], writes=[jk, st])
                          kb.op('dve', lambda e: e.tensor_scalar_mul(out=st[:, 2:4], in0=st[:, 0:2], scalar1=1.0 / D), reads=[st], writes=[st])
                          kb.op('dve', lambda e: e.tensor_tensor(out=st[:, 4:5], in0=st[:, 2:3], in1=st[:, 2:3], op=ALU.mult), reads=[st], writes=[st])
                          kb.op('dve', lambda e: e.tensor_tensor(out=st[:, 5:6], in0=st[:, 3:4], in1=st[:, 4:5], op=ALU.subtract), reads=[st], writes=[st])
                          kb.op('act', lambda e: e.activation(out=st[:, 6:7], in_=st[:, 5:6], func=AF.Sqrt, bias=epsl[:, 0:1]), reads=[st, epsl], writes=[st])
                          kb.op('dve', lambda e: e.reciprocal(out=st[:, 6:7], in_=st[:, 6:7]), reads=[st], writes=[st])
                          kb.op('dve', lambda e, xo_=xo_: e.tensor_scalar(out=xo_[:], in0=rr[:], scalar1=st[:, 2:3], scalar2=st[:, 6:7], op0=ALU.subtract, op1=ALU.mult), reads=[rr, st], writes=[xo_])
                          kb.op('dve', lambda e, xo_=xo_: e.tensor_tensor(out=xo_[:], in0=xo_[:], in1=lng[:, 0, :], op=ALU.mult), reads=[xo_, lng], writes=[xo_])
                          kb.op('dve', lambda e, xo_=xo_: e.tensor_tensor(out=xo_[:], in0=xo_[:], in1=lng[:, 1, :], op=ALU.add), reads=[xo_, lng], writes=[xo_])
                          if layer == DEPTH - 1:
                              kb.dma('sp', OUT[ti * 128:(ti + 1) * 128, :], xo_[:], reads=[xo_])
                          else:
                              kb.dma('sp', S['xcur'][ti], xo_[:], reads=[xo_], writes=[D_['xcur']])
              if debug == 'L' and layer == nlayers - 1:
                  kb.barrier()
                  dbg_out['xcur'] = nc.dram_tensor('dbg_xcur', [NT, 128, D], F32, kind="ExternalOutput").ap()
                  kb.dma('sp', dbg_out['xcur'], S['xcur'], reads=[D_['xcur']])
              kb.barrier()

        kb.barrier()
    return nc, dbg_out


def host_consts():
    c = {}
    c['ident'] = np.eye(128, dtype=np.float32)
    t = np.arange(SEQ)
    prow, pcol = t // 64, t % 64
    inv = (10000.0 ** (-np.arange(8, dtype=np.float32) / 8)).astype(np.float32)
    cosr = np.ones((T, 16), np.float32); sinr = np.zeros((T, 16), np.float32)
    ar = prow[:, None].astype(np.float32) * inv[None, :]
    ac = pcol[:, None].astype(np.float32) * inv[None, :]
    cosr[:SEQ, 0:8] = np.cos(ar); cosr[:SEQ, 8:16] = np.cos(ac)
    sinr[:SEQ, 0:8] = np.sin(ar); sinr[:SEQ, 8:16] = np.sin(ac)
    c['ropec'] = np.ascontiguousarray(cosr.reshape(NT, 128, 16).transpose(1, 0, 2))
    c['ropes'] = np.ascontiguousarray(sinr.reshape(NT, 128, 16).transpose(1, 0, 2))
    c['iota128'] = np.arange(128, dtype=np.float32)[None, :]
    pp = np.arange(128)[:, None, None]
    c['base1'] = ((np.arange(8)[None, :, None] * 128 + pp) * 7 + np.arange(7)[None, None, :]).reshape(128, 56).astype(np.float32)
    c['base2'] = (np.arange(28)[None, :] * 128 + np.arange(128)[:, None]).astype(np.float32)
    c['utri'] = np.triu(np.ones((128, 128), np.float32), 1)
    c['bstart'] = (np.arange(NBLK, dtype=np.float32) * 512.0)[None, :]
    m32 = np.zeros((128, 6), np.float32)
    for p_ in range(128):
        m32[p_, p_ // 32] = 1.0
        m32[p_, 4 + p_ // 64] = 1.0
    c['m32'] = m32
    c['iotaT'] = np.stack([np.arange(T, dtype=np.float32), (T - 1) - np.arange(T, dtype=np.float32)])
    cc = np.arange(NT, dtype=np.float32)
    c['cv'] = np.stack([127.0 - 128.0 * cc, 1.0 + 128.0 * cc]).astype(np.float32)
    mC = np.zeros((4, 128, 128), np.float32)
    for b in range(4):
        for co in range(128):
            for st in range(128):
                if co // 16 == b * 2 + st // 64:
                    mC[b, co, st] = 1.0
    c['maskC'] = mC
    c['maskB'] = np.ascontiguousarray(mC.transpose(0, 2, 1))
    return c


def rpb_toeplitz(rpb):
    kc = np.arange(64)[:, None]; qc = np.arange(64)[None, :]
    c0 = np.clip(qc - 8, 0, 48)
    inwin = (kc >= c0) & (kc <= c0 + 15)
    idx = np.clip(kc - qc + 15, 0, 30)
    g = rpb[:, :, :, idx]
    return np.where(inwin[None, None, None], g, np.float32(-30000.0)).astype(np.float32)


_CACHE = {}


def make_in_maps(inputs, ncores=8):
    consts = host_consts()
    shared = {}
    for k, v in inputs.items():
        if k in ('x', 'c', 'ctx', 'c_ctx', 'na_rpb'):
            continue
        shared[k] = np.ascontiguousarray(np.asarray(v, dtype=np.float32))
    shared['rpbT'] = rpb_toeplitz(np.asarray(inputs['na_rpb'], dtype=np.float32))
    shared['c_ctx'] = np.asarray(inputs['c_ctx'], np.float32).reshape(1, D)
    shared.update(consts)
    maps = []
    for b in range(ncores):
        m = dict(shared)
        m['x'] = np.ascontiguousarray(np.asarray(inputs['x'][b], np.float32))
        m['ctx'] = np.ascontiguousarray(np.asarray(inputs['ctx'][b], np.float32))
        m['c'] = np.asarray(inputs['c'][b], np.float32).reshape(1, D)
        maps.append(m)
    return maps


def kernel(**inputs):
    if 'nc' not in _CACHE:
        _CACHE['nc'] = build_program()[0]
    nc = _CACHE['nc']
    maps = make_in_maps(inputs, 8)
    res = run_bass_kernel_spmd(nc, maps, core_ids=list(range(8)))
    return np.stack([np.asarray(r['out'], dtype=np.float32) for r in res.results], axis=0)
```

```python
import math
import contextlib
import numpy as np
import ml_dtypes
import concourse.bass as bass
import concourse.mybir as mybir
from concourse.bass_utils import run_bass_kernel_spmd

F32 = mybir.dt.float32
BF16 = mybir.dt.bfloat16
ALU = mybir.AluOpType
AF = mybir.ActivationFunctionType
AX = mybir.AxisListType

D = 1024
SEQ = 4096
CTX = 256
T = SEQ + CTX
NT = T // 128
NLT = SEQ // 128
DEPTH = 4
IN_COLS = 2208
D_FF = 3584
NEXP = 8
ALPHA = (2 * DEPTH) ** 0.25
LN_EPS = 1e-5
RMS_EPS = 1e-6
NA_SCALE = 64 ** -0.5
MLA_SCALE = 96 ** -0.5
DIFF_SCALE = 32 ** -0.5
C_NAQ, C_NAK, C_S5U, C_MQ, C_MK, C_DQ, C_DK = 0, 2, 4, 6, 10, 14, 18
NFM = 22
BS = 1024
NBLK = 17
NSLOT = NBLK * BS
I32 = mybir.dt.int32


class KB:
    def __init__(self, nc, es):
        self.nc = nc
        self.es = es
        self.eng = {'pe': nc.tensor, 'act': nc.scalar, 'dve': nc.vector, 'pool': nc.gpsimd, 'sp': nc.sync}
        self.sem = {}
        self.cnt = {}
        for e in ('pe', 'act', 'dve', 'pool'):
            self.sem[e] = es.enter_context(nc.semaphore('sem_' + e))
            self.cnt[e] = 0
        self.KD = 24
        self.dsem = {}
        self.dcnt = {}
        for q in ('sp', 'pool'):
            self.dsem[q] = [es.enter_context(nc.semaphore('dsem_%s%d' % (q, i))) for i in range(self.KD)]
            self.dcnt[q] = 0
        self.waited = {e: {} for e in self.eng}
        self.semobj = {}
        for e in self.sem:
            self.semobj[e] = self.sem[e]
        for q in self.dsem:
            for i, s in enumerate(self.dsem[q]):
                self.semobj[(q, i)] = s

    def _wait(self, e, tok):
        key, val = tok
        if self.waited[e].get(key, 0) >= val:
            return
        self.eng[e].wait_ge(self.semobj[key], val)
        self.waited[e][key] = val

    def _deps(self, e, reads, writes):
        deps = {}

        def add(tok):
            if tok is None:
                return
            k, v = tok
            if e == 'pe' and k == 'pe':
                return
            if deps.get(k, 0) < v:
                deps[k] = v
        for t in reads:
            add(t.w)
        for t in writes:
            add(t.w)
            for k, v in t.r.items():
                add((k, v))
        for k, v in deps.items():
            self._wait(e, (k, v))

    def _mark(self, tok, reads, writes):
        k, v = tok
        for t in reads:
            if t.r.get(k, 0) < v:
                t.r[k] = v
        for t in writes:
            t.w = tok
            t.r = {}

    def op(self, e, fn, reads=(), writes=()):
        self._deps(e, reads, writes)
        inst = fn(self.eng[e])
        self.cnt[e] += 1
        inst.then_inc(self.sem[e], 1)
        self._mark((e, self.cnt[e]), reads, writes)

    def dma(self, q, out, in_, reads=(), writes=()):
        self._deps(q, reads, writes)
        n = self.dcnt[q]
        k = n % self.KD
        gen = n // self.KD + 1
        if gen > 1:
            self._wait(q, ((q, k), 16 * (gen - 1)))
        self.eng[q].dma_start(out=out, in_=in_).then_inc(self.dsem[q][k], 16)
        self.dcnt[q] += 1
        self._mark(((q, k), 16 * gen), reads, writes)

    def idma(self, out, out_offset, in_, in_offset, reads=(), writes=()):
        q = 'pool'
        self._deps(q, reads, writes)
        n = self.dcnt[q]
        k = n % self.KD
        gen = n // self.KD + 1
        if gen > 1:
            self._wait(q, ((q, k), 16 * (gen - 1)))
        self.eng[q].indirect_dma_start(out=out, out_offset=out_offset, in_=in_, in_offset=in_offset).then_inc(self.dsem[q][k], 16)
        self.dcnt[q] += 1
        self._mark(((q, k), 16 * gen), reads, writes)

    def barrier(self):
        toks = [(e, self.cnt[e]) for e in self.cnt if self.cnt[e] > 0]
        for q in self.dsem:
            n = self.dcnt[q]
            for k in range(self.KD):
                uses = (n - k + self.KD - 1) // self.KD if n > k else 0
                if uses > 0:
                    toks.append(((q, k), 16 * uses))
        for e in self.eng:
            for tok in toks:
                self._wait(e, tok)


class Tl:
    def __init__(self, t):
        self.t = t
        self.w = None
        self.r = {}

    def __getitem__(self, key):
        return self.t[key]


def build_program(nlayers=DEPTH, debug=None):
    nc = bass.Bass("TRN2", target_bir_lowering=False)
    es0 = contextlib.ExitStack()

    def din(name, shape, dt=F32):
        return nc.dram_tensor(name, list(shape), dt, kind="ExternalInput").ap()

    def dscr(name, shape, dt):
        return nc.dram_tensor(name, list(shape), dt, kind="Internal").ap()

    I = {}
    I['x'] = din('x', [SEQ, D]); I['ctx'] = din('ctx', [CTX, D]); I['c'] = din('c', [1, D]); I['c_ctx'] = din('c_ctx', [1, D])
    I['w_ada'] = din('w_ada', [DEPTH, D, 6 * D]); I['b_ada'] = din('b_ada', [DEPTH, 6 * D])
    I['w_in'] = din('w_in', [DEPTH, D, IN_COLS])
    I['rpbT'] = din('rpbT', [DEPTH, 4, 15, 64, 64])
    I['mla_q_norm'] = din('mla_q_norm', [DEPTH, 256]); I['mla_kv_norm'] = din('mla_kv_norm', [DEPTH, 128])
    I['mla_w_uq'] = din('mla_w_uq', [DEPTH, 256, 384]); I['mla_w_ukv'] = din('mla_w_ukv', [DEPTH, 128, 512])
    for nm in ('s5_lam_re', 's5_lam_im'):
        I[nm] = din(nm, [DEPTH, 2, 16, 64])
    I['s5_log_dt'] = din('s5_log_dt', [DEPTH, 2, 16])
    for nm in ('s5_b_re', 's5_b_im'):
        I[nm] = din(nm, [DEPTH, 2, 16, 64, 16])
    for nm in ('s5_c_re', 's5_c_im'):
        I[nm] = din(nm, [DEPTH, 2, 16, 16, 64])
    I['s5_d'] = din('s5_d', [DEPTH, 256]); I['s5_w_glu'] = din('s5_w_glu', [DEPTH, 256, 512])
    for nm in ('diff_lam_q1', 'diff_lam_k1', 'diff_lam_q2', 'diff_lam_k2'):
        I[nm] = din(nm, [DEPTH, 32])
    I['diff_subln'] = din('diff_subln', [DEPTH, 64])
    I['w_branch'] = din('w_branch', [DEPTH, 4, 256, D]); I['w_gate'] = din('w_gate', [DEPTH, D, 4 * D]); I['b_gate'] = din('b_gate', [DEPTH, 4 * D])
    I['w_out'] = din('w_out', [DEPTH, D, D])
    for nm in ('ln1_g', 'ln1_b', 'ln2_g', 'ln2_b'):
        I[nm] = din(nm, [DEPTH, D])
    for nm in ('ffn_w1', 'ffn_w3'):
        I[nm] = din(nm, [2, D, D_FF])
    I['ffn_w2'] = din('ffn_w2', [2, D_FF, D])
    I['moe_router'] = din('moe_router', [2, D, NEXP])
    for nm in ('moe_w1', 'moe_w3'):
        I[nm] = din(nm, [2, NEXP, D, D_FF])
    I['moe_w2'] = din('moe_w2', [2, NEXP, D_FF, D])
    I['ident'] = din('ident', [128, 128]); I['ropec'] = din('ropec', [128, NT, 16]); I['ropes'] = din('ropes', [128, NT, 16])
    I['iota128'] = din('iota128', [1, 128]); I['base1'] = din('base1', [128, 56]); I['base2'] = din('base2', [128, 28]); I['utri'] = din('utri', [128, 128]); I['bstart'] = din('bstart', [1, NBLK]); I['m32'] = din('m32', [128, 6]); I['iotaT'] = din('iotaT', [2, T]); I['cv'] = din('cv', [2, NT]); I['maskC'] = din('maskC', [4, 128, 128]); I['maskB'] = din('maskB', [4, 128, 128])

    OUT = nc.dram_tensor('out', [SEQ, D], F32, kind="ExternalOutput").ap()
    dbg_out = {}

    S = {}
    S['xcur'] = dscr('xcur', [NT, 128, D], F32)
    S['x1'] = dscr('x1s', [NT, 128, D], F32)
    S['hT'] = dscr('hTs', [NT, 128, 8, 128], BF16)
    S['h2T'] = dscr('h2Ts', [128, 8, T], BF16)
    S['FM'] = dscr('FMs', [NFM, 128, T], BF16)
    S['VV'] = dscr('VVs', [NT, 128, 12, 128], BF16)
    S['oT'] = dscr('oTs', [4, 2, 128, T], BF16)
    S['mod'] = dscr('mods', [2, 6 * D], F32)
    S['h2tm'] = dscr('h2tms', [NT, 128, D], BF16)
    S['Xs'] = dscr('Xss', [NSLOT, D], BF16)
    S['Ys'] = dscr('Yss', [NSLOT, D], F32)
    D_ = {k: Tl(v) for k, v in S.items()}

    with es0:
        kb = KB(nc, es0)
        ucnt = [0]

        def sb(es, name, shape, dt):
            ucnt[0] += 1
            return Tl(es.enter_context(nc.sbuf_tensor('%s_u%d' % (name, ucnt[0]), list(shape), dt)))

        def ps(es, name, shape, dt):
            return Tl(es.enter_context(nc.psum_tensor(name, list(shape), dt)))

        ident_f = sb(es0, 'ident_f', [128, 128], F32)
        ident = sb(es0, 'ident_b', [128, 128], BF16)
        ones_b = sb(es0, 'ones_b', [128, 128], BF16)
        condT = sb(es0, 'condT', [128, 8, 2], BF16)
        ctmp = sb(es0, 'ctmp', [128, 8, 2], F32)
        kb.dma('sp', ident_f[:], I['ident'][:], writes=[ident_f])
        kb.op('dve', lambda e: e.tensor_copy(out=ident[:], in_=ident_f[:]), reads=[ident_f], writes=[ident])
        kb.op('dve', lambda e: e.memset(ones_b[:], 1.0), writes=[ones_b])
        with nc.allow_non_contiguous_dma(reason="tiny cond vector"):
            kb.dma('sp', ctmp[:, :, 0], I['c'][0].rearrange("(k p) -> p k", p=128), writes=[ctmp])
            kb.dma('sp', ctmp[:, :, 1], I['c_ctx'][0].rearrange("(k p) -> p k", p=128), writes=[ctmp])
        kb.op('act', lambda e: e.activation(out=condT[:], in_=ctmp[:], func=AF.Silu), reads=[ctmp], writes=[condT])

        pbig = [ps(es0, 'pbig%d' % i, [128, 1024], F32) for i in range(2)]
        pbank = [ps(es0, 'pb%d' % i, [128, 512], F32) for i in range(4)]
        pbank += [Tl(pbig[0].t[:, 0:512]), Tl(pbig[0].t[:, 512:1024])]
        ptr = [Tl(pbig[1].t[:, 0:512].bitcast(BF16)), Tl(pbig[1].t[:, 512:1024].bitcast(BF16))]

        for layer in range(nlayers):
            ctx_out = layer < DEPTH - 1
            lam_init = 0.8 - 0.6 * math.exp(-0.3 * layer)
            with contextlib.ExitStack() as es:
                wst = [sb(es, 'wada%d' % i, [128, 8, 512], BF16) for i in range(2)]
                modsb = sb(es, 'modsb', [2, 6 * D], F32)
                bada = sb(es, 'bada', [2, 6 * D], F32)
                kb.dma('sp', bada[:], I['b_ada'][layer:layer + 1, :].partition_broadcast(2) if False else I['b_ada'][layer:layer + 1, :].to_broadcast([2, 6 * D]), writes=[bada])
                for cc in range(12):
                    w = wst[cc % 2]
                    kb.dma('pool', w[:], I['w_ada'][layer, :, cc * 512:(cc + 1) * 512].rearrange("(k p) n -> p k n", p=128), writes=[w])
                    pb = pbank[cc % 2]
                    for kc in range(8):
                        kb.op('pe', lambda e, kc=kc, w=w, pb=pb: e.matmul(pb[0:2, :], lhsT=condT[:, kc, :], rhs=w[:, kc, :], start=(kc == 0), stop=(kc == 7)),
                              reads=[condT, w], writes=[pb])
                    kb.op('dve', lambda e, cc=cc, pb=pb: e.tensor_tensor(out=modsb[:, cc * 512:(cc + 1) * 512], in0=pb[0:2, :], in1=bada[:, cc * 512:(cc + 1) * 512], op=ALU.add),
                          reads=[pb, bada], writes=[modsb])
                kb.dma('sp', S['mod'][:], modsb[:], reads=[modsb], writes=[D_['mod']])
                if debug == 'M':
                    dbg_out['mod'] = nc.dram_tensor('dbg_mod', [2, 6 * D], F32, kind="ExternalOutput").ap()
                    kb.dma('sp', dbg_out['mod'][:], modsb[:], reads=[modsb])
                kb.barrier()
            if debug == 'M':
                break
            with contextlib.ExitStack() as es:
                win = sb(es, 'win', [128, 8, IN_COLS], BF16)
                wuq = sb(es, 'wuq', [128, 2, 384], BF16)
                wukv = sb(es, 'wukv', [128, 512], BF16)
                kb.dma('pool', win[:], I['w_in'][layer].rearrange("(k p) n -> p k n", p=128), writes=[win])
                kb.dma('pool', wuq[:], I['mla_w_uq'][layer].rearrange("(k p) n -> p k n", p=128), writes=[wuq])
                kb.dma('pool', wukv[:], I['mla_w_ukv'][layer], writes=[wukv])
                modb = sb(es, 'modbA', [128, 2, 2, D], F32)
                for kind in range(2):
                    for j in range(2):
                        kb.dma('sp', modb[:, kind, j, :], S['mod'][kind:kind + 1, j * D:(j + 1) * D].to_broadcast([128, D]), reads=[D_['mod']], writes=[modb])
                kb.op('dve', lambda e: e.tensor_scalar_add(out=modb[:, :, 1, :], in0=modb[:, :, 1, :], scalar1=1.0), reads=[modb], writes=[modb])
                qg = sb(es, 'qg', [128, 256], F32); kvg = sb(es, 'kvg', [128, 128], F32)
                kb.dma('sp', qg[:], I['mla_q_norm'][layer:layer + 1, :].to_broadcast([128, 256]), writes=[qg])
                kb.dma('sp', kvg[:], I['mla_kv_norm'][layer:layer + 1, :].to_broadcast([128, 128]), writes=[kvg])
                rc = sb(es, 'rc', [128, NT, 16], F32); rs = sb(es, 'rs', [128, NT, 16], F32)
                kb.dma('sp', rc[:], I['ropec'][:], writes=[rc]); kb.dma('sp', rs[:], I['ropes'][:], writes=[rs])
                epsq = sb(es, 'epsq', [128, 1], F32)
                kb.op('dve', lambda e: e.memset(epsq[:], RMS_EPS), writes=[epsq])
                xt = [sb(es, 'xt%d' % i, [128, D], F32) for i in range(2)]
                htmp = sb(es, 'htmp', [128, D], F32)
                hb = sb(es, 'hb', [128, D], BF16)
                hTt = [sb(es, 'hTt%d' % i, [128, 8, 128], BF16) for i in range(2)]
                z = sb(es, 'z', [128, IN_COLS], F32)
                zb = sb(es, 'zb', [128, IN_COLS], BF16)
                fm = [sb(es, 'fm%d' % i, [128, NFM, 256], BF16) for i in range(2)]
                vv = [sb(es, 'vv%d' % i, [128, 12, 128], BF16) for i in range(2)]
                for i in range(2):
                    kb.op('pool', lambda e, i=i: e.memset(vv[i][:], 1.0), writes=[vv[i]])
                    kb.op('pool', lambda e, i=i: e.memset(fm[i][:], 0.0), writes=[fm[i]])
                ss = sb(es, 'ss', [128, 4], F32)
                junk = sb(es, 'junk', [128, 256], F32)
                qn = sb(es, 'qn', [128, 384], BF16)
                qnT = sb(es, 'qnT', [128, 3, 128], BF16)
                qf = sb(es, 'qf', [128, 384], F32)
                kvf = sb(es, 'kvf', [128, 512], F32)
                Qb = sb(es, 'Qb', [128, 4, 96], BF16)
                Kb = sb(es, 'Kb', [128, 4, 96], BF16)
                krr = sb(es, 'krr', [128, 32], F32)
                dqk = sb(es, 'dqk', [128, 512], BF16)
                rt = [sb(es, 'rt%d' % i, [128, 256], F32) for i in range(4)]
                ptoggle = [0]

                def rope(src_ap, dst_ap, G, ti, reads, writes):
                    sv = src_ap.rearrange("p (g h two f) -> p g h two f", g=G, h=2, two=2, f=8)
                    dv = dst_ap.rearrange("p (g h two f) -> p g h two f", g=G, h=2, two=2, f=8)
                    cb = rc[:, ti, :].rearrange("p (h f) -> p h f", h=2).unsqueeze(1).to_broadcast([128, G, 2, 8])
                    sn = rs[:, ti, :].rearrange("p (h f) -> p h f", h=2).unsqueeze(1).to_broadcast([128, G, 2, 8])
                    tv = [r_[:, 0:G * 16].rearrange("p (g h f) -> p g h f", g=G, h=2, f=8) for r_ in rt]
                    z1 = sv[:, :, :, 0, :]; z2 = sv[:, :, :, 1, :]
                    kb.op('dve', lambda e: e.tensor_tensor(out=tv[0], in0=z1, in1=cb, op=ALU.mult), reads=reads + [rc], writes=[rt[0]])
                    kb.op('dve', lambda e: e.tensor_tensor(out=tv[1], in0=z2, in1=sn, op=ALU.mult), reads=reads + [rs], writes=[rt[1]])
                    kb.op('dve', lambda e: e.tensor_tensor(out=tv[2], in0=z1, in1=sn, op=ALU.mult), reads=reads + [rs], writes=[rt[2]])
                    kb.op('dve', lambda e: e.tensor_tensor(out=tv[3], in0=z2, in1=cb, op=ALU.mult), reads=reads + [rc], writes=[rt[3]])
                    kb.op('dve', lambda e: e.tensor_tensor(out=dv[:, :, :, 0, :], in0=tv[0], in1=tv[1], op=ALU.subtract), reads=[rt[0], rt[1]], writes=writes)
                    kb.op('dve', lambda e: e.tensor_tensor(out=dv[:, :, :, 1, :], in0=tv[2], in1=tv[3], op=ALU.add), reads=[rt[2], rt[3]], writes=writes)

                def transposes(items, fmt, j):
                    for b0 in range(0, len(items), 8):
                        batch = items[b0:b0 + 8]
                        pt = ptr[ptoggle[0] % 2]; ptoggle[0] += 1
                        for i, (st, sap, n, ch) in enumerate(batch):
                            kb.op('pe', lambda e, i=i, sap=sap, n=n, pt=pt: e.transpose(out=pt[0:n, i * 128:(i + 1) * 128], in_=sap, identity=ident[:]),
                                  reads=[st, ident], writes=[pt])
                        for i, (st, sap, n, ch) in enumerate(batch):
                            eng = 'act' if (i % 2 == 0) else 'pool_'
                            if eng == 'act':
                                kb.op('act', lambda e, i=i, n=n, ch=ch, pt=pt: e.copy(out=fmt[0:n, ch, j * 128:(j + 1) * 128], in_=pt[0:n, i * 128:(i + 1) * 128]), reads=[pt], writes=[fmt])
                            else:
                                kb.op('dve', lambda e, i=i, n=n, ch=ch, pt=pt: e.tensor_copy(out=fmt[0:n, ch, j * 128:(j + 1) * 128], in_=pt[0:n, i * 128:(i + 1) * 128]), reads=[pt], writes=[fmt])

                for ti in range(NT):
                    kind = 0 if ti < NLT else 1
                    g, j = ti // 2, ti % 2
                    fmt = fm[g % 2]; vt = vv[ti % 2]; x_t = xt[ti % 2]; hT_t = hTt[ti % 2]
                    if layer == 0:
                        src = I['x'][ti * 128:(ti + 1) * 128, :] if kind == 0 else I['ctx'][(ti - NLT) * 128:(ti - NLT + 1) * 128, :]
                        kb.dma('sp', x_t[:], src, writes=[x_t])
                    else:
                        kb.dma('sp', x_t[:], S['xcur'][ti], reads=[D_['xcur']], writes=[x_t])
                    kb.op('dve', lambda e: e.tensor_tensor(out=htmp[:], in0=x_t[:], in1=modb[:, kind, 1, :], op=ALU.mult), reads=[x_t, modb], writes=[htmp])
                    kb.op('dve', lambda e: e.tensor_tensor(out=hb[:], in0=htmp[:], in1=modb[:, kind, 0, :], op=ALU.add), reads=[htmp, modb], writes=[hb])
                    pt = ptr[ptoggle[0] % 2]; ptoggle[0] += 1
                    for kc in range(8):
                        kb.op('pe', lambda e, kc=kc, pt=pt: e.transpose(out=pt[:, kc * 128:(kc + 1) * 128], in_=hb[:, kc * 128:(kc + 1) * 128], identity=ident[:]), reads=[hb, ident], writes=[pt])
                    kb.op('act', lambda e, pt=pt: e.copy(out=hT_t[:].rearrange("p k t -> p (k t)"), in_=pt[:]), reads=[pt], writes=[hT_t])
                    kb.dma('sp', S['hT'][ti], hT_t[:], reads=[hT_t], writes=[D_['hT']])
                    for cg in range(5):
                        c0 = cg * 512; n = min(512, IN_COLS - c0)
                        pb = pbank[cg % 4]
                        for kc in range(8):
                            kb.op('pe', lambda e, kc=kc, pb=pb, c0=c0, n=n: e.matmul(pb[:, 0:n], lhsT=hT_t[:, kc, :], rhs=win[:, kc, c0:c0 + n], start=(kc == 0), stop=(kc == 7)),
                                  reads=[hT_t, win], writes=[pb])
                        kb.op('act', lambda e, pb=pb, c0=c0, n=n: e.copy(out=z[:, c0:c0 + n], in_=pb[:, 0:n]), reads=[pb], writes=[z])
                    kb.op('pool', lambda e: e.tensor_copy(out=zb[:], in_=z[:]), reads=[z], writes=[zb])
                    kb.op('pool', lambda e: e.tensor_copy(out=vt[:, 0:4, 0:64], in_=z[:, 512:768].rearrange("p (h d) -> p h d", h=4)), reads=[z], writes=[vt])
                    kb.op('pool', lambda e: e.tensor_copy(out=vt[:, 8:12, 0:64], in_=z[:, 1952:2208].rearrange("p (h d) -> p h d", h=4)), reads=[z], writes=[vt])
                    kb.op('act', lambda e: e.activation(out=junk[:, 0:256], in_=z[:, 768:1024], func=AF.Square, accum_out=ss[:, 0:1]), reads=[z], writes=[junk, ss])
                    kb.op('act', lambda e: e.activation(out=junk[:, 0:128], in_=z[:, 1024:1152], func=AF.Square, accum_out=ss[:, 1:2]), reads=[z], writes=[junk, ss])
                    kb.op('act', lambda e: e.activation(out=ss[:, 2:3], in_=ss[:, 0:1], func=AF.Sqrt, scale=1.0 / 256, bias=epsq[:, 0:1]), reads=[ss, epsq], writes=[ss])
                    kb.op('act', lambda e: e.activation(out=ss[:, 3:4], in_=ss[:, 1:2], func=AF.Sqrt, scale=1.0 / 128, bias=epsq[:, 0:1]), reads=[ss, epsq], writes=[ss])
                    kb.op('dve', lambda e: e.reciprocal(out=ss[:, 2:4], in_=ss[:, 2:4]), reads=[ss], writes=[ss])
                    kb.op('dve', lambda e: e.scalar_tensor_tensor(out=qn[:, 0:256], in0=z[:, 768:1024], scalar=ss[:, 2:3], in1=qg[:], op0=ALU.mult, op1=ALU.mult), reads=[z, ss, qg], writes=[qn])
                    kb.op('dve', lambda e: e.scalar_tensor_tensor(out=qn[:, 256:384], in0=z[:, 1024:1152], scalar=ss[:, 3:4], in1=kvg[:], op0=ALU.mult, op1=ALU.mult), reads=[z, ss, kvg], writes=[qn])
                    pt = ptr[ptoggle[0] % 2]; ptoggle[0] += 1
                    for c3 in range(3):
                        kb.op('pe', lambda e, c3=c3, pt=pt: e.transpose(out=pt[:, c3 * 128:(c3 + 1) * 128], in_=qn[:, c3 * 128:(c3 + 1) * 128], identity=ident[:]), reads=[qn, ident], writes=[pt])
                    kb.op('act', lambda e, pt=pt: e.copy(out=qnT[:].rearrange("p k t -> p (k t)"), in_=pt[:, 0:384]), reads=[pt], writes=[qnT])
                    pq = pbank[4]; pk = pbank[5]
                    for c2 in range(2):
                        kb.op('pe', lambda e, c2=c2: e.matmul(pq[:, 0:384], lhsT=qnT[:, c2, :], rhs=wuq[:, c2, :], start=(c2 == 0), stop=(c2 == 1)), reads=[qnT, wuq], writes=[pq])
                    kb.op('pe', lambda e: e.matmul(pk[:, :], lhsT=qnT[:, 2, :], rhs=wukv[:], start=True, stop=True), reads=[qnT, wukv], writes=[pk])
                    kb.op('act', lambda e: e.copy(out=qf[:], in_=pq[:, 0:384]), reads=[pq], writes=[qf])
                    kb.op('act', lambda e: e.copy(out=kvf[:], in_=pk[:]), reads=[pk], writes=[kvf])
                    qf3 = qf[:].rearrange("p (h d) -> p h d", h=4); kv3 = kvf[:].rearrange("p (h d) -> p h d", h=4)
                    kb.op('pool', lambda e: e.tensor_copy(out=Qb[:, :, 0:64], in_=qf3[:, :, 0:64]), reads=[qf], writes=[Qb])
                    kb.op('pool', lambda e: e.tensor_copy(out=Kb[:, :, 0:64], in_=kv3[:, :, 0:64]), reads=[kvf], writes=[Kb])
                    kb.op('pool', lambda e: e.tensor_copy(out=vt[:, 4:8, 0:64], in_=kv3[:, :, 64:128]), reads=[kvf], writes=[vt])
                    for h in range(4):
                        rope(qf[:, h * 96 + 64:h * 96 + 96], Qb[:, h, 64:96], 1, ti, [qf], [Qb])
                    rope(z[:, 1152:1184], krr[:, :], 1, ti, [z], [krr])
                    kb.op('pool', lambda e: e.tensor_copy(out=Kb[:, :, 64:96], in_=krr[:].unsqueeze(1).to_broadcast([128, 4, 32])), reads=[krr], writes=[Kb])
                    rope(z[:, 1440:1696], dqk[:, 0:256], 8, ti, [z], [dqk])
                    rope(z[:, 1696:1952], dqk[:, 256:512], 8, ti, [z], [dqk])
                    items = []
                    for c2 in range(2):
                        items.append((zb, zb[:, c2 * 128:(c2 + 1) * 128], 128, C_NAQ + c2))
                        items.append((zb, zb[:, 256 + c2 * 128:256 + (c2 + 1) * 128], 128, C_NAK + c2))
                        items.append((zb, zb[:, 1184 + c2 * 128:1184 + (c2 + 1) * 128], 128, C_S5U + c2))
                    for h in range(4):
                        items.append((Qb, Qb[:, h, :], 96, C_MQ + h))
                        items.append((Kb, Kb[:, h, :], 96, C_MK + h))
                    for c2 in range(2):
                        items.append((dqk, dqk[:, c2 * 128:(c2 + 1) * 128], 128, C_DQ + c2))
                        items.append((dqk, dqk[:, 256 + c2 * 128:256 + (c2 + 1) * 128], 128, C_DK + c2))
                    transposes(items, fmt, j)
                    kb.dma('sp', S['VV'][ti], vt[:], reads=[vt], writes=[D_['VV']])
                    if j == 1:
                        kb.dma('sp', S['FM'][:, :, g * 256:(g + 1) * 256].rearrange("c p t -> p c t"), fmt[:], reads=[fmt], writes=[D_['FM']])
                if debug == 'A':
                    kb.barrier()
                    for nm, shp, dt in (('FM', [NFM, 128, T], BF16), ('VV', [NT, 128, 12, 128], BF16), ('hT', [NT, 128, 8, 128], BF16)):
                        dbg_out[nm] = nc.dram_tensor('dbg_' + nm, shp, dt, kind="ExternalOutput").ap()
                        kb.dma('sp', dbg_out[nm], S[nm], reads=[D_[nm]])
                kb.barrier()
            if debug == 'A':
                break
            with contextlib.ExitStack() as es:
                KT = sb(es, 'KT', [128, T], BF16); QT = sb(es, 'QT', [128, T], BF16)
                V = sb(es, 'Vt', [128, NT, 128], BF16)
                rd = sb(es, 'rd', [128, 512], F32)
                onb = sb(es, 'onb', [128, 512], BF16)
                o1 = sb(es, 'o1', [128, 512], F32); o2 = sb(es, 'o2', [128, 512], F32); osq = sb(es, 'osq', [128, 512], BF16)
                rs2 = sb(es, 'rs2', [128, 512], F32)
                Wt3 = [sb(es, 'Wt3_%d' % i, [128, 4, 8, 512], BF16) for i in range(3)]
                QM = [sb(es, 'QM%d' % i, [128, T], BF16) for i in range(4)]
                m32 = sb(es, 'm32', [128, 6], F32)
                kb.dma('sp', m32[:], I['m32'][:], writes=[m32])
                Grev = sb(es, 'Grev', [128, 4, 15, 64], BF16)
                lamv = sb(es, 'lamv', [128, 4, 32], F32); lamt = sb(es, 'lamt', [128, 8], F32)
                gsub = sb(es, 'gsub', [128, 1], F32); epsd = sb(es, 'epsd', [128, 1], F32)
                ecnt = [0]
                for i4, nm in enumerate(('diff_lam_q1', 'diff_lam_k1', 'diff_lam_q2', 'diff_lam_k2')):
                    kb.dma('sp', lamv[:, i4, :], I[nm][layer:layer + 1, :].to_broadcast([128, 32]), writes=[lamv])
                kb.op('dve', lambda e: e.tensor_tensor(out=lamv[:, 0, :], in0=lamv[:, 0, :], in1=lamv[:, 1, :], op=ALU.mult), reads=[lamv], writes=[lamv])
                kb.op('dve', lambda e: e.tensor_tensor(out=lamv[:, 2, :], in0=lamv[:, 2, :], in1=lamv[:, 3, :], op=ALU.mult), reads=[lamv], writes=[lamv])
                kb.op('dve', lambda e: e.reduce_sum(out=lamt[:, 0:1], in_=lamv[:, 0, :], axis=AX.X), reads=[lamv], writes=[lamt])
                kb.op('dve', lambda e: e.reduce_sum(out=lamt[:, 1:2], in_=lamv[:, 2, :], axis=AX.X), reads=[lamv], writes=[lamt])
                kb.op('act', lambda e: e.activation(out=lamt[:, 2:4], in_=lamt[:, 0:2], func=AF.Exp), reads=[lamt], writes=[lamt])
                kb.op('dve', lambda e: e.tensor_tensor(out=lamt[:, 4:5], in0=lamt[:, 3:4], in1=lamt[:, 2:3], op=ALU.subtract), reads=[lamt], writes=[lamt])
                kb.op('dve', lambda e: e.tensor_scalar_add(out=lamt[:, 5:6], in0=lamt[:, 4:5], scalar1=-lam_init), reads=[lamt], writes=[lamt])
                with nc.allow_non_contiguous_dma(reason="tiny"):
                    kb.dma('sp', gsub[0:64, :], I['diff_subln'][layer].rearrange("(p o) -> p o", o=1), writes=[gsub])
                kb.op('dve', lambda e: e.tensor_scalar_mul(out=gsub[0:64, :], in0=gsub[0:64, :], scalar1=(1.0 - lam_init)), reads=[gsub], writes=[gsub])
                kb.op('dve', lambda e: e.memset(epsd[:], RMS_EPS), writes=[epsd])
                with contextlib.ExitStack() as es2:
                    graw = sb(es2, 'graw', [128, 4, 15, 64], F32)
                    for half in range(2):
                        kb.dma('sp', graw[half * 64:(half + 1) * 64], I['rpbT'][layer].rearrange("h r k q -> k h r q"), writes=[graw])
                    for m in range(15):
                        kb.op('act', lambda e, m=m: e.activation(out=Grev[:, :, m, :], in_=graw[:, :, 14 - m, :], func=AF.Exp), reads=[graw], writes=[Grev])
                    kb.barrier()

                def build_W(jq, Wt):
                    R0 = 8 * jq; KR0 = min(max(R0 - 4, 0), 48)
                    kb.op('pool', lambda e: e.memset(Wt[:], 0.0), writes=[Wt])
                    for i in range(8):
                        for half in range(2):
                            kr = KR0 + 2 * i + half
                            al = []
                            for a in range(8):
                                r0 = min(max(R0 + a - 4, 0), 56)
                                if r0 <= kr <= r0 + 7:
                                    al.append(a)
                            if not al:
                                continue
                            a0, a1 = al[0], al[-1] + 1
                            m0 = (R0 + a0) - kr + 7
                            kb.op('pool', lambda e, i=i, half=half, a0=a0, a1=a1, m0=m0: e.tensor_copy(
                                out=Wt[half * 64:(half + 1) * 64, :, i, a0 * 64:a1 * 64].rearrange("p h (a q) -> p h a q", q=64),
                                in_=Grev[half * 64:(half + 1) * 64, :, m0:m0 + (a1 - a0), :]), reads=[Grev], writes=[Wt])
                for ci_, jq_ in enumerate((0, 1, 7)):
                    build_W(jq_, Wt3[ci_])

                E2 = [sb(es, 'E2_%d' % i, [128, 1024], BF16) for i in range(3)]

                def attn_block(kts, kr0, kd, q0, nq, scale, pso, wfn=None, Qs=None, Wsrc=None):
                    Qs = QT if Qs is None else Qs
                    pairs = [kts[i:i + 2] for i in range(0, len(kts), 2)]
                    npair = len(pairs)
                    base = ecnt[0]; ecnt[0] += npair

                    def smm(pi):
                        sc = pbig[(base + pi) % 2]
                        for j, kt in enumerate(pairs[pi]):
                            kb.op('pe', lambda e, j=j, kt=kt: e.matmul(sc[:, j * 512:j * 512 + nq], lhsT=KT[kr0:kr0 + kd, kt * 128:(kt + 1) * 128], rhs=Qs[kr0:kr0 + kd, q0:q0 + nq], start=True, stop=True),
                                  reads=[KT, Qs], writes=[sc])
                    smm(0)
                    for pi, pr in enumerate(pairs):
                        sc = pbig[(base + pi) % 2]; Et = E2[(base + pi) % 3]
                        w = len(pr)
                        if nq == 512:
                            kb.op('act', lambda e, sc=sc, Et=Et, w=w: e.activation(out=Et[:, 0:w * 512], in_=sc[:, 0:w * 512], func=AF.Exp, scale=scale), reads=[sc], writes=[Et])
                        else:
                            kb.op('act', lambda e, sc=sc, Et=Et, w=w: e.activation(out=Et[:, :].rearrange("p (j n) -> p j n", n=512)[:, 0:w, 0:nq],
                                  in_=sc[:, :].rearrange("p (j n) -> p j n", n=512)[:, 0:w, 0:nq], func=AF.Exp, scale=scale), reads=[sc], writes=[Et])
                        if pi + 1 < npair:
                            smm(pi + 1)
                        for j, kt in enumerate(pr):
                            idx = pi * 2 + j
                            wm = wfn(idx) if wfn is not None else None
                            if wm is not None:
                                kb.op('dve', lambda e, Et=Et, wm=wm, j=j: e.tensor_tensor(out=Et[:, j * 512:j * 512 + nq], in0=Et[:, j * 512:j * 512 + nq], in1=wm, op=ALU.mult), reads=[Et, Wsrc], writes=[Et])
                        for j, kt in enumerate(pr):
                            idx = pi * 2 + j
                            kb.op('pe', lambda e, kt=kt, Et=Et, idx=idx, j=j: e.matmul(pso[:, 0:nq], lhsT=V[:, kt, :], rhs=Et[:, j * 512:j * 512 + nq], start=(idx == 0), stop=(idx == len(kts) - 1)),
                                  reads=[V, Et], writes=[pso])

                def norm_store(pso, nq, dst_tile, dst_ap, dt_out_tile=None):
                    kb.op('dve', lambda e: e.reciprocal(out=rd[64:128, 0:nq], in_=pso[64:128, 0:nq]), reads=[pso], writes=[rd])
                    kb.op('dve', lambda e: e.tensor_tensor(out=dst_ap, in0=pso[0:64, 0:nq], in1=rd[64:128, 0:nq], op=ALU.mult), reads=[pso, rd], writes=[dst_tile])

                qchunks = [(j * 512, 512, True) for j in range(8)] + ([(SEQ, 256, False)] if ctx_out else [])
                all_kt = list(range(NT)); ctx_kt = [32, 33]
                pcnt = [0]
                kt0_holder = [0]
                for h in range(4):
                    kb.dma('sp', KT[:], S['FM'][C_MK + h], reads=[D_['FM']], writes=[KT])
                    kb.dma('sp', QT[:], S['FM'][C_MQ + h], reads=[D_['FM']], writes=[QT])
                    kb.dma('sp', V[:], S['VV'][:, :, 4 + h, :].rearrange("t p c -> p t c"), reads=[D_['VV']], writes=[V])
                    for (q0, nq, lat) in qchunks:
                        pso = pbank[2 + pcnt[0] % 2]; pcnt[0] += 1
                        attn_block(all_kt if lat else ctx_kt, 0, 96, q0, nq, MLA_SCALE, pso)
                        norm_store(pso, nq, onb, onb[0:64, 0:nq])
                        kb.dma('sp', S['oT'][1, h // 2, (h % 2) * 64:(h % 2) * 64 + 64, q0:q0 + nq], onb[0:64, 0:nq], reads=[onb], writes=[D_['oT']])
                for c2 in range(2):
                    kb.dma('sp', KT[:], S['FM'][C_DK + c2], reads=[D_['FM']], writes=[KT])
                    kb.dma('sp', QT[:], S['FM'][C_DQ + c2], reads=[D_['FM']], writes=[QT])
                    for i4 in range(4):
                        kb.op('dve', lambda e, i4=i4: e.tensor_scalar_mul(out=QM[i4][:], in0=QT[:], scalar1=m32[:, i4:i4 + 1]), reads=[QT, m32], writes=[QM[i4]])
                    for hh in range(2):
                        h = 2 * c2 + hh
                        kb.dma('sp', V[:], S['VV'][:, :, 8 + h, :].rearrange("t p c -> p t c"), reads=[D_['VV']], writes=[V])
                        for (q0, nq, lat) in qchunks:
                            kts = all_kt if lat else ctx_kt
                            attn_block(kts, 0, 128, q0, nq, DIFF_SCALE, pbank[2], Qs=QM[2 * hh])
                            attn_block(kts, 0, 128, q0, nq, DIFF_SCALE, pbank[3], Qs=QM[2 * hh + 1])
                            norm_store(pbank[2], nq, o1, o1[0:64, 0:nq])
                            norm_store(pbank[3], nq, o2, o2[0:64, 0:nq])
                            kb.op('dve', lambda e: e.scalar_tensor_tensor(out=o1[0:64, 0:nq], in0=o2[0:64, 0:nq], scalar=lamt[0:64, 5:6], in1=o1[0:64, 0:nq], op0=ALU.mult, op1=ALU.add),
                                  reads=[o1, o2, lamt], writes=[o1])
                            kb.op('dve', lambda e: e.tensor_tensor(out=osq[0:64, 0:nq], in0=o1[0:64, 0:nq], in1=o1[0:64, 0:nq], op=ALU.mult), reads=[o1], writes=[osq])
                            kb.op('pe', lambda e: e.matmul(pbank[0][0:64, 0:nq], lhsT=ones_b[0:64, 0:64], rhs=osq[0:64, 0:nq], start=True, stop=True), reads=[ones_b, osq], writes=[pbank[0]])
                            kb.op('act', lambda e: e.activation(out=rs2[0:64, 0:nq], in_=pbank[0][0:64, 0:nq], func=AF.Sqrt, scale=1.0 / 64, bias=epsd[0:64, 0:1]), reads=[pbank[0], epsd], writes=[rs2])
                            kb.op('dve', lambda e: e.reciprocal(out=rs2[0:64, 0:nq], in_=rs2[0:64, 0:nq]), reads=[rs2], writes=[rs2])
                            kb.op('dve', lambda e: e.scalar_tensor_tensor(out=onb[0:64, 0:nq], in0=o1[0:64, 0:nq], scalar=gsub[0:64, 0:1], in1=rs2[0:64, 0:nq], op0=ALU.mult, op1=ALU.mult),
                                  reads=[o1, gsub, rs2], writes=[onb])
                            kb.dma('sp', S['oT'][3, h // 2, (h % 2) * 64:(h % 2) * 64 + 64, q0:q0 + nq], onb[0:64, 0:nq], reads=[onb], writes=[D_['oT']])
                for c2 in range(2):
                    kb.dma('sp', KT[:], S['FM'][C_NAK + c2], reads=[D_['FM']], writes=[KT])
                    kb.dma('sp', QT[:], S['FM'][C_NAQ + c2], reads=[D_['FM']], writes=[QT])
                    for hh in range(2):
                        kb.op('dve', lambda e, hh=hh: e.tensor_scalar_mul(out=QM[hh][:], in0=QT[:], scalar1=m32[:, 4 + hh:5 + hh]), reads=[QT, m32], writes=[QM[hh]])
                    for hh in range(2):
                        h = 2 * c2 + hh
                        kb.dma('sp', V[:], S['VV'][:, :, h, :].rearrange("t p c -> p t c"), reads=[D_['VV']], writes=[V])
                        for (q0, nq, lat) in qchunks:
                            pso = pbank[2 + pcnt[0] % 2]; pcnt[0] += 1
                            if lat:
                                jq = q0 // 512
                                Wc = Wt3[0] if jq == 0 else (Wt3[2] if jq == 7 else Wt3[1])
                                kt0 = min(max(8 * jq - 4, 0), 48) // 2
                                kts = list(range(kt0, kt0 + 8)) + ctx_kt
                                wfn = (lambda idx, h=h, Wc=Wc: Wc[:, h, idx, :] if idx < 8 else None)
                                attn_block(kts, 0, 128, q0, nq, NA_SCALE, pso, wfn, Qs=QM[hh], Wsrc=Wc)
                            else:
                                attn_block(ctx_kt, 0, 128, q0, nq, NA_SCALE, pso, Qs=QM[hh])
                            norm_store(pso, nq, onb, onb[0:64, 0:nq])
                            kb.dma('sp', S['oT'][0, h // 2, (h % 2) * 64:(h % 2) * 64 + 64, q0:q0 + nq], onb[0:64, 0:nq], reads=[onb], writes=[D_['oT']])
                if debug == 'B':
                    kb.barrier()
                    dbg_out['oT'] = nc.dram_tensor('dbg_oT', [4, 2, 128, T], BF16, kind="ExternalOutput").ap()
                    kb.dma('sp', dbg_out['oT'], S['oT'], reads=[D_['oT']])
                kb.barrier()
            if debug == 'B':
                break
            TWO_PI = 2.0 * math.pi
            with contextlib.ExitStack() as es:
                uTn = sb(es, 'uTn', [128, T], BF16)
                iot = sb(es, 'iot', [128, T], F32)
                kb.dma('sp', iot[:, :], I['iotaT'][0:1, :].to_broadcast([128, T]), writes=[iot])
                P = sb(es, 's5p', [128, 24, 16], F32)
                LRE, LIM, DT, LR, TH, RR, M1, SN, CS, ARE, AIM, NRE, NIM, DEN, CRE, CIM, TMP = range(17)
                with nc.allow_non_contiguous_dma(reason="small s5 params"):
                    kb.dma('sp', P[:, LRE, :].rearrange("p (d k) -> p d k", d=2), I['s5_lam_re'][layer].rearrange("d (k two) p -> (two p) d k", two=2), writes=[P])
                    kb.dma('sp', P[:, LIM, :].rearrange("p (d k) -> p d k", d=2), I['s5_lam_im'][layer].rearrange("d (k two) p -> (two p) d k", two=2), writes=[P])
                    for two in range(2):
                        kb.dma('sp', P[two * 64:(two + 1) * 64, DT, :].rearrange("p (d k) -> p d k", d=2),
                               I['s5_log_dt'][layer].rearrange("d (k two) -> two d k", two=2)[two:two + 1].to_broadcast([64, 2, 8]), writes=[P])
                def sm(fn, *a, **k):
                    kb.op('dve', fn, reads=[P], writes=[P])
                kb.op('act', lambda e: e.activation(out=P[:, DT, :], in_=P[:, DT, :], func=AF.Exp), reads=[P], writes=[P])
                sm(lambda e: e.tensor_tensor(out=P[:, LR, :], in0=P[:, LRE, :], in1=P[:, DT, :], op=ALU.mult))
                sm(lambda e: e.tensor_tensor(out=P[:, TH, :], in0=P[:, LIM, :], in1=P[:, DT, :], op=ALU.mult))
                kb.op('act', lambda e: e.activation(out=P[:, RR, :], in_=P[:, LR, :], func=AF.Exp), reads=[P], writes=[P])
                sm(lambda e: e.tensor_scalar(out=P[:, TMP, :], in0=P[:, TH, :], scalar1=1.0 / TWO_PI, scalar2=12582912.0, op0=ALU.mult, op1=ALU.add))
                sm(lambda e: e.tensor_scalar_add(out=P[:, TMP, :], in0=P[:, TMP, :], scalar1=-12582912.0))
                sm(lambda e: e.scalar_tensor_tensor(out=P[:, M1, :], in0=P[:, TMP, :], scalar=-TWO_PI, in1=P[:, TH, :], op0=ALU.mult, op1=ALU.add))
                sm(lambda e: e.tensor_scalar(out=P[:, M1, :], in0=P[:, M1, :], scalar1=-3.14159, scalar2=3.14159, op0=ALU.max, op1=ALU.min))
                kb.op('act', lambda e: e.activation(out=P[:, SN, :], in_=P[:, M1, :], func=AF.Sin), reads=[P], writes=[P])
                sm(lambda e: e.tensor_scalar_add(out=P[:, CS, :], in0=P[:, TH, :], scalar1=0.5 * math.pi))
                sm(lambda e: e.tensor_scalar(out=P[:, TMP, :], in0=P[:, CS, :], scalar1=1.0 / TWO_PI, scalar2=12582912.0, op0=ALU.mult, op1=ALU.add))
                sm(lambda e: e.tensor_scalar_add(out=P[:, TMP, :], in0=P[:, TMP, :], scalar1=-12582912.0))
                sm(lambda e: e.scalar_tensor_tensor(out=P[:, M1, :], in0=P[:, TMP, :], scalar=-TWO_PI, in1=P[:, CS, :], op0=ALU.mult, op1=ALU.add))
                sm(lambda e: e.tensor_scalar(out=P[:, M1, :], in0=P[:, M1, :], scalar1=-3.14159, scalar2=3.14159, op0=ALU.max, op1=ALU.min))
                kb.op('act', lambda e: e.activation(out=P[:, CS, :], in_=P[:, M1, :], func=AF.Sin), reads=[P], writes=[P])
                sm(lambda e: e.tensor_tensor(out=P[:, ARE, :], in0=P[:, RR, :], in1=P[:, CS, :], op=ALU.mult))
                sm(lambda e: e.tensor_tensor(out=P[:, AIM, :], in0=P[:, RR, :], in1=P[:, SN, :], op=ALU.mult))
                sm(lambda e: e.tensor_scalar_add(out=P[:, ARE, :], in0=P[:, ARE, :], scalar1=-1.0))
                sm(lambda e: e.tensor_tensor(out=P[:, NRE, :], in0=P[:, ARE, :], in1=P[:, LRE, :], op=ALU.mult))
                sm(lambda e: e.tensor_tensor(out=P[:, TMP, :], in0=P[:, AIM, :], in1=P[:, LIM, :], op=ALU.mult))
                sm(lambda e: e.tensor_tensor(out=P[:, NRE, :], in0=P[:, NRE, :], in1=P[:, TMP, :], op=ALU.add))
                sm(lambda e: e.tensor_tensor(out=P[:, NIM, :], in0=P[:, AIM, :], in1=P[:, LRE, :], op=ALU.mult))
                sm(lambda e: e.tensor_tensor(out=P[:, TMP, :], in0=P[:, ARE, :], in1=P[:, LIM, :], op=ALU.mult))
                sm(lambda e: e.tensor_tensor(out=P[:, NIM, :], in0=P[:, NIM, :], in1=P[:, TMP, :], op=ALU.subtract))
                sm(lambda e: e.tensor_tensor(out=P[:, DEN, :], in0=P[:, LRE, :], in1=P[:, LRE, :], op=ALU.mult))
                sm(lambda e: e.tensor_tensor(out=P[:, TMP, :], in0=P[:, LIM, :], in1=P[:, LIM, :], op=ALU.mult))
                sm(lambda e: e.tensor_tensor(out=P[:, DEN, :], in0=P[:, DEN, :], in1=P[:, TMP, :], op=ALU.add))
                sm(lambda e: e.reciprocal(out=P[:, DEN, :], in_=P[:, DEN, :]))
                sm(lambda e: e.tensor_tensor(out=P[:, CRE, :], in0=P[:, NRE, :], in1=P[:, DEN, :], op=ALU.mult))
                sm(lambda e: e.tensor_tensor(out=P[:, CIM, :], in0=P[:, NIM, :], in1=P[:, DEN, :], op=ALU.mult))
                BbT = sb(es, 'BbT', [128, 16, 2, 128], BF16); CT = sb(es, 'CT', [128, 16, 2, 128], BF16)
                with contextlib.ExitStack() as es2:
                    braw = sb(es2, 'braw', [128, 2, 16, 16], F32)
                    bbar = sb(es2, 'bbar', [128, 2, 16, 16], F32)
                    btmp = sb(es2, 'btmp', [128, 16, 16], F32)
                    craw = sb(es2, 'craw', [128, 2, 4, 64], F32)
                    mB = sb(es2, 'mB', [128, 4, 128], F32); mC = sb(es2, 'mC', [128, 4, 128], F32)
                    kb.dma('sp', mB[:], I['maskB'].rearrange("b p q -> p b q"), writes=[mB]); kb.dma('sp', mC[:], I['maskC'].rearrange("b p q -> p b q"), writes=[mC])
                    with nc.allow_non_contiguous_dma(reason="s5 small"):
                        for ri, nm in enumerate(('s5_b_re', 's5_b_im')):
                            for dd in range(2):
                                kb.dma('sp', braw[:, ri, dd * 8:(dd + 1) * 8, :], I[nm][layer, dd].rearrange("(k two) p c -> (two p) k c", two=2), writes=[braw])
                        for ri, nm in enumerate(('s5_c_re', 's5_c_im')):
                            for dd in range(2):
                                kb.dma('sp', craw[:, ri, dd * 2:(dd + 1) * 2, :], I[nm][layer, dd].rearrange("(gh gl) c p -> (gl c) gh p", gl=8), writes=[craw])
                    cre_b = P[:, CRE, :].unsqueeze(2).to_broadcast([128, 16, 16]); cim_b = P[:, CIM, :].unsqueeze(2).to_broadcast([128, 16, 16])
                    kb.op('dve', lambda e: e.tensor_tensor(out=bbar[:, 0], in0=braw[:, 0], in1=cre_b, op=ALU.mult), reads=[braw, P], writes=[bbar])
                    kb.op('dve', lambda e: e.tensor_tensor(out=btmp[:], in0=braw[:, 1], in1=cim_b, op=ALU.mult), reads=[braw, P], writes=[btmp])
                    kb.op('dve', lambda e: e.tensor_tensor(out=bbar[:, 0], in0=bbar[:, 0], in1=btmp[:], op=ALU.subtract), reads=[bbar, btmp], writes=[bbar])
                    kb.op('dve', lambda e: e.tensor_tensor(out=bbar[:, 1], in0=braw[:, 1], in1=cre_b, op=ALU.mult), reads=[braw, P], writes=[bbar])
                    kb.op('dve', lambda e: e.tensor_tensor(out=btmp[:], in0=braw[:, 0], in1=cim_b, op=ALU.mult), reads=[braw, P], writes=[btmp])
                    kb.op('dve', lambda e: e.tensor_tensor(out=bbar[:, 1], in0=bbar[:, 1], in1=btmp[:], op=ALU.add), reads=[bbar, btmp], writes=[bbar])
                    pad = [sb(es2, 'pad%d' % i, [128, 128], BF16) for i in range(2)]
                    pc = [0]
                    for tile in range(16):
                        dd, k = tile // 8, tile % 8
                        for ri in range(2):
                            pd = pad[pc[0] % 2]; pt = ptr[pc[0] % 2]; pc[0] += 1
                            kb.op('dve', lambda e, pd=pd, tile=tile, ri=ri, k=k: e.tensor_tensor(out=pd[:].rearrange("p (a c) -> p a c", c=16), in0=bbar[:, ri, tile, :].unsqueeze(1).to_broadcast([128, 8, 16]),
                                  in1=mB[:, k % 4, :].rearrange("p (a c) -> p a c", c=16), op=ALU.mult), reads=[bbar, mB], writes=[pd])
                            kb.op('pe', lambda e, pd=pd, pt=pt: e.transpose(out=pt[:, 0:128], in_=pd[:], identity=ident[:]), reads=[pd, ident], writes=[pt])
                            kb.op('act', lambda e, pt=pt, tile=tile, ri=ri: e.copy(out=BbT[:, tile, ri, :], in_=pt[:, 0:128]), reads=[pt], writes=[BbT])
                            pd = pad[pc[0] % 2]; pt = ptr[pc[0] % 2]; pc[0] += 1
                            kb.op('dve', lambda e, pd=pd, dd=dd, ri=ri, k=k: e.scalar_tensor_tensor(out=pd[:].rearrange("p (a c) -> p a c", c=64), in0=craw[:, ri, dd * 2 + k // 4, :].unsqueeze(1).to_broadcast([128, 2, 64]),
                                  scalar=(1.0 if ri == 0 else -1.0), in1=mC[:, k % 4, :].rearrange("p (a c) -> p a c", c=64), op0=ALU.mult, op1=ALU.mult), reads=[craw, mC], writes=[pd])
                            kb.op('pe', lambda e, pd=pd, pt=pt: e.transpose(out=pt[:, 0:128], in_=pd[:], identity=ident[:]), reads=[pd, ident], writes=[pt])
                            kb.op('act', lambda e, pt=pt, tile=tile, ri=ri: e.copy(out=CT[:, tile, ri, :], in_=pt[:, 0:128]), reads=[pt], writes=[CT])
                    kb.barrier()
                yacc = sb(es, 'yacc', [128, 2, T], F32)
                dsk = sb(es, 'dsk', [128, 2], F32)
                with nc.allow_non_contiguous_dma(reason="tiny"):
                    kb.dma('sp', dsk[:], I['s5_d'][layer].rearrange("(c p) -> p c", p=128), writes=[dsk])
                for c2 in range(2):
                    kb.dma('sp', uTn[:], S['FM'][C_S5U + c2], reads=[D_['FM']], writes=[uTn])
                    kb.op('pool', lambda e, c2=c2: e.tensor_scalar_mul(out=yacc[:, c2, :], in0=uTn[:, :], scalar1=dsk[:, c2:c2 + 1]), reads=[uTn, dsk], writes=[yacc])
                cur_c = [1]
                cosT = sb(es, 'cosT', [128, T], F32); sinT = sb(es, 'sinT', [128, T], F32)
                dre = sb(es, 'dre', [128, T], F32); dim_ = sb(es, 'dim', [128, T], F32)
                gre = sb(es, 'gre', [128, T], F32); gim = sb(es, 'gim', [128, T], F32)
                hreb = sb(es, 'hreb', [128, T], BF16); himb = sb(es, 'himb', [128, T], BF16)
                lat_chunks = [(j * 512, 512) for j in range(8)]
                for tile in range(16):
                    dd, k = tile // 8, tile % 8
                    cch = k // 4
                    seq = ([(SEQ, 256)] + lat_chunks) if dd == 0 else (lat_chunks + [(SEQ, 256)])
                    if cur_c[0] != cch:
                        kb.dma('sp', uTn[:], S['FM'][C_S5U + cch], reads=[D_['FM']], writes=[uTn])
                        cur_c[0] = cch
                    for (buf, off) in ((sinT, 0.0), (cosT, 0.5)):
                        kb.op('pool', lambda e, buf=buf, off=off: e.tensor_scalar(out=buf[:], in0=(iot[:, :] if dd == 0 else iot[:, ::-1]), scalar1=P[:, TH, tile:tile + 1], scalar2=off * math.pi, op0=ALU.mult, op1=ALU.add), reads=[iot, P], writes=[buf])
                        kb.op('dve', lambda e, buf=buf: e.tensor_scalar(out=dre[:], in0=buf[:], scalar1=1.0 / TWO_PI, scalar2=12582912.0, op0=ALU.mult, op1=ALU.add), reads=[buf], writes=[dre])
                        kb.op('dve', lambda e: e.tensor_scalar_add(out=dre[:], in0=dre[:], scalar1=-12582912.0), reads=[dre], writes=[dre])
                        kb.op('dve', lambda e, buf=buf: e.scalar_tensor_tensor(out=buf[:], in0=dre[:], scalar=-TWO_PI, in1=buf[:], op0=ALU.mult, op1=ALU.add), reads=[dre, buf], writes=[buf])
                        kb.op('dve', lambda e, buf=buf: e.tensor_scalar(out=buf[:], in0=buf[:], scalar1=-3.14159, scalar2=3.14159, op0=ALU.max, op1=ALU.min), reads=[buf], writes=[buf])
                        kb.op('act', lambda e, buf=buf: e.activation(out=buf[:], in_=buf[:], func=AF.Sin), reads=[buf], writes=[buf])
                    so = 0
                    for ci, (to, n) in enumerate(seq):
                        pr = pbank[0 + (ci % 2) * 2]; pi_ = pbank[1 + (ci % 2) * 2]
                        kb.op('pe', lambda e, pr=pr, to=to, n=n: e.matmul(pr[:, 0:n], lhsT=BbT[:, tile, 0, :], rhs=uTn[:, to:to + n], start=True, stop=True), reads=[BbT, uTn], writes=[pr])
                        kb.op('pe', lambda e, pi_=pi_, to=to, n=n: e.matmul(pi_[:, 0:n], lhsT=BbT[:, tile, 1, :], rhs=uTn[:, to:to + n], start=True, stop=True), reads=[BbT, uTn], writes=[pi_])
                        sl = slice(so, so + n)
                        kb.op('act', lambda e, pr=pr, sl=sl, n=n: e.copy(out=gre[:, sl], in_=pr[:, 0:n]), reads=[pr], writes=[gre])
                        kb.op('act', lambda e, pi_=pi_, sl=sl, n=n: e.copy(out=gim[:, sl], in_=pi_[:, 0:n]), reads=[pi_], writes=[gim])
                        so += n
                    kb.op('dve', lambda e: e.tensor_tensor(out=dre[:], in0=gre[:], in1=cosT[:], op=ALU.mult), reads=[gre, cosT], writes=[dre])
                    kb.op('pool', lambda e: e.tensor_tensor(out=dim_[:], in0=gim[:], in1=cosT[:], op=ALU.mult), reads=[gim, cosT], writes=[dim_])
                    kb.op('dve', lambda e: e.tensor_tensor(out=gim[:], in0=gim[:], in1=sinT[:], op=ALU.mult), reads=[gim, sinT], writes=[gim])
                    kb.op('dve', lambda e: e.tensor_tensor(out=gre[:], in0=gre[:], in1=sinT[:], op=ALU.mult), reads=[gre, sinT], writes=[gre])
                    kb.op('dve', lambda e: e.tensor_tensor(out=dre[:], in0=dre[:], in1=gim[:], op=ALU.add), reads=[dre, gim], writes=[dre])
                    kb.op('dve', lambda e: e.tensor_tensor(out=dim_[:], in0=dim_[:], in1=gre[:], op=ALU.subtract), reads=[dim_, gre], writes=[dim_])
                    rb = P[:, RR, tile:tile + 1].to_broadcast([128, T])
                    if dd == 0:
                        kb.op('dve', lambda e: e.tensor_tensor_scan(out=gre[:], data0=rb, data1=dre[:], initial=0.0, op0=ALU.mult, op1=ALU.add), reads=[dre, P], writes=[gre])
                        kb.op('dve', lambda e: e.tensor_tensor_scan(out=gim[:], data0=rb, data1=dim_[:], initial=0.0, op0=ALU.mult, op1=ALU.add), reads=[dim_, P], writes=[gim])
                    else:
                        kb.op('dve', lambda e: e.tensor_tensor_scan(out=gre[:, ::-1], data0=rb, data1=dre[:, ::-1], initial=0.0, op0=ALU.mult, op1=ALU.add), reads=[dre, P], writes=[gre])
                        kb.op('dve', lambda e: e.tensor_tensor_scan(out=gim[:, ::-1], data0=rb, data1=dim_[:, ::-1], initial=0.0, op0=ALU.mult, op1=ALU.add), reads=[dim_, P], writes=[gim])
                    kb.op('pool', lambda e: e.tensor_tensor(out=dre[:], in0=gre[:], in1=cosT[:], op=ALU.mult), reads=[gre, cosT], writes=[dre])
                    kb.op('dve', lambda e: e.tensor_tensor(out=dim_[:], in0=gim[:], in1=cosT[:], op=ALU.mult), reads=[gim, cosT], writes=[dim_])
                    kb.op('dve', lambda e: e.tensor_tensor(out=gim[:], in0=gim[:], in1=sinT[:], op=ALU.mult), reads=[gim, sinT], writes=[gim])
                    kb.op('dve', lambda e: e.tensor_tensor(out=gre[:], in0=gre[:], in1=sinT[:], op=ALU.mult), reads=[gre, sinT], writes=[gre])
                    kb.op('dve', lambda e: e.tensor_tensor(out=hreb[:], in0=dre[:], in1=gim[:], op=ALU.subtract), reads=[dre, gim], writes=[hreb])
                    kb.op('dve', lambda e: e.tensor_tensor(out=himb[:], in0=gre[:], in1=dim_[:], op=ALU.add), reads=[gre, dim_], writes=[himb])
                    so = 0
                    for ci, (to, n) in enumerate(seq):
                        sl = slice(so, so + n)
                        py = pbank[4 + ci % 2]
                        kb.op('pe', lambda e, py=py, sl=sl, n=n: e.matmul(py[:, 0:n], lhsT=CT[:, tile, 0, :], rhs=hreb[:, sl], start=True, stop=False), reads=[CT, hreb], writes=[py])
                        kb.op('pe', lambda e, py=py, sl=sl, n=n: e.matmul(py[:, 0:n], lhsT=CT[:, tile, 1, :], rhs=himb[:, sl], start=False, stop=True), reads=[CT, himb], writes=[py])
                        kb.op('dve', lambda e, py=py, to=to, n=n: e.tensor_tensor(out=yacc[:, cch, to:to + n], in0=py[:, 0:n], in1=yacc[:, cch, to:to + n], op=ALU.add), reads=[py, yacc], writes=[yacc])
                        so += n
                wglu = sb(es, 'wglu', [128, 2, 512], BF16)
                kb.dma('pool', wglu[:], I['s5_w_glu'][layer].rearrange("(c p) n -> p c n", p=128), writes=[wglu])
                yb = sb(es, 'yb', [128, 2, 512], BF16)
                sg = sb(es, 'sg', [128, 512], F32)
                og = sb(es, 'og', [128, 2, 512], BF16)
                for (to, n) in lat_chunks + [(SEQ, 256)]:
                    kb.op('pool', lambda e, to=to, n=n: e.tensor_copy(out=yb[:, :, 0:n], in_=yacc[:, :, to:to + n]), reads=[yacc], writes=[yb])
                    for oc in range(2):
                        pv = pbank[0]; pg = pbank[1]
                        for c2 in range(2):
                            kb.op('pe', lambda e, c2=c2, oc=oc, n=n: e.matmul(pv[:, 0:n], lhsT=wglu[:, c2, oc * 128:(oc + 1) * 128], rhs=yb[:, c2, 0:n], start=(c2 == 0), stop=(c2 == 1)), reads=[wglu, yb], writes=[pv])
                        for c2 in range(2):
                            kb.op('pe', lambda e, c2=c2, oc=oc, n=n: e.matmul(pg[:, 0:n], lhsT=wglu[:, c2, 256 + oc * 128:256 + (oc + 1) * 128], rhs=yb[:, c2, 0:n], start=(c2 == 0), stop=(c2 == 1)), reads=[wglu, yb], writes=[pg])
                        kb.op('act', lambda e, n=n: e.activation(out=sg[:, 0:n], in_=pg[:, 0:n], func=AF.Sigmoid), reads=[pg], writes=[sg])
                        kb.op('dve', lambda e, oc=oc, n=n: e.tensor_tensor(out=og[:, oc, 0:n], in0=pv[:, 0:n], in1=sg[:, 0:n], op=ALU.mult), reads=[pv, sg], writes=[og])
                    kb.dma('sp', S['oT'][2, :, :, to:to + n].rearrange("c p t -> p c t"), og[:, :, 0:n], reads=[og], writes=[D_['oT']])
                if debug == 'C':
                    kb.barrier()
                    dbg_out['oT'] = nc.dram_tensor('dbg_oT', [4, 2, 128, T], BF16, kind="ExternalOutput").ap()
                    kb.dma('sp', dbg_out['oT'], S['oT'], reads=[D_['oT']])
                kb.barrier()
            if debug == 'C':
                break
            with contextlib.ExitStack() as es:
                wg = sb(es, 'wg', [128, 8, 4 * D], BF16)
                wbr = sb(es, 'wbr', [128, 4, 2, D], BF16)
                wo = sb(es, 'wo', [128, 8, D], BF16)
                for kc in range(8):
                    kb.dma('pool', wg[:, kc, :], I['w_gate'][layer, kc * 128:(kc + 1) * 128, :], writes=[wg])
                kb.dma('pool', wbr[:], I['w_branch'][layer].rearrange("b (c p) n -> p b c n", p=128), writes=[wbr])
                kb.dma('pool', wo[:], I['w_out'][layer].rearrange("(k p) n -> p k n", p=128), writes=[wo])
                bg = sb(es, 'bg', [128, 4 * D], F32)
                kb.dma('sp', bg[:], I['b_gate'][layer:layer + 1, :].to_broadcast([128, 4 * D]), writes=[bg])
                md = sb(es, 'mdD', [128, 2, 3, D], F32)
                for kind in range(2):
                    for jj, mi in enumerate((2, 3, 4)):
                        kb.dma('sp', md[:, kind, jj, :], S['mod'][kind:kind + 1, mi * D:(mi + 1) * D].to_broadcast([128, D]), reads=[D_['mod']], writes=[md])
                kb.op('dve', lambda e: e.tensor_scalar_add(out=md[:, :, 2, :], in0=md[:, :, 2, :], scalar1=1.0), reads=[md], writes=[md])
                lng = sb(es, 'lng', [128, 2, D], F32)
                kb.dma('sp', lng[:, 0, :], I['ln1_g'][layer:layer + 1, :].to_broadcast([128, D]), writes=[lng])
                kb.dma('sp', lng[:, 1, :], I['ln1_b'][layer:layer + 1, :].to_broadcast([128, D]), writes=[lng])
                epsl = sb(es, 'epsl', [128, 1], F32)
                kb.op('dve', lambda e: e.memset(epsl[:], LN_EPS), writes=[epsl])
                hTm = [sb(es, 'hTm%d' % i, [128, 8, 128], BF16) for i in range(2)]
                oTm = [sb(es, 'oTm%d' % i, [128, 4, 2, 128], BF16) for i in range(2)]
                xm = [sb(es, 'xm%d' % i, [128, D], F32) for i in range(2)]
                gt = sb(es, 'gt', [128, D], F32); macc = sb(es, 'macc', [128, D], F32); tt = sb(es, 'tt', [128, D], F32)
                mbb = sb(es, 'mbb', [128, D], BF16); mT = sb(es, 'mT', [128, 8, 128], BF16)
                st = sb(es, 'stD', [128, 8], F32); jk = sb(es, 'jkD', [128, D], F32)
                h2b = sb(es, 'h2b', [128, D], BF16); h2T = sb(es, 'h2Tt', [128, 8, 128], BF16)
                pcd = [0]

                def layer_norm(r_t, g_ap, b_ap, out_t):
                    kb.op('act', lambda e: e.activation(out=jk[:], in_=r_t[:], func=AF.Identity, accum_out=st[:, 0:1]), reads=[r_t], writes=[jk, st])
                    kb.op('act', lambda e: e.activation(out=jk[:], in_=r_t[:], func=AF.Square, accum_out=st[:, 1:2]), reads=[r_t], writes=[jk, st])
                    kb.op('dve', lambda e: e.tensor_scalar_mul(out=st[:, 2:4], in0=st[:, 0:2], scalar1=1.0 / D), reads=[st], writes=[st])
                    kb.op('dve', lambda e: e.tensor_tensor(out=st[:, 4:5], in0=st[:, 2:3], in1=st[:, 2:3], op=ALU.mult), reads=[st], writes=[st])
                    kb.op('dve', lambda e: e.tensor_tensor(out=st[:, 5:6], in0=st[:, 3:4], in1=st[:, 4:5], op=ALU.subtract), reads=[st], writes=[st])
                    kb.op('act', lambda e: e.activation(out=st[:, 6:7], in_=st[:, 5:6], func=AF.Sqrt, bias=epsl[:, 0:1]), reads=[st, epsl], writes=[st])
                    kb.op('dve', lambda e: e.reciprocal(out=st[:, 6:7], in_=st[:, 6:7]), reads=[st], writes=[st])
                    kb.op('dve', lambda e: e.tensor_scalar(out=out_t[:], in0=r_t[:], scalar1=st[:, 2:3], scalar2=st[:, 6:7], op0=ALU.subtract, op1=ALU.mult), reads=[r_t, st], writes=[out_t])
                    kb.op('dve', lambda e: e.tensor_tensor(out=out_t[:], in0=out_t[:], in1=g_ap, op=ALU.mult), reads=[out_t, lng], writes=[out_t])
                    kb.op('dve', lambda e: e.tensor_tensor(out=out_t[:], in0=out_t[:], in1=b_ap, op=ALU.add), reads=[out_t, lng], writes=[out_t])

                for ti in range(NT):
                    kind = 0 if ti < NLT else 1
                    hT_t = hTm[ti % 2]; oT_t = oTm[ti % 2]; x_t = xm[ti % 2]
                    kb.dma('sp', hT_t[:], S['hT'][ti], reads=[D_['hT']], writes=[hT_t])
                    kb.dma('sp', oT_t[:], S['oT'][:, :, :, ti * 128:(ti + 1) * 128].rearrange("b c p t -> p b c t"), reads=[D_['oT']], writes=[oT_t])
                    if layer == 0:
                        src = I['x'][ti * 128:(ti + 1) * 128, :] if kind == 0 else I['ctx'][(ti - NLT) * 128:(ti - NLT + 1) * 128, :]
                        kb.dma('sp', x_t[:], src, writes=[x_t])
                    else:
                        kb.dma('sp', x_t[:], S['xcur'][ti], reads=[D_['xcur']], writes=[x_t])
                    for br in range(4):
                        for hf in range(2):
                            pgt = pbank[hf]
                            for kc in range(8):
                                kb.op('pe', lambda e, kc=kc, pgt=pgt, br=br, hf=hf: e.matmul(pgt[:, :], lhsT=hT_t[:, kc, :], rhs=wg[:, kc, br * D + hf * 512:br * D + (hf + 1) * 512], start=(kc == 0), stop=(kc == 7)),
                                      reads=[hT_t, wg], writes=[pgt])
                            kb.op('dve', lambda e, pgt=pgt, br=br, hf=hf: e.tensor_tensor(out=gt[:, hf * 512:(hf + 1) * 512], in0=pgt[:, :], in1=bg[:, br * D + hf * 512:br * D + (hf + 1) * 512], op=ALU.add), reads=[pgt, bg], writes=[gt])
                        kb.op('act', lambda e: e.activation(out=gt[:], in_=gt[:], func=AF.Sigmoid), reads=[gt], writes=[gt])
                        for hf in range(2):
                            pbt = pbank[2 + hf]
                            for c2 in range(2):
                                kb.op('pe', lambda e, c2=c2, pbt=pbt, br=br, hf=hf: e.matmul(pbt[:, :], lhsT=oT_t[:, br, c2, :], rhs=wbr[:, br, c2, hf * 512:(hf + 1) * 512], start=(c2 == 0), stop=(c2 == 1)),
                                      reads=[oT_t, wbr], writes=[pbt])
                            dst = macc if br == 0 else tt
                            kb.op('dve', lambda e, pbt=pbt, hf=hf, dst=dst: e.tensor_tensor(out=dst[:, hf * 512:(hf + 1) * 512], in0=pbt[:, :], in1=gt[:, hf * 512:(hf + 1) * 512], op=ALU.mult), reads=[pbt, gt], writes=[dst])
                        if br > 0:
                            kb.op('pool', lambda e: e.tensor_tensor(out=macc[:], in0=macc[:], in1=tt[:], op=ALU.add), reads=[macc, tt], writes=[macc])
                    kb.op('pool', lambda e: e.tensor_copy(out=mbb[:], in_=macc[:]), reads=[macc], writes=[mbb])
                    pt = ptr[pcd[0] % 2]; pcd[0] += 1
                    for kc in range(8):
                        kb.op('pe', lambda e, kc=kc, pt=pt: e.transpose(out=pt[:, kc * 128:(kc + 1) * 128], in_=mbb[:, kc * 128:(kc + 1) * 128], identity=ident[:]), reads=[mbb, ident], writes=[pt])
                    kb.op('act', lambda e, pt=pt: e.copy(out=mT[:].rearrange("p k t -> p (k t)"), in_=pt[:]), reads=[pt], writes=[mT])
                    for hf in range(2):
                        py = pbank[4 + hf]
                        for kc in range(8):
                            kb.op('pe', lambda e, kc=kc, py=py, hf=hf: e.matmul(py[:, :], lhsT=mT[:, kc, :], rhs=wo[:, kc, hf * 512:(hf + 1) * 512], start=(kc == 0), stop=(kc == 7)), reads=[mT, wo], writes=[py])
                        kb.op('dve', lambda e, py=py, hf=hf: e.tensor_tensor(out=tt[:, hf * 512:(hf + 1) * 512], in0=py[:, :], in1=md[:, kind, 0, hf * 512:(hf + 1) * 512], op=ALU.mult), reads=[py, md], writes=[tt])
                    kb.op('dve', lambda e: e.scalar_tensor_tensor(out=tt[:], in0=x_t[:], scalar=ALPHA, in1=tt[:], op0=ALU.mult, op1=ALU.add), reads=[x_t, tt], writes=[tt])
                    layer_norm(tt, lng[:, 0, :], lng[:, 1, :], macc)
                    kb.dma('sp', S['x1'][ti], macc[:], reads=[macc], writes=[D_['x1']])
                    kb.op('dve', lambda e: e.tensor_tensor(out=tt[:], in0=macc[:], in1=md[:, kind, 2, :], op=ALU.mult), reads=[macc, md], writes=[tt])
                    kb.op('dve', lambda e: e.tensor_tensor(out=h2b[:], in0=tt[:], in1=md[:, kind, 1, :], op=ALU.add), reads=[tt, md], writes=[h2b])
                    if layer % 2 == 1:
                        kb.dma('sp', S['h2tm'][ti], h2b[:], reads=[h2b], writes=[D_['h2tm']])
                    pt = ptr[pcd[0] % 2]; pcd[0] += 1
                    for kc in range(8):
                        kb.op('pe', lambda e, kc=kc, pt=pt: e.transpose(out=pt[:, kc * 128:(kc + 1) * 128], in_=h2b[:, kc * 128:(kc + 1) * 128], identity=ident[:]), reads=[h2b, ident], writes=[pt])
                    kb.op('act', lambda e, pt=pt: e.copy(out=h2T[:].rearrange("p k t -> p (k t)"), in_=pt[:]), reads=[pt], writes=[h2T])
                    kb.dma('sp', S['h2T'][:, :, ti * 128:(ti + 1) * 128], h2T[:], reads=[h2T], writes=[D_['h2T']])
                kb.barrier()
            if layer % 2 == 1:
              with contextlib.ExitStack() as es:
                jl = layer // 2
                gk = sb(es, 'gkS', [128, NT, 2], F32); bei = sb(es, 'beiS', [128, NBLK], I32); posi = sb(es, 'posiS', [128, 2, NT], I32)
                ix1 = sb(es, 'ix1S', [128, NBLK, 56], I32); ix2 = sb(es, 'ix2S', [128, NBLK, 28], I32)
                es_r = contextlib.ExitStack(); es_r.__enter__()
                hblk = sb(es_r, 'hblkS', [128, 8, 1024], BF16)
                wr = sb(es_r, 'wrS', [128, 8, NEXP], BF16)
                kb.dma('pool', wr[:], I['moe_router'][jl].rearrange("(k p) n -> p k n", p=128), writes=[wr])
                oh = sb(es_r, 'ohS', [128, 2, NT, NEXP], F32)
                mkb = sb(es_r, 'mkbS', [128, NT, NEXP], BF16)
                rank = sb(es_r, 'rankS', [128, NT, NEXP], F32)
                run = sb(es_r, 'runS', [128, NEXP], F32)
                lg = sb(es_r, 'lgS', [128, 4, NEXP], F32); sc = sb(es_r, 'scS', [128, 8], F32)
                utri = sb(es_r, 'utriS', [128, 128], BF16); utf = sb(es_r, 'utfS', [128, 128], F32)
                kb.dma('sp', utf[:], I['utri'][:], writes=[utf])
                kb.op('dve', lambda e: e.tensor_copy(out=utri[:], in_=utf[:]), reads=[utf], writes=[utri])
                bst = sb(es_r, 'bstS', [128, NBLK], F32)
                kb.dma('sp', bst[:], I['bstart'][0:1, :].to_broadcast([128, NBLK]), writes=[bst])
                kb.op('dve', lambda e: e.memset(run[:], 0.0), writes=[run])
                for (t0, nb) in [(b_ * 1024, min(1024, T - b_ * 1024)) for b_ in range((T + 1023) // 1024)]:
                    kb.dma('sp', hblk[:, :, 0:nb], S['h2T'][:, :, t0:t0 + nb], reads=[D_['h2T']], writes=[hblk])
                    for s_ in range(nb // 128):
                        ti = t0 // 128 + s_
                        pl = pbank[s_ % 2]
                        for kc in range(8):
                            kb.op('pe', lambda e, kc=kc, pl=pl, s_=s_: e.matmul(pl[:, 0:NEXP], lhsT=hblk[:, kc, s_ * 128:(s_ + 1) * 128], rhs=wr[:, kc, :], start=(kc == 0), stop=(kc == 7)), reads=[hblk, wr], writes=[pl])
                        kb.op('dve', lambda e, pl=pl: e.tensor_copy(out=lg[:, 0, :], in_=pl[:, 0:NEXP]), reads=[pl], writes=[lg])
                        kb.op('dve', lambda e: e.reduce_max(out=sc[:, 0:1], in_=lg[:, 0, :], axis=AX.X), reads=[lg], writes=[sc])
                        kb.op('dve', lambda e, ti=ti: e.tensor_scalar(out=oh[:, 0, ti, :], in0=lg[:, 0, :], scalar1=sc[:, 0:1], scalar2=None, op0=ALU.is_equal), reads=[lg, sc], writes=[oh])
                        kb.op('dve', lambda e, ti=ti: e.scalar_tensor_tensor(out=lg[:, 2, :], in0=oh[:, 0, ti, :], scalar=-1e30, in1=lg[:, 0, :], op0=ALU.mult, op1=ALU.add), reads=[lg, oh], writes=[lg])
                        kb.op('dve', lambda e: e.reduce_max(out=sc[:, 1:2], in_=lg[:, 2, :], axis=AX.X), reads=[lg], writes=[sc])
                        kb.op('dve', lambda e, ti=ti: e.tensor_scalar(out=oh[:, 1, ti, :], in0=lg[:, 2, :], scalar1=sc[:, 1:2], scalar2=None, op0=ALU.is_equal), reads=[lg, sc], writes=[oh])
                        kb.op('dve', lambda e: e.tensor_tensor(out=sc[:, 2:3], in0=sc[:, 1:2], in1=sc[:, 0:1], op=ALU.subtract), reads=[sc], writes=[sc])
                        kb.op('act', lambda e: e.activation(out=sc[:, 3:4], in_=sc[:, 2:3], func=AF.Exp), reads=[sc], writes=[sc])
                        kb.op('dve', lambda e: e.tensor_scalar_add(out=sc[:, 4:5], in0=sc[:, 3:4], scalar1=1.0), reads=[sc], writes=[sc])
                        kb.op('dve', lambda e, ti=ti: e.reciprocal(out=gk[:, ti, 0:1], in_=sc[:, 4:5]), reads=[sc], writes=[gk])
                        kb.op('dve', lambda e, ti=ti: e.tensor_tensor(out=gk[:, ti, 1:2], in0=sc[:, 3:4], in1=gk[:, ti, 0:1], op=ALU.mult), reads=[sc, gk], writes=[gk])
                        kb.op('dve', lambda e, ti=ti: e.tensor_tensor(out=mkb[:, ti, :], in0=oh[:, 0, ti, :], in1=oh[:, 1, ti, :], op=ALU.add), reads=[oh], writes=[mkb])
                        pr_ = pbank[2 + s_ % 2]
                        kb.op('pe', lambda e, pr_=pr_, ti=ti: e.matmul(pr_[:, 0:NEXP], lhsT=utri[:], rhs=mkb[:, ti, :], start=True, stop=True), reads=[utri, mkb], writes=[pr_])
                        kb.op('pe', lambda e, pr_=pr_, ti=ti: e.matmul(pr_[:, NEXP:2 * NEXP], lhsT=ones_b[:], rhs=mkb[:, ti, :], start=True, stop=True), reads=[ones_b, mkb], writes=[pr_])
                        kb.op('dve', lambda e, pr_=pr_, ti=ti: e.tensor_tensor(out=rank[:, ti, :], in0=pr_[:, 0:NEXP], in1=run[:], op=ALU.add), reads=[pr_, run], writes=[rank])
                        kb.op('dve', lambda e, pr_=pr_: e.tensor_tensor(out=run[:], in0=pr_[:, NEXP:2 * NEXP], in1=run[:], op=ALU.add), reads=[pr_, run], writes=[run])
                cw = sb(es_r, 'cwS', [128, 6, NEXP], F32)
                MAGIC = 12582912.0
                kb.op('dve', lambda e: e.tensor_scalar(out=cw[:, 3, :], in0=run[:], scalar1=float(BS - 1), scalar2=1.0 / BS, op0=ALU.add, op1=ALU.mult), reads=[run], writes=[cw])
                kb.op('dve', lambda e: e.tensor_scalar(out=cw[:, 3, :], in0=cw[:, 3, :], scalar1=(-0.5 + 0.5 / BS), scalar2=MAGIC, op0=ALU.add, op1=ALU.add), reads=[cw], writes=[cw])
                kb.op('dve', lambda e: e.tensor_scalar(out=cw[:, 0, :], in0=cw[:, 3, :], scalar1=-MAGIC, scalar2=float(BS), op0=ALU.add, op1=ALU.mult), reads=[cw], writes=[cw])
                kb.op('dve', lambda e: e.tensor_copy(out=cw[:, 1, 0:1], in_=cw[:, 0, 0:1]), reads=[cw], writes=[cw])
                for e_ in range(1, NEXP):
                    kb.op('dve', lambda e, e_=e_: e.tensor_tensor(out=cw[:, 1, e_:e_ + 1], in0=cw[:, 1, e_ - 1:e_], in1=cw[:, 0, e_:e_ + 1], op=ALU.add), reads=[cw], writes=[cw])
                kb.op('dve', lambda e: e.tensor_tensor(out=cw[:, 2, :], in0=cw[:, 1, :], in1=cw[:, 0, :], op=ALU.subtract), reads=[cw], writes=[cw])
                cmpb = sb(es_r, 'cmpbS', [128, NBLK, NEXP], F32)
                bef = sb(es_r, 'befS', [128, NBLK], F32)
                kb.op('dve', lambda e: e.tensor_tensor(out=cmpb[:], in0=cw[:, 1, :].unsqueeze(1).to_broadcast([128, NBLK, NEXP]), in1=bst[:].unsqueeze(2).to_broadcast([128, NBLK, NEXP]), op=ALU.is_le), reads=[cw, bst], writes=[cmpb])
                kb.op('dve', lambda e: e.reduce_sum(out=bef[:], in_=cmpb[:], axis=AX.X), reads=[cmpb], writes=[bef])
                kb.op('dve', lambda e: e.tensor_scalar_min(out=bef[:], in0=bef[:], scalar1=float(NEXP - 1)), reads=[bef], writes=[bef])
                kb.op('dve', lambda e: e.tensor_copy(out=bei[:], in_=bef[:]), reads=[bef], writes=[bei])
                b1 = sb(es_r, 'b1S', [128, 56], F32); b2 = sb(es_r, 'b2S', [128, 28], F32)
                kb.dma('sp', b1[:], I['base1'][:], writes=[b1]); kb.dma('sp', b2[:], I['base2'][:], writes=[b2])
                ix1f = sb(es_r, 'ix1fS', [128, NBLK, 56], F32); ix2f = sb(es_r, 'ix2fS', [128, NBLK, 28], F32)
                kb.op('dve', lambda e: e.scalar_tensor_tensor(out=ix1f[:], in0=bef[:].unsqueeze(2).to_broadcast([128, NBLK, 56]), scalar=7168.0, in1=b1[:].unsqueeze(1).to_broadcast([128, NBLK, 56]), op0=ALU.mult, op1=ALU.add), reads=[bef, b1], writes=[ix1f])
                kb.op('dve', lambda e: e.scalar_tensor_tensor(out=ix2f[:], in0=bef[:].unsqueeze(2).to_broadcast([128, NBLK, 28]), scalar=3584.0, in1=b2[:].unsqueeze(1).to_broadcast([128, NBLK, 28]), op0=ALU.mult, op1=ALU.add), reads=[bef, b2], writes=[ix2f])
                if jl > 0:
                    kb.op('dve', lambda e: e.tensor_scalar_add(out=ix1f[:], in0=ix1f[:], scalar1=float(jl * NEXP * D * 7)), reads=[ix1f], writes=[ix1f])
                    kb.op('dve', lambda e: e.tensor_scalar_add(out=ix2f[:], in0=ix2f[:], scalar1=float(jl * NEXP * D_FF)), reads=[ix2f], writes=[ix2f])
                kb.op('dve', lambda e: e.tensor_copy(out=ix1[:], in_=ix1f[:]), reads=[ix1f], writes=[ix1])
                kb.op('dve', lambda e: e.tensor_copy(out=ix2[:], in_=ix2f[:]), reads=[ix2f], writes=[ix2])
                posf = sb(es_r, 'posfS', [128, 2, NT], F32)
                kb.op('dve', lambda e: e.tensor_tensor(out=rank[:], in0=rank[:], in1=cw[:, 2, :].unsqueeze(1).to_broadcast([128, NT, NEXP]), op=ALU.add), reads=[rank, cw], writes=[rank])
                for k2 in range(2):
                    kb.op('dve', lambda e, k2=k2: e.tensor_tensor(out=oh[:, k2], in0=oh[:, k2], in1=rank[:], op=ALU.mult), reads=[oh, rank], writes=[oh])
                    kb.op('dve', lambda e, k2=k2: e.reduce_sum(out=posf[:, k2, :], in_=oh[:, k2], axis=AX.X), reads=[oh], writes=[posf])
                kb.op('dve', lambda e: e.tensor_copy(out=posi[:], in_=posf[:]), reads=[posf], writes=[posi])
                zt = sb(es_r, 'ztS', [128, 4 * D], BF16)
                kb.op('pool', lambda e: e.memset(zt[:], 0.0), writes=[zt])
                for a_ in range(NSLOT // 512):
                    kb.dma('sp', S['Xs'][a_ * 512:(a_ + 1) * 512, :].rearrange("(p j) d -> p (j d)", j=4), zt[:], reads=[zt], writes=[D_['Xs']])
                h2t = [sb(es_r, 'h2tS%d' % i, [128, D], BF16) for i in range(2)]
                for ti in range(NT):
                    ht_ = h2t[ti % 2]
                    kb.dma('sp', ht_[:], S['h2tm'][ti], reads=[D_['h2tm']], writes=[ht_])
                    for k2 in range(2):
                        kb.idma(S['Xs'][:, :], bass.IndirectOffsetOnAxis(ap=posi[:, k2, ti:ti + 1].bitcast(mybir.dt.uint32), axis=0), ht_[:], None, reads=[ht_, posi, D_['Xs']], writes=[D_['Xs']])
                kb.barrier()
                es_r.close()
                es2 = contextlib.ExitStack(); es2.__enter__()
                NS = BS // 128
                xb = sb(es2, 'xbS', [128, NS, D], BF16)
                hb2 = sb(es2, 'hb2S', [128, 8, BS], BF16)
                acc = sb(es2, 'accS', [128, NS, D], F32)
                w1c = [sb(es2, 'w1cS%d' % i, [128, 8, 512], BF16) for i in range(2)]
                w3c = [sb(es2, 'w3cS%d' % i, [128, 8, 512], BF16) for i in range(2)]
                w2c = [sb(es2, 'w2cS%d' % i, [128, 4, D], BF16) for i in range(2)]
                gT = [sb(es2, 'gTS%d' % i, [128, 4, BS], BF16) for i in range(2)]
                sil = sb(es2, 'silS', [128, 512], F32)
                W1v = I['moe_w1'].rearrange("j e k (c n) -> (j e k c) n", n=512); W3v = I['moe_w3'].rearrange("j e k (c n) -> (j e k c) n", n=512); W2v = I['moe_w2'].rearrange("j e f n -> (j e f) n")
                U32 = mybir.dt.uint32
                wcnt = [0]; pcs = [0]
                for b_ in range(NBLK):
                    kb.dma('sp', xb[:], S['Xs'][b_ * BS:(b_ + 1) * BS, :].rearrange("(s p) d -> p s d", p=128), reads=[D_['Xs']], writes=[xb])
                    for s_ in range(NS):
                        pt = ptr[pcs[0] % 2]; pcs[0] += 1
                        for kc in range(8):
                            kb.op('pe', lambda e, kc=kc, pt=pt, s_=s_: e.transpose(out=pt[:, kc * 128:(kc + 1) * 128], in_=xb[:, s_, kc * 128:(kc + 1) * 128], identity=ident[:]), reads=[xb, ident], writes=[pt])
                        kb.op('act', lambda e, pt=pt, s_=s_: e.copy(out=hb2[:, :, s_ * 128:(s_ + 1) * 128], in_=pt[:].rearrange("p (k t) -> p k t", k=8)), reads=[pt], writes=[hb2])
                    kb.op('pool', lambda e: e.memset(acc[:], 0.0), writes=[acc])
                    for fc in range(D_FF // 512):
                        a1 = w1c[wcnt[0] % 2]; a3 = w3c[wcnt[0] % 2]; a2 = w2c[wcnt[0] % 2]; g_ = gT[wcnt[0] % 2]; wcnt[0] += 1
                        for kc in range(8):
                            kb.idma(a1[:, kc, :], None, W1v[:, :], bass.IndirectOffsetOnAxis(ap=ix1[:, b_, kc * 7 + fc:kc * 7 + fc + 1].bitcast(U32), axis=0), reads=[ix1], writes=[a1])
                            kb.idma(a3[:, kc, :], None, W3v[:, :], bass.IndirectOffsetOnAxis(ap=ix1[:, b_, kc * 7 + fc:kc * 7 + fc + 1].bitcast(U32), axis=0), reads=[ix1], writes=[a3])
                        for f in range(4):
                            kb.idma(a2[:, f, :], None, W2v[:, :], bass.IndirectOffsetOnAxis(ap=ix2[:, b_, fc * 4 + f:fc * 4 + f + 1].bitcast(U32), axis=0), reads=[ix2], writes=[a2])
                        for f in range(4):
                            for th in range(BS // 512):
                                c0 = th * 512
                                p1 = pbank[0 + ((f * 2 + th) % 2) * 2]; p3 = pbank[1 + ((f * 2 + th) % 2) * 2]
                                for kc in range(8):
                                    kb.op('pe', lambda e, kc=kc, p1=p1, a1=a1, f=f, c0=c0: e.matmul(p1[:, :], lhsT=a1[:, kc, f * 128:(f + 1) * 128], rhs=hb2[:, kc, c0:c0 + 512], start=(kc == 0), stop=(kc == 7)), reads=[a1, hb2], writes=[p1])
                                for kc in range(8):
                                    kb.op('pe', lambda e, kc=kc, p3=p3, a3=a3, f=f, c0=c0: e.matmul(p3[:, :], lhsT=a3[:, kc, f * 128:(f + 1) * 128], rhs=hb2[:, kc, c0:c0 + 512], start=(kc == 0), stop=(kc == 7)), reads=[a3, hb2], writes=[p3])
                                kb.op('act', lambda e, p1=p1: e.activation(out=sil[:], in_=p1[:, :], func=AF.Silu), reads=[p1], writes=[sil])
                                kb.op('dve', lambda e, p3=p3, g_=g_, f=f, c0=c0: e.tensor_tensor(out=g_[:, f, c0:c0 + 512], in0=p3[:, :], in1=sil[:], op=ALU.mult), reads=[p3, sil], writes=[g_])
                        for s_ in range(NS):
                            for hf in range(2):
                                py = pbank[4 + (s_ * 2 + hf) % 2]
                                for f in range(4):
                                    kb.op('pe', lambda e, f=f, py=py, g_=g_, a2=a2, s_=s_, hf=hf: e.matmul(py[:, :], lhsT=g_[:, f, s_ * 128:(s_ + 1) * 128], rhs=a2[:, f, hf * 512:(hf + 1) * 512], start=(f == 0), stop=(f == 3)), reads=[g_, a2], writes=[py])
                                kb.op('dve', lambda e, py=py, s_=s_, hf=hf: e.tensor_tensor(out=acc[:, s_, hf * 512:(hf + 1) * 512], in0=py[:, :], in1=acc[:, s_, hf * 512:(hf + 1) * 512], op=ALU.add), reads=[py, acc], writes=[acc])
                    kb.dma('sp', S['Ys'][b_ * BS:(b_ + 1) * BS, :].rearrange("(s p) d -> p s d", p=128), acc[:], reads=[acc], writes=[D_['Ys']])
                kb.barrier()
                es2.close()
                es2 = es
                md5 = sb(es2, 'md5S', [128, 2, D], F32)
                for kind in range(2):
                    kb.dma('sp', md5[:, kind, :], S['mod'][kind:kind + 1, 5 * D:6 * D].to_broadcast([128, D]), reads=[D_['mod']], writes=[md5])
                lng = sb(es2, 'lng2S', [128, 2, D], F32)
                kb.dma('sp', lng[:, 0, :], I['ln2_g'][layer:layer + 1, :].to_broadcast([128, D]), writes=[lng])
                kb.dma('sp', lng[:, 1, :], I['ln2_b'][layer:layer + 1, :].to_broadcast([128, D]), writes=[lng])
                epsl = sb(es2, 'epsl2S', [128, 1], F32)
                kb.op('dve', lambda e: e.memset(epsl[:], LN_EPS), writes=[epsl])
                y0 = [sb(es2, 'y0S%d' % i, [128, D], F32) for i in range(2)]; y1 = [sb(es2, 'y1S%d' % i, [128, D], F32) for i in range(2)]
                x1t = [sb(es2, 'x1tS%d' % i, [128, D], F32) for i in range(2)]
                rr = sb(es2, 'rrS', [128, D], F32); xo = [sb(es2, 'xoS%d' % i, [128, D], F32) for i in range(2)]
                st = sb(es2, 'stS', [128, 8], F32); jk = sb(es2, 'jkS', [128, D], F32)
                for ti in range(NT):
                    kind = 0 if ti < NLT else 1
                    if layer == DEPTH - 1 and kind == 1:
                        continue
                    x1_ = x1t[ti % 2]; xo_ = xo[ti % 2]; ya = y0[ti % 2]; yb_ = y1[ti % 2]
                    kb.dma('sp', x1_[:], S['x1'][ti], reads=[D_['x1']], writes=[x1_])
                    kb.idma(ya[:], None, S['Ys'][:, :], bass.IndirectOffsetOnAxis(ap=posi[:, 0, ti:ti + 1].bitcast(mybir.dt.uint32), axis=0), reads=[posi, D_['Ys']], writes=[ya])
                    kb.idma(yb_[:], None, S['Ys'][:, :], bass.IndirectOffsetOnAxis(ap=posi[:, 1, ti:ti + 1].bitcast(mybir.dt.uint32), axis=0), reads=[posi, D_['Ys']], writes=[yb_])
                    kb.op('dve', lambda e, ya=ya, ti=ti: e.tensor_scalar_mul(out=rr[:], in0=ya[:], scalar1=gk[:, ti, 0:1]), reads=[ya, gk], writes=[rr])
                    kb.op('dve', lambda e, yb_=yb_, ti=ti: e.scalar_tensor_tensor(out=rr[:], in0=yb_[:], scalar=gk[:, ti, 1:2], in1=rr[:], op0=ALU.mult, op1=ALU.add), reads=[yb_, gk, rr], writes=[rr])
                    kb.op('dve', lambda e, kind=kind: e.tensor_tensor(out=rr[:], in0=rr[:], in1=md5[:, kind, :], op=ALU.mult), reads=[rr, md5], writes=[rr])
                    kb.op('dve', lambda e, x1_=x1_: e.scalar_tensor_tensor(out=rr[:], in0=x1_[:], scalar=ALPHA, in1=rr[:], op0=ALU.mult, op1=ALU.add), reads=[x1_, rr], writes=[rr])
                    kb.op('act', lambda e: e.activation(out=jk[:], in_=rr[:], func=AF.Identity, accum_out=st[:, 0:1]), reads=[rr], writes=[jk, st])
                    kb.op('act', lambda e: e.activation(out=jk[:], in_=rr[:], func=AF.Square, accum_out=st[:, 1:2]), reads=[rr], writes=[jk, st])
                    kb.op('dve', lambda e: e.tensor_scalar_mul(out=st[:, 2:4], in0=st[:, 0:2], scalar1=1.0 / D), reads=[st], writes=[st])
                    kb.op('dve', lambda e: e.tensor_tensor(out=st[:, 4:5], in0=st[:, 2:3], in1=st[:, 2:3], op=ALU.mult), reads=[st], writes=[st])
                    kb.op('dve', lambda e: e.tensor_tensor(out=st[:, 5:6], in0=st[:, 3:4], in1=st[:, 4:5], op=ALU.subtract), reads=[st], writes=[st])
                    kb.op('act', lambda e: e.activation(out=st[:, 6:7], in_=st[:, 5:6], func=AF.Sqrt, bias=epsl[:, 0:1]), reads=[st, epsl], writes=[st])
                    kb.op('dve', lambda e: e.reciprocal(out=st[:, 6:7], in_=st[:, 6:7]), reads=[st], writes=[st])
                    kb.op('dve', lambda e, xo_=xo_: e.tensor_scalar(out=xo_[:], in0=rr[:], scalar1=st[:, 2:3], scalar2=st[:, 6:7], op0=ALU.subtract, op1=ALU.mult), reads=[rr, st], writes=[xo_])
                    kb.op('dve', lambda e, xo_=xo_: e.tensor_tensor(out=xo_[:], in0=xo_[:], in1=lng[:, 0, :], op=ALU.mult), reads=[xo_, lng], writes=[xo_])
                    kb.op('dve', lambda e, xo_=xo_: e.tensor_tensor(out=xo_[:], in0=xo_[:], in1=lng[:, 1, :], op=ALU.add), reads=[xo_, lng], writes=[xo_])
                    if layer == DEPTH - 1:
                        kb.dma('sp', OUT[ti * 128:(ti + 1) * 128, :], xo_[:], reads=[xo_])
                    else:
                        kb.dma('sp', S['xcur'][ti], xo_[:], reads=[xo_], writes=[D_['xcur']])
                kb.barrier()
            with contextlib.ExitStack() as es:
              if layer % 2 == 0:
                  is_moe = (layer % 2 == 1)
                  jl = layer // 2
                  nexp = NEXP if is_moe else 1
                  TB = 1536
                  hblk = sb(es, 'hblk', [128, 8, TB], BF16)
                  acc = sb(es, 'accE', [128, TB // 128, D], F32)
                  w1c = [sb(es, 'w1c%d' % i, [128, 8, 512], BF16) for i in range(2)]
                  w3c = [sb(es, 'w3c%d' % i, [128, 8, 512], BF16) for i in range(2)]
                  w2c = [sb(es, 'w2c%d' % i, [128, 4, D], BF16) for i in range(2)]
                  gT = [sb(es, 'gT%d' % i, [128, 4, TB], BF16) for i in range(2)]
                  sil = sb(es, 'sil', [128, 512], F32)
                  gates = sb(es, 'gates', [128, TB // 128, NEXP], F32)
                  wr = sb(es, 'wr', [128, 8, NEXP], BF16)
                  lg = sb(es, 'lg', [128, 4, NEXP], F32); sc = sb(es, 'scE', [128, 8], F32)
                  md5 = sb(es, 'md5', [128, 2, D], F32)
                  for kind in range(2):
                      kb.dma('sp', md5[:, kind, :], S['mod'][kind:kind + 1, 5 * D:6 * D].to_broadcast([128, D]), reads=[D_['mod']], writes=[md5])
                  lng = sb(es, 'lng2', [128, 2, D], F32)
                  kb.dma('sp', lng[:, 0, :], I['ln2_g'][layer:layer + 1, :].to_broadcast([128, D]), writes=[lng])
                  kb.dma('sp', lng[:, 1, :], I['ln2_b'][layer:layer + 1, :].to_broadcast([128, D]), writes=[lng])
                  epsl = sb(es, 'epsl2', [128, 1], F32)
                  kb.op('dve', lambda e: e.memset(epsl[:], LN_EPS), writes=[epsl])
                  x1t = [sb(es, 'x1t%d' % i, [128, D], F32) for i in range(2)]
                  rr = sb(es, 'rrE', [128, D], F32); xo = [sb(es, 'xoE%d' % i, [128, D], F32) for i in range(2)]
                  st = sb(es, 'stE', [128, 8], F32); jk = sb(es, 'jkE', [128, D], F32)
                  if is_moe:
                      kb.dma('pool', wr[:], I['moe_router'][jl].rearrange("(k p) n -> p k n", p=128), writes=[wr])
                  wcnt = [0]
                  blocks = [(b * TB, min(TB, T - b * TB)) for b in range((T + TB - 1) // TB)]
                  for (t0, nb) in blocks:
                      ntl = nb // 128
                      kb.dma('sp', hblk[:, :, 0:nb], S['h2T'][:, :, t0:t0 + nb], reads=[D_['h2T']], writes=[hblk])
                      kb.op('pool', lambda e: e.memset(acc[:], 0.0), writes=[acc])
                      if is_moe:
                          for s_ in range(ntl):
                              pl = pbank[4 + s_ % 2]
                              for kc in range(8):
                                  kb.op('pe', lambda e, kc=kc, pl=pl, s_=s_: e.matmul(pl[:, 0:NEXP], lhsT=hblk[:, kc, s_ * 128:(s_ + 1) * 128], rhs=wr[:, kc, :], start=(kc == 0), stop=(kc == 7)), reads=[hblk, wr], writes=[pl])
                              kb.op('dve', lambda e, pl=pl: e.tensor_copy(out=lg[:, 0, :], in_=pl[:, 0:NEXP]), reads=[pl], writes=[lg])
                              kb.op('dve', lambda e: e.reduce_max(out=sc[:, 0:1], in_=lg[:, 0, :], axis=AX.X), reads=[lg], writes=[sc])
                              kb.op('dve', lambda e: e.tensor_scalar(out=lg[:, 1, :], in0=lg[:, 0, :], scalar1=sc[:, 0:1], scalar2=None, op0=ALU.is_equal), reads=[lg, sc], writes=[lg])
                              kb.op('dve', lambda e: e.scalar_tensor_tensor(out=lg[:, 2, :], in0=lg[:, 1, :], scalar=-1e30, in1=lg[:, 0, :], op0=ALU.mult, op1=ALU.add), reads=[lg], writes=[lg])
                              kb.op('dve', lambda e: e.reduce_max(out=sc[:, 1:2], in_=lg[:, 2, :], axis=AX.X), reads=[lg], writes=[sc])
                              kb.op('dve', lambda e: e.tensor_scalar(out=lg[:, 3, :], in0=lg[:, 2, :], scalar1=sc[:, 1:2], scalar2=None, op0=ALU.is_equal), reads=[lg, sc], writes=[lg])
                              kb.op('dve', lambda e: e.tensor_tensor(out=sc[:, 2:3], in0=sc[:, 1:2], in1=sc[:, 0:1], op=ALU.subtract), reads=[sc], writes=[sc])
                              kb.op('act', lambda e: e.activation(out=sc[:, 3:4], in_=sc[:, 2:3], func=AF.Exp), reads=[sc], writes=[sc])
                              kb.op('dve', lambda e: e.tensor_scalar_add(out=sc[:, 4:5], in0=sc[:, 3:4], scalar1=1.0), reads=[sc], writes=[sc])
                              kb.op('dve', lambda e: e.reciprocal(out=sc[:, 4:5], in_=sc[:, 4:5]), reads=[sc], writes=[sc])
                              kb.op('dve', lambda e: e.tensor_tensor(out=sc[:, 5:6], in0=sc[:, 3:4], in1=sc[:, 4:5], op=ALU.mult), reads=[sc], writes=[sc])
                              kb.op('dve', lambda e: e.tensor_scalar_mul(out=lg[:, 1, :], in0=lg[:, 1, :], scalar1=sc[:, 4:5]), reads=[lg, sc], writes=[lg])
                              kb.op('dve', lambda e, s_=s_: e.scalar_tensor_tensor(out=gates[:, s_, :], in0=lg[:, 3, :], scalar=sc[:, 5:6], in1=lg[:, 1, :], op0=ALU.mult, op1=ALU.add), reads=[lg, sc], writes=[gates])
                      for ex in range(nexp):
                          if is_moe:
                              W1 = I['moe_w1'][jl, ex]; W3 = I['moe_w3'][jl, ex]; W2 = I['moe_w2'][jl, ex]
                          else:
                              W1 = I['ffn_w1'][jl]; W3 = I['ffn_w3'][jl]; W2 = I['ffn_w2'][jl]
                          for fc in range(D_FF // 512):
                              a1 = w1c[wcnt[0] % 2]; a3 = w3c[wcnt[0] % 2]; a2 = w2c[wcnt[0] % 2]; g_ = gT[wcnt[0] % 2]; wcnt[0] += 1
                              kb.dma('pool', a1[:], W1[:, fc * 512:(fc + 1) * 512].rearrange("(k p) n -> p k n", p=128), writes=[a1])
                              kb.dma('pool', a3[:], W3[:, fc * 512:(fc + 1) * 512].rearrange("(k p) n -> p k n", p=128), writes=[a3])
                              kb.dma('pool', a2[:], W2[fc * 512:(fc + 1) * 512, :].rearrange("(f p) n -> p f n", p=128), writes=[a2])
                              for f in range(4):
                                  for th in range((nb + 511) // 512):
                                      c0 = th * 512; n = min(512, nb - c0)
                                      p1 = pbank[0 + (f * 2 + th) % 2 * 2]; p3 = pbank[1 + (f * 2 + th) % 2 * 2]
                                      for kc in range(8):
                                          kb.op('pe', lambda e, kc=kc, p1=p1, a1=a1, f=f, c0=c0, n=n: e.matmul(p1[:, 0:n], lhsT=a1[:, kc, f * 128:(f + 1) * 128], rhs=hblk[:, kc, c0:c0 + n], start=(kc == 0), stop=(kc == 7)), reads=[a1, hblk], writes=[p1])
                                      for kc in range(8):
                                          kb.op('pe', lambda e, kc=kc, p3=p3, a3=a3, f=f, c0=c0, n=n: e.matmul(p3[:, 0:n], lhsT=a3[:, kc, f * 128:(f + 1) * 128], rhs=hblk[:, kc, c0:c0 + n], start=(kc == 0), stop=(kc == 7)), reads=[a3, hblk], writes=[p3])
                                      kb.op('act', lambda e, p1=p1, n=n: e.activation(out=sil[:, 0:n], in_=p1[:, 0:n], func=AF.Silu), reads=[p1], writes=[sil])
                                      kb.op('dve', lambda e, p3=p3, g_=g_, f=f, c0=c0, n=n: e.tensor_tensor(out=g_[:, f, c0:c0 + n], in0=p3[:, 0:n], in1=sil[:, 0:n], op=ALU.mult), reads=[p3, sil], writes=[g_])
                              for s_ in range(ntl):
                                  for hf in range(2):
                                      py = pbank[4 + (s_ * 2 + hf) % 2]
                                      for f in range(4):
                                          kb.op('pe', lambda e, f=f, py=py, g_=g_, a2=a2, s_=s_, hf=hf: e.matmul(py[:, :], lhsT=g_[:, f, s_ * 128:(s_ + 1) * 128], rhs=a2[:, f, hf * 512:(hf + 1) * 512], start=(f == 0), stop=(f == 3)), reads=[g_, a2], writes=[py])
                                      if is_moe:
                                          kb.op('dve', lambda e, py=py, s_=s_, hf=hf, ex=ex: e.scalar_tensor_tensor(out=acc[:, s_, hf * 512:(hf + 1) * 512], in0=py[:, :], scalar=gates[:, s_, ex:ex + 1], in1=acc[:, s_, hf * 512:(hf + 1) * 512], op0=ALU.mult, op1=ALU.add), reads=[py, gates, acc], writes=[acc])
                                      else:
                                          kb.op('dve', lambda e, py=py, s_=s_, hf=hf: e.tensor_tensor(out=acc[:, s_, hf * 512:(hf + 1) * 512], in0=py[:, :], in1=acc[:, s_, hf * 512:(hf + 1) * 512], op=ALU.add), reads=[py, acc], writes=[acc])
                      for s_ in range(ntl):
                          ti = t0 // 128 + s_
                          kind = 0 if ti < NLT else 1
                          if layer == DEPTH - 1 and kind == 1:
                              continue
                          x1_ = x1t[ti % 2]; xo_ = xo[ti % 2]
                          kb.dma('sp', x1_[:], S['x1'][ti], reads=[D_['x1']], writes=[x1_])
                          kb.op('dve', lambda e, s_=s_, kind=kind: e.tensor_tensor(out=rr[:], in0=acc[:, s_, :], in1=md5[:, kind, :], op=ALU.mult), reads=[acc, md5], writes=[rr])
                          kb.op('dve', lambda e, x1_=x1_: e.scalar_tensor_tensor(out=rr[:], in0=x1_[:], scalar=ALPHA, in1=rr[:], op0=ALU.mult, op1=ALU.add), reads=[x1_, rr], writes=[rr])
                          kb.op('act', lambda e: e.activation(out=jk[:], in_=rr[:], func=AF.Identity, accum_out=st[:, 0:1]), reads=[rr], writes=[jk, st])
                          kb.op('act', lambda e: e.activation(out=jk[:], in_=rr[:], func=AF.Square, accum_out=st[:, 1:2]), reads=[rr], writes=[jk, st])
                          kb.op('dve', lambda e: e.tensor_scalar_mul(out=st[:, 2:4], in0=st[:, 0:2], scalar1=1.0 / D), reads=[st], writes=[st])
                          kb.op('dve', lambda e: e.tensor_tensor(out=st[:, 4:5], in0=st[:, 2:3], in1=st[:, 2:3], op=ALU.mult), reads=[st], writes=[st])
                          kb.op('dve', lambda e: e.tensor_tensor(out=st[:, 5:6], in0=st[:, 3:4], in1=st[:, 4:5], op=ALU.subtract), reads=[st], writes=[st])
                          kb.op('act', lambda e: e.activation(out=st[:, 6:7], in_=st[:, 5:6], func=AF.Sqrt, bias=epsl[:, 0:1]), reads=[st, epsl], writes=[st])
                          kb.op('dve', lambda e: e.reciprocal(out=st[:, 6:7], in_=st[:, 6:7]), reads=[st], writes=[st])
                          kb.op('dve', lambda e, xo_=xo_: e.tensor_scalar(out=xo_[:], in0=rr[:], scalar1=st[:, 2:3], scalar2=st[:, 6:7], op0=ALU.subtract, op1=ALU.mult), reads=[rr, st], writes=[xo_])
                          kb.op('dve', lambda e, xo_=xo_: e.tensor_tensor(out=xo_[:], in0=xo_[:], in1=lng[:, 0, :], op=ALU.mult), reads=[xo_, lng], writes=[xo_])
                          kb.op('dve', lambda e, xo_=xo_: e.tensor_tensor(out=xo_[:], in0=xo_[:], in1=lng[:, 1, :], op=ALU.add), reads=[xo_, lng], writes=[xo_])
                          if layer == DEPTH - 1:
                              kb.dma('sp', OUT[ti * 128:(ti + 1) * 128, :], xo_[:], reads=[xo_])
                          else:
                              kb.dma('sp', S['xcur'][ti], xo_[:], reads=[xo_], writes=[D_['xcur']])
              if debug == 'L' and layer == nlayers - 1:
                  kb.barrier()
                  dbg_out['xcur'] = nc.dram_tensor('dbg_xcur', [NT, 128, D], F32, kind="ExternalOutput").ap()
                  kb.dma('sp', dbg_out['xcur'], S['xcur'], reads=[D_['xcur']])
              kb.barrier()

        kb.barrier()
    return nc, dbg_out


def host_consts():
    c = {}
    c['ident'] = np.eye(128, dtype=np.float32)
    t = np.arange(SEQ)
    prow, pcol = t // 64, t % 64
    inv = (10000.0 ** (-np.arange(8, dtype=np.float32) / 8)).astype(np.float32)
    cosr = np.ones((T, 16), np.float32); sinr = np.zeros((T, 16), np.float32)
    ar = prow[:, None].astype(np.float32) * inv[None, :]
    ac = pcol[:, None].astype(np.float32) * inv[None, :]
    cosr[:SEQ, 0:8] = np.cos(ar); cosr[:SEQ, 8:16] = np.cos(ac)
    sinr[:SEQ, 0:8] = np.sin(ar); sinr[:SEQ, 8:16] = np.sin(ac)
    c['ropec'] = np.ascontiguousarray(cosr.reshape(NT, 128, 16).transpose(1, 0, 2))
    c['ropes'] = np.ascontiguousarray(sinr.reshape(NT, 128, 16).transpose(1, 0, 2))
    c['iota128'] = np.arange(128, dtype=np.float32)[None, :]
    pp = np.arange(128)[:, None, None]
    c['base1'] = ((np.arange(8)[None, :, None] * 128 + pp) * 7 + np.arange(7)[None, None, :]).reshape(128, 56).astype(np.float32)
    c['base2'] = (np.arange(28)[None, :] * 128 + np.arange(128)[:, None]).astype(np.float32)
    c['utri'] = np.triu(np.ones((128, 128), np.float32), 1)
    c['bstart'] = (np.arange(NBLK, dtype=np.float32) * float(BS))[None, :]
    m32 = np.zeros((128, 6), np.float32)
    for p_ in range(128):
        m32[p_, p_ // 32] = 1.0
        m32[p_, 4 + p_ // 64] = 1.0
    c['m32'] = m32
    c['iotaT'] = np.stack([np.arange(T, dtype=np.float32), (T - 1) - np.arange(T, dtype=np.float32)])
    cc = np.arange(NT, dtype=np.float32)
    c['cv'] = np.stack([127.0 - 128.0 * cc, 1.0 + 128.0 * cc]).astype(np.float32)
    mC = np.zeros((4, 128, 128), np.float32)
    for b in range(4):
        for co in range(128):
            for st in range(128):
                if co // 16 == b * 2 + st // 64:
                    mC[b, co, st] = 1.0
    c['maskC'] = mC
    c['maskB'] = np.ascontiguousarray(mC.transpose(0, 2, 1))
    return c


def rpb_toeplitz(rpb):
    kc = np.arange(64)[:, None]; qc = np.arange(64)[None, :]
    c0 = np.clip(qc - 8, 0, 48)
    inwin = (kc >= c0) & (kc <= c0 + 15)
    idx = np.clip(kc - qc + 15, 0, 30)
    g = rpb[:, :, :, idx]
    return np.where(inwin[None, None, None], g, np.float32(-30000.0)).astype(np.float32)


_CACHE = {}


def make_in_maps(inputs, ncores=8):
    consts = host_consts()
    shared = {}
    for k, v in inputs.items():
        if k in ('x', 'c', 'ctx', 'c_ctx', 'na_rpb'):
            continue
        shared[k] = np.ascontiguousarray(np.asarray(v, dtype=np.float32))
    shared['rpbT'] = rpb_toeplitz(np.asarray(inputs['na_rpb'], dtype=np.float32))
    shared['c_ctx'] = np.asarray(inputs['c_ctx'], np.float32).reshape(1, D)
    shared.update(consts)
    maps = []
    for b in range(ncores):
        m = dict(shared)
        m['x'] = np.ascontiguousarray(np.asarray(inputs['x'][b], np.float32))
        m['ctx'] = np.ascontiguousarray(np.asarray(inputs['ctx'][b], np.float32))
        m['c'] = np.asarray(inputs['c'][b], np.float32).reshape(1, D)
        maps.append(m)
    return maps


def kernel(**inputs):
    if 'nc' not in _CACHE:
        _CACHE['nc'] = build_program()[0]
    nc = _CACHE['nc']
    maps = make_in_maps(inputs, 8)
    res = run_bass_kernel_spmd(nc, maps, core_ids=list(range(8)))
    return np.stack([np.asarray(r['out'], dtype=np.float32) for r in res.results], axis=0)
```

```python
import math
import contextlib
import numpy as np
import ml_dtypes
import concourse.bass as bass
import concourse.mybir as mybir
from concourse.bass_utils import run_bass_kernel_spmd

F32 = mybir.dt.float32
BF16 = mybir.dt.bfloat16
ALU = mybir.AluOpType
AF = mybir.ActivationFunctionType
AX = mybir.AxisListType

D = 1024
SEQ = 4096
CTX = 256
T = SEQ + CTX
NT = T // 128
NLT = SEQ // 128
DEPTH = 4
IN_COLS = 2208
D_FF = 3584
NEXP = 8
ALPHA = (2 * DEPTH) ** 0.25
LN_EPS = 1e-5
RMS_EPS = 1e-6
NA_SCALE = 64 ** -0.5
MLA_SCALE = 96 ** -0.5
DIFF_SCALE = 32 ** -0.5
C_NAQ, C_NAK, C_S5U, C_MQ, C_MK, C_DQ, C_DK = 0, 2, 4, 6, 10, 14, 18
NFM = 22
BS = 1024
NBLK = 17
NSLOT = NBLK * BS
I32 = mybir.dt.int32


class KB:
    def __init__(self, nc, es):
        self.nc = nc
        self.es = es
        self.eng = {'pe': nc.tensor, 'act': nc.scalar, 'dve': nc.vector, 'pool': nc.gpsimd, 'sp': nc.sync}
        self.sem = {}
        self.cnt = {}
        for e in ('pe', 'act', 'dve', 'pool'):
            self.sem[e] = es.enter_context(nc.semaphore('sem_' + e))
            self.cnt[e] = 0
        self.KD = 24
        self.dsem = {}
        self.dcnt = {}
        for q in ('sp', 'pool'):
            self.dsem[q] = [es.enter_context(nc.semaphore('dsem_%s%d' % (q, i))) for i in range(self.KD)]
            self.dcnt[q] = 0
        self.waited = {e: {} for e in self.eng}
        self.semobj = {}
        for e in self.sem:
            self.semobj[e] = self.sem[e]
        for q in self.dsem:
            for i, s in enumerate(self.dsem[q]):
                self.semobj[(q, i)] = s

    def _wait(self, e, tok):
        key, val = tok
        if self.waited[e].get(key, 0) >= val:
            return
        self.eng[e].wait_ge(self.semobj[key], val)
        self.waited[e][key] = val

    def _deps(self, e, reads, writes):
        deps = {}

        def add(tok):
            if tok is None:
                return
            k, v = tok
            if e == 'pe' and k == 'pe':
                return
            if deps.get(k, 0) < v:
                deps[k] = v
        for t in reads:
            add(t.w)
        for t in writes:
            add(t.w)
            for k, v in t.r.items():
                add((k, v))
        for k, v in deps.items():
            self._wait(e, (k, v))

    def _mark(self, tok, reads, writes):
        k, v = tok
        for t in reads:
            if t.r.get(k, 0) < v:
                t.r[k] = v
        for t in writes:
            t.w = tok
            t.r = {}

    def op(self, e, fn, reads=(), writes=()):
        self._deps(e, reads, writes)
        inst = fn(self.eng[e])
        self.cnt[e] += 1
        inst.then_inc(self.sem[e], 1)
        self._mark((e, self.cnt[e]), reads, writes)

    def dma(self, q, out, in_, reads=(), writes=()):
        self._deps(q, reads, writes)
        n = self.dcnt[q]
        k = n % self.KD
        gen = n // self.KD + 1
        if gen > 1:
            self._wait(q, ((q, k), 16 * (gen - 1)))
        self.eng[q].dma_start(out=out, in_=in_).then_inc(self.dsem[q][k], 16)
        self.dcnt[q] += 1
        self._mark(((q, k), 16 * gen), reads, writes)

    def idma(self, out, out_offset, in_, in_offset, reads=(), writes=()):
        q = 'pool'
        self._deps(q, reads, writes)
        n = self.dcnt[q]
        k = n % self.KD
        gen = n // self.KD + 1
        if gen > 1:
            self._wait(q, ((q, k), 16 * (gen - 1)))
        self.eng[q].indirect_dma_start(out=out, out_offset=out_offset, in_=in_, in_offset=in_offset).then_inc(self.dsem[q][k], 16)
        self.dcnt[q] += 1
        self._mark(((q, k), 16 * gen), reads, writes)

    def barrier(self):
        toks = [(e, self.cnt[e]) for e in self.cnt if self.cnt[e] > 0]
        for q in self.dsem:
            n = self.dcnt[q]
            for k in range(self.KD):
                uses = (n - k + self.KD - 1) // self.KD if n > k else 0
                if uses > 0:
                    toks.append(((q, k), 16 * uses))
        for e in self.eng:
            for tok in toks:
                self._wait(e, tok)


class Tl:
    def __init__(self, t):
        self.t = t
        self.w = None
        self.r = {}

    def __getitem__(self, key):
        return self.t[key]


def build_program(nlayers=DEPTH, debug=None):
    nc = bass.Bass("TRN2", target_bir_lowering=False)
    es0 = contextlib.ExitStack()

    def din(name, shape, dt=F32):
        return nc.dram_tensor(name, list(shape), dt, kind="ExternalInput").ap()

    def dscr(name, shape, dt):
        return nc.dram_tensor(name, list(shape), dt, kind="Internal").ap()

    I = {}
    I['x'] = din('x', [SEQ, D]); I['ctx'] = din('ctx', [CTX, D]); I['c'] = din('c', [1, D]); I['c_ctx'] = din('c_ctx', [1, D])
    I['w_ada'] = din('w_ada', [DEPTH, D, 6 * D]); I['b_ada'] = din('b_ada', [DEPTH, 6 * D])
    I['w_in'] = din('w_in', [DEPTH, D, IN_COLS])
    I['rpbT'] = din('rpbT', [DEPTH, 4, 15, 64, 64])
    I['mla_q_norm'] = din('mla_q_norm', [DEPTH, 256]); I['mla_kv_norm'] = din('mla_kv_norm', [DEPTH, 128])
    I['mla_w_uq'] = din('mla_w_uq', [DEPTH, 256, 384]); I['mla_w_ukv'] = din('mla_w_ukv', [DEPTH, 128, 512])
    for nm in ('s5_lam_re', 's5_lam_im'):
        I[nm] = din(nm, [DEPTH, 2, 16, 64])
    I['s5_log_dt'] = din('s5_log_dt', [DEPTH, 2, 16])
    for nm in ('s5_b_re', 's5_b_im'):
        I[nm] = din(nm, [DEPTH, 2, 16, 64, 16])
    for nm in ('s5_c_re', 's5_c_im'):
        I[nm] = din(nm, [DEPTH, 2, 16, 16, 64])
    I['s5_d'] = din('s5_d', [DEPTH, 256]); I['s5_w_glu'] = din('s5_w_glu', [DEPTH, 256, 512])
    for nm in ('diff_lam_q1', 'diff_lam_k1', 'diff_lam_q2', 'diff_lam_k2'):
        I[nm] = din(nm, [DEPTH, 32])
    I['diff_subln'] = din('diff_subln', [DEPTH, 64])
    I['w_branch'] = din('w_branch', [DEPTH, 4, 256, D]); I['w_gate'] = din('w_gate', [DEPTH, D, 4 * D]); I['b_gate'] = din('b_gate', [DEPTH, 4 * D])
    I['w_out'] = din('w_out', [DEPTH, D, D])
    for nm in ('ln1_g', 'ln1_b', 'ln2_g', 'ln2_b'):
        I[nm] = din(nm, [DEPTH, D])
    for nm in ('ffn_w1', 'ffn_w3'):
        I[nm] = din(nm, [2, D, D_FF])
    I['ffn_w2'] = din('ffn_w2', [2, D_FF, D])
    I['moe_router'] = din('moe_router', [2, D, NEXP])
    for nm in ('moe_w1', 'moe_w3'):
        I[nm] = din(nm, [2, NEXP, D, D_FF])
    I['moe_w2'] = din('moe_w2', [2, NEXP, D_FF, D])
    I['ident'] = din('ident', [128, 128]); I['ropec'] = din('ropec', [128, NT, 16]); I['ropes'] = din('ropes', [128, NT, 16])
    I['iota128'] = din('iota128', [1, 128]); I['base1'] = din('base1', [128, 56]); I['base2'] = din('base2', [128, 28]); I['utri'] = din('utri', [128, 128]); I['bstart'] = din('bstart', [1, NBLK]); I['m32'] = din('m32', [128, 6]); I['iotaT'] = din('iotaT', [2, T]); I['cv'] = din('cv', [2, NT]); I['maskC'] = din('maskC', [4, 128, 128]); I['maskB'] = din('maskB', [4, 128, 128])

    OUT = nc.dram_tensor('out', [SEQ, D], F32, kind="ExternalOutput").ap()
    dbg_out = {}

    S = {}
    S['xcur'] = dscr('xcur', [NT, 128, D], F32)
    S['x1'] = dscr('x1s', [NT, 128, D], F32)
    S['hT'] = dscr('hTs', [NT, 128, 8, 128], BF16)
    S['h2T'] = dscr('h2Ts', [128, 8, T], BF16)
    S['FM'] = dscr('FMs', [NFM, 128, T], BF16)
    S['VV'] = dscr('VVs', [NT, 128, 12, 128], BF16)
    S['oT'] = dscr('oTs', [4, 2, 128, T], BF16)
    S['mod'] = dscr('mods', [2, 6 * D], F32)
    S['h2tm'] = dscr('h2tms', [NT, 128, D], BF16)
    S['Xs'] = dscr('Xss', [NSLOT, D], BF16)
    S['Ys'] = dscr('Yss', [NSLOT, D], F32)
    D_ = {k: Tl(v) for k, v in S.items()}

    with es0:
        kb = KB(nc, es0)
        ucnt = [0]

        def sb(es, name, shape, dt):
            ucnt[0] += 1
            return Tl(es.enter_context(nc.sbuf_tensor('%s_u%d' % (name, ucnt[0]), list(shape), dt)))

        def ps(es, name, shape, dt):
            return Tl(es.enter_context(nc.psum_tensor(name, list(shape), dt)))

        ident_f = sb(es0, 'ident_f', [128, 128], F32)
        ident = sb(es0, 'ident_b', [128, 128], BF16)
        ones_b = sb(es0, 'ones_b', [128, 128], BF16)
        condT = sb(es0, 'condT', [128, 8, 2], BF16)
        ctmp = sb(es0, 'ctmp', [128, 8, 2], F32)
        kb.dma('sp', ident_f[:], I['ident'][:], writes=[ident_f])
        kb.op('dve', lambda e: e.tensor_copy(out=ident[:], in_=ident_f[:]), reads=[ident_f], writes=[ident])
        kb.op('dve', lambda e: e.memset(ones_b[:], 1.0), writes=[ones_b])
        with nc.allow_non_contiguous_dma(reason="tiny cond vector"):
            kb.dma('sp', ctmp[:, :, 0], I['c'][0].rearrange("(k p) -> p k", p=128), writes=[ctmp])
            kb.dma('sp', ctmp[:, :, 1], I['c_ctx'][0].rearrange("(k p) -> p k", p=128), writes=[ctmp])
        kb.op('act', lambda e: e.activation(out=condT[:], in_=ctmp[:], func=AF.Silu), reads=[ctmp], writes=[condT])

        pbig = [ps(es0, 'pbig%d' % i, [128, 1024], F32) for i in range(2)]
        pbank = [ps(es0, 'pb%d' % i, [128, 512], F32) for i in range(4)]
        pbank += [Tl(pbig[0].t[:, 0:512]), Tl(pbig[0].t[:, 512:1024])]
        ptr = [Tl(pbig[1].t[:, 0:512].bitcast(BF16)), Tl(pbig[1].t[:, 512:1024].bitcast(BF16))]

        for layer in range(nlayers):
            ctx_out = layer < DEPTH - 1
            lam_init = 0.8 - 0.6 * math.exp(-0.3 * layer)
            with contextlib.ExitStack() as es:
                wst = [sb(es, 'wada%d' % i, [128, 8, 512], BF16) for i in range(2)]
                modsb = sb(es, 'modsb', [2, 6 * D], F32)
                bada = sb(es, 'bada', [2, 6 * D], F32)
                kb.dma('sp', bada[:], I['b_ada'][layer:layer + 1, :].partition_broadcast(2) if False else I['b_ada'][layer:layer + 1, :].to_broadcast([2, 6 * D]), writes=[bada])
                for cc in range(12):
                    w = wst[cc % 2]
                    kb.dma('pool', w[:], I['w_ada'][layer, :, cc * 512:(cc + 1) * 512].rearrange("(k p) n -> p k n", p=128), writes=[w])
                    pb = pbank[cc % 2]
                    for kc in range(8):
                        kb.op('pe', lambda e, kc=kc, w=w, pb=pb: e.matmul(pb[0:2, :], lhsT=condT[:, kc, :], rhs=w[:, kc, :], start=(kc == 0), stop=(kc == 7)),
                              reads=[condT, w], writes=[pb])
                    kb.op('dve', lambda e, cc=cc, pb=pb: e.tensor_tensor(out=modsb[:, cc * 512:(cc + 1) * 512], in0=pb[0:2, :], in1=bada[:, cc * 512:(cc + 1) * 512], op=ALU.add),
                          reads=[pb, bada], writes=[modsb])
                kb.dma('sp', S['mod'][:], modsb[:], reads=[modsb], writes=[D_['mod']])
                if debug == 'M':
                    dbg_out['mod'] = nc.dram_tensor('dbg_mod', [2, 6 * D], F32, kind="ExternalOutput").ap()
                    kb.dma('sp', dbg_out['mod'][:], modsb[:], reads=[modsb])
                kb.barrier()
            if debug == 'M':
                break
            with contextlib.ExitStack() as es:
                win = sb(es, 'win', [128, 8, IN_COLS], BF16)
                wuq = sb(es, 'wuq', [128, 2, 384], BF16)
                wukv = sb(es, 'wukv', [128, 512], BF16)
                kb.dma('pool', win[:], I['w_in'][layer].rearrange("(k p) n -> p k n", p=128), writes=[win])
                kb.dma('pool', wuq[:], I['mla_w_uq'][layer].rearrange("(k p) n -> p k n", p=128), writes=[wuq])
                kb.dma('pool', wukv[:], I['mla_w_ukv'][layer], writes=[wukv])
                modb = sb(es, 'modbA', [128, 2, 2, D], F32)
                for kind in range(2):
                    for j in range(2):
                        kb.dma('sp', modb[:, kind, j, :], S['mod'][kind:kind + 1, j * D:(j + 1) * D].to_broadcast([128, D]), reads=[D_['mod']], writes=[modb])
                kb.op('dve', lambda e: e.tensor_scalar_add(out=modb[:, :, 1, :], in0=modb[:, :, 1, :], scalar1=1.0), reads=[modb], writes=[modb])
                qg = sb(es, 'qg', [128, 256], F32); kvg = sb(es, 'kvg', [128, 128], F32)
                kb.dma('sp', qg[:], I['mla_q_norm'][layer:layer + 1, :].to_broadcast([128, 256]), writes=[qg])
                kb.dma('sp', kvg[:], I['mla_kv_norm'][layer:layer + 1, :].to_broadcast([128, 128]), writes=[kvg])
                rc = sb(es, 'rc', [128, NT, 16], F32); rs = sb(es, 'rs', [128, NT, 16], F32)
                kb.dma('sp', rc[:], I['ropec'][:], writes=[rc]); kb.dma('sp', rs[:], I['ropes'][:], writes=[rs])
                epsq = sb(es, 'epsq', [128, 1], F32)
                kb.op('dve', lambda e: e.memset(epsq[:], RMS_EPS), writes=[epsq])
                xt = [sb(es, 'xt%d' % i, [128, D], F32) for i in range(2)]
                htmp_2 = [sb(es, 'htmp%d' % i_, [128, D], F32) for i_ in range(2)]
                hb_2 = [sb(es, 'hb%d' % i_, [128, D], BF16) for i_ in range(2)]
                hTt = [sb(es, 'hTt%d' % i, [128, 8, 128], BF16) for i in range(2)]
                z_2 = [sb(es, 'z%d' % i_, [128, IN_COLS], F32) for i_ in range(2)]
                zb_2 = [sb(es, 'zb%d' % i_, [128, IN_COLS], BF16) for i_ in range(2)]
                fm = [sb(es, 'fm%d' % i, [128, NFM, 256], BF16) for i in range(2)]
                vv = [sb(es, 'vv%d' % i, [128, 12, 128], BF16) for i in range(2)]
                for i in range(2):
                    kb.op('pool', lambda e, i=i: e.memset(vv[i][:], 1.0), writes=[vv[i]])
                    kb.op('pool', lambda e, i=i: e.memset(fm[i][:], 0.0), writes=[fm[i]])
                ss_2 = [sb(es, 'ss%d' % i_, [128, 4], F32) for i_ in range(2)]
                junk_2 = [sb(es, 'junk%d' % i_, [128, 256], F32) for i_ in range(2)]
                qn_2 = [sb(es, 'qn%d' % i_, [128, 384], BF16) for i_ in range(2)]
                qnT_2 = [sb(es, 'qnT%d' % i_, [128, 3, 128], BF16) for i_ in range(2)]
                qf_2 = [sb(es, 'qf%d' % i_, [128, 384], F32) for i_ in range(2)]
                kvf_2 = [sb(es, 'kvf%d' % i_, [128, 512], F32) for i_ in range(2)]
                Qb_2 = [sb(es, 'Qb%d' % i_, [128, 4, 96], BF16) for i_ in range(2)]
                Kb_2 = [sb(es, 'Kb%d' % i_, [128, 4, 96], BF16) for i_ in range(2)]
                krr_2 = [sb(es, 'krr%d' % i_, [128, 32], F32) for i_ in range(2)]
                dqk_2 = [sb(es, 'dqk%d' % i_, [128, 512], BF16) for i_ in range(2)]
                rt = [sb(es, 'rt%d' % i, [128, 256], F32) for i in range(4)]
                ptoggle = [0]

                def rope(src_ap, dst_ap, G, ti, reads, writes):
                    sv = src_ap.rearrange("p (g h two f) -> p g h two f", g=G, h=2, two=2, f=8)
                    dv = dst_ap.rearrange("p (g h two f) -> p g h two f", g=G, h=2, two=2, f=8)
                    cb = rc[:, ti, :].rearrange("p (h f) -> p h f", h=2).unsqueeze(1).to_broadcast([128, G, 2, 8])
                    sn = rs[:, ti, :].rearrange("p (h f) -> p h f", h=2).unsqueeze(1).to_broadcast([128, G, 2, 8])
                    tv = [r_[:, 0:G * 16].rearrange("p (g h f) -> p g h f", g=G, h=2, f=8) for r_ in rt]
                    z1 = sv[:, :, :, 0, :]; z2 = sv[:, :, :, 1, :]
                    kb.op('dve', lambda e: e.tensor_tensor(out=tv[0], in0=z1, in1=cb, op=ALU.mult), reads=reads + [rc], writes=[rt[0]])
                    kb.op('dve', lambda e: e.tensor_tensor(out=tv[1], in0=z2, in1=sn, op=ALU.mult), reads=reads + [rs], writes=[rt[1]])
                    kb.op('dve', lambda e: e.tensor_tensor(out=tv[2], in0=z1, in1=sn, op=ALU.mult), reads=reads + [rs], writes=[rt[2]])
                    kb.op('dve', lambda e: e.tensor_tensor(out=tv[3], in0=z2, in1=cb, op=ALU.mult), reads=reads + [rc], writes=[rt[3]])
                    kb.op('dve', lambda e: e.tensor_tensor(out=dv[:, :, :, 0, :], in0=tv[0], in1=tv[1], op=ALU.subtract), reads=[rt[0], rt[1]], writes=writes)
                    kb.op('dve', lambda e: e.tensor_tensor(out=dv[:, :, :, 1, :], in0=tv[2], in1=tv[3], op=ALU.add), reads=[rt[2], rt[3]], writes=writes)

                def transposes(items, fmt, j):
                    for b0 in range(0, len(items), 8):
                        batch = items[b0:b0 + 8]
                        pt = ptr[ptoggle[0] % 2]; ptoggle[0] += 1
                        for i, (st, sap, n, ch) in enumerate(batch):
                            kb.op('pe', lambda e, i=i, sap=sap, n=n, pt=pt: e.transpose(out=pt[0:n, i * 128:(i + 1) * 128], in_=sap, identity=ident[:]),
                                  reads=[st, ident], writes=[pt])
                        for i, (st, sap, n, ch) in enumerate(batch):
                            eng = 'act' if (i % 2 == 0) else 'pool_'
                            if eng == 'act':
                                kb.op('act', lambda e, i=i, n=n, ch=ch, pt=pt: e.copy(out=fmt[0:n, ch, j * 128:(j + 1) * 128], in_=pt[0:n, i * 128:(i + 1) * 128]), reads=[pt], writes=[fmt])
                            else:
                                kb.op('dve', lambda e, i=i, n=n, ch=ch, pt=pt: e.tensor_copy(out=fmt[0:n, ch, j * 128:(j + 1) * 128], in_=pt[0:n, i * 128:(i + 1) * 128]), reads=[pt], writes=[fmt])

                def loadxA(tj):
                    xd = xt[tj % 2]
                    if layer == 0:
                        src = I['x'][tj * 128:(tj + 1) * 128, :] if tj < NLT else I['ctx'][(tj - NLT) * 128:(tj - NLT + 1) * 128, :]
                        kb.dma('sp', xd[:], src, writes=[xd])
                    else:
                        kb.dma('sp', xd[:], S['xcur'][tj], reads=[D_['xcur']], writes=[xd])
                for ti in range(NT):
                    kind = 0 if ti < NLT else 1
                    g, j = ti // 2, ti % 2
                    fmt = fm[g % 2]; vt = vv[ti % 2]; x_t = xt[ti % 2]; hT_t = hTt[ti % 2]
                    htmp = htmp_2[ti % 2]; hb = hb_2[ti % 2]; z = z_2[ti % 2]; zb = zb_2[ti % 2]; ss = ss_2[ti % 2]; junk = junk_2[ti % 2]; qn = qn_2[ti % 2]; qnT = qnT_2[ti % 2]; qf = qf_2[ti % 2]; kvf = kvf_2[ti % 2]; Qb = Qb_2[ti % 2]; Kb = Kb_2[ti % 2]; krr = krr_2[ti % 2]; dqk = dqk_2[ti % 2]
                    if ti == 0:
                        loadxA(0)
                    if ti + 1 < NT:
                        loadxA(ti + 1)
                    kb.op('dve', lambda e: e.tensor_tensor(out=htmp[:], in0=x_t[:], in1=modb[:, kind, 1, :], op=ALU.mult), reads=[x_t, modb], writes=[htmp])
                    kb.op('dve', lambda e: e.tensor_tensor(out=hb[:], in0=htmp[:], in1=modb[:, kind, 0, :], op=ALU.add), reads=[htmp, modb], writes=[hb])
                    pt = ptr[ptoggle[0] % 2]; ptoggle[0] += 1
                    for kc in range(8):
                        kb.op('pe', lambda e, kc=kc, pt=pt: e.transpose(out=pt[:, kc * 128:(kc + 1) * 128], in_=hb[:, kc * 128:(kc + 1) * 128], identity=ident[:]), reads=[hb, ident], writes=[pt])
                    kb.op('act', lambda e, pt=pt: e.copy(out=hT_t[:].rearrange("p k t -> p (k t)"), in_=pt[:]), reads=[pt], writes=[hT_t])
                    kb.dma('sp', S['hT'][ti], hT_t[:], reads=[hT_t], writes=[D_['hT']])
                    for cg in range(5):
                        c0 = cg * 512; n = min(512, IN_COLS - c0)
                        pb = pbank[cg % 4]
                        for kc in range(8):
                            kb.op('pe', lambda e, kc=kc, pb=pb, c0=c0, n=n: e.matmul(pb[:, 0:n], lhsT=hT_t[:, kc, :], rhs=win[:, kc, c0:c0 + n], start=(kc == 0), stop=(kc == 7)),
                                  reads=[hT_t, win], writes=[pb])
                        kb.op('act', lambda e, pb=pb, c0=c0, n=n: e.copy(out=z[:, c0:c0 + n], in_=pb[:, 0:n]), reads=[pb], writes=[z])
                    kb.op('pool', lambda e: e.tensor_copy(out=zb[:], in_=z[:]), reads=[z], writes=[zb])
                    kb.op('pool', lambda e: e.tensor_copy(out=vt[:, 0:4, 0:64], in_=z[:, 512:768].rearrange("p (h d) -> p h d", h=4)), reads=[z], writes=[vt])
                    kb.op('pool', lambda e: e.tensor_copy(out=vt[:, 8:12, 0:64], in_=z[:, 1952:2208].rearrange("p (h d) -> p h d", h=4)), reads=[z], writes=[vt])
                    kb.op('act', lambda e: e.activation(out=junk[:, 0:256], in_=z[:, 768:1024], func=AF.Square, accum_out=ss[:, 0:1]), reads=[z], writes=[junk, ss])
                    kb.op('act', lambda e: e.activation(out=junk[:, 0:128], in_=z[:, 1024:1152], func=AF.Square, accum_out=ss[:, 1:2]), reads=[z], writes=[junk, ss])
                    kb.op('act', lambda e: e.activation(out=ss[:, 2:3], in_=ss[:, 0:1], func=AF.Sqrt, scale=1.0 / 256, bias=epsq[:, 0:1]), reads=[ss, epsq], writes=[ss])
                    kb.op('act', lambda e: e.activation(out=ss[:, 3:4], in_=ss[:, 1:2], func=AF.Sqrt, scale=1.0 / 128, bias=epsq[:, 0:1]), reads=[ss, epsq], writes=[ss])
                    kb.op('dve', lambda e: e.reciprocal(out=ss[:, 2:4], in_=ss[:, 2:4]), reads=[ss], writes=[ss])
                    kb.op('dve', lambda e: e.scalar_tensor_tensor(out=qn[:, 0:256], in0=z[:, 768:1024], scalar=ss[:, 2:3], in1=qg[:], op0=ALU.mult, op1=ALU.mult), reads=[z, ss, qg], writes=[qn])
                    kb.op('dve', lambda e: e.scalar_tensor_tensor(out=qn[:, 256:384], in0=z[:, 1024:1152], scalar=ss[:, 3:4], in1=kvg[:], op0=ALU.mult, op1=ALU.mult), reads=[z, ss, kvg], writes=[qn])
                    pt = ptr[ptoggle[0] % 2]; ptoggle[0] += 1
                    for c3 in range(3):
                        kb.op('pe', lambda e, c3=c3, pt=pt: e.transpose(out=pt[:, c3 * 128:(c3 + 1) * 128], in_=qn[:, c3 * 128:(c3 + 1) * 128], identity=ident[:]), reads=[qn, ident], writes=[pt])
                    kb.op('act', lambda e, pt=pt: e.copy(out=qnT[:].rearrange("p k t -> p (k t)"), in_=pt[:, 0:384]), reads=[pt], writes=[qnT])
                    pq = pbank[4]; pk = pbank[5]
                    for c2 in range(2):
                        kb.op('pe', lambda e, c2=c2: e.matmul(pq[:, 0:384], lhsT=qnT[:, c2, :], rhs=wuq[:, c2, :], start=(c2 == 0), stop=(c2 == 1)), reads=[qnT, wuq], writes=[pq])
                    kb.op('pe', lambda e: e.matmul(pk[:, :], lhsT=qnT[:, 2, :], rhs=wukv[:], start=True, stop=True), reads=[qnT, wukv], writes=[pk])
                    kb.op('act', lambda e: e.copy(out=qf[:], in_=pq[:, 0:384]), reads=[pq], writes=[qf])
                    kb.op('act', lambda e: e.copy(out=kvf[:], in_=pk[:]), reads=[pk], writes=[kvf])
                    qf3 = qf[:].rearrange("p (h d) -> p h d", h=4); kv3 = kvf[:].rearrange("p (h d) -> p h d", h=4)
                    kb.op('pool', lambda e: e.tensor_copy(out=Qb[:, :, 0:64], in_=qf3[:, :, 0:64]), reads=[qf], writes=[Qb])
                    kb.op('pool', lambda e: e.tensor_copy(out=Kb[:, :, 0:64], in_=kv3[:, :, 0:64]), reads=[kvf], writes=[Kb])
                    kb.op('pool', lambda e: e.tensor_copy(out=vt[:, 4:8, 0:64], in_=kv3[:, :, 64:128]), reads=[kvf], writes=[vt])
                    for h in range(4):
                        rope(qf[:, h * 96 + 64:h * 96 + 96], Qb[:, h, 64:96], 1, ti, [qf], [Qb])
                    rope(z[:, 1152:1184], krr[:, :], 1, ti, [z], [krr])
                    kb.op('pool', lambda e: e.tensor_copy(out=Kb[:, :, 64:96], in_=krr[:].unsqueeze(1).to_broadcast([128, 4, 32])), reads=[krr], writes=[Kb])
                    rope(z[:, 1440:1696], dqk[:, 0:256], 8, ti, [z], [dqk])
                    rope(z[:, 1696:1952], dqk[:, 256:512], 8, ti, [z], [dqk])
                    items = []
                    for c2 in range(2):
                        items.append((zb, zb[:, c2 * 128:(c2 + 1) * 128], 128, C_NAQ + c2))
                        items.append((zb, zb[:, 256 + c2 * 128:256 + (c2 + 1) * 128], 128, C_NAK + c2))
                        items.append((zb, zb[:, 1184 + c2 * 128:1184 + (c2 + 1) * 128], 128, C_S5U + c2))
                    for h in range(4):
                        items.append((Qb, Qb[:, h, :], 96, C_MQ + h))
                        items.append((Kb, Kb[:, h, :], 96, C_MK + h))
                    for c2 in range(2):
                        items.append((dqk, dqk[:, c2 * 128:(c2 + 1) * 128], 128, C_DQ + c2))
                        items.append((dqk, dqk[:, 256 + c2 * 128:256 + (c2 + 1) * 128], 128, C_DK + c2))
                    transposes(items, fmt, j)
                    kb.dma('sp', S['VV'][ti], vt[:], reads=[vt], writes=[D_['VV']])
                    if j == 1:
                        kb.dma('sp', S['FM'][:, :, g * 256:(g + 1) * 256].rearrange("c p t -> p c t"), fmt[:], reads=[fmt], writes=[D_['FM']])
                if debug == 'A':
                    kb.barrier()
                    for nm, shp, dt in (('FM', [NFM, 128, T], BF16), ('VV', [NT, 128, 12, 128], BF16), ('hT', [NT, 128, 8, 128], BF16)):
                        dbg_out[nm] = nc.dram_tensor('dbg_' + nm, shp, dt, kind="ExternalOutput").ap()
                        kb.dma('sp', dbg_out[nm], S[nm], reads=[D_[nm]])
                kb.barrier()
            if debug == 'A':
                break
            with contextlib.ExitStack() as es:
                KT = sb(es, 'KT', [128, T], BF16); QT = sb(es, 'QT', [128, T], BF16)
                V = sb(es, 'Vt', [128, NT, 128], BF16)
                rd = sb(es, 'rd', [128, 512], F32)
                onb = sb(es, 'onb', [128, 512], BF16)
                o1 = sb(es, 'o1', [128, 512], F32); o2 = sb(es, 'o2', [128, 512], F32); osq = sb(es, 'osq', [128, 512], BF16)
                rs2 = sb(es, 'rs2', [128, 512], F32)
                Wt3 = [sb(es, 'Wt3_%d' % i, [128, 4, 8, 512], BF16) for i in range(3)]
                QM = [sb(es, 'QM%d' % i, [128, T], BF16) for i in range(4)]
                m32 = sb(es, 'm32', [128, 6], F32)
                kb.dma('sp', m32[:], I['m32'][:], writes=[m32])
                Grev = sb(es, 'Grev', [128, 4, 15, 64], BF16)
                lamv = sb(es, 'lamv', [128, 4, 32], F32); lamt = sb(es, 'lamt', [128, 8], F32)
                gsub = sb(es, 'gsub', [128, 1], F32); epsd = sb(es, 'epsd', [128, 1], F32)
                ecnt = [0]
                for i4, nm in enumerate(('diff_lam_q1', 'diff_lam_k1', 'diff_lam_q2', 'diff_lam_k2')):
                    kb.dma('sp', lamv[:, i4, :], I[nm][layer:layer + 1, :].to_broadcast([128, 32]), writes=[lamv])
                kb.op('dve', lambda e: e.tensor_tensor(out=lamv[:, 0, :], in0=lamv[:, 0, :], in1=lamv[:, 1, :], op=ALU.mult), reads=[lamv], writes=[lamv])
                kb.op('dve', lambda e: e.tensor_tensor(out=lamv[:, 2, :], in0=lamv[:, 2, :], in1=lamv[:, 3, :], op=ALU.mult), reads=[lamv], writes=[lamv])
                kb.op('dve', lambda e: e.reduce_sum(out=lamt[:, 0:1], in_=lamv[:, 0, :], axis=AX.X), reads=[lamv], writes=[lamt])
                kb.op('dve', lambda e: e.reduce_sum(out=lamt[:, 1:2], in_=lamv[:, 2, :], axis=AX.X), reads=[lamv], writes=[lamt])
                kb.op('act', lambda e: e.activation(out=lamt[:, 2:4], in_=lamt[:, 0:2], func=AF.Exp), reads=[lamt], writes=[lamt])
                kb.op('dve', lambda e: e.tensor_tensor(out=lamt[:, 4:5], in0=lamt[:, 3:4], in1=lamt[:, 2:3], op=ALU.subtract), reads=[lamt], writes=[lamt])
                kb.op('dve', lambda e: e.tensor_scalar_add(out=lamt[:, 5:6], in0=lamt[:, 4:5], scalar1=-lam_init), reads=[lamt], writes=[lamt])
                with nc.allow_non_contiguous_dma(reason="tiny"):
                    kb.dma('sp', gsub[0:64, :], I['diff_subln'][layer].rearrange("(p o) -> p o", o=1), writes=[gsub])
                kb.op('dve', lambda e: e.tensor_scalar_mul(out=gsub[0:64, :], in0=gsub[0:64, :], scalar1=(1.0 - lam_init)), reads=[gsub], writes=[gsub])
                kb.op('dve', lambda e: e.memset(epsd[:], RMS_EPS), writes=[epsd])
                with contextlib.ExitStack() as es2:
                    graw = sb(es2, 'graw', [128, 4, 15, 64], F32)
                    for half in range(2):
                        kb.dma('sp', graw[half * 64:(half + 1) * 64], I['rpbT'][layer].rearrange("h r k q -> k h r q"), writes=[graw])
                    for m in range(15):
                        kb.op('act', lambda e, m=m: e.activation(out=Grev[:, :, m, :], in_=graw[:, :, 14 - m, :], func=AF.Exp), reads=[graw], writes=[Grev])
                    kb.barrier()

                def build_W(jq, Wt):
                    R0 = 8 * jq; KR0 = min(max(R0 - 4, 0), 48)
                    kb.op('pool', lambda e: e.memset(Wt[:], 0.0), writes=[Wt])
                    for i in range(8):
                        for half in range(2):
                            kr = KR0 + 2 * i + half
                            al = []
                            for a in range(8):
                                r0 = min(max(R0 + a - 4, 0), 56)
                                if r0 <= kr <= r0 + 7:
                                    al.append(a)
                            if not al:
                                continue
                            a0, a1 = al[0], al[-1] + 1
                            m0 = (R0 + a0) - kr + 7
                            kb.op('pool', lambda e, i=i, half=half, a0=a0, a1=a1, m0=m0: e.tensor_copy(
                                out=Wt[half * 64:(half + 1) * 64, :, i, a0 * 64:a1 * 64].rearrange("p h (a q) -> p h a q", q=64),
                                in_=Grev[half * 64:(half + 1) * 64, :, m0:m0 + (a1 - a0), :]), reads=[Grev], writes=[Wt])
                for ci_, jq_ in enumerate((0, 1, 7)):
                    build_W(jq_, Wt3[ci_])

                E2 = [sb(es, 'E2_%d' % i, [128, 1024], BF16) for i in range(3)]

                def attn_block(kts, kr0, kd, q0, nq, scale, pso, wfn=None, Qs=None, Wsrc=None):
                    Qs = QT if Qs is None else Qs
                    pairs = [kts[i:i + 2] for i in range(0, len(kts), 2)]
                    npair = len(pairs)
                    base = ecnt[0]; ecnt[0] += npair

                    def smm(pi):
                        sc = pbig[(base + pi) % 2]
                        for j, kt in enumerate(pairs[pi]):
                            kb.op('pe', lambda e, j=j, kt=kt: e.matmul(sc[:, j * 512:j * 512 + nq], lhsT=KT[kr0:kr0 + kd, kt * 128:(kt + 1) * 128], rhs=Qs[kr0:kr0 + kd, q0:q0 + nq], start=True, stop=True),
                                  reads=[KT, Qs], writes=[sc])
                    smm(0)
                    for pi, pr in enumerate(pairs):
                        sc = pbig[(base + pi) % 2]; Et = E2[(base + pi) % 3]
                        w = len(pr)
                        if nq == 512:
                            kb.op('act', lambda e, sc=sc, Et=Et, w=w: e.activation(out=Et[:, 0:w * 512], in_=sc[:, 0:w * 512], func=AF.Exp, scale=scale), reads=[sc], writes=[Et])
                        else:
                            kb.op('act', lambda e, sc=sc, Et=Et, w=w: e.activation(out=Et[:, :].rearrange("p (j n) -> p j n", n=512)[:, 0:w, 0:nq],
                                  in_=sc[:, :].rearrange("p (j n) -> p j n", n=512)[:, 0:w, 0:nq], func=AF.Exp, scale=scale), reads=[sc], writes=[Et])
                        if pi + 1 < npair:
                            smm(pi + 1)
                        for j, kt in enumerate(pr):
                            idx = pi * 2 + j
                            wm = wfn(idx) if wfn is not None else None
                            if wm is not None:
                                kb.op('dve', lambda e, Et=Et, wm=wm, j=j: e.tensor_tensor(out=Et[:, j * 512:j * 512 + nq], in0=Et[:, j * 512:j * 512 + nq], in1=wm, op=ALU.mult), reads=[Et, Wsrc], writes=[Et])
                        for j, kt in enumerate(pr):
                            idx = pi * 2 + j
                            kb.op('pe', lambda e, kt=kt, Et=Et, idx=idx, j=j: e.matmul(pso[:, 0:nq], lhsT=V[:, kt, :], rhs=Et[:, j * 512:j * 512 + nq], start=(idx == 0), stop=(idx == len(kts) - 1)),
                                  reads=[V, Et], writes=[pso])

                def norm_store(pso, nq, dst_tile, dst_ap, dt_out_tile=None):
                    kb.op('dve', lambda e: e.reciprocal(out=rd[64:128, 0:nq], in_=pso[64:128, 0:nq]), reads=[pso], writes=[rd])
                    kb.op('dve', lambda e: e.tensor_tensor(out=dst_ap, in0=pso[0:64, 0:nq], in1=rd[64:128, 0:nq], op=ALU.mult), reads=[pso, rd], writes=[dst_tile])

                qchunks = [(j * 512, 512, True) for j in range(8)] + ([(SEQ, 256, False)] if ctx_out else [])
                all_kt = list(range(NT)); ctx_kt = [32, 33]
                pcnt = [0]
                kt0_holder = [0]
                for h in range(4):
                    kb.dma('sp', KT[:], S['FM'][C_MK + h], reads=[D_['FM']], writes=[KT])
                    kb.dma('sp', QT[:], S['FM'][C_MQ + h], reads=[D_['FM']], writes=[QT])
                    kb.dma('sp', V[:], S['VV'][:, :, 4 + h, :].rearrange("t p c -> p t c"), reads=[D_['VV']], writes=[V])
                    for (q0, nq, lat) in qchunks:
                        pso = pbank[2 + pcnt[0] % 2]; pcnt[0] += 1
                        attn_block(all_kt if lat else ctx_kt, 0, 96, q0, nq, MLA_SCALE, pso)
                        norm_store(pso, nq, onb, onb[0:64, 0:nq])
                        kb.dma('sp', S['oT'][1, h // 2, (h % 2) * 64:(h % 2) * 64 + 64, q0:q0 + nq], onb[0:64, 0:nq], reads=[onb], writes=[D_['oT']])
                for c2 in range(2):
                    kb.dma('sp', KT[:], S['FM'][C_DK + c2], reads=[D_['FM']], writes=[KT])
                    kb.dma('sp', QT[:], S['FM'][C_DQ + c2], reads=[D_['FM']], writes=[QT])
                    for i4 in range(4):
                        kb.op('dve', lambda e, i4=i4: e.tensor_scalar_mul(out=QM[i4][:], in0=QT[:], scalar1=m32[:, i4:i4 + 1]), reads=[QT, m32], writes=[QM[i4]])
                    for hh in range(2):
                        h = 2 * c2 + hh
                        kb.dma('sp', V[:], S['VV'][:, :, 8 + h, :].rearrange("t p c -> p t c"), reads=[D_['VV']], writes=[V])
                        for (q0, nq, lat) in qchunks:
                            kts = all_kt if lat else ctx_kt
                            attn_block(kts, 0, 128, q0, nq, DIFF_SCALE, pbank[2], Qs=QM[2 * hh])
                            attn_block(kts, 0, 128, q0, nq, DIFF_SCALE, pbank[3], Qs=QM[2 * hh + 1])
                            norm_store(pbank[2], nq, o1, o1[0:64, 0:nq])
                            norm_store(pbank[3], nq, o2, o2[0:64, 0:nq])
                            kb.op('dve', lambda e: e.scalar_tensor_tensor(out=o1[0:64, 0:nq], in0=o2[0:64, 0:nq], scalar=lamt[0:64, 5:6], in1=o1[0:64, 0:nq], op0=ALU.mult, op1=ALU.add),
                                  reads=[o1, o2, lamt], writes=[o1])
                            kb.op('dve', lambda e: e.tensor_tensor(out=osq[0:64, 0:nq], in0=o1[0:64, 0:nq], in1=o1[0:64, 0:nq], op=ALU.mult), reads=[o1], writes=[osq])
                            kb.op('pe', lambda e: e.matmul(pbank[0][0:64, 0:nq], lhsT=ones_b[0:64, 0:64], rhs=osq[0:64, 0:nq], start=True, stop=True), reads=[ones_b, osq], writes=[pbank[0]])
                            kb.op('act', lambda e: e.activation(out=rs2[0:64, 0:nq], in_=pbank[0][0:64, 0:nq], func=AF.Sqrt, scale=1.0 / 64, bias=epsd[0:64, 0:1]), reads=[pbank[0], epsd], writes=[rs2])
                            kb.op('dve', lambda e: e.reciprocal(out=rs2[0:64, 0:nq], in_=rs2[0:64, 0:nq]), reads=[rs2], writes=[rs2])
                            kb.op('dve', lambda e: e.scalar_tensor_tensor(out=onb[0:64, 0:nq], in0=o1[0:64, 0:nq], scalar=gsub[0:64, 0:1], in1=rs2[0:64, 0:nq], op0=ALU.mult, op1=ALU.mult),
                                  reads=[o1, gsub, rs2], writes=[onb])
                            kb.dma('sp', S['oT'][3, h // 2, (h % 2) * 64:(h % 2) * 64 + 64, q0:q0 + nq], onb[0:64, 0:nq], reads=[onb], writes=[D_['oT']])
                for c2 in range(2):
                    kb.dma('sp', KT[:], S['FM'][C_NAK + c2], reads=[D_['FM']], writes=[KT])
                    kb.dma('sp', QT[:], S['FM'][C_NAQ + c2], reads=[D_['FM']], writes=[QT])
                    for hh in range(2):
                        kb.op('dve', lambda e, hh=hh: e.tensor_scalar_mul(out=QM[hh][:], in0=QT[:], scalar1=m32[:, 4 + hh:5 + hh]), reads=[QT, m32], writes=[QM[hh]])
                    for hh in range(2):
                        h = 2 * c2 + hh
                        kb.dma('sp', V[:], S['VV'][:, :, h, :].rearrange("t p c -> p t c"), reads=[D_['VV']], writes=[V])
                        for (q0, nq, lat) in qchunks:
                            pso = pbank[2 + pcnt[0] % 2]; pcnt[0] += 1
                            if lat:
                                jq = q0 // 512
                                Wc = Wt3[0] if jq == 0 else (Wt3[2] if jq == 7 else Wt3[1])
                                kt0 = min(max(8 * jq - 4, 0), 48) // 2
                                kts = list(range(kt0, kt0 + 8)) + ctx_kt
                                wfn = (lambda idx, h=h, Wc=Wc: Wc[:, h, idx, :] if idx < 8 else None)
                                attn_block(kts, 0, 128, q0, nq, NA_SCALE, pso, wfn, Qs=QM[hh], Wsrc=Wc)
                            else:
                                attn_block(ctx_kt, 0, 128, q0, nq, NA_SCALE, pso, Qs=QM[hh])
                            norm_store(pso, nq, onb, onb[0:64, 0:nq])
                            kb.dma('sp', S['oT'][0, h // 2, (h % 2) * 64:(h % 2) * 64 + 64, q0:q0 + nq], onb[0:64, 0:nq], reads=[onb], writes=[D_['oT']])
                if debug == 'B':
                    kb.barrier()
                    dbg_out['oT'] = nc.dram_tensor('dbg_oT', [4, 2, 128, T], BF16, kind="ExternalOutput").ap()
                    kb.dma('sp', dbg_out['oT'], S['oT'], reads=[D_['oT']])
                kb.barrier()
            if debug == 'B':
                break
            TWO_PI = 2.0 * math.pi
            with contextlib.ExitStack() as es:
                uTn = sb(es, 'uTn', [128, T], BF16)
                iot = sb(es, 'iot', [128, T], F32)
                kb.dma('sp', iot[:, :], I['iotaT'][0:1, :].to_broadcast([128, T]), writes=[iot])
                P = sb(es, 's5p', [128, 24, 16], F32)
                LRE, LIM, DT, LR, TH, RR, M1, SN, CS, ARE, AIM, NRE, NIM, DEN, CRE, CIM, TMP = range(17)
                with nc.allow_non_contiguous_dma(reason="small s5 params"):
                    kb.dma('sp', P[:, LRE, :].rearrange("p (d k) -> p d k", d=2), I['s5_lam_re'][layer].rearrange("d (k two) p -> (two p) d k", two=2), writes=[P])
                    kb.dma('sp', P[:, LIM, :].rearrange("p (d k) -> p d k", d=2), I['s5_lam_im'][layer].rearrange("d (k two) p -> (two p) d k", two=2), writes=[P])
                    for two in range(2):
                        kb.dma('sp', P[two * 64:(two + 1) * 64, DT, :].rearrange("p (d k) -> p d k", d=2),
                               I['s5_log_dt'][layer].rearrange("d (k two) -> two d k", two=2)[two:two + 1].to_broadcast([64, 2, 8]), writes=[P])
                def sm(fn, *a, **k):
                    kb.op('dve', fn, reads=[P], writes=[P])
                kb.op('act', lambda e: e.activation(out=P[:, DT, :], in_=P[:, DT, :], func=AF.Exp), reads=[P], writes=[P])
                sm(lambda e: e.tensor_tensor(out=P[:, LR, :], in0=P[:, LRE, :], in1=P[:, DT, :], op=ALU.mult))
                sm(lambda e: e.tensor_tensor(out=P[:, TH, :], in0=P[:, LIM, :], in1=P[:, DT, :], op=ALU.mult))
                kb.op('act', lambda e: e.activation(out=P[:, RR, :], in_=P[:, LR, :], func=AF.Exp), reads=[P], writes=[P])
                sm(lambda e: e.tensor_scalar(out=P[:, TMP, :], in0=P[:, TH, :], scalar1=1.0 / TWO_PI, scalar2=12582912.0, op0=ALU.mult, op1=ALU.add))
                sm(lambda e: e.tensor_scalar_add(out=P[:, TMP, :], in0=P[:, TMP, :], scalar1=-12582912.0))
                sm(lambda e: e.scalar_tensor_tensor(out=P[:, M1, :], in0=P[:, TMP, :], scalar=-TWO_PI, in1=P[:, TH, :], op0=ALU.mult, op1=ALU.add))
                sm(lambda e: e.tensor_scalar(out=P[:, M1, :], in0=P[:, M1, :], scalar1=-3.14159, scalar2=3.14159, op0=ALU.max, op1=ALU.min))
                kb.op('act', lambda e: e.activation(out=P[:, SN, :], in_=P[:, M1, :], func=AF.Sin), reads=[P], writes=[P])
                sm(lambda e: e.tensor_scalar_add(out=P[:, CS, :], in0=P[:, TH, :], scalar1=0.5 * math.pi))
                sm(lambda e: e.tensor_scalar(out=P[:, TMP, :], in0=P[:, CS, :], scalar1=1.0 / TWO_PI, scalar2=12582912.0, op0=ALU.mult, op1=ALU.add))
                sm(lambda e: e.tensor_scalar_add(out=P[:, TMP, :], in0=P[:, TMP, :], scalar1=-12582912.0))
                sm(lambda e: e.scalar_tensor_tensor(out=P[:, M1, :], in0=P[:, TMP, :], scalar=-TWO_PI, in1=P[:, CS, :], op0=ALU.mult, op1=ALU.add))
                sm(lambda e: e.tensor_scalar(out=P[:, M1, :], in0=P[:, M1, :], scalar1=-3.14159, scalar2=3.14159, op0=ALU.max, op1=ALU.min))
                kb.op('act', lambda e: e.activation(out=P[:, CS, :], in_=P[:, M1, :], func=AF.Sin), reads=[P], writes=[P])
                sm(lambda e: e.tensor_tensor(out=P[:, ARE, :], in0=P[:, RR, :], in1=P[:, CS, :], op=ALU.mult))
                sm(lambda e: e.tensor_tensor(out=P[:, AIM, :], in0=P[:, RR, :], in1=P[:, SN, :], op=ALU.mult))
                sm(lambda e: e.tensor_scalar_add(out=P[:, ARE, :], in0=P[:, ARE, :], scalar1=-1.0))
                sm(lambda e: e.tensor_tensor(out=P[:, NRE, :], in0=P[:, ARE, :], in1=P[:, LRE, :], op=ALU.mult))
                sm(lambda e: e.tensor_tensor(out=P[:, TMP, :], in0=P[:, AIM, :], in1=P[:, LIM, :], op=ALU.mult))
                sm(lambda e: e.tensor_tensor(out=P[:, NRE, :], in0=P[:, NRE, :], in1=P[:, TMP, :], op=ALU.add))
                sm(lambda e: e.tensor_tensor(out=P[:, NIM, :], in0=P[:, AIM, :], in1=P[:, LRE, :], op=ALU.mult))
                sm(lambda e: e.tensor_tensor(out=P[:, TMP, :], in0=P[:, ARE, :], in1=P[:, LIM, :], op=ALU.mult))
                sm(lambda e: e.tensor_tensor(out=P[:, NIM, :], in0=P[:, NIM, :], in1=P[:, TMP, :], op=ALU.subtract))
                sm(lambda e: e.tensor_tensor(out=P[:, DEN, :], in0=P[:, LRE, :], in1=P[:, LRE, :], op=ALU.mult))
                sm(lambda e: e.tensor_tensor(out=P[:, TMP, :], in0=P[:, LIM, :], in1=P[:, LIM, :], op=ALU.mult))
                sm(lambda e: e.tensor_tensor(out=P[:, DEN, :], in0=P[:, DEN, :], in1=P[:, TMP, :], op=ALU.add))
                sm(lambda e: e.reciprocal(out=P[:, DEN, :], in_=P[:, DEN, :]))
                sm(lambda e: e.tensor_tensor(out=P[:, CRE, :], in0=P[:, NRE, :], in1=P[:, DEN, :], op=ALU.mult))
                sm(lambda e: e.tensor_tensor(out=P[:, CIM, :], in0=P[:, NIM, :], in1=P[:, DEN, :], op=ALU.mult))
                BbT = sb(es, 'BbT', [128, 16, 2, 128], BF16); CT = sb(es, 'CT', [128, 16, 2, 128], BF16)
                with contextlib.ExitStack() as es2:
                    braw = sb(es2, 'braw', [128, 2, 16, 16], F32)
                    bbar = sb(es2, 'bbar', [128, 2, 16, 16], F32)
                    btmp = sb(es2, 'btmp', [128, 16, 16], F32)
                    craw = sb(es2, 'craw', [128, 2, 4, 64], F32)
                    mB = sb(es2, 'mB', [128, 4, 128], F32); mC = sb(es2, 'mC', [128, 4, 128], F32)
                    kb.dma('sp', mB[:], I['maskB'].rearrange("b p q -> p b q"), writes=[mB]); kb.dma('sp', mC[:], I['maskC'].rearrange("b p q -> p b q"), writes=[mC])
                    with nc.allow_non_contiguous_dma(reason="s5 small"):
                        for ri, nm in enumerate(('s5_b_re', 's5_b_im')):
                            for dd in range(2):
                                kb.dma('sp', braw[:, ri, dd * 8:(dd + 1) * 8, :], I[nm][layer, dd].rearrange("(k two) p c -> (two p) k c", two=2), writes=[braw])
                        for ri, nm in enumerate(('s5_c_re', 's5_c_im')):
                            for dd in range(2):
                                kb.dma('sp', craw[:, ri, dd * 2:(dd + 1) * 2, :], I[nm][layer, dd].rearrange("(gh gl) c p -> (gl c) gh p", gl=8), writes=[craw])
                    cre_b = P[:, CRE, :].unsqueeze(2).to_broadcast([128, 16, 16]); cim_b = P[:, CIM, :].unsqueeze(2).to_broadcast([128, 16, 16])
                    kb.op('dve', lambda e: e.tensor_tensor(out=bbar[:, 0], in0=braw[:, 0], in1=cre_b, op=ALU.mult), reads=[braw, P], writes=[bbar])
                    kb.op('dve', lambda e: e.tensor_tensor(out=btmp[:], in0=braw[:, 1], in1=cim_b, op=ALU.mult), reads=[braw, P], writes=[btmp])
                    kb.op('dve', lambda e: e.tensor_tensor(out=bbar[:, 0], in0=bbar[:, 0], in1=btmp[:], op=ALU.subtract), reads=[bbar, btmp], writes=[bbar])
                    kb.op('dve', lambda e: e.tensor_tensor(out=bbar[:, 1], in0=braw[:, 1], in1=cre_b, op=ALU.mult), reads=[braw, P], writes=[bbar])
                    kb.op('dve', lambda e: e.tensor_tensor(out=btmp[:], in0=braw[:, 0], in1=cim_b, op=ALU.mult), reads=[braw, P], writes=[btmp])
                    kb.op('dve', lambda e: e.tensor_tensor(out=bbar[:, 1], in0=bbar[:, 1], in1=btmp[:], op=ALU.add), reads=[bbar, btmp], writes=[bbar])
                    pad = [sb(es2, 'pad%d' % i, [128, 128], BF16) for i in range(2)]
                    pc = [0]
                    for tile in range(16):
                        dd, k = tile // 8, tile % 8
                        for ri in range(2):
                            pd = pad[pc[0] % 2]; pt = ptr[pc[0] % 2]; pc[0] += 1
                            kb.op('dve', lambda e, pd=pd, tile=tile, ri=ri, k=k: e.tensor_tensor(out=pd[:].rearrange("p (a c) -> p a c", c=16), in0=bbar[:, ri, tile, :].unsqueeze(1).to_broadcast([128, 8, 16]),
                                  in1=mB[:, k % 4, :].rearrange("p (a c) -> p a c", c=16), op=ALU.mult), reads=[bbar, mB], writes=[pd])
                            kb.op('pe', lambda e, pd=pd, pt=pt: e.transpose(out=pt[:, 0:128], in_=pd[:], identity=ident[:]), reads=[pd, ident], writes=[pt])
                            kb.op('act', lambda e, pt=pt, tile=tile, ri=ri: e.copy(out=BbT[:, tile, ri, :], in_=pt[:, 0:128]), reads=[pt], writes=[BbT])
                            pd = pad[pc[0] % 2]; pt = ptr[pc[0] % 2]; pc[0] += 1
                            kb.op('dve', lambda e, pd=pd, dd=dd, ri=ri, k=k: e.scalar_tensor_tensor(out=pd[:].rearrange("p (a c) -> p a c", c=64), in0=craw[:, ri, dd * 2 + k // 4, :].unsqueeze(1).to_broadcast([128, 2, 64]),
                                  scalar=(1.0 if ri == 0 else -1.0), in1=mC[:, k % 4, :].rearrange("p (a c) -> p a c", c=64), op0=ALU.mult, op1=ALU.mult), reads=[craw, mC], writes=[pd])
                            kb.op('pe', lambda e, pd=pd, pt=pt: e.transpose(out=pt[:, 0:128], in_=pd[:], identity=ident[:]), reads=[pd, ident], writes=[pt])
                            kb.op('act', lambda e, pt=pt, tile=tile, ri=ri: e.copy(out=CT[:, tile, ri, :], in_=pt[:, 0:128]), reads=[pt], writes=[CT])
                    kb.barrier()
                yacc = sb(es, 'yacc', [128, 2, T], F32)
                dsk = sb(es, 'dsk', [128, 2], F32)
                with nc.allow_non_contiguous_dma(reason="tiny"):
                    kb.dma('sp', dsk[:], I['s5_d'][layer].rearrange("(c p) -> p c", p=128), writes=[dsk])
                for c2 in range(2):
                    kb.dma('sp', uTn[:], S['FM'][C_S5U + c2], reads=[D_['FM']], writes=[uTn])
                    kb.op('pool', lambda e, c2=c2: e.tensor_scalar_mul(out=yacc[:, c2, :], in0=uTn[:, :], scalar1=dsk[:, c2:c2 + 1]), reads=[uTn, dsk], writes=[yacc])
                cur_c = [1]
                cosT = sb(es, 'cosT', [128, T], F32); sinT = sb(es, 'sinT', [128, T], F32)
                dre = sb(es, 'dre', [128, T], F32); dim_ = sb(es, 'dim', [128, T], F32)
                gre = sb(es, 'gre', [128, T], F32); gim = sb(es, 'gim', [128, T], F32)
                hreb = sb(es, 'hreb', [128, T], BF16); himb = sb(es, 'himb', [128, T], BF16)
                lat_chunks = [(j * 512, 512) for j in range(8)]
                for tile in range(16):
                    dd, k = tile // 8, tile % 8
                    cch = k // 4
                    seq = ([(SEQ, 256)] + lat_chunks) if dd == 0 else (lat_chunks + [(SEQ, 256)])
                    if cur_c[0] != cch:
                        kb.dma('sp', uTn[:], S['FM'][C_S5U + cch], reads=[D_['FM']], writes=[uTn])
                        cur_c[0] = cch
                    for (buf, off) in ((sinT, 0.0), (cosT, 0.5)):
                        kb.op('pool', lambda e, buf=buf, off=off: e.tensor_scalar(out=buf[:], in0=(iot[:, :] if dd == 0 else iot[:, ::-1]), scalar1=P[:, TH, tile:tile + 1], scalar2=off * math.pi, op0=ALU.mult, op1=ALU.add), reads=[iot, P], writes=[buf])
                        kb.op('dve', lambda e, buf=buf: e.tensor_scalar(out=dre[:], in0=buf[:], scalar1=1.0 / TWO_PI, scalar2=12582912.0, op0=ALU.mult, op1=ALU.add), reads=[buf], writes=[dre])
                        kb.op('dve', lambda e: e.tensor_scalar_add(out=dre[:], in0=dre[:], scalar1=-12582912.0), reads=[dre], writes=[dre])
                        kb.op('dve', lambda e, buf=buf: e.scalar_tensor_tensor(out=buf[:], in0=dre[:], scalar=-TWO_PI, in1=buf[:], op0=ALU.mult, op1=ALU.add), reads=[dre, buf], writes=[buf])
                        kb.op('dve', lambda e, buf=buf: e.tensor_scalar(out=buf[:], in0=buf[:], scalar1=-3.14159, scalar2=3.14159, op0=ALU.max, op1=ALU.min), reads=[buf], writes=[buf])
                        kb.op('act', lambda e, buf=buf: e.activation(out=buf[:], in_=buf[:], func=AF.Sin), reads=[buf], writes=[buf])
                    so = 0
                    for ci, (to, n) in enumerate(seq):
                        pr = pbank[0 + (ci % 2) * 2]; pi_ = pbank[1 + (ci % 2) * 2]
                        kb.op('pe', lambda e, pr=pr, to=to, n=n: e.matmul(pr[:, 0:n], lhsT=BbT[:, tile, 0, :], rhs=uTn[:, to:to + n], start=True, stop=True), reads=[BbT, uTn], writes=[pr])
                        kb.op('pe', lambda e, pi_=pi_, to=to, n=n: e.matmul(pi_[:, 0:n], lhsT=BbT[:, tile, 1, :], rhs=uTn[:, to:to + n], start=True, stop=True), reads=[BbT, uTn], writes=[pi_])
                        sl = slice(so, so + n)
                        kb.op('act', lambda e, pr=pr, sl=sl, n=n: e.copy(out=gre[:, sl], in_=pr[:, 0:n]), reads=[pr], writes=[gre])
                        kb.op('act', lambda e, pi_=pi_, sl=sl, n=n: e.copy(out=gim[:, sl], in_=pi_[:, 0:n]), reads=[pi_], writes=[gim])
                        so += n
                    kb.op('dve', lambda e: e.tensor_tensor(out=dre[:], in0=gre[:], in1=cosT[:], op=ALU.mult), reads=[gre, cosT], writes=[dre])
                    kb.op('pool', lambda e: e.tensor_tensor(out=dim_[:], in0=gim[:], in1=cosT[:], op=ALU.mult), reads=[gim, cosT], writes=[dim_])
                    kb.op('dve', lambda e: e.tensor_tensor(out=gim[:], in0=gim[:], in1=sinT[:], op=ALU.mult), reads=[gim, sinT], writes=[gim])
                    kb.op('dve', lambda e: e.tensor_tensor(out=gre[:], in0=gre[:], in1=sinT[:], op=ALU.mult), reads=[gre, sinT], writes=[gre])
                    kb.op('dve', lambda e: e.tensor_tensor(out=dre[:], in0=dre[:], in1=gim[:], op=ALU.add), reads=[dre, gim], writes=[dre])
                    kb.op('dve', lambda e: e.tensor_tensor(out=dim_[:], in0=dim_[:], in1=gre[:], op=ALU.subtract), reads=[dim_, gre], writes=[dim_])
                    rb = P[:, RR, tile:tile + 1].to_broadcast([128, T])
                    if dd == 0:
                        kb.op('dve', lambda e: e.tensor_tensor_scan(out=gre[:], data0=rb, data1=dre[:], initial=0.0, op0=ALU.mult, op1=ALU.add), reads=[dre, P], writes=[gre])
                        kb.op('dve', lambda e: e.tensor_tensor_scan(out=gim[:], data0=rb, data1=dim_[:], initial=0.0, op0=ALU.mult, op1=ALU.add), reads=[dim_, P], writes=[gim])
                    else:
                        kb.op('dve', lambda e: e.tensor_tensor_scan(out=gre[:, ::-1], data0=rb, data1=dre[:, ::-1], initial=0.0, op0=ALU.mult, op1=ALU.add), reads=[dre, P], writes=[gre])
                        kb.op('dve', lambda e: e.tensor_tensor_scan(out=gim[:, ::-1], data0=rb, data1=dim_[:, ::-1], initial=0.0, op0=ALU.mult, op1=ALU.add), reads=[dim_, P], writes=[gim])
                    kb.op('pool', lambda e: e.tensor_tensor(out=dre[:], in0=gre[:], in1=cosT[:], op=ALU.mult), reads=[gre, cosT], writes=[dre])
                    kb.op('dve', lambda e: e.tensor_tensor(out=dim_[:], in0=gim[:], in1=cosT[:], op=ALU.mult), reads=[gim, cosT], writes=[dim_])
                    kb.op('dve', lambda e: e.tensor_tensor(out=gim[:], in0=gim[:], in1=sinT[:], op=ALU.mult), reads=[gim, sinT], writes=[gim])
                    kb.op('dve', lambda e: e.tensor_tensor(out=gre[:], in0=gre[:], in1=sinT[:], op=ALU.mult), reads=[gre, sinT], writes=[gre])
                    kb.op('dve', lambda e: e.tensor_tensor(out=hreb[:], in0=dre[:], in1=gim[:], op=ALU.subtract), reads=[dre, gim], writes=[hreb])
                    kb.op('dve', lambda e: e.tensor_tensor(out=himb[:], in0=gre[:], in1=dim_[:], op=ALU.add), reads=[gre, dim_], writes=[himb])
                    so = 0
                    for ci, (to, n) in enumerate(seq):
                        sl = slice(so, so + n)
                        py = pbank[4 + ci % 2]
                        kb.op('pe', lambda e, py=py, sl=sl, n=n: e.matmul(py[:, 0:n], lhsT=CT[:, tile, 0, :], rhs=hreb[:, sl], start=True, stop=False), reads=[CT, hreb], writes=[py])
                        kb.op('pe', lambda e, py=py, sl=sl, n=n: e.matmul(py[:, 0:n], lhsT=CT[:, tile, 1, :], rhs=himb[:, sl], start=False, stop=True), reads=[CT, himb], writes=[py])
                        kb.op('dve', lambda e, py=py, to=to, n=n: e.tensor_tensor(out=yacc[:, cch, to:to + n], in0=py[:, 0:n], in1=yacc[:, cch, to:to + n], op=ALU.add), reads=[py, yacc], writes=[yacc])
                        so += n
                wglu = sb(es, 'wglu', [128, 2, 512], BF16)
                kb.dma('pool', wglu[:], I['s5_w_glu'][layer].rearrange("(c p) n -> p c n", p=128), writes=[wglu])
                yb = sb(es, 'yb', [128, 2, 512], BF16)
                sg = sb(es, 'sg', [128, 512], F32)
                og = sb(es, 'og', [128, 2, 512], BF16)
                for (to, n) in lat_chunks + [(SEQ, 256)]:
                    kb.op('pool', lambda e, to=to, n=n: e.tensor_copy(out=yb[:, :, 0:n], in_=yacc[:, :, to:to + n]), reads=[yacc], writes=[yb])
                    for oc in range(2):
                        pv = pbank[0]; pg = pbank[1]
                        for c2 in range(2):
                            kb.op('pe', lambda e, c2=c2, oc=oc, n=n: e.matmul(pv[:, 0:n], lhsT=wglu[:, c2, oc * 128:(oc + 1) * 128], rhs=yb[:, c2, 0:n], start=(c2 == 0), stop=(c2 == 1)), reads=[wglu, yb], writes=[pv])
                        for c2 in range(2):
                            kb.op('pe', lambda e, c2=c2, oc=oc, n=n: e.matmul(pg[:, 0:n], lhsT=wglu[:, c2, 256 + oc * 128:256 + (oc + 1) * 128], rhs=yb[:, c2, 0:n], start=(c2 == 0), stop=(c2 == 1)), reads=[wglu, yb], writes=[pg])
                        kb.op('act', lambda e, n=n: e.activation(out=sg[:, 0:n], in_=pg[:, 0:n], func=AF.Sigmoid), reads=[pg], writes=[sg])
                        kb.op('dve', lambda e, oc=oc, n=n: e.tensor_tensor(out=og[:, oc, 0:n], in0=pv[:, 0:n], in1=sg[:, 0:n], op=ALU.mult), reads=[pv, sg], writes=[og])
                    kb.dma('sp', S['oT'][2, :, :, to:to + n].rearrange("c p t -> p c t"), og[:, :, 0:n], reads=[og], writes=[D_['oT']])
                if debug == 'C':
                    kb.barrier()
                    dbg_out['oT'] = nc.dram_tensor('dbg_oT', [4, 2, 128, T], BF16, kind="ExternalOutput").ap()
                    kb.dma('sp', dbg_out['oT'], S['oT'], reads=[D_['oT']])
                kb.barrier()
            if debug == 'C':
                break
            with contextlib.ExitStack() as es:
                wg = sb(es, 'wg', [128, 8, 4 * D], BF16)
                wbr = sb(es, 'wbr', [128, 4, 2, D], BF16)
                wo = sb(es, 'wo', [128, 8, D], BF16)
                for kc in range(8):
                    kb.dma('pool', wg[:, kc, :], I['w_gate'][layer, kc * 128:(kc + 1) * 128, :], writes=[wg])
                kb.dma('pool', wbr[:], I['w_branch'][layer].rearrange("b (c p) n -> p b c n", p=128), writes=[wbr])
                kb.dma('pool', wo[:], I['w_out'][layer].rearrange("(k p) n -> p k n", p=128), writes=[wo])
                bg = sb(es, 'bg', [128, 4 * D], F32)
                kb.dma('sp', bg[:], I['b_gate'][layer:layer + 1, :].to_broadcast([128, 4 * D]), writes=[bg])
                md = sb(es, 'mdD', [128, 2, 3, D], F32)
                for kind in range(2):
                    for jj, mi in enumerate((2, 3, 4)):
                        kb.dma('sp', md[:, kind, jj, :], S['mod'][kind:kind + 1, mi * D:(mi + 1) * D].to_broadcast([128, D]), reads=[D_['mod']], writes=[md])
                kb.op('dve', lambda e: e.tensor_scalar_add(out=md[:, :, 2, :], in0=md[:, :, 2, :], scalar1=1.0), reads=[md], writes=[md])
                lng = sb(es, 'lng', [128, 2, D], F32)
                kb.dma('sp', lng[:, 0, :], I['ln1_g'][layer:layer + 1, :].to_broadcast([128, D]), writes=[lng])
                kb.dma('sp', lng[:, 1, :], I['ln1_b'][layer:layer + 1, :].to_broadcast([128, D]), writes=[lng])
                epsl = sb(es, 'epsl', [128, 1], F32)
                kb.op('dve', lambda e: e.memset(epsl[:], LN_EPS), writes=[epsl])
                hTm = [sb(es, 'hTm%d' % i, [128, 8, 128], BF16) for i in range(2)]
                oTm = [sb(es, 'oTm%d' % i, [128, 4, 2, 128], BF16) for i in range(2)]
                xm = [sb(es, 'xm%d' % i, [128, D], F32) for i in range(2)]
                gt_2 = [sb(es, 'gt%d' % i, [128, D], F32) for i in range(2)]; macc_2 = [sb(es, 'macc%d' % i, [128, D], F32) for i in range(2)]; tt_2 = [sb(es, 'tt%d' % i, [128, D], F32) for i in range(2)]
                mbb_2 = [sb(es, 'mbb%d' % i, [128, D], BF16) for i in range(2)]; mT_2 = [sb(es, 'mT%d' % i, [128, 8, 128], BF16) for i in range(2)]
                st_2 = [sb(es, 'stD%d' % i, [128, 8], F32) for i in range(2)]; jk = sb(es, 'jkD', [128, D], F32)
                h2b_2 = [sb(es, 'h2b%d' % i, [128, D], BF16) for i in range(2)]; h2T_2 = [sb(es, 'h2Tt%d' % i, [128, 8, 128], BF16) for i in range(2)]
                st = st_2[0]
                pcd = [0]

                def layer_norm(r_t, g_ap, b_ap, out_t):
                    kb.op('act', lambda e: e.activation(out=jk[:], in_=r_t[:], func=AF.Identity, accum_out=st[:, 0:1]), reads=[r_t], writes=[jk, st])
                    kb.op('act', lambda e: e.activation(out=jk[:], in_=r_t[:], func=AF.Square, accum_out=st[:, 1:2]), reads=[r_t], writes=[jk, st])
                    kb.op('dve', lambda e: e.tensor_scalar_mul(out=st[:, 2:4], in0=st[:, 0:2], scalar1=1.0 / D), reads=[st], writes=[st])
                    kb.op('dve', lambda e: e.tensor_tensor(out=st[:, 4:5], in0=st[:, 2:3], in1=st[:, 2:3], op=ALU.mult), reads=[st], writes=[st])
                    kb.op('dve', lambda e: e.tensor_tensor(out=st[:, 5:6], in0=st[:, 3:4], in1=st[:, 4:5], op=ALU.subtract), reads=[st], writes=[st])
                    kb.op('act', lambda e: e.activation(out=st[:, 6:7], in_=st[:, 5:6], func=AF.Sqrt, bias=epsl[:, 0:1]), reads=[st, epsl], writes=[st])
                    kb.op('dve', lambda e: e.reciprocal(out=st[:, 6:7], in_=st[:, 6:7]), reads=[st], writes=[st])
                    kb.op('dve', lambda e: e.tensor_scalar(out=out_t[:], in0=r_t[:], scalar1=st[:, 2:3], scalar2=st[:, 6:7], op0=ALU.subtract, op1=ALU.mult), reads=[r_t, st], writes=[out_t])
                    kb.op('dve', lambda e: e.tensor_tensor(out=out_t[:], in0=out_t[:], in1=g_ap, op=ALU.mult), reads=[out_t, lng], writes=[out_t])
                    kb.op('dve', lambda e: e.tensor_tensor(out=out_t[:], in0=out_t[:], in1=b_ap, op=ALU.add), reads=[out_t, lng], writes=[out_t])

                def loadD(tj):
                    hd = hTm[tj % 2]; od = oTm[tj % 2]; xd = xm[tj % 2]
                    kb.dma('sp', hd[:], S['hT'][tj], reads=[D_['hT']], writes=[hd])
                    kb.dma('sp', od[:], S['oT'][:, :, :, tj * 128:(tj + 1) * 128].rearrange("b c p t -> p b c t"), reads=[D_['oT']], writes=[od])
                    if layer == 0:
                        src = I['x'][tj * 128:(tj + 1) * 128, :] if tj < NLT else I['ctx'][(tj - NLT) * 128:(tj - NLT + 1) * 128, :]
                        kb.dma('sp', xd[:], src, writes=[xd])
                    else:
                        kb.dma('sp', xd[:], S['xcur'][tj], reads=[D_['xcur']], writes=[xd])
                for ti in range(NT):
                    kind = 0 if ti < NLT else 1
                    hT_t = hTm[ti % 2]; oT_t = oTm[ti % 2]; x_t = xm[ti % 2]
                    gt = gt_2[ti % 2]; macc = macc_2[ti % 2]; tt = tt_2[ti % 2]; mbb = mbb_2[ti % 2]; mT = mT_2[ti % 2]; st = st_2[ti % 2]; h2b = h2b_2[ti % 2]; h2T = h2T_2[ti % 2]
                    if ti == 0:
                        loadD(0)
                    if ti + 1 < NT:
                        loadD(ti + 1)
                    for br in range(4):
                        for hf in range(2):
                            pgt = pbank[hf]
                            for kc in range(8):
                                kb.op('pe', lambda e, kc=kc, pgt=pgt, br=br, hf=hf: e.matmul(pgt[:, :], lhsT=hT_t[:, kc, :], rhs=wg[:, kc, br * D + hf * 512:br * D + (hf + 1) * 512], start=(kc == 0), stop=(kc == 7)),
                                      reads=[hT_t, wg], writes=[pgt])
                            kb.op('dve', lambda e, pgt=pgt, br=br, hf=hf: e.tensor_tensor(out=gt[:, hf * 512:(hf + 1) * 512], in0=pgt[:, :], in1=bg[:, br * D + hf * 512:br * D + (hf + 1) * 512], op=ALU.add), reads=[pgt, bg], writes=[gt])
                        kb.op('act', lambda e: e.activation(out=gt[:], in_=gt[:], func=AF.Sigmoid), reads=[gt], writes=[gt])
                        for hf in range(2):
                            pbt = pbank[2 + hf]
                            for c2 in range(2):
                                kb.op('pe', lambda e, c2=c2, pbt=pbt, br=br, hf=hf: e.matmul(pbt[:, :], lhsT=oT_t[:, br, c2, :], rhs=wbr[:, br, c2, hf * 512:(hf + 1) * 512], start=(c2 == 0), stop=(c2 == 1)),
                                      reads=[oT_t, wbr], writes=[pbt])
                            dst = macc if br == 0 else tt
                            kb.op('dve', lambda e, pbt=pbt, hf=hf, dst=dst: e.tensor_tensor(out=dst[:, hf * 512:(hf + 1) * 512], in0=pbt[:, :], in1=gt[:, hf * 512:(hf + 1) * 512], op=ALU.mult), reads=[pbt, gt], writes=[dst])
                        if br > 0:
                            kb.op('pool', lambda e: e.tensor_tensor(out=macc[:], in0=macc[:], in1=tt[:], op=ALU.add), reads=[macc, tt], writes=[macc])
                    kb.op('pool', lambda e: e.tensor_copy(out=mbb[:], in_=macc[:]), reads=[macc], writes=[mbb])
                    pt = ptr[pcd[0] % 2]; pcd[0] += 1
                    for kc in range(8):
                        kb.op('pe', lambda e, kc=kc, pt=pt: e.transpose(out=pt[:, kc * 128:(kc + 1) * 128], in_=mbb[:, kc * 128:(kc + 1) * 128], identity=ident[:]), reads=[mbb, ident], writes=[pt])
                    kb.op('act', lambda e, pt=pt: e.copy(out=mT[:].rearrange("p k t -> p (k t)"), in_=pt[:]), reads=[pt], writes=[mT])
                    for hf in range(2):
                        py = pbank[4 + hf]
                        for kc in range(8):
                            kb.op('pe', lambda e, kc=kc, py=py, hf=hf: e.matmul(py[:, :], lhsT=mT[:, kc, :], rhs=wo[:, kc, hf * 512:(hf + 1) * 512], start=(kc == 0), stop=(kc == 7)), reads=[mT, wo], writes=[py])
                        kb.op('dve', lambda e, py=py, hf=hf: e.tensor_tensor(out=tt[:, hf * 512:(hf + 1) * 512], in0=py[:, :], in1=md[:, kind, 0, hf * 512:(hf + 1) * 512], op=ALU.mult), reads=[py, md], writes=[tt])
                    kb.op('dve', lambda e: e.scalar_tensor_tensor(out=tt[:], in0=x_t[:], scalar=ALPHA, in1=tt[:], op0=ALU.mult, op1=ALU.add), reads=[x_t, tt], writes=[tt])
                    layer_norm(tt, lng[:, 0, :], lng[:, 1, :], macc)
                    kb.dma('sp', S['x1'][ti], macc[:], reads=[macc], writes=[D_['x1']])
                    kb.op('dve', lambda e: e.tensor_tensor(out=tt[:], in0=macc[:], in1=md[:, kind, 2, :], op=ALU.mult), reads=[macc, md], writes=[tt])
                    kb.op('dve', lambda e: e.tensor_tensor(out=h2b[:], in0=tt[:], in1=md[:, kind, 1, :], op=ALU.add), reads=[tt, md], writes=[h2b])
                    if layer % 2 == 1:
                        kb.dma('sp', S['h2tm'][ti], h2b[:], reads=[h2b], writes=[D_['h2tm']])
                    pt = ptr[pcd[0] % 2]; pcd[0] += 1
                    for kc in range(8):
                        kb.op('pe', lambda e, kc=kc, pt=pt: e.transpose(out=pt[:, kc * 128:(kc + 1) * 128], in_=h2b[:, kc * 128:(kc + 1) * 128], identity=ident[:]), reads=[h2b, ident], writes=[pt])
                    kb.op('act', lambda e, pt=pt: e.copy(out=h2T[:].rearrange("p k t -> p (k t)"), in_=pt[:]), reads=[pt], writes=[h2T])
                    kb.dma('sp', S['h2T'][:, :, ti * 128:(ti + 1) * 128], h2T[:], reads=[h2T], writes=[D_['h2T']])
                kb.barrier()
            if layer % 2 == 1:
              with contextlib.ExitStack() as es:
                jl = layer // 2
                gk = sb(es, 'gkS', [128, NT, 2], F32); bei = sb(es, 'beiS', [128, NBLK], I32); posi = sb(es, 'posiS', [128, 2, NT], I32)
                ix1 = sb(es, 'ix1S', [128, NBLK, 56], I32); ix2 = sb(es, 'ix2S', [128, NBLK, 28], I32)
                es_r = contextlib.ExitStack(); es_r.__enter__()
                hblk = sb(es_r, 'hblkS', [128, 8, 1024], BF16)
                wr = sb(es_r, 'wrS', [128, 8, NEXP], BF16)
                kb.dma('pool', wr[:], I['moe_router'][jl].rearrange("(k p) n -> p k n", p=128), writes=[wr])
                oh = sb(es_r, 'ohS', [128, 2, NT, NEXP], F32)
                mkb = sb(es_r, 'mkbS', [128, NT, NEXP], BF16)
                rank = sb(es_r, 'rankS', [128, NT, NEXP], F32)
                run = sb(es_r, 'runS', [128, NEXP], F32)
                lg = sb(es_r, 'lgS', [128, 4, NEXP], F32); sc = sb(es_r, 'scS', [128, 8], F32)
                utri = sb(es_r, 'utriS', [128, 128], BF16); utf = sb(es_r, 'utfS', [128, 128], F32)
                kb.dma('sp', utf[:], I['utri'][:], writes=[utf])
                kb.op('dve', lambda e: e.tensor_copy(out=utri[:], in_=utf[:]), reads=[utf], writes=[utri])
                bst = sb(es_r, 'bstS', [128, NBLK], F32)
                kb.dma('sp', bst[:], I['bstart'][0:1, :].to_broadcast([128, NBLK]), writes=[bst])
                kb.op('dve', lambda e: e.memset(run[:], 0.0), writes=[run])
                for (t0, nb) in [(b_ * 1024, min(1024, T - b_ * 1024)) for b_ in range((T + 1023) // 1024)]:
                    kb.dma('sp', hblk[:, :, 0:nb], S['h2T'][:, :, t0:t0 + nb], reads=[D_['h2T']], writes=[hblk])
                    for s_ in range(nb // 128):
                        ti = t0 // 128 + s_
                        pl = pbank[s_ % 2]
                        for kc in range(8):
                            kb.op('pe', lambda e, kc=kc, pl=pl, s_=s_: e.matmul(pl[:, 0:NEXP], lhsT=hblk[:, kc, s_ * 128:(s_ + 1) * 128], rhs=wr[:, kc, :], start=(kc == 0), stop=(kc == 7)), reads=[hblk, wr], writes=[pl])
                        kb.op('dve', lambda e, pl=pl: e.tensor_copy(out=lg[:, 0, :], in_=pl[:, 0:NEXP]), reads=[pl], writes=[lg])
                        kb.op('dve', lambda e: e.reduce_max(out=sc[:, 0:1], in_=lg[:, 0, :], axis=AX.X), reads=[lg], writes=[sc])
                        kb.op('dve', lambda e, ti=ti: e.tensor_scalar(out=oh[:, 0, ti, :], in0=lg[:, 0, :], scalar1=sc[:, 0:1], scalar2=None, op0=ALU.is_equal), reads=[lg, sc], writes=[oh])
                        kb.op('dve', lambda e, ti=ti: e.scalar_tensor_tensor(out=lg[:, 2, :], in0=oh[:, 0, ti, :], scalar=-1e30, in1=lg[:, 0, :], op0=ALU.mult, op1=ALU.add), reads=[lg, oh], writes=[lg])
                        kb.op('dve', lambda e: e.reduce_max(out=sc[:, 1:2], in_=lg[:, 2, :], axis=AX.X), reads=[lg], writes=[sc])
                        kb.op('dve', lambda e, ti=ti: e.tensor_scalar(out=oh[:, 1, ti, :], in0=lg[:, 2, :], scalar1=sc[:, 1:2], scalar2=None, op0=ALU.is_equal), reads=[lg, sc], writes=[oh])
                        kb.op('dve', lambda e: e.tensor_tensor(out=sc[:, 2:3], in0=sc[:, 1:2], in1=sc[:, 0:1], op=ALU.subtract), reads=[sc], writes=[sc])
                        kb.op('act', lambda e: e.activation(out=sc[:, 3:4], in_=sc[:, 2:3], func=AF.Exp), reads=[sc], writes=[sc])
                        kb.op('dve', lambda e: e.tensor_scalar_add(out=sc[:, 4:5], in0=sc[:, 3:4], scalar1=1.0), reads=[sc], writes=[sc])
                        kb.op('dve', lambda e, ti=ti: e.reciprocal(out=gk[:, ti, 0:1], in_=sc[:, 4:5]), reads=[sc], writes=[gk])
                        kb.op('dve', lambda e, ti=ti: e.tensor_tensor(out=gk[:, ti, 1:2], in0=sc[:, 3:4], in1=gk[:, ti, 0:1], op=ALU.mult), reads=[sc, gk], writes=[gk])
                        kb.op('dve', lambda e, ti=ti: e.tensor_tensor(out=mkb[:, ti, :], in0=oh[:, 0, ti, :], in1=oh[:, 1, ti, :], op=ALU.add), reads=[oh], writes=[mkb])
                        pr_ = pbank[2 + s_ % 2]
                        kb.op('pe', lambda e, pr_=pr_, ti=ti: e.matmul(pr_[:, 0:NEXP], lhsT=utri[:], rhs=mkb[:, ti, :], start=True, stop=True), reads=[utri, mkb], writes=[pr_])
                        kb.op('pe', lambda e, pr_=pr_, ti=ti: e.matmul(pr_[:, NEXP:2 * NEXP], lhsT=ones_b[:], rhs=mkb[:, ti, :], start=True, stop=True), reads=[ones_b, mkb], writes=[pr_])
                        kb.op('dve', lambda e, pr_=pr_, ti=ti: e.tensor_tensor(out=rank[:, ti, :], in0=pr_[:, 0:NEXP], in1=run[:], op=ALU.add), reads=[pr_, run], writes=[rank])
                        kb.op('dve', lambda e, pr_=pr_: e.tensor_tensor(out=run[:], in0=pr_[:, NEXP:2 * NEXP], in1=run[:], op=ALU.add), reads=[pr_, run], writes=[run])
                cw = sb(es_r, 'cwS', [128, 6, NEXP], F32)
                MAGIC = 12582912.0
                kb.op('dve', lambda e: e.tensor_scalar(out=cw[:, 3, :], in0=run[:], scalar1=float(BS - 1), scalar2=1.0 / BS, op0=ALU.add, op1=ALU.mult), reads=[run], writes=[cw])
                kb.op('dve', lambda e: e.tensor_scalar(out=cw[:, 3, :], in0=cw[:, 3, :], scalar1=(-0.5 + 0.5 / BS), scalar2=MAGIC, op0=ALU.add, op1=ALU.add), reads=[cw], writes=[cw])
                kb.op('dve', lambda e: e.tensor_scalar(out=cw[:, 0, :], in0=cw[:, 3, :], scalar1=-MAGIC, scalar2=float(BS), op0=ALU.add, op1=ALU.mult), reads=[cw], writes=[cw])
                kb.op('dve', lambda e: e.tensor_copy(out=cw[:, 1, 0:1], in_=cw[:, 0, 0:1]), reads=[cw], writes=[cw])
                for e_ in range(1, NEXP):
                    kb.op('dve', lambda e, e_=e_: e.tensor_tensor(out=cw[:, 1, e_:e_ + 1], in0=cw[:, 1, e_ - 1:e_], in1=cw[:, 0, e_:e_ + 1], op=ALU.add), reads=[cw], writes=[cw])
                kb.op('dve', lambda e: e.tensor_tensor(out=cw[:, 2, :], in0=cw[:, 1, :], in1=cw[:, 0, :], op=ALU.subtract), reads=[cw], writes=[cw])
                cmpb = sb(es_r, 'cmpbS', [128, NBLK, NEXP], F32)
                bef = sb(es_r, 'befS', [128, NBLK], F32)
                kb.op('dve', lambda e: e.tensor_tensor(out=cmpb[:], in0=cw[:, 1, :].unsqueeze(1).to_broadcast([128, NBLK, NEXP]), in1=bst[:].unsqueeze(2).to_broadcast([128, NBLK, NEXP]), op=ALU.is_le), reads=[cw, bst], writes=[cmpb])
                kb.op('dve', lambda e: e.reduce_sum(out=bef[:], in_=cmpb[:], axis=AX.X), reads=[cmpb], writes=[bef])
                kb.op('dve', lambda e: e.tensor_scalar_min(out=bef[:], in0=bef[:], scalar1=float(NEXP - 1)), reads=[bef], writes=[bef])
                kb.op('dve', lambda e: e.tensor_copy(out=bei[:], in_=bef[:]), reads=[bef], writes=[bei])
                b1 = sb(es_r, 'b1S', [128, 56], F32); b2 = sb(es_r, 'b2S', [128, 28], F32)
                kb.dma('sp', b1[:], I['base1'][:], writes=[b1]); kb.dma('sp', b2[:], I['base2'][:], writes=[b2])
                ix1f = sb(es_r, 'ix1fS', [128, NBLK, 56], F32); ix2f = sb(es_r, 'ix2fS', [128, NBLK, 28], F32)
                kb.op('dve', lambda e: e.scalar_tensor_tensor(out=ix1f[:], in0=bef[:].unsqueeze(2).to_broadcast([128, NBLK, 56]), scalar=7168.0, in1=b1[:].unsqueeze(1).to_broadcast([128, NBLK, 56]), op0=ALU.mult, op1=ALU.add), reads=[bef, b1], writes=[ix1f])
                kb.op('dve', lambda e: e.scalar_tensor_tensor(out=ix2f[:], in0=bef[:].unsqueeze(2).to_broadcast([128, NBLK, 28]), scalar=3584.0, in1=b2[:].unsqueeze(1).to_broadcast([128, NBLK, 28]), op0=ALU.mult, op1=ALU.add), reads=[bef, b2], writes=[ix2f])
                if jl > 0:
                    kb.op('dve', lambda e: e.tensor_scalar_add(out=ix1f[:], in0=ix1f[:], scalar1=float(jl * NEXP * D * 7)), reads=[ix1f], writes=[ix1f])
                    kb.op('dve', lambda e: e.tensor_scalar_add(out=ix2f[:], in0=ix2f[:], scalar1=float(jl * NEXP * D_FF)), reads=[ix2f], writes=[ix2f])
                kb.op('dve', lambda e: e.tensor_copy(out=ix1[:], in_=ix1f[:]), reads=[ix1f], writes=[ix1])
                kb.op('dve', lambda e: e.tensor_copy(out=ix2[:], in_=ix2f[:]), reads=[ix2f], writes=[ix2])
                posf = sb(es_r, 'posfS', [128, 2, NT], F32)
                kb.op('dve', lambda e: e.tensor_tensor(out=rank[:], in0=rank[:], in1=cw[:, 2, :].unsqueeze(1).to_broadcast([128, NT, NEXP]), op=ALU.add), reads=[rank, cw], writes=[rank])
                for k2 in range(2):
                    kb.op('dve', lambda e, k2=k2: e.tensor_tensor(out=oh[:, k2], in0=oh[:, k2], in1=rank[:], op=ALU.mult), reads=[oh, rank], writes=[oh])
                    kb.op('dve', lambda e, k2=k2: e.reduce_sum(out=posf[:, k2, :], in_=oh[:, k2], axis=AX.X), reads=[oh], writes=[posf])
                kb.op('dve', lambda e: e.tensor_copy(out=posi[:], in_=posf[:]), reads=[posf], writes=[posi])
                zt = sb(es_r, 'ztS', [128, 4 * D], BF16)
                kb.op('pool', lambda e: e.memset(zt[:], 0.0), writes=[zt])
                for a_ in range(NSLOT // 512):
                    kb.dma('sp', S['Xs'][a_ * 512:(a_ + 1) * 512, :].rearrange("(p j) d -> p (j d)", j=4), zt[:], reads=[zt], writes=[D_['Xs']])
                h2t = [sb(es_r, 'h2tS%d' % i, [128, D], BF16) for i in range(2)]
                for ti in range(NT):
                    ht_ = h2t[ti % 2]
                    kb.dma('sp', ht_[:], S['h2tm'][ti], reads=[D_['h2tm']], writes=[ht_])
                    for k2 in range(2):
                        kb.idma(S['Xs'][:, :], bass.IndirectOffsetOnAxis(ap=posi[:, k2, ti:ti + 1].bitcast(mybir.dt.uint32), axis=0), ht_[:], None, reads=[ht_, posi, D_['Xs']], writes=[D_['Xs']])
                kb.barrier()
                es_r.close()
                es2 = contextlib.ExitStack(); es2.__enter__()
                NS = BS // 128
                xb = sb(es2, 'xbS', [128, NS, D], BF16)
                hb2 = sb(es2, 'hb2S', [128, 8, BS], BF16)
                acc = sb(es2, 'accS', [128, NS, D], F32)
                w1c = [sb(es2, 'w1cS%d' % i, [128, 8, 512], BF16) for i in range(2)]
                w3c = [sb(es2, 'w3cS%d' % i, [128, 8, 512], BF16) for i in range(2)]
                w2c = [sb(es2, 'w2cS%d' % i, [128, 4, D], BF16) for i in range(2)]
                gT = [sb(es2, 'gTS%d' % i, [128, 4, BS], BF16) for i in range(2)]
                sil = sb(es2, 'silS', [128, 512], F32)
                W1v = I['moe_w1'].rearrange("j e k (c n) -> (j e k c) n", n=512); W3v = I['moe_w3'].rearrange("j e k (c n) -> (j e k c) n", n=512); W2v = I['moe_w2'].rearrange("j e f n -> (j e f) n")
                U32 = mybir.dt.uint32
                wcnt = [0]; pcs = [0]
                for b_ in range(NBLK):
                    kb.dma('sp', xb[:], S['Xs'][b_ * BS:(b_ + 1) * BS, :].rearrange("(s p) d -> p s d", p=128), reads=[D_['Xs']], writes=[xb])
                    for s_ in range(NS):
                        pt = ptr[pcs[0] % 2]; pcs[0] += 1
                        for kc in range(8):
                            kb.op('pe', lambda e, kc=kc, pt=pt, s_=s_: e.transpose(out=pt[:, kc * 128:(kc + 1) * 128], in_=xb[:, s_, kc * 128:(kc + 1) * 128], identity=ident[:]), reads=[xb, ident], writes=[pt])
                        kb.op('act', lambda e, pt=pt, s_=s_: e.copy(out=hb2[:, :, s_ * 128:(s_ + 1) * 128], in_=pt[:].rearrange("p (k t) -> p k t", k=8)), reads=[pt], writes=[hb2])
                    kb.op('pool', lambda e: e.memset(acc[:], 0.0), writes=[acc])
                    for fc in range(D_FF // 512):
                        a1 = w1c[wcnt[0] % 2]; a3 = w3c[wcnt[0] % 2]; a2 = w2c[wcnt[0] % 2]; g_ = gT[wcnt[0] % 2]; wcnt[0] += 1
                        for kc in range(8):
                            kb.idma(a1[:, kc, :], None, W1v[:, :], bass.IndirectOffsetOnAxis(ap=ix1[:, b_, kc * 7 + fc:kc * 7 + fc + 1].bitcast(U32), axis=0), reads=[ix1], writes=[a1])
                            kb.idma(a3[:, kc, :], None, W3v[:, :], bass.IndirectOffsetOnAxis(ap=ix1[:, b_, kc * 7 + fc:kc * 7 + fc + 1].bitcast(U32), axis=0), reads=[ix1], writes=[a3])
                        for f in range(4):
                            kb.idma(a2[:, f, :], None, W2v[:, :], bass.IndirectOffsetOnAxis(ap=ix2[:, b_, fc * 4 + f:fc * 4 + f + 1].bitcast(U32), axis=0), reads=[ix2], writes=[a2])
                        for f in range(4):
                            for th in range(BS // 512):
                                c0 = th * 512
                                p1 = pbank[0 + ((f * 2 + th) % 2) * 2]; p3 = pbank[1 + ((f * 2 + th) % 2) * 2]
                                for kc in range(8):
                                    kb.op('pe', lambda e, kc=kc, p1=p1, a1=a1, f=f, c0=c0: e.matmul(p1[:, :], lhsT=a1[:, kc, f * 128:(f + 1) * 128], rhs=hb2[:, kc, c0:c0 + 512], start=(kc == 0), stop=(kc == 7)), reads=[a1, hb2], writes=[p1])
                                for kc in range(8):
                                    kb.op('pe', lambda e, kc=kc, p3=p3, a3=a3, f=f, c0=c0: e.matmul(p3[:, :], lhsT=a3[:, kc, f * 128:(f + 1) * 128], rhs=hb2[:, kc, c0:c0 + 512], start=(kc == 0), stop=(kc == 7)), reads=[a3, hb2], writes=[p3])
                                kb.op('act', lambda e, p1=p1: e.activation(out=sil[:], in_=p1[:, :], func=AF.Silu), reads=[p1], writes=[sil])
                                kb.op('dve', lambda e, p3=p3, g_=g_, f=f, c0=c0: e.tensor_tensor(out=g_[:, f, c0:c0 + 512], in0=p3[:, :], in1=sil[:], op=ALU.mult), reads=[p3, sil], writes=[g_])
                        for s_ in range(NS):
                            for hf in range(2):
                                py = pbank[4 + (s_ * 2 + hf) % 2]
                                for f in range(4):
                                    kb.op('pe', lambda e, f=f, py=py, g_=g_, a2=a2, s_=s_, hf=hf: e.matmul(py[:, :], lhsT=g_[:, f, s_ * 128:(s_ + 1) * 128], rhs=a2[:, f, hf * 512:(hf + 1) * 512], start=(f == 0), stop=(f == 3)), reads=[g_, a2], writes=[py])
                                kb.op('dve', lambda e, py=py, s_=s_, hf=hf: e.tensor_tensor(out=acc[:, s_, hf * 512:(hf + 1) * 512], in0=py[:, :], in1=acc[:, s_, hf * 512:(hf + 1) * 512], op=ALU.add), reads=[py, acc], writes=[acc])
                    kb.dma('sp', S['Ys'][b_ * BS:(b_ + 1) * BS, :].rearrange("(s p) d -> p s d", p=128), acc[:], reads=[acc], writes=[D_['Ys']])
                kb.barrier()
                es2.close()
                es2 = es
                md5 = sb(es2, 'md5S', [128, 2, D], F32)
                for kind in range(2):
                    kb.dma('sp', md5[:, kind, :], S['mod'][kind:kind + 1, 5 * D:6 * D].to_broadcast([128, D]), reads=[D_['mod']], writes=[md5])
                lng = sb(es2, 'lng2S', [128, 2, D], F32)
                kb.dma('sp', lng[:, 0, :], I['ln2_g'][layer:layer + 1, :].to_broadcast([128, D]), writes=[lng])
                kb.dma('sp', lng[:, 1, :], I['ln2_b'][layer:layer + 1, :].to_broadcast([128, D]), writes=[lng])
                epsl = sb(es2, 'epsl2S', [128, 1], F32)
                kb.op('dve', lambda e: e.memset(epsl[:], LN_EPS), writes=[epsl])
                y0 = [sb(es2, 'y0S%d' % i, [128, D], F32) for i in range(2)]; y1 = [sb(es2, 'y1S%d' % i, [128, D], F32) for i in range(2)]
                x1t = [sb(es2, 'x1tS%d' % i, [128, D], F32) for i in range(2)]
                rr = sb(es2, 'rrS', [128, D], F32); xo = [sb(es2, 'xoS%d' % i, [128, D], F32) for i in range(2)]
                st = sb(es2, 'stS', [128, 8], F32); jk = sb(es2, 'jkS', [128, D], F32)
                for ti in range(NT):
                    kind = 0 if ti < NLT else 1
                    if layer == DEPTH - 1 and kind == 1:
                        continue
                    x1_ = x1t[ti % 2]; xo_ = xo[ti % 2]; ya = y0[ti % 2]; yb_ = y1[ti % 2]
                    kb.dma('sp', x1_[:], S['x1'][ti], reads=[D_['x1']], writes=[x1_])
                    kb.idma(ya[:], None, S['Ys'][:, :], bass.IndirectOffsetOnAxis(ap=posi[:, 0, ti:ti + 1].bitcast(mybir.dt.uint32), axis=0), reads=[posi, D_['Ys']], writes=[ya])
                    kb.idma(yb_[:], None, S['Ys'][:, :], bass.IndirectOffsetOnAxis(ap=posi[:, 1, ti:ti + 1].bitcast(mybir.dt.uint32), axis=0), reads=[posi, D_['Ys']], writes=[yb_])
                    kb.op('dve', lambda e, ya=ya, ti=ti: e.tensor_scalar_mul(out=rr[:], in0=ya[:], scalar1=gk[:, ti, 0:1]), reads=[ya, gk], writes=[rr])
                    kb.op('dve', lambda e, yb_=yb_, ti=ti: e.scalar_tensor_tensor(out=rr[:], in0=yb_[:], scalar=gk[:, ti, 1:2], in1=rr[:], op0=ALU.mult, op1=ALU.add), reads=[yb_, gk, rr], writes=[rr])
                    kb.op('dve', lambda e, kind=kind: e.tensor_tensor(out=rr[:], in0=rr[:], in1=md5[:, kind, :], op=ALU.mult), reads=[rr, md5], writes=[rr])
                    kb.op('dve', lambda e, x1_=x1_: e.scalar_tensor_tensor(out=rr[:], in0=x1_[:], scalar=ALPHA, in1=rr[:], op0=ALU.mult, op1=ALU.add), reads=[x1_, rr], writes=[rr])
                    kb.op('act', lambda e: e.activation(out=jk[:], in_=rr[:], func=AF.Identity, accum_out=st[:, 0:1]), reads=[rr], writes=[jk, st])
                    kb.op('act', lambda e: e.activation(out=jk[:], in_=rr[:], func=AF.Square, accum_out=st[:, 1:2]), reads=[rr], writes=[jk, st])
                    kb.op('dve', lambda e: e.tensor_scalar_mul(out=st[:, 2:4], in0=st[:, 0:2], scalar1=1.0 / D), reads=[st], writes=[st])
                    kb.op('dve', lambda e: e.tensor_tensor(out=st[:, 4:5], in0=st[:, 2:3], in1=st[:, 2:3], op=ALU.mult), reads=[st], writes=[st])
                    kb.op('dve', lambda e: e.tensor_tensor(out=st[:, 5:6], in0=st[:, 3:4], in1=st[:, 4:5], op=ALU.subtract), reads=[st], writes=[st])
                    kb.op('act', lambda e: e.activation(out=st[:, 6:7], in_=st[:, 5:6], func=AF.Sqrt, bias=epsl[:, 0:1]), reads=[st, epsl], writes=[st])
                    kb.op('dve', lambda e: e.reciprocal(out=st[:, 6:7], in_=st[:, 6:7]), reads=[st], writes=[st])
                    kb.op('dve', lambda e, xo_=xo_: e.tensor_scalar(out=xo_[:], in0=rr[:], scalar1=st[:, 2:3], scalar2=st[:, 6:7], op0=ALU.subtract, op1=ALU.mult), reads=[rr, st], writes=[xo_])
                    kb.op('dve', lambda e, xo_=xo_: e.tensor_tensor(out=xo_[:], in0=xo_[:], in1=lng[:, 0, :], op=ALU.mult), reads=[xo_, lng], writes=[xo_])
                    kb.op('dve', lambda e, xo_=xo_: e.tensor_tensor(out=xo_[:], in0=xo_[:], in1=lng[:, 1, :], op=ALU.add), reads=[xo_, lng], writes=[xo_])
                    if layer == DEPTH - 1:
                        kb.dma('sp', OUT[ti * 128:(ti + 1) * 128, :], xo_[:], reads=[xo_])
                    else:
                        kb.dma('sp', S['xcur'][ti], xo_[:], reads=[xo_], writes=[D_['xcur']])
                kb.barrier()
            with contextlib.ExitStack() as es:
              if layer % 2 == 0:
                  is_moe = (layer % 2 == 1)
                  jl = layer // 2
                  nexp = NEXP if is_moe else 1
                  TB = 1536
                  hblk = sb(es, 'hblk', [128, 8, TB], BF16)
                  acc = sb(es, 'accE', [128, TB // 128, D], F32)
                  w1c = [sb(es, 'w1c%d' % i, [128, 8, 512], BF16) for i in range(2)]
                  w3c = [sb(es, 'w3c%d' % i, [128, 8, 512], BF16) for i in range(2)]
                  w2c = [sb(es, 'w2c%d' % i, [128, 4, D], BF16) for i in range(2)]
                  gT = [sb(es, 'gT%d' % i, [128, 4, TB], BF16) for i in range(2)]
                  sil = sb(es, 'sil', [128, 512], F32)
                  gates = sb(es, 'gates', [128, TB // 128, NEXP], F32)
                  wr = sb(es, 'wr', [128, 8, NEXP], BF16)
                  lg = sb(es, 'lg', [128, 4, NEXP], F32); sc = sb(es, 'scE', [128, 8], F32)
                  md5 = sb(es, 'md5', [128, 2, D], F32)
                  for kind in range(2):
                      kb.dma('sp', md5[:, kind, :], S['mod'][kind:kind + 1, 5 * D:6 * D].to_broadcast([128, D]), reads=[D_['mod']], writes=[md5])
                  lng = sb(es, 'lng2', [128, 2, D], F32)
                  kb.dma('sp', lng[:, 0, :], I['ln2_g'][layer:layer + 1, :].to_broadcast([128, D]), writes=[lng])
                  kb.dma('sp', lng[:, 1, :], I['ln2_b'][layer:layer + 1, :].to_broadcast([128, D]), writes=[lng])
                  epsl = sb(es, 'epsl2', [128, 1], F32)
                  kb.op('dve', lambda e: e.memset(epsl[:], LN_EPS), writes=[epsl])
                  x1t = [sb(es, 'x1t%d' % i, [128, D], F32) for i in range(2)]
                  rr = sb(es, 'rrE', [128, D], F32); xo = [sb(es, 'xoE%d' % i, [128, D], F32) for i in range(2)]
                  st = sb(es, 'stE', [128, 8], F32); jk = sb(es, 'jkE', [128, D], F32)
                  if is_moe:
                      kb.dma('pool', wr[:], I['moe_router'][jl].rearrange("(k p) n -> p k n", p=128), writes=[wr])
                  wcnt = [0]
                  blocks = [(b * TB, min(TB, T - b * TB)) for b in range((T + TB - 1) // TB)]
                  for (t0, nb) in blocks:
                      ntl = nb // 128
                      kb.dma('sp', hblk[:, :, 0:nb], S['h2T'][:, :, t0:t0 + nb], reads=[D_['h2T']], writes=[hblk])
                      kb.op('pool', lambda e: e.memset(acc[:], 0.0), writes=[acc])
                      if is_moe:
                          for s_ in range(ntl):
                              pl = pbank[4 + s_ % 2]
                              for kc in range(8):
                                  kb.op('pe', lambda e, kc=kc, pl=pl, s_=s_: e.matmul(pl[:, 0:NEXP], lhsT=hblk[:, kc, s_ * 128:(s_ + 1) * 128], rhs=wr[:, kc, :], start=(kc == 0), stop=(kc == 7)), reads=[hblk, wr], writes=[pl])
                              kb.op('dve', lambda e, pl=pl: e.tensor_copy(out=lg[:, 0, :], in_=pl[:, 0:NEXP]), reads=[pl], writes=[lg])
                              kb.op('dve', lambda e: e.reduce_max(out=sc[:, 0:1], in_=lg[:, 0, :], axis=AX.X), reads=[lg], writes=[sc])
                              kb.op('dve', lambda e: e.tensor_scalar(out=lg[:, 1, :], in0=lg[:, 0, :], scalar1=sc[:, 0:1], scalar2=None, op0=ALU.is_equal), reads=[lg, sc], writes=[lg])
                              kb.op('dve', lambda e: e.scalar_tensor_tensor(out=lg[:, 2, :], in0=lg[:, 1, :], scalar=-1e30, in1=lg[:, 0, :], op0=ALU.mult, op1=ALU.add), reads=[lg], writes=[lg])
                              kb.op('dve', lambda e: e.reduce_max(out=sc[:, 1:2], in_=lg[:, 2, :], axis=AX.X), reads=[lg], writes=[sc])
                              kb.op('dve', lambda e: e.tensor_scalar(out=lg[:, 3, :], in0=lg[:, 2, :], scalar1=sc[:, 1:2], scalar2=None, op0=ALU.is_equal), reads=[lg, sc], writes=[lg])
                              kb.op('dve', lambda e: e.tensor_tensor(out=sc[:, 2:3], in0=sc[:, 1:2], in1=sc[:, 0:1], op=ALU.subtract), reads=[sc], writes=[sc])
                              kb.op('act', lambda e: e.activation(out=sc[:, 3:4], in_=sc[:, 2:3], func=AF.Exp), reads=[sc], writes=[sc])
                              kb.op('dve', lambda e: e.tensor_scalar_add(out=sc[:, 4:5], in0=sc[:, 3:4], scalar1=1.0), reads=[sc], writes=[sc])
                              kb.op('dve', lambda e: e.reciprocal(out=sc[:, 4:5], in_=sc[:, 4:5]), reads=[sc], writes=[sc])
                              kb.op('dve', lambda e: e.tensor_tensor(out=sc[:, 5:6], in0=sc[:, 3:4], in1=sc[:, 4:5], op=ALU.mult), reads=[sc], writes=[sc])
                              kb.op('dve', lambda e: e.tensor_scalar_mul(out=lg[:, 1, :], in0=lg[:, 1, :], scalar1=sc[:, 4:5]), reads=[lg, sc], writes=[lg])
                              kb.op('dve', lambda e, s_=s_: e.scalar_tensor_tensor(out=gates[:, s_, :], in0=lg[:, 3, :], scalar=sc[:, 5:6], in1=lg[:, 1, :], op0=ALU.mult, op1=ALU.add), reads=[lg, sc], writes=[gates])
                      for ex in range(nexp):
                          if is_moe:
                              W1 = I['moe_w1'][jl, ex]; W3 = I['moe_w3'][jl, ex]; W2 = I['moe_w2'][jl, ex]
                          else:
                              W1 = I['ffn_w1'][jl]; W3 = I['ffn_w3'][jl]; W2 = I['ffn_w2'][jl]
                          for fc in range(D_FF // 512):
                              a1 = w1c[wcnt[0] % 2]; a3 = w3c[wcnt[0] % 2]; a2 = w2c[wcnt[0] % 2]; g_ = gT[wcnt[0] % 2]; wcnt[0] += 1
                              kb.dma('pool', a1[:], W1[:, fc * 512:(fc + 1) * 512].rearrange("(k p) n -> p k n", p=128), writes=[a1])
                              kb.dma('pool', a3[:], W3[:, fc * 512:(fc + 1) * 512].rearrange("(k p) n -> p k n", p=128), writes=[a3])
                              kb.dma('pool', a2[:], W2[fc * 512:(fc + 1) * 512, :].rearrange("(f p) n -> p f n", p=128), writes=[a2])
                              for f in range(4):
                                  for th in range((nb + 511) // 512):
                                      c0 = th * 512; n = min(512, nb - c0)
                                      p1 = pbank[0 + (f * 2 + th) % 2 * 2]; p3 = pbank[1 + (f * 2 + th) % 2 * 2]
                                      for kc in range(8):
                                          kb.op('pe', lambda e, kc=kc, p1=p1, a1=a1, f=f, c0=c0, n=n: e.matmul(p1[:, 0:n], lhsT=a1[:, kc, f * 128:(f + 1) * 128], rhs=hblk[:, kc, c0:c0 + n], start=(kc == 0), stop=(kc == 7)), reads=[a1, hblk], writes=[p1])
                                      for kc in range(8):
                                          kb.op('pe', lambda e, kc=kc, p3=p3, a3=a3, f=f, c0=c0, n=n: e.matmul(p3[:, 0:n], lhsT=a3[:, kc, f * 128:(f + 1) * 128], rhs=hblk[:, kc, c0:c0 + n], start=(kc == 0), stop=(kc == 7)), reads=[a3, hblk], writes=[p3])
                                      kb.op('act', lambda e, p1=p1, n=n: e.activation(out=sil[:, 0:n], in_=p1[:, 0:n], func=AF.Silu), reads=[p1], writes=[sil])
                                      kb.op('dve', lambda e, p3=p3, g_=g_, f=f, c0=c0, n=n: e.tensor_tensor(out=g_[:, f, c0:c0 + n], in0=p3[:, 0:n], in1=sil[:, 0:n], op=ALU.mult), reads=[p3, sil], writes=[g_])
                              for s_ in range(ntl):
                                  for hf in range(2):
                                      py = pbank[4 + (s_ * 2 + hf) % 2]
                                      for f in range(4):
                                          kb.op('pe', lambda e, f=f, py=py, g_=g_, a2=a2, s_=s_, hf=hf: e.matmul(py[:, :], lhsT=g_[:, f, s_ * 128:(s_ + 1) * 128], rhs=a2[:, f, hf * 512:(hf + 1) * 512], start=(f == 0), stop=(f == 3)), reads=[g_, a2], writes=[py])
                                      if is_moe:
                                          kb.op('dve', lambda e, py=py, s_=s_, hf=hf, ex=ex: e.scalar_tensor_tensor(out=acc[:, s_, hf * 512:(hf + 1) * 512], in0=py[:, :], scalar=gates[:, s_, ex:ex + 1], in1=acc[:, s_, hf * 512:(hf + 1) * 512], op0=ALU.mult, op1=ALU.add), reads=[py, gates, acc], writes=[acc])
                                      else:
                                          kb.op('dve', lambda e, py=py, s_=s_, hf=hf: e.tensor_tensor(out=acc[:, s_, hf * 512:(hf + 1) * 512], in0=py[:, :], in1=acc[:, s_, hf * 512:(hf + 1) * 512], op=ALU.add), reads=[py, acc], writes=[acc])
                      for s_ in range(ntl):
                          ti = t0 // 128 + s_
                          kind = 0 if ti < NLT else 1
                          if layer == DEPTH - 1 and kind == 1:
                              continue
                          x1_ = x1t[ti % 2]; xo_ = xo[ti % 2]
                          kb.dma('sp', x1_[:], S['x1'][ti], reads=[D_['x1']], writes=[x1_])
                          kb.op('dve', lambda e, s_=s_, kind=kind: e.tensor_tensor(out=rr[:], in0=acc[:, s_, :], in1=md5[:, kind, :], op=ALU.mult), reads=[acc, md5], writes=[rr])
                          kb.op('dve', lambda e, x1_=x1_: e.scalar_tensor_tensor(out=rr[:], in0=x1_[:], scalar=ALPHA, in1=rr[:], op0=ALU.mult, op1=ALU.add), reads=[x1_, rr], writes=[rr])
                          kb.op('act', lambda e: e.activation(out=jk[:], in_=rr[:], func=AF.Identity, accum_out=st[:, 0:1]), reads=[rr], writes=[jk, st])
                          kb.op('act', lambda e: e.activation(out=jk[:], in_=rr[:], func=AF.Square, accum_out=st[:, 1:2]), reads=[rr], writes=[jk, st])
                          kb.op('dve', lambda e: e.tensor_scalar_mul(out=st[:, 2:4], in0=st[:, 0:2], scalar1=1.0 / D), reads=[st], writes=[st])
                          kb.op('dve', lambda e: e.tensor_tensor(out=st[:, 4:5], in0=st[:, 2:3], in1=st[:, 2:3], op=ALU.mult), reads=[st], writes=[st])
                          kb.op('dve', lambda e: e.tensor_tensor(out=st[:, 5:6], in0=st[:, 3:4], in1=st[:, 4:5], op=ALU.subtract), reads=[st], writes=[st])
                          kb.op('act', lambda e: e.activation(out=st[:, 6:7], in_=st[:, 5:6], func=AF.Sqrt, bias=epsl[:, 0:1]), reads=[st, epsl], writes=[st])
                          kb.op('dve', lambda e: e.reciprocal(out=st[:, 6:7], in_=st[:, 6:7]), reads=[st], writes=[st])
                          kb.op('dve', lambda e, xo_=xo_: e.tensor_scalar(out=xo_[:], in0=rr[:], scalar1=st[:, 2:3], scalar2=st[:, 6:7], op0=ALU.subtract, op1=ALU.mult), reads=[rr, st], writes=[xo_])
                          kb.op('dve', lambda e, xo_=xo_: e.tensor_tensor(out=xo_[:], in0=xo_[:], in1=lng[:, 0, :], op=ALU.mult), reads=[xo_, lng], writes=[xo_])
                          kb.op('dve', lambda e, xo_=xo_: e.tensor_tensor(out=xo_[:], in0=xo_[:], in1=lng[:, 1, :], op=ALU.add), reads=[xo_, lng], writes=[xo_])
                          if layer == DEPTH - 1:
                              kb.dma('sp', OUT[ti * 128:(ti + 1) * 128, :], xo_[:], reads=[xo_])
                          else:
                              kb.dma('sp', S['xcur'][ti], xo_[:], reads=[xo_], writes=[D_['xcur']])
              if debug == 'L' and layer == nlayers - 1:
                  kb.barrier()
                  dbg_out['xcur'] = nc.dram_tensor('dbg_xcur', [NT, 128, D], F32, kind="ExternalOutput").ap()
                  kb.dma('sp', dbg_out['xcur'], S['xcur'], reads=[D_['xcur']])
              kb.barrier()

        kb.barrier()
    return nc, dbg_out


def host_consts():
    c = {}
    c['ident'] = np.eye(128, dtype=np.float32)
    t = np.arange(SEQ)
    prow, pcol = t // 64, t % 64
    inv = (10000.0 ** (-np.arange(8, dtype=np.float32) / 8)).astype(np.float32)
    cosr = np.ones((T, 16), np.float32); sinr = np.zeros((T, 16), np.float32)
    ar = prow[:, None].astype(np.float32) * inv[None, :]
    ac = pcol[:, None].astype(np.float32) * inv[None, :]
    cosr[:SEQ, 0:8] = np.cos(ar); cosr[:SEQ, 8:16] = np.cos(ac)
    sinr[:SEQ, 0:8] = np.sin(ar); sinr[:SEQ, 8:16] = np.sin(ac)
    c['ropec'] = np.ascontiguousarray(cosr.reshape(NT, 128, 16).transpose(1, 0, 2))
    c['ropes'] = np.ascontiguousarray(sinr.reshape(NT, 128, 16).transpose(1, 0, 2))
    c['iota128'] = np.arange(128, dtype=np.float32)[None, :]
    pp = np.arange(128)[:, None, None]
    c['base1'] = ((np.arange(8)[None, :, None] * 128 + pp) * 7 + np.arange(7)[None, None, :]).reshape(128, 56).astype(np.float32)
    c['base2'] = (np.arange(28)[None, :] * 128 + np.arange(128)[:, None]).astype(np.float32)
    c['utri'] = np.triu(np.ones((128, 128), np.float32), 1)
    c['bstart'] = (np.arange(NBLK, dtype=np.float32) * float(BS))[None, :]
    m32 = np.zeros((128, 6), np.float32)
    for p_ in range(128):
        m32[p_, p_ // 32] = 1.0
        m32[p_, 4 + p_ // 64] = 1.0
    c['m32'] = m32
    c['iotaT'] = np.stack([np.arange(T, dtype=np.float32), (T - 1) - np.arange(T, dtype=np.float32)])
    cc = np.arange(NT, dtype=np.float32)
    c['cv'] = np.stack([127.0 - 128.0 * cc, 1.0 + 128.0 * cc]).astype(np.float32)
    mC = np.zeros((4, 128, 128), np.float32)
    for b in range(4):
        for co in range(128):
            for st in range(128):
                if co // 16 == b * 2 + st // 64:
                    mC[b, co, st] = 1.0
    c['maskC'] = mC
    c['maskB'] = np.ascontiguousarray(mC.transpose(0, 2, 1))
    return c


def rpb_toeplitz(rpb):
    kc = np.arange(64)[:, None]; qc = np.arange(64)[None, :]
    c0 = np.clip(qc - 8, 0, 48)
    inwin = (kc >= c0) & (kc <= c0 + 15)
    idx = np.clip(kc - qc + 15, 0, 30)
    g = rpb[:, :, :, idx]
    return np.where(inwin[None, None, None], g, np.float32(-30000.0)).astype(np.float32)


_CACHE = {}


def make_in_maps(inputs, ncores=8):
    consts = host_consts()
    shared = {}
    for k, v in inputs.items():
        if k in ('x', 'c', 'ctx', 'c_ctx', 'na_rpb'):
            continue
        shared[k] = np.ascontiguousarray(np.asarray(v, dtype=np.float32))
    shared['rpbT'] = rpb_toeplitz(np.asarray(inputs['na_rpb'], dtype=np.float32))
    shared['c_ctx'] = np.asarray(inputs['c_ctx'], np.float32).reshape(1, D)
    shared.update(consts)
    maps = []
    for b in range(ncores):
        m = dict(shared)
        m['x'] = np.ascontiguousarray(np.asarray(inputs['x'][b], np.float32))
        m['ctx'] = np.ascontiguousarray(np.asarray(inputs['ctx'][b], np.float32))
        m['c'] = np.asarray(inputs['c'][b], np.float32).reshape(1, D)
        maps.append(m)
    return maps


def kernel(**inputs):
    if 'nc' not in _CACHE:
        _CACHE['nc'] = build_program()[0]
    nc = _CACHE['nc']
    maps = make_in_maps(inputs, 8)
    res = run_bass_kernel_spmd(nc, maps, core_ids=list(range(8)))
    return np.stack([np.asarray(r['out'], dtype=np.float32) for r in res.results], axis=0)
```
